# Optimizing a Trainium2 kernel written in Bass

```python
import math
import jax, jax.numpy as jnp
from jax import lax
import numpy as np

D_MODEL = 1024
BATCH = 8
SEQ = 4096
DEPTH = 2

N_MIXERS = 2
NORM_EPS = 1e-6
ADA_STD = 0.5
MLA_HEADS = 8
Q_LORA = 256
KV_LORA = 256
QK_NOPE = 128
QK_ROPE = 64
V_HEAD = 128
ROPE_THETA = 10000.0
Q_BLOCK = 128
MLA_SCALE = (QK_NOPE + QK_ROPE) ** -0.5
MLA_IN_DIM = Q_LORA + KV_LORA + QK_ROPE
SSM_INNER = 2 * D_MODEL
SSM_HEAD_DIM = 64
SSM_HEADS = SSM_INNER // SSM_HEAD_DIM
SSM_GROUPS = 8
SSM_HPG = SSM_HEADS // SSM_GROUPS
SSM_STATE = 128
SSM_CONV = 4
SSM_CONV_DIM = SSM_INNER + 2 * SSM_GROUPS * SSM_STATE
SSM_IN_DIM = SSM_INNER + SSM_CONV_DIM + SSM_HEADS
SSM_CHUNK = 128
N_EXPERTS = 32
TOP_K = 4
EXPERT_FF = D_MODEL
SWIGLU_LIMIT = 7.0
SWIGLU_ALPHA = 1.702
MOE_BLOCK = 256

kernel_name = "hybrid_mla_ssd_moe_adaln"


def rmsnorm(x, g):
    xf = x.astype(jnp.float32)
    xf = xf * lax.rsqrt(jnp.mean(xf * xf, axis=-1, keepdims=True) + NORM_EPS)
    return (xf * g.astype(jnp.float32)).astype(x.dtype)


def rope_cos_sin(positions):
    inv_freq = 1.0 / (ROPE_THETA ** (jnp.arange(0, QK_ROPE, 2, dtype=jnp.float32) / QK_ROPE))
    ang = positions.astype(jnp.float32)[..., None] * inv_freq
    return jnp.cos(ang), jnp.sin(ang)


def apply_rope(x, cos, sin):
    x1, x2 = jnp.split(x, 2, axis=-1)
    return jnp.concatenate([x1 * cos - x2 * sin, x1 * sin + x2 * cos], axis=-1).astype(x.dtype)


def mla_mixer(h, positions, w_in, g_q, g_kv, w_q_up, w_kv_up, w_out):
    bsz, seqlen, _ = h.shape
    lat = h @ w_in
    q_lat = rmsnorm(lat[..., :Q_LORA], g_q)
    kv_lat = rmsnorm(lat[..., Q_LORA:Q_LORA + KV_LORA], g_kv)
    cos, sin = rope_cos_sin(positions)
    k_rope = apply_rope(lat[..., Q_LORA + KV_LORA:], cos, sin)
    q = (q_lat @ w_q_up).reshape(bsz, seqlen, MLA_HEADS, QK_NOPE + QK_ROPE)
    q_nope = q[..., :QK_NOPE]
    q_rope = apply_rope(q[..., QK_NOPE:], cos[:, :, None], sin[:, :, None])
    kv = (kv_lat @ w_kv_up).reshape(bsz, seqlen, MLA_HEADS, QK_NOPE + V_HEAD)
    k_nope = kv[..., :QK_NOPE]
    v = kv[..., QK_NOPE:]
    outs = []
    for blk in range(seqlen // Q_BLOCK):
        q0 = blk * Q_BLOCK
        q1 = q0 + Q_BLOCK
        s = (jnp.einsum('bqhd,bkhd->bhqk', q_nope[:, q0:q1], k_nope[:, :q1])
             + jnp.einsum('bqhr,bkr->bhqk', q_rope[:, q0:q1], k_rope[:, :q1]))
        s = s.astype(jnp.float32) * MLA_SCALE
        causal = (q0 + jnp.arange(Q_BLOCK))[:, None] >= jnp.arange(q1)[None, :]
        p = jax.nn.softmax(jnp.where(causal, s, -jnp.inf), axis=-1).astype(v.dtype)
        outs.append(jnp.einsum('bhqk,bkhd->bqhd', p, v[:, :q1]))
    o = jnp.concatenate(outs, axis=1).reshape(bsz, seqlen, MLA_HEADS * V_HEAD)
    return o @ w_out


def causal_depthwise_conv(u, w, b):
    k = w.shape[0]
    out = lax.conv_general_dilated(u, w[:, None, :].astype(u.dtype), window_strides=(1,),
                                   padding=[(k - 1, 0)], dimension_numbers=('NWC', 'WIO', 'NWC'),
                                   feature_group_count=u.shape[-1])
    return out + b


def ssd_chunked(x, a_dt, bm, cm):
    bsz, seqlen, ng, nr, hp = x.shape
    nc = seqlen // SSM_CHUNK
    x = x.reshape(bsz, nc, SSM_CHUNK, ng, nr, hp)
    bm = bm.reshape(bsz, nc, SSM_CHUNK, ng, SSM_STATE)
    cm = cm.reshape(bsz, nc, SSM_CHUNK, ng, SSM_STATE)
    a = a_dt.reshape(bsz, nc, SSM_CHUNK, ng, nr).transpose(0, 3, 4, 1, 2)
    a_cum = jnp.cumsum(a, axis=-1)
    seg = a_cum[..., :, None] - a_cum[..., None, :]
    causal = jnp.tril(jnp.ones((SSM_CHUNK, SSM_CHUNK), dtype=bool))
    decay = jnp.exp(jnp.where(causal, seg, -jnp.inf))
    cb = jnp.einsum('bclgn,bcsgn->bgcls', cm, bm)
    y_diag = jnp.einsum('bgrcls,bcsgrp->bclgrp', (cb[:, :, None] * decay).astype(x.dtype), x)
    decay_to_end = jnp.exp(a_cum[..., -1:] - a_cum).astype(x.dtype)
    states = jnp.einsum('bclgn,bgrcl,bclgrp->bcgrpn', bm, decay_to_end, x)
    chunk_decay = jnp.exp(a_cum[..., -1]).astype(x.dtype)

    def step(carry, inp):
        st, dec = inp
        return carry * dec[..., None, None] + st, carry

    init = jnp.zeros((bsz, ng, nr, hp, SSM_STATE), x.dtype)
    _, prev = lax.scan(step, init, (states.transpose(1, 0, 2, 3, 4, 5), chunk_decay.transpose(3, 0, 1, 2)))
    prev = prev.transpose(1, 0, 2, 3, 4, 5)
    y_off = jnp.einsum('bclgn,bcgrpn,bgrcl->bclgrp', cm, prev, jnp.exp(a_cum).astype(x.dtype))
    return (y_diag + y_off).reshape(bsz, seqlen, ng, nr, hp)


def gated_group_rmsnorm(y, z, g):
    yf = (y * jax.nn.silu(z)).astype(jnp.float32)
    yf = yf.reshape(*y.shape[:-1], SSM_GROUPS, SSM_INNER // SSM_GROUPS)
    yf = yf * lax.rsqrt(jnp.mean(yf * yf, axis=-1, keepdims=True) + NORM_EPS)
    return (yf.reshape(y.shape) * g.astype(jnp.float32)).astype(y.dtype)


def ssd_mixer(h, w_in, conv_w, conv_b, dt_bias, a_log, d_skip, g_norm, w_out):
    bsz, seqlen, _ = h.shape
    zxbcdt = h @ w_in
    z = zxbcdt[..., :SSM_INNER]
    xbc = zxbcdt[..., SSM_INNER:SSM_INNER + SSM_CONV_DIM]
    dt_raw = zxbcdt[..., SSM_INNER + SSM_CONV_DIM:]
    xbc = jax.nn.silu(causal_depthwise_conv(xbc, conv_w, conv_b))
    gn = SSM_GROUPS * SSM_STATE
    xs = xbc[..., :SSM_INNER].reshape(bsz, seqlen, SSM_GROUPS, SSM_HPG, SSM_HEAD_DIM)
    bm = xbc[..., SSM_INNER:SSM_INNER + gn].reshape(bsz, seqlen, SSM_GROUPS, SSM_STATE)
    cm = xbc[..., SSM_INNER + gn:].reshape(bsz, seqlen, SSM_GROUPS, SSM_STATE)
    dt = jax.nn.softplus(dt_raw.astype(jnp.float32) + dt_bias.astype(jnp.float32))
    dt = dt.reshape(bsz, seqlen, SSM_GROUPS, SSM_HPG)
    a = -jnp.exp(a_log.astype(jnp.float32)).reshape(SSM_GROUPS, SSM_HPG)
    x_dt = (xs * dt[..., None]).astype(xs.dtype)
    y = ssd_chunked(x_dt, dt * a, bm, cm)
    y = y + d_skip.reshape(SSM_GROUPS, SSM_HPG)[..., None] * xs
    y = y.reshape(bsz, seqlen, SSM_INNER).astype(h.dtype)
    return gated_group_rmsnorm(y, z, g_norm) @ w_out


def clamped_swiglu(gu):
    g, lin = jnp.split(gu, 2, axis=-1)
    g = jnp.minimum(g, SWIGLU_LIMIT)
    lin = jnp.clip(lin, -SWIGLU_LIMIT, SWIGLU_LIMIT)
    return g * jax.nn.sigmoid(SWIGLU_ALPHA * g) * (lin + 1.0)


def moe_ffn(h, w_router, b_router, w_gate_up, b_gate_up, w_down, b_down):
    n_tok, d = h.shape
    logits = (h @ w_router + b_router).astype(jnp.float32)
    top_logit, top_e = lax.top_k(logits, TOP_K)
    gate = jax.nn.softmax(top_logit, axis=-1)
    n_pair = n_tok * TOP_K
    pair_e = top_e.reshape(-1).astype(jnp.int32)
    pair_tok = jnp.arange(n_pair, dtype=jnp.int32) // TOP_K
    pair_gate = gate.reshape(-1)
    order = jnp.argsort(pair_e, stable=True)
    e_sorted = pair_e[order]
    counts = jnp.bincount(pair_e, length=N_EXPERTS).astype(jnp.int32)
    padded = (counts + MOE_BLOCK - 1) // MOE_BLOCK * MOE_BLOCK
    pad_end = jnp.cumsum(padded)
    pad_start = pad_end - padded
    start = jnp.cumsum(counts) - counts
    slot = pad_start[e_sorted] + jnp.arange(n_pair, dtype=jnp.int32) - start[e_sorted]
    n_rows = -(-n_pair // MOE_BLOCK) * MOE_BLOCK + N_EXPERTS * MOE_BLOCK
    n_blk = n_rows // MOE_BLOCK
    row_tok = jnp.zeros((n_rows,), jnp.int32).at[slot].set(pair_tok[order])
    row_gate = jnp.zeros((n_rows,), jnp.float32).at[slot].set(pair_gate[order])
    blk_start = jnp.arange(n_blk, dtype=jnp.int32) * MOE_BLOCK
    blk_e = jnp.minimum(jnp.searchsorted(pad_end, blk_start, side='right'), N_EXPERTS - 1)

    def expert_block(args):
        tok, e = args
        gu = h[tok] @ w_gate_up[e] + b_gate_up[e]
        return clamped_swiglu(gu) @ w_down[e] + b_down[e]

    y_rows = lax.map(expert_block, (row_tok.reshape(n_blk, MOE_BLOCK), blk_e)).reshape(n_rows, d)
    y_rows = y_rows * row_gate[:, None].astype(y_rows.dtype)
    return jnp.zeros_like(h).at[row_tok].add(y_rows.astype(h.dtype))


def setup_inputs(seed: int = 0) -> dict:
    key = jax.random.key(seed)
    ks = iter(jax.random.split(key, 32))
    f32 = jnp.float32
    n_mla = (DEPTH + 1) // N_MIXERS
    n_ssm = DEPTH // N_MIXERS

    def normal(shape, std):
        return std * jax.random.normal(next(ks), shape, f32)

    def gain(shape):
        return 1.0 + 0.1 * jax.random.normal(next(ks), shape, f32)

    x = normal((BATCH, SEQ, D_MODEL), 1.0)
    c = normal((BATCH, D_MODEL), 1.0)
    offsets = jax.random.randint(next(ks), (BATCH, 1), 0, 2048, dtype=jnp.int32)
    positions = offsets + jnp.arange(SEQ, dtype=jnp.int32)[None, :]
    w_mod = normal((DEPTH, D_MODEL, 6 * D_MODEL), ADA_STD * D_MODEL ** -0.5)
    b_mod = normal((DEPTH, 6 * D_MODEL), 0.02)
    g_mix_norm = gain((DEPTH, D_MODEL))
    g_ffn_norm = gain((DEPTH, D_MODEL))
    mla_w_in = normal((n_mla, D_MODEL, MLA_IN_DIM), D_MODEL ** -0.5)
    mla_g_q = gain((n_mla, Q_LORA))
    mla_g_kv = gain((n_mla, KV_LORA))
    mla_w_q_up = normal((n_mla, Q_LORA, MLA_HEADS * (QK_NOPE + QK_ROPE)), Q_LORA ** -0.5)
    mla_w_kv_up = normal((n_mla, KV_LORA, MLA_HEADS * (QK_NOPE + V_HEAD)), KV_LORA ** -0.5)
    mla_w_out = normal((n_mla, MLA_HEADS * V_HEAD, D_MODEL), (MLA_HEADS * V_HEAD) ** -0.5)
    ssm_w_in = normal((n_ssm, D_MODEL, SSM_IN_DIM), D_MODEL ** -0.5)
    ssm_conv_w = normal((n_ssm, SSM_CONV, SSM_CONV_DIM), SSM_CONV ** -0.5)
    ssm_conv_b = normal((n_ssm, SSM_CONV_DIM), 0.02)
    dt0 = jnp.exp(jax.random.uniform(next(ks), (n_ssm, SSM_HEADS), f32, math.log(1e-3), math.log(1e-1)))
    ssm_dt_bias = dt0 + jnp.log(-jnp.expm1(-dt0))
    ssm_a_log = jnp.log(jax.random.uniform(next(ks), (n_ssm, SSM_HEADS), f32, 1.0, 16.0))
    ssm_d = gain((n_ssm, SSM_HEADS))
    ssm_g_norm = gain((n_ssm, SSM_INNER))
    ssm_w_out = normal((n_ssm, SSM_INNER, D_MODEL), SSM_INNER ** -0.5)
    moe_w_router = normal((DEPTH, D_MODEL, N_EXPERTS), D_MODEL ** -0.5)
    moe_b_router = normal((DEPTH, N_EXPERTS), 0.01)
    moe_w_gate_up = normal((DEPTH, N_EXPERTS, D_MODEL, 2 * EXPERT_FF), D_MODEL ** -0.5)
    moe_b_gate_up = normal((DEPTH, N_EXPERTS, 2 * EXPERT_FF), 0.02)
    moe_w_down = normal((DEPTH, N_EXPERTS, EXPERT_FF, D_MODEL), EXPERT_FF ** -0.5)
    moe_b_down = normal((DEPTH, N_EXPERTS, D_MODEL), 0.02)
    g_final = gain((D_MODEL,))
    return {"x": x, "c": c, "positions": positions,
            "w_mod": w_mod, "b_mod": b_mod, "g_mix_norm": g_mix_norm, "g_ffn_norm": g_ffn_norm,
            "mla_w_in": mla_w_in, "mla_g_q": mla_g_q, "mla_g_kv": mla_g_kv,
            "mla_w_q_up": mla_w_q_up, "mla_w_kv_up": mla_w_kv_up, "mla_w_out": mla_w_out,
            "ssm_w_in": ssm_w_in, "ssm_conv_w": ssm_conv_w, "ssm_conv_b": ssm_conv_b,
            "ssm_dt_bias": ssm_dt_bias, "ssm_a_log": ssm_a_log, "ssm_d": ssm_d,
            "ssm_g_norm": ssm_g_norm, "ssm_w_out": ssm_w_out,
            "moe_w_router": moe_w_router, "moe_b_router": moe_b_router,
            "moe_w_gate_up": moe_w_gate_up, "moe_b_gate_up": moe_b_gate_up,
            "moe_w_down": moe_w_down, "moe_b_down": moe_b_down,
            "g_final": g_final}


def reference(x, c, positions, w_mod, b_mod, g_mix_norm, g_ffn_norm,
              mla_w_in, mla_g_q, mla_g_kv, mla_w_q_up, mla_w_kv_up, mla_w_out,
              ssm_w_in, ssm_conv_w, ssm_conv_b, ssm_dt_bias, ssm_a_log, ssm_d, ssm_g_norm, ssm_w_out,
              moe_w_router, moe_b_router, moe_w_gate_up, moe_b_gate_up, moe_w_down, moe_b_down,
              g_final):
    bsz, seqlen, d = x.shape
    cond = jax.nn.silu(c)
    for i in range(DEPTH):
        mod = cond @ w_mod[i] + b_mod[i]
        sh1, sc1, gt1, sh2, sc2, gt2 = [m[:, None, :] for m in jnp.split(mod, 6, axis=-1)]
        h = rmsnorm(x, g_mix_norm[i]) * (1.0 + sc1) + sh1
        j = i // N_MIXERS
        if i % N_MIXERS == 0:
            y = mla_mixer(h, positions, mla_w_in[j], mla_g_q[j], mla_g_kv[j],
                          mla_w_q_up[j], mla_w_kv_up[j], mla_w_out[j])
        else:
            y = ssd_mixer(h, ssm_w_in[j], ssm_conv_w[j], ssm_conv_b[j], ssm_dt_bias[j],
                          ssm_a_log[j], ssm_d[j], ssm_g_norm[j], ssm_w_out[j])
        x = x + (gt1 * y).astype(x.dtype)
        h = rmsnorm(x, g_ffn_norm[i]) * (1.0 + sc2) + sh2
        y = moe_ffn(h.reshape(bsz * seqlen, d), moe_w_router[i], moe_b_router[i],
                    moe_w_gate_up[i], moe_b_gate_up[i], moe_w_down[i], moe_b_down[i])
        x = x + (gt2 * y.reshape(bsz, seqlen, d)).astype(x.dtype)
    return rmsnorm(x, g_final)
```

```python
import numpy as np
import concourse.bass as bass
import concourse.mybir as mybir
from concourse.bass_utils import run_bass_kernel_spmd

F32 = mybir.dt.float32
F32R = mybir.dt.float32r
BF16 = mybir.dt.bfloat16
I32 = mybir.dt.int32
U32 = mybir.dt.uint32
ALU = mybir.AluOpType
AF = mybir.ActivationFunctionType
AX = mybir.AxisListType

EPOCH = 20000


class Buf:
    __slots__ = ("t", "name", "lastw", "reads", "dsem", "dcount", "is_dram")

    def __init__(self, t, name, is_dram=False):
        self.t = t
        self.name = name
        self.lastw = None
        self.reads = []
        self.is_dram = is_dram

    def __getitem__(self, idx):
        return self.t[idx]


class FW:
    def __init__(self, nc):
        self.nc = nc
        self.eng = {"pe": nc.tensor, "dve": nc.vector, "act": nc.scalar,
                    "pool": nc.gpsimd, "sp": nc.sync}
        self.sem = {}
        self.cnt = {}
        self.nsem = 0
        self.waited = {e: {} for e in self.eng}
        self.semobjs = {}
        self.ctx = []
        self.bctx = []
        self.last = {e: None for e in self.eng}
        self.pend = {e: [] for e in self.eng}
        self.store_q = "pool"
        self.dma_toks = []
        self.dma_pool = []
        self.dma_free = []
        for e in ("pe", "dve", "act", "pool"):
            self._new_epoch(e)

    def _mksem(self, name):
        cm = self.nc.semaphore(name)
        s = cm.__enter__()
        self.ctx.append(cm)
        self.nsem += 1
        self.semobjs[id(s)] = s
        return s

    def _new_epoch(self, e):
        self.sem[e] = self._mksem(f"s_{e}_{self.nsem}")
        self.cnt[e] = 0

    def sb(self, name, shape, dt=F32):
        self.uid = getattr(self, "uid", 0) + 1
        cm = self.nc.sbuf_tensor(f"{name}_u{self.uid}", shape, dt)
        t = cm.__enter__()
        self.bctx.append(cm)
        return Buf(t, name)

    def ps(self, name, shape, dt=F32):
        cm = self.nc.psum_tensor(name, shape, dt)
        t = cm.__enter__()
        self.ctx.append(cm)
        return Buf(t, name)

    def dram(self, name, shape, dt=F32, kind="Internal"):
        t = self.nc.dram_tensor(name, shape, dt, kind=kind)
        return Buf(t.ap(), name, is_dram=True)

    def _wait(self, e, tok):
        if tok is None:
            return
        sem, val, _ = tok
        w = self.waited[e]
        if w.get(id(sem), 0) >= val:
            return
        w[id(sem)] = val
        self.eng[e].wait_ge(sem, val)

    def _deps(self, e, reads, writes, accumulate=False):
        for tok in self.pend[e]:
            self._wait(e, tok)
        self.pend[e] = []
        for b in reads:
            if b is None:
                continue
            if b.lastw is not None:
                self._wait(e, b.lastw)
        for b in writes:
            if b is None:
                continue
            if b.lastw is not None and not (accumulate and b.lastw[2] == e):
                if b.lastw[2] != e or b.lastw[2] in ("sp", "poolq", "actq"):
                    self._wait(e, b.lastw)
            for tok in b.reads:
                if tok[2] != e:
                    self._wait(e, tok)

    def _commit(self, tok, reads, writes):
        for b in reads:
            if b is not None:
                b.reads.append(tok)
        for b in writes:
            if b is not None:
                b.lastw = tok
                b.reads = []

    def op(self, e, fn, reads=(), writes=(), accumulate=False):
        self._deps(e, reads, writes, accumulate)
        if self.cnt[e] >= EPOCH:
            self._new_epoch(e)
        inst = fn(self.eng[e])
        self.cnt[e] += 1
        inst.then_inc(self.sem[e], 1)
        tok = (self.sem[e], self.cnt[e], e)
        self.last[e] = tok
        self._commit(tok, reads, writes)
        return tok

    def _dma_sem(self):
        if self.dma_free:
            return self.dma_free.pop()
        s = [self._mksem(f"s_dma_{self.nsem}"), 0]
        self.dma_pool.append(s)
        return s

    def dma(self, q, fn, reads=(), writes=(), semslot=None):
        if q == "sp" and not [w for w in writes if w is not None] and getattr(self, "store_q", None):
            q = self.store_q
        self._deps(q, reads, writes)
        if semslot is None:
            semslot = self._rot_sem(q)
        if semslot[1] > 0:
            self._wait(q, (semslot[0], semslot[1], "dmaq"))
        inst = fn(self.eng[q])
        semslot[1] += 16
        inst.then_inc(semslot[0], 16)
        tok = (semslot[0], semslot[1], "dmaq")
        self._commit(tok, reads, writes)
        self.dma_toks.append(tok)
        if len(self.dma_toks) > 64:
            self.dma_toks = self.dma_toks[-64:]
        return tok

    NROT = 16

    def _rot_sem(self, q="sp"):
        if not hasattr(self, "_rotq"):
            self._rotq = {}
        if q not in self._rotq:
            self._rotq[q] = {"sems": [[self._mksem(f"s_rot_{q}_{i}"), 0] for i in range(self.NROT)], "i": 0}
        pool = self._rotq[q]
        i = pool["i"]
        pool["i"] = (i + 1) % self.NROT
        return _RotSlot(pool["sems"], i)

    def barrier(self):
        toks = [t for t in self.last.values() if t is not None]
        toks += self.dma_toks
        for pool in getattr(self, "_rotq", {}).values():
            for s in pool["sems"]:
                if s[1] > 0:
                    toks.append((s[0], s[1], "dmaq"))
        for e in self.eng:
            self.pend[e] = list(toks)
        self.dma_toks = []

    def finish(self):
        self.barrier()
        for e in self.eng:
            for tok in self.pend[e]:
                self._wait(e, tok)
            self.pend[e] = []

    def mark(self):
        return len(self.bctx)

    def release(self, m):
        while len(self.bctx) > m:
            self.bctx.pop().__exit__(None, None, None)

    def close(self):
        self.release(0)
        for cm in reversed(self.ctx):
            cm.__exit__(None, None, None)
        self.ctx = []


class _RotSlot(list):
    def __init__(self, sems, i):
        super().__init__(sems[i])
        self.sems = sems
        self.i = i

    def __setitem__(self, k, v):
        super().__setitem__(k, v)
        self.sems[self.i][k] = v


T = 4096
D = 1024
NT = T // 128
EPS = 1e-6
MLA_SCALE = 192 ** -0.5
PI = float(np.pi)
TWO_PI = float(2 * np.pi)


class K:
    pass


def mm(fw, ob, o_ap, lb, l_ap, rb, r_ap, start, stop):
    return fw.op("pe", lambda e: e.matmul(o_ap, l_ap, r_ap, start=start, stop=stop),
                 reads=[lb, rb], writes=[ob], accumulate=not start)


def tr(fw, ob, o_ap, ib, i_ap, ident, id_ap, first=True):
    return fw.op("pe", lambda e: e.transpose(o_ap, i_ap, id_ap), reads=[ib, ident], writes=[ob],
                 accumulate=not first)


def build(stop="all", n_layers=2, debug=False):
    nc = bass.Bass("TRN2", target_bir_lowering=False)
    fw = FW(nc)
    k = K()

    def din(name, shape, dt=F32):
        return nc.dram_tensor(name, shape, dt, kind="ExternalInput").ap()

    x_in = din("x", [T, D])
    c_in = din("c", [128, 8])
    pos_in = din("pos", [1, T], I32)
    w_mod = din("w_mod", [2, D, 6 * D])
    b_mod = din("b_mod", [2, 6 * D])
    g_mix = din("g_mix", [2, D])
    g_ffn = din("g_ffn", [2, D])
    mla_w_in = din("mla_w_in", [D, 640])
    mla_gq = din("mla_gq", [128, 2])
    mla_gkv = din("mla_gkv", [128, 2])
    mla_wq = din("mla_wq", [256, 2048])
    mla_wkv = din("mla_wkv", [256, 2048])
    mla_wo = din("mla_wo", [D, D])
    w_router = din("w_router", [2, D, 32])
    b_router = din("b_router", [2, 32])
    w_gu = din("w_gu", [2 * 32 * 1024, 2 * D])
    b_gu = din("b_gu", [64, 2 * D])
    w_dn = din("w_dn", [2 * 32 * 1024, D])
    b_dn = din("b_dn", [64, D])
    g_final = din("g_final", [1, D])
    ssm_w_in = din("ssm_w_in", [D, 6176])
    ssm_cw = din("ssm_cw", [128, 32, 4])
    ssm_cb = din("ssm_cb", [128, 32])
    ssm_hv = din("ssm_hv", [32, 2])
    ssm_dexp = din("ssm_dexp", [1, 2048])
    ssm_gn = din("ssm_gn", [1, 2048])
    ssm_wo = din("ssm_wo", [2048, D])
    cst = din("cst", [128, 1024])
    invf = din("invf", [64, 2])
    out_d = nc.dram_tensor("out", [T, D], F32, kind="ExternalOutput").ap()
    dbg_d = nc.dram_tensor("dbg", [T, D], F32, kind="ExternalOutput").ap() if debug else None

    def dscr(name, shape, dt=F32):
        return nc.dram_tensor(name, shape, dt, kind="Internal").ap()

    xs = dscr("xs", [T, D])
    qT_d = dscr("qT_d", [8, 192, T], BF16)
    kT_d = dscr("kT_d", [8, 128, T], BF16)
    krT_d = dscr("krT_d", [64, T], BF16)
    v_d = dscr("v_d", [8, T, 128], BF16)
    oT_d = dscr("oT_d", [8, 128, T], BF16)
    yacc = dscr("yacc", [T, D])
    h2_d = dscr("h2_d", [T, D])
    xsort_d = dscr("xsort_d", [4 * T + 64 * 256, D])
    ysort_d = dscr("ysort_d", [4 * T + 64 * 256, D])
    sxs_d = dscr("sxs_d", [T, 2048])
    szs_d = dscr("szs_d", [T, 2048])
    sy_d = dscr("sy_d", [T, 2048])
    sbc_d = dscr("sbc_d", [16, 128, T], BF16)
    sac_d = dscr("sac_d", [32, T])
    synT_d = dscr("synT_d", [16, 128, T], BF16)

    C_ = fw.sb("cst_s", [128, 1024])
    fw.dma("sp", lambda e: e.dma_start(out=C_[:], in_=cst), writes=[C_])
    ident = C_
    ID = C_[:, 0:128]
    ONES = C_[:, 128:256]
    TRI = C_[:, 256:384]
    SL = C_[:, 384:512]
    onesr = fw.sb("onesr", [128, 128], F32R)
    fw.op("act", lambda e: e.activation(out=onesr[:], in_=C_[:, 128:256], func=AF.Copy), reads=[C_], writes=[onesr])
    trib = fw.sb("trib", [128, 128], BF16)
    fw.op("act", lambda e: e.activation(out=trib[:], in_=C_[:, 256:384], func=AF.Copy), reads=[C_], writes=[trib])
    epsc = fw.sb("epsc", [128, 1])
    fw.op("dve", lambda e: e.memset(epsc[:], EPS), writes=[epsc])
    PS = [fw.ps(f"ps{i}", [128, 512]) for i in range(8)]

    cond = fw.sb("cond", [128, 8])
    fw.dma("sp", lambda e: e.dma_start(out=cond[:], in_=c_in), writes=[cond])
    sg = fw.sb("sg", [128, 8])
    fw.op("act", lambda e: e.activation(out=sg[:], in_=cond[:], func=AF.Sigmoid), reads=[cond], writes=[sg])
    fw.op("dve", lambda e: e.tensor_tensor(out=cond[:], in0=cond[:], in1=sg[:], op=ALU.mult), reads=[cond, sg], writes=[cond])
    condb = fw.sb("condb", [128, 8, 128])
    for c in range(8):
        fw.op("dve", lambda e, c=c: e.tensor_scalar(out=condb[:, c, :], in0=C_[:, 128:256], scalar1=cond[:, c:c + 1],
                                                    scalar2=None, op0=ALU.mult), reads=[C_, cond], writes=[condb])
    MOD = fw.sb("MOD", [128, 6, D])

    def compute_mod(L):
        mkm = fw.mark()
        wmt = [fw.sb(f"wmt{i}", [128, 8, 512]) for i in range(2)]
        bmt = [fw.sb(f"bmt{i}", [128, 512]) for i in range(2)]
        gbc = fw.sb("gbc", [128, D])
        for n in range(12):
            wt = wmt[n % 2]
            bt = bmt[n % 2]
            fw.dma("sp", lambda e: e.dma_start(out=wt[:], in_=w_mod[L, :, n * 512:(n + 1) * 512].rearrange("(c p) n -> p c n", p=128)), writes=[wt])
            fw.dma("sp", lambda e: e.dma_start(out=bt[:], in_=b_mod[L:L + 1, n * 512:(n + 1) * 512].broadcast_to([128, 512])), writes=[bt])
            p = PS[n % 2]
            for c in range(8):
                mm(fw, p, p[:], condb, condb[:, c, :], wt, wt[:, c, :], c == 0, c == 7)
            fw.op("dve", lambda e: e.tensor_tensor(out=MOD[:, n // 2, (n % 2) * 512:(n % 2 + 1) * 512], in0=p[:], in1=bt[:], op=ALU.add),
                  reads=[p, bt], writes=[MOD])
        for (slot, g) in ((1, g_mix), (4, g_ffn)):
            fw.dma("sp", lambda e: e.dma_start(out=gbc[:], in_=g[L:L + 1, :].broadcast_to([128, D])), writes=[gbc])
            fw.op("dve", lambda e: e.scalar_tensor_tensor(out=MOD[:, slot, :], in0=MOD[:, slot, :], scalar=1.0, in1=gbc[:],
                                                          op0=ALU.add, op1=ALU.mult), reads=[MOD, gbc], writes=[MOD])
        fw.barrier()
        fw.release(mkm)

    xt = [fw.sb(f"xt{i}", [128, D]) for i in range(2)]
    ht = [fw.sb(f"ht{i}", [128, D]) for i in range(2)]
    junk = fw.sb("junk", [128, D])
    st = [fw.sb(f"st{i}", [128, 4]) for i in range(2)]

    def norm_mod(xb, hb, sb_, a_slot, s_slot):
        fw.op("act", lambda e: e.activation(out=junk[:], in_=xb[:], func=AF.Square, accum_out=sb_[:, 0:1]), reads=[xb], writes=[junk, sb_])
        fw.op("act", lambda e: e.activation(out=sb_[:, 1:2], in_=sb_[:, 0:1], func=AF.Sqrt, bias=epsc[:], scale=1.0 / D), reads=[sb_, epsc], writes=[sb_])
        fw.op("dve", lambda e: e.reciprocal(out=sb_[:, 2:3], in_=sb_[:, 1:2]), reads=[sb_], writes=[sb_])
        fw.op("dve", lambda e: e.scalar_tensor_tensor(out=hb[:], in0=xb[:], scalar=sb_[:, 2:3], in1=MOD[:, a_slot, :], op0=ALU.mult, op1=ALU.mult),
              reads=[xb, sb_, MOD], writes=[hb])
        if s_slot is not None:
            fw.op("dve", lambda e: e.tensor_tensor(out=hb[:], in0=hb[:], in1=MOD[:, s_slot, :], op=ALU.add), reads=[hb, MOD], writes=[hb])

    def transpose_to(hb, dstb, dst_fn, pa, pb):
        for half, p in ((0, pa), (1, pb)):
            for c in range(4):
                cc = half * 4 + c
                tr(fw, p, p[:, c * 128:(c + 1) * 128], hb, hb[:, cc * 128:(cc + 1) * 128], C_, ID, first=(c == 0))
            fw.op("act", lambda e, half=half, p=p: e.activation(out=dst_fn(half), in_=p[:].rearrange("p (c t) -> p c t", c=4), func=AF.Copy),
                  reads=[p], writes=[dstb])

    k.__dict__.update(locals())
    return k


def mla_layer(k, L, src_first):
    fw = k.fw; nc = k.nc; PS = k.PS; C_ = k.C_; MOD = k.MOD
    x_src = k.x_in if src_first else k.xs
    mk0 = fw.mark()
    w_in_s = fw.sb("mla_w_in_s", [128, 8, 640], F32R)
    for h2 in range(2):
        fw.dma("pool", lambda e: e.dma_start(out=w_in_s[:, h2 * 4:(h2 + 1) * 4, :], in_=k.mla_w_in[h2 * 512:(h2 + 1) * 512, :].rearrange("(c p) n -> p c n", p=128)), writes=[w_in_s])
    wq_s = fw.sb("wq_s", [128, 2, 2048], F32R)
    wkv_s = fw.sb("wkv_s", [128, 2, 2048], F32R)
    for c in range(2):
        fw.dma("pool", lambda e: e.dma_start(out=wq_s[:, c, :], in_=k.mla_wq[c * 128:(c + 1) * 128, :]), writes=[wq_s])
        fw.dma("pool", lambda e: e.dma_start(out=wkv_s[:, c, :], in_=k.mla_wkv[c * 128:(c + 1) * 128, :]), writes=[wkv_s])
    gq = fw.sb("gq", [128, 2]); gkv = fw.sb("gkv", [128, 2]); invf = fw.sb("invf_s", [64, 2])
    fw.dma("sp", lambda e: e.dma_start(out=gq[:], in_=k.mla_gq), writes=[gq])
    fw.dma("sp", lambda e: e.dma_start(out=gkv[:], in_=k.mla_gkv), writes=[gkv])
    fw.dma("sp", lambda e: e.dma_start(out=invf[:], in_=k.invf), writes=[invf])
    hT = fw.sb("hT", [128, 8, 512], F32R)
    sq = fw.sb("sq", [128, 2, 512], F32R)
    rstd = fw.sb("rstd", [128, 512])
    latn = fw.sb("latn", [128, 4, 512], F32R)
    posi = fw.sb("posi", [64, 512], I32)
    ang = fw.sb("ang", [64, 512]); kf = fw.sb("kf", [64, 512]); ki = fw.sb("ki", [64, 512], I32)
    rr = fw.sb("rr", [64, 512]); mwrap = fw.sb("mwrap", [64, 512])
    Ct = fw.sb("Ct", [64, 512]); St = fw.sb("St", [64, 512])
    t1 = fw.sb("t1", [64, 512]); t2 = fw.sb("t2", [64, 512])
    stq = [fw.sb(f"stq{i}", [128, 512], BF16) for i in range(2)]
    str_ = [fw.sb(f"str{i}", [64, 512], BF16) for i in range(2)]
    stv = [fw.sb(f"stv{i}", [128, 1024], BF16) for i in range(2)]

    def wrap_sin(dst, src, shift):
        fw.op("dve", lambda e: e.tensor_scalar(out=kf[:], in0=src[:], scalar1=shift, scalar2=1.0 / TWO_PI, op0=ALU.add, op1=ALU.mult), reads=[src], writes=[kf])
        fw.op("dve", lambda e: e.tensor_copy(out=ki[:], in_=kf[:]), reads=[kf], writes=[ki])
        fw.op("dve", lambda e: e.tensor_copy(out=kf[:], in_=ki[:]), reads=[ki], writes=[kf])
        fw.op("dve", lambda e: e.scalar_tensor_tensor(out=rr[:], in0=kf[:], scalar=-TWO_PI, in1=src[:], op0=ALU.mult, op1=ALU.add), reads=[kf, src], writes=[rr])
        if shift != 0.0:
            fw.op("dve", lambda e: e.tensor_scalar(out=rr[:], in0=rr[:], scalar1=shift, scalar2=None, op0=ALU.add), reads=[rr], writes=[rr])
        fw.op("dve", lambda e: e.tensor_scalar(out=mwrap[:], in0=rr[:], scalar1=PI, scalar2=-TWO_PI, op0=ALU.is_gt, op1=ALU.mult), reads=[rr], writes=[mwrap])
        fw.op("dve", lambda e: e.tensor_tensor(out=rr[:], in0=rr[:], in1=mwrap[:], op=ALU.add), reads=[rr, mwrap], writes=[rr])
        fw.op("dve", lambda e: e.tensor_scalar(out=mwrap[:], in0=rr[:], scalar1=-PI, scalar2=TWO_PI, op0=ALU.is_lt, op1=ALU.mult), reads=[rr], writes=[mwrap])
        fw.op("dve", lambda e: e.tensor_tensor(out=rr[:], in0=rr[:], in1=mwrap[:], op=ALU.add), reads=[rr, mwrap], writes=[rr])
        fw.op("dve", lambda e: e.tensor_scalar(out=rr[:], in0=rr[:], scalar1=PI, scalar2=-PI, op0=ALU.min, op1=ALU.max), reads=[rr], writes=[rr])
        fw.op("act", lambda e: e.activation(out=dst[:], in_=rr[:], func=AF.Sin), reads=[rr], writes=[dst])

    for j in range(T // 512):
        t0 = j * 512
        for r in range(4):
            xb = k.xt[r % 2]; hb = k.ht[r % 2]; sb_ = k.st[r % 2]
            fw.dma("sp", lambda e: e.dma_start(out=xb[:], in_=x_src[t0 + r * 128:t0 + (r + 1) * 128, :]), writes=[xb])
            if src_first:
                fw.dma("sp", lambda e: e.dma_start(out=k.xs[t0 + r * 128:t0 + (r + 1) * 128, :], in_=xb[:]), reads=[xb])
            k.norm_mod(xb, hb, sb_, 1, 0)
            k.transpose_to(hb, hT, lambda half, r=r: hT[:, half * 4:(half + 1) * 4, r * 128:(r + 1) * 128], PS[0], PS[1])
        fw.dma("sp", lambda e: e.dma_start(out=posi[:], in_=k.pos_in[0:1, t0:t0 + 512].broadcast_to([64, 512])), writes=[posi])
        fw.op("dve", lambda e: e.tensor_copy(out=ang[:], in_=posi[:]), reads=[posi], writes=[ang])
        fw.op("dve", lambda e: e.tensor_scalar(out=ang[:], in0=ang[:], scalar1=invf[:, 0:1], scalar2=None, op0=ALU.mult), reads=[ang, invf], writes=[ang])
        wrap_sin(St, ang, 0.0)
        wrap_sin(Ct, ang, PI / 2)
        fw.op("dve", lambda e: e.tensor_scalar(out=St[:], in0=St[:], scalar1=invf[:, 1:2], scalar2=None, op0=ALU.mult), reads=[St, invf], writes=[St])
        for oc in range(4):
            p = PS[2 + oc]
            for c in range(8):
                mm(fw, p, p[:], w_in_s, w_in_s[:, c, oc * 128:(oc + 1) * 128], hT, hT[:, c, :], c == 0, c == 7)
        for oc in range(2):
            p = PS[6 + oc]
            for c in range(8):
                mm(fw, p, p[0:64, :], w_in_s, w_in_s[:, c, 512 + oc * 64:512 + (oc + 1) * 64], hT, hT[:, c, :], c == 0, c == 7)
        for grp, gvec in ((0, gq), (1, gkv)):
            for c2 in range(2):
                p = PS[2 + grp * 2 + c2]
                fw.op("act", lambda e, p=p, c2=c2: e.activation(out=sq[:, c2, :], in_=p[:], func=AF.Square), reads=[p], writes=[sq])
            for c2 in range(2):
                mm(fw, PS[0], PS[0][:], k.onesr, k.onesr[:], sq, sq[:, c2, :], c2 == 0, c2 == 1)
            fw.op("act", lambda e: e.activation(out=rstd[:], in_=PS[0][:], func=AF.Sqrt, bias=k.epsc[:], scale=1.0 / 256), reads=[PS[0], k.epsc], writes=[rstd])
            fw.op("dve", lambda e: e.reciprocal(out=rstd[:], in_=rstd[:]), reads=[rstd], writes=[rstd])
            for c2 in range(2):
                p = PS[2 + grp * 2 + c2]
                fw.op("dve", lambda e, p=p, c2=c2, grp=grp, gvec=gvec: e.scalar_tensor_tensor(out=latn[:, grp * 2 + c2, :], in0=p[:], scalar=gvec[:, c2:c2 + 1], in1=rstd[:],
                                                                                     op0=ALU.mult, op1=ALU.mult), reads=[p, gvec, rstd], writes=[latn])
        sr = str_[0]
        fw.op("dve", lambda e: e.tensor_tensor(out=t1[:], in0=PS[6][0:64, :], in1=Ct[:], op=ALU.mult), reads=[PS[6], Ct], writes=[t1])
        fw.op("dve", lambda e: e.tensor_tensor(out=t2[:], in0=PS[7][0:64, :], in1=St[:], op=ALU.mult), reads=[PS[7], St], writes=[t2])
        fw.op("dve", lambda e: e.tensor_tensor(out=sr[:], in0=t1[:], in1=t2[:], op=ALU.add), reads=[t1, t2], writes=[sr])
        fw.dma("sp", lambda e: e.dma_start(out=k.krT_d[:, t0:t0 + 512], in_=sr[:]), reads=[sr])
        for h in range(8):
            pq = PS[2 + (h % 2) * 3]; pr = PS[3 + (h % 2) * 3]; prs = PS[4 + (h % 2) * 3]
            for c in range(2):
                mm(fw, pq, pq[:], wq_s, wq_s[:, c, h * 192:h * 192 + 128], latn, latn[:, c, :], c == 0, c == 1)
            for c in range(2):
                mm(fw, pr, pr[0:64, :], wq_s, wq_s[:, c, h * 192 + 128:h * 192 + 192], latn, latn[:, c, :], c == 0, c == 1)
            for c in range(2):
                mm(fw, prs, prs[0:64, :], wq_s, wq_s[:, c, 1536 + h * 64:1536 + (h + 1) * 64], latn, latn[:, c, :], c == 0, c == 1)
            sq_ = stq[h % 2]; sr = str_[(h + 1) % 2]
            fw.op("act", lambda e, pq=pq, sq_=sq_: e.activation(out=sq_[:], in_=pq[:], func=AF.Copy, scale=MLA_SCALE), reads=[pq], writes=[sq_])
            fw.dma("sp", lambda e, sq_=sq_, h=h: e.dma_start(out=k.qT_d[h, 0:128, t0:t0 + 512], in_=sq_[:]), reads=[sq_])
            fw.op("dve", lambda e, pr=pr: e.scalar_tensor_tensor(out=t1[:], in0=pr[0:64, :], scalar=MLA_SCALE, in1=Ct[:], op0=ALU.mult, op1=ALU.mult), reads=[pr, Ct], writes=[t1])
            fw.op("dve", lambda e, prs=prs: e.scalar_tensor_tensor(out=t2[:], in0=prs[0:64, :], scalar=MLA_SCALE, in1=St[:], op0=ALU.mult, op1=ALU.mult), reads=[prs, St], writes=[t2])
            fw.op("dve", lambda e, sr=sr: e.tensor_tensor(out=sr[:], in0=t1[:], in1=t2[:], op=ALU.add), reads=[t1, t2], writes=[sr])
            fw.dma("sp", lambda e, sr=sr, h=h: e.dma_start(out=k.qT_d[h, 128:192, t0:t0 + 512], in_=sr[:]), reads=[sr])
        for h in range(8):
            pk = PS[2 + (h % 2)]
            for c in range(2):
                mm(fw, pk, pk[:], wkv_s, wkv_s[:, c, h * 128:(h + 1) * 128], latn, latn[:, 2 + c, :], c == 0, c == 1)
            sk = stq[h % 2]
            fw.op("act", lambda e, pk=pk, sk=sk: e.activation(out=sk[:], in_=pk[:], func=AF.Copy), reads=[pk], writes=[sk])
            fw.dma("sp", lambda e, sk=sk, h=h: e.dma_start(out=k.kT_d[h, :, t0:t0 + 512], in_=sk[:]), reads=[sk])
        for r in range(4):
            sv = stv[r % 2]
            for half in range(2):
                p = PS[4 + half]
                for c in range(2):
                    mm(fw, p, p[:], latn, latn[:, 2 + c, r * 128:(r + 1) * 128], wkv_s, wkv_s[:, c, 1024 + half * 512:1024 + (half + 1) * 512], c == 0, c == 1)
                fw.op("act", lambda e, p=p, sv=sv, half=half: e.activation(out=sv[:, half * 512:(half + 1) * 512], in_=p[:], func=AF.Copy), reads=[p], writes=[sv])
            fw.dma("sp", lambda e, sv=sv, r=r: e.dma_start(out=k.v_d[:, t0 + r * 128:t0 + (r + 1) * 128, :].rearrange("h t d -> t h d"),
                                                            in_=sv[:].rearrange("t (h d) -> t h d", h=8)), reads=[sv])
    fw.barrier()
    fw.release(mk0)
    if k.stop == "A":
        return
    krT = fw.sb("krT", [64, T], BF16)
    fw.dma("sp", lambda e: e.dma_start(out=krT[:], in_=k.krT_d), writes=[krT])
    kTs = [fw.sb(f"kTs{i}", [128, T], BF16) for i in range(2)]
    vs = [fw.sb(f"vs{i}", [128, 32, 132], BF16) for i in range(2)]
    for i in range(2):
        fw.op("dve", lambda e, i=i: e.memset(vs[i][:, :, 128:129], 1.0), writes=[vs[i]])
    qn = [fw.sb(f"qn{i}", [128, 512], BF16) for i in range(2)]
    qr = [fw.sb(f"qr{i}", [64, 512], BF16) for i in range(2)]
    pT = [fw.sb(f"pT{i}", [128, 512], BF16) for i in range(3)]
    rs = fw.sb("rs", [128, 4])
    on = [fw.sb(f"on{i}", [128, 128]) for i in range(2)]
    oT = [fw.sb(f"oT{i}", [128, 512], BF16) for i in range(2)]
    blk = 0
    for h in range(8):
        kb = kTs[h % 2]; vb = vs[h % 2]
        fw.dma("sp", lambda e: e.dma_start(out=kb[:], in_=k.kT_d[h]), writes=[kb])
        for q4 in range(4):
            fw.dma("sp", lambda e, q4=q4: e.dma_start(out=vb[:, q4 * 8:(q4 + 1) * 8, 0:128],
                                                      in_=k.v_d[h, q4 * 1024:(q4 + 1) * 1024, :].rearrange("(c p) d -> p c d", p=128)), writes=[vb])
        for jq in range(8):
            qnb = qn[jq % 2]; qrb = qr[jq % 2]
            fw.dma("sp", lambda e: e.dma_start(out=qnb[:], in_=k.qT_d[h, 0:128, jq * 512:(jq + 1) * 512]), writes=[qnb])
            fw.dma("sp", lambda e: e.dma_start(out=qrb[:], in_=k.qT_d[h, 128:192, jq * 512:(jq + 1) * 512]), writes=[qrb])
            nk = 4 * jq + 4
            for kc in range(nk):
                r = max(0, kc - 4 * jq)
                q0 = r * 128
                sp_ = PS[blk % 3]; pb = pT[blk % 3]; blk += 1
                mm(fw, sp_, sp_[:, q0:512], kb, kb[:, kc * 128:(kc + 1) * 128], qnb, qnb[:, q0:512], True, False)
                mm(fw, sp_, sp_[:, q0:512], krT, krT[:, kc * 128:(kc + 1) * 128], qrb, qrb[:, q0:512], False, True)
                fw.op("act", lambda e, sp_=sp_, pb=pb, q0=q0: e.activation(out=pb[:, q0:512], in_=sp_[:, q0:512], func=AF.Exp), reads=[sp_], writes=[pb])
                if kc >= 4 * jq:
                    fw.op("dve", lambda e, pb=pb, q0=q0: e.tensor_tensor(out=pb[:, q0:q0 + 128], in0=pb[:, q0:q0 + 128], in1=k.trib[:], op=ALU.mult),
                          reads=[pb, k.trib], writes=[pb])
                for s_ in range(r, 4):
                    acc = PS[3 + s_]
                    mm(fw, acc, acc[:, 0:129], pb, pb[:, s_ * 128:(s_ + 1) * 128], vb, vb[:, kc, 0:129], kc == 0, kc == 4 * jq + s_)
            ob = oT[jq % 2]
            for s_ in range(4):
                acc = PS[3 + s_]; onb = on[s_ % 2]
                fw.op("dve", lambda e, acc=acc, s_=s_: e.reciprocal(out=rs[:, s_:s_ + 1], in_=acc[:, 128:129]), reads=[acc], writes=[rs])
                fw.op("act", lambda e, acc=acc, onb=onb, s_=s_: e.activation(out=onb[:], in_=acc[:, 0:128], func=AF.Copy, scale=rs[:, s_:s_ + 1]), reads=[acc, rs], writes=[onb])
                tr(fw, PS[7], PS[7][:, s_ * 128:(s_ + 1) * 128], onb, onb[:], C_, k.ID, first=(s_ == 0))
            fw.op("act", lambda e, ob=ob: e.activation(out=ob[:], in_=PS[7][:], func=AF.Copy), reads=[PS[7]], writes=[ob])
            fw.dma("sp", lambda e, ob=ob: e.dma_start(out=k.oT_d[h, :, jq * 512:(jq + 1) * 512], in_=ob[:]), reads=[ob])
    fw.barrier()
    fw.release(mk0)
    if k.stop == "B":
        return


def mixer_out_and_moe_router(k, L, w_out_d, n_kc, oT_src):
    fw = k.fw; PS = k.PS; C_ = k.C_; MOD = k.MOD
    mk0 = fw.mark()
    wo = fw.sb(f"wo_{L}", [128, n_kc, D], BF16)
    for c in range(n_kc):
        fw.dma("pool", lambda e, c=c: e.dma_start(out=wo[:, c, :], in_=w_out_d[c * 128:(c + 1) * 128, :]), writes=[wo])
    oTt = [fw.sb(f"oTt{L}_{i}", [128, n_kc, 512], BF16) for i in range(2)]
    ytmp = fw.sb(f"ytmp{L}", [128, D])
    for j in range(T // 512):
        ob = oTt[j % 2]
        fw.dma("sp", lambda e: e.dma_start(out=ob[:], in_=oT_src[:, :, j * 512:(j + 1) * 512].rearrange("h p t -> p h t")), writes=[ob])
        for r in range(4):
            t0 = j * 512 + r * 128
            xb = k.xt[r % 2]
            fw.dma("sp", lambda e: e.dma_start(out=xb[:], in_=k.xs[t0:t0 + 128, :]), writes=[xb])
            for half in range(2):
                p = PS[half]
                for c in range(n_kc):
                    mm(fw, p, p[:], ob, ob[:, c, r * 128:(r + 1) * 128], wo, wo[:, c, half * 512:(half + 1) * 512], c == 0, c == n_kc - 1)
                fw.op("dve", lambda e, p=p, half=half: e.tensor_tensor(out=ytmp[:, half * 512:(half + 1) * 512], in0=p[:], in1=MOD[:, 2, half * 512:(half + 1) * 512], op=ALU.mult),
                      reads=[p, MOD], writes=[ytmp])
            fw.op("dve", lambda e: e.tensor_tensor(out=xb[:], in0=xb[:], in1=ytmp[:], op=ALU.add), reads=[xb, ytmp], writes=[xb])
            fw.dma("sp", lambda e: e.dma_start(out=k.xs[t0:t0 + 128, :], in_=xb[:]), reads=[xb])
    fw.barrier()
    fw.release(mk0)


def moe_layer(k, L):
    fw = k.fw; PS = k.PS; C_ = k.C_; MOD = k.MOD; nc = k.nc
    mk0 = fw.mark()
    yacc = k.yacc
    h2T = fw.sb("h2T", [128, 8, T], BF16)
    G_all = fw.sb("G_all", [128, NT, 32])
    wr = fw.sb("wr", [128, 8, 32])
    fw.dma("sp", lambda e: e.dma_start(out=wr[:], in_=k.w_router[L].rearrange("(c p) n -> p c n", p=128)), writes=[wr])
    brb = fw.sb("brb", [128, 32])
    fw.dma("sp", lambda e: e.dma_start(out=brb[:], in_=k.b_router[L:L + 1, :].broadcast_to([128, 32])), writes=[brb])
    h2f = fw.sb("h2f", [128, 8, 128])
    lg = fw.sb("lg", [128, 32]); m8 = fw.sb("m8", [128, 8]); msk = fw.sb("msk", [128, 32]); ex = fw.sb("ex", [128, 32])
    sm = fw.sb("sm", [128, 4])
    MS = ""
    for i in range(NT):
        xb = k.xt[i % 2]; hb = k.ht[i % 2]; sb_ = k.st[i % 2]
        fw.dma("sp", lambda e: e.dma_start(out=xb[:], in_=k.xs[i * 128:(i + 1) * 128, :]), writes=[xb])
        if MS == "DL":
            continue
        k.norm_mod(xb, hb, sb_, 4, 3)
        if MS == "D0":
            continue
        for half, p in ((0, PS[0]), (1, PS[1])):
            for c in range(4):
                cc = half * 4 + c
                tr(fw, p, p[:, c * 128:(c + 1) * 128], hb, hb[:, cc * 128:(cc + 1) * 128], C_, k.ID, first=(c == 0))
            fw.op("act", lambda e, half=half, p=p: e.activation(out=h2f[:, half * 4:(half + 1) * 4, :], in_=p[:].rearrange("p (c t) -> p c t", c=4), func=AF.Copy),
                  reads=[p], writes=[h2f])
        fw.op("dve", lambda e: e.tensor_copy(out=h2T[:, :, i * 128:(i + 1) * 128], in_=h2f[:]), reads=[h2f], writes=[h2T])
        if MS == "D1":
            continue
        pl = PS[2]
        for c in range(8):
            mm(fw, pl, pl[:, 0:32], h2f, h2f[:, c, :], wr, wr[:, c, :], c == 0, c == 7)
        fw.op("dve", lambda e: e.tensor_tensor(out=lg[:], in0=pl[:, 0:32], in1=brb[:], op=ALU.add), reads=[pl, brb], writes=[lg])
        if MS == "D2":
            continue
        fw.op("dve", lambda e: e.max(out=m8[:], in_=lg[:]), reads=[lg], writes=[m8])
        fw.op("dve", lambda e: e.tensor_scalar(out=msk[:], in0=lg[:], scalar1=m8[:, 3:4], scalar2=None, op0=ALU.is_ge), reads=[lg, m8], writes=[msk])
        fw.op("dve", lambda e: e.tensor_scalar(out=sm[:, 0:1], in0=m8[:, 0:1], scalar1=-1.0, scalar2=None, op0=ALU.mult), reads=[m8], writes=[sm])
        fw.op("act", lambda e: e.activation(out=ex[:], in_=lg[:], func=AF.Exp, bias=sm[:, 0:1], scale=1.0), reads=[lg, sm], writes=[ex])
        fw.op("dve", lambda e: e.tensor_tensor(out=ex[:], in0=ex[:], in1=msk[:], op=ALU.mult), reads=[ex, msk], writes=[ex])
        fw.op("dve", lambda e: e.reduce_sum(out=sm[:, 1:2], in_=ex[:], axis=AX.X), reads=[ex], writes=[sm])
        fw.op("dve", lambda e: e.reciprocal(out=sm[:, 2:3], in_=sm[:, 1:2]), reads=[sm], writes=[sm])
        fw.op("dve", lambda e: e.tensor_scalar(out=G_all[:, i, :], in0=ex[:], scalar1=sm[:, 2:3], scalar2=None, op0=ALU.mult), reads=[ex, sm], writes=[G_all])
    fw.barrier()
    if MS:
        fw.release(mk0)
        return
    wgu = fw.sb("wgu", [128, 8, 2048], BF16)
    wdn = fw.sb("wdn", [128, 8, 1024], BF16)
    bgu = fw.sb("bgu", [128, 16])
    bdb = fw.sb("bdb", [128, 1024])
    hact = fw.sb("hact", [128, 8, 512], BF16)
    g_ = fw.sb("g_", [128, 512]); sg_ = fw.sb("sg_", [128, 512]); l_ = fw.sb("l_", [128, 512])
    yo = [fw.sb(f"yo{i}", [128, 1024]) for i in range(2)]
    for ex_i in range(32):
        for c in range(8):
            fw.dma("pool", lambda e, c=c: e.dma_start(out=wgu[:, c, :], in_=k.w_gu[L, ex_i, c * 128:(c + 1) * 128, :]), writes=[wgu])
        for c in range(8):
            fw.dma("pool", lambda e, c=c: e.dma_start(out=wdn[:, c, :], in_=k.w_dn[L, ex_i, c * 128:(c + 1) * 128, :]), writes=[wdn])
        with nc.allow_non_contiguous_dma(reason="small bias relayout"):
            fw.dma("sp", lambda e: e.dma_start(out=bgu[:], in_=k.b_gu[L, ex_i, :].rearrange("(c p) -> p c", p=128)), writes=[bgu])
        fw.dma("sp", lambda e: e.dma_start(out=bdb[:], in_=k.b_dn[L, ex_i:ex_i + 1, :].broadcast_to([128, 1024])), writes=[bdb])
        for jt in range(T // 512):
            for fc in range(8):
                pg = PS[(fc % 2) * 2]; pl2 = PS[(fc % 2) * 2 + 1]
                for c in range(8):
                    mm(fw, pg, pg[:], wgu, wgu[:, c, fc * 128:(fc + 1) * 128], h2T, h2T[:, c, jt * 512:(jt + 1) * 512], c == 0, c == 7)
                for c in range(8):
                    mm(fw, pl2, pl2[:], wgu, wgu[:, c, 1024 + fc * 128:1024 + (fc + 1) * 128], h2T, h2T[:, c, jt * 512:(jt + 1) * 512], c == 0, c == 7)
                fw.op("dve", lambda e, pg=pg, fc=fc: e.tensor_scalar(out=g_[:], in0=pg[:], scalar1=bgu[:, fc:fc + 1], scalar2=7.0, op0=ALU.add, op1=ALU.min), reads=[pg, bgu], writes=[g_])
                fw.op("act", lambda e: e.activation(out=sg_[:], in_=g_[:], func=AF.Sigmoid, scale=1.702), reads=[g_], writes=[sg_])
                fw.op("dve", lambda e, pl2=pl2, fc=fc: e.tensor_scalar(out=l_[:], in0=pl2[:], scalar1=bgu[:, 8 + fc:9 + fc], scalar2=7.0, op0=ALU.add, op1=ALU.min), reads=[pl2, bgu], writes=[l_])
                fw.op("dve", lambda e: e.tensor_scalar(out=l_[:], in0=l_[:], scalar1=-7.0, scalar2=1.0, op0=ALU.max, op1=ALU.add), reads=[l_], writes=[l_])
                fw.op("dve", lambda e: e.tensor_tensor(out=g_[:], in0=g_[:], in1=sg_[:], op=ALU.mult), reads=[g_, sg_], writes=[g_])
                fw.op("dve", lambda e, fc=fc: e.tensor_tensor(out=hact[:, fc, :], in0=g_[:], in1=l_[:], op=ALU.mult), reads=[g_, l_], writes=[hact])
            for r in range(4):
                ti = jt * 4 + r
                yb = yo[r % 2]
                for half in range(2):
                    p = PS[4 + half]
                    for fc in range(8):
                        mm(fw, p, p[:], hact, hact[:, fc, r * 128:(r + 1) * 128], wdn, wdn[:, fc, half * 512:(half + 1) * 512], fc == 0, fc == 7)
                    fw.op("dve", lambda e, p=p, half=half, yb=yb: e.tensor_tensor(out=yb[:, half * 512:(half + 1) * 512], in0=p[:], in1=bdb[:, half * 512:(half + 1) * 512], op=ALU.add),
                          reads=[p, bdb], writes=[yb])
                fw.op("dve", lambda e, yb=yb, ti=ti: e.tensor_scalar(out=yb[:], in0=yb[:], scalar1=G_all[:, ti, ex_i:ex_i + 1], scalar2=None, op0=ALU.mult), reads=[yb, G_all], writes=[yb])
                if ex_i == 0:
                    fw.dma("sp", lambda e, yb=yb, ti=ti: e.dma_start(out=yacc[ti * 128:(ti + 1) * 128, :], in_=yb[:]), reads=[yb])
                else:
                    fw.dma("pool", lambda e, yb=yb, ti=ti: e.dma_start(out=yacc[ti * 128:(ti + 1) * 128, :], in_=yb[:], accum_op=ALU.add), reads=[yb])
        fw.barrier()
    for i in range(NT):
        xb = k.xt[i % 2]; yb = yo[i % 2]
        fw.dma("sp", lambda e: e.dma_start(out=xb[:], in_=k.xs[i * 128:(i + 1) * 128, :]), writes=[xb])
        fw.dma("sp", lambda e: e.dma_start(out=yb[:], in_=yacc[i * 128:(i + 1) * 128, :]), writes=[yb])
        fw.op("dve", lambda e: e.tensor_tensor(out=yb[:], in0=yb[:], in1=MOD[:, 5, :], op=ALU.mult), reads=[yb, MOD], writes=[yb])
        fw.op("dve", lambda e: e.tensor_tensor(out=xb[:], in0=xb[:], in1=yb[:], op=ALU.add), reads=[xb, yb], writes=[xb])
        fw.dma("sp", lambda e: e.dma_start(out=k.xs[i * 128:(i + 1) * 128, :], in_=xb[:]), reads=[xb])
    fw.barrier()
    fw.release(mk0)


def final_norm(k):
    fw = k.fw
    gfb = fw.sb("gfb", [128, D])
    fw.dma("sp", lambda e: e.dma_start(out=gfb[:], in_=k.g_final[0:1, :].broadcast_to([128, D])), writes=[gfb])
    fw.op("dve", lambda e: e.tensor_copy(out=k.MOD[:, 1, :], in_=gfb[:]), reads=[gfb], writes=[k.MOD])
    for i in range(NT):
        xb = k.xt[i % 2]; hb = k.ht[i % 2]; sb_ = k.st[i % 2]
        fw.dma("sp", lambda e: e.dma_start(out=xb[:], in_=k.xs[i * 128:(i + 1) * 128, :]), writes=[xb])
        k.norm_mod(xb, hb, sb_, 1, None)
        fw.dma("sp", lambda e: e.dma_start(out=k.out_d[i * 128:(i + 1) * 128, :], in_=hb[:]), reads=[hb])


def ssd_layer(k, L):
    fw = k.fw; nc = k.nc; PS = k.PS; C_ = k.C_; MOD = k.MOD
    mk0 = fw.mark()
    w_in = k.ssm_w_in
    dt_tm = fw.sb("dt_tm", [128, NT, 32]); nac_tm = fw.sb("nac_tm", [128, NT, 32])
    mk_a = fw.mark()
    dtT = fw.sb("dtT", [32, T]); adtT = fw.sb("adtT", [32, T])
    mkA = fw.mark()
    hT = fw.sb("s_hT", [128, 8, 512], F32R)
    wsl = [fw.sb(f"wsl{i}", [128, 8, 512], F32R) for i in range(2)]
    wdt = fw.sb("wdt", [128, 8, 32], F32R)
    fw.dma("pool", lambda e: e.dma_start(out=wdt[:], in_=w_in[:, 6144:6176].rearrange("(c p) n -> p c n", p=128)), writes=[wdt])
    cw = fw.sb("cw", [128, 32, 4]); cb = fw.sb("cb", [128, 32])
    fw.dma("sp", lambda e: e.dma_start(out=cw[:], in_=k.ssm_cw), writes=[cw])
    fw.dma("sp", lambda e: e.dma_start(out=cb[:], in_=k.ssm_cb), writes=[cb])
    hv = fw.sb("hv", [32, 4])
    fw.dma("sp", lambda e: e.dma_start(out=hv[:, 0:2], in_=k.ssm_hv), writes=[hv])
    fw.op("act", lambda e: e.activation(out=hv[:, 2:3], in_=hv[:, 1:2], func=AF.Exp), reads=[hv], writes=[hv])
    fw.op("dve", lambda e: e.tensor_scalar(out=hv[:, 3:4], in0=hv[:, 2:3], scalar1=-1.0, scalar2=None, op0=ALU.mult), reads=[hv], writes=[hv])
    halo = fw.sb("halo", [128, 32, 4])
    fw.op("dve", lambda e: e.memset(halo[:], 0.0), writes=[halo])
    ub = [fw.sb(f"ub{i}", [128, 516]) for i in range(2)]
    acc = [fw.sb(f"cacc{i}", [128, 512]) for i in range(2)]
    xact = [fw.sb(f"xact{i}", [128, 512]) for i in range(2)]
    bcst = [fw.sb(f"bcst{i}", [128, 512], BF16) for i in range(2)]
    xtm = [fw.sb(f"xtm{i}", [128, 4, 512]) for i in range(2)]
    zst = [fw.sb(f"zst{i}", [128, 512]) for i in range(2)]
    et = fw.sb("et", [32, 512])
    nslab = 0
    for j in range(T // 512):
        t0 = j * 512
        for r in range(4):
            xb = k.xt[r % 2]; hb = k.ht[r % 2]; sb_ = k.st[r % 2]
            fw.dma("sp", lambda e: e.dma_start(out=xb[:], in_=k.xs[t0 + r * 128:t0 + (r + 1) * 128, :]), writes=[xb])
            k.norm_mod(xb, hb, sb_, 1, 0)
            k.transpose_to(hb, hT, lambda half, r=r: hT[:, half * 4:(half + 1) * 4, r * 128:(r + 1) * 128], PS[0], PS[1])
        pd = PS[2]
        for c in range(8):
            mm(fw, pd, pd[0:32, :], wdt, wdt[:, c, :], hT, hT[:, c, :], c == 0, c == 7)
        fw.op("act", lambda e: e.activation(out=et[:], in_=pd[0:32, :], func=AF.Exp, bias=hv[:, 0:1], scale=1.0), reads=[pd, hv], writes=[et])
        fw.op("act", lambda e: e.activation(out=dtT[:, t0:t0 + 512], in_=et[:], func=AF.Ln, bias=1.0, scale=1.0), reads=[et], writes=[dtT])
        fw.op("dve", lambda e: e.tensor_scalar(out=adtT[:, t0:t0 + 512], in0=dtT[:, t0:t0 + 512], scalar1=hv[:, 3:4], scalar2=None, op0=ALU.mult), reads=[dtT, hv], writes=[adtT])
        for sl in range(8):
            wb = wsl[nslab % 2]; nslab += 1
            for hf in range(2):
                fw.dma("pool", lambda e, hf=hf: e.dma_start(out=wb[:, hf * 4:(hf + 1) * 4, :],
                                                             in_=w_in[hf * 512:(hf + 1) * 512, 2048 + sl * 512:2048 + (sl + 1) * 512].rearrange("(c p) n -> p c n", p=128)), writes=[wb])
            for c4 in range(4):
                cc = sl * 4 + c4
                p = PS[3 + (cc % 2)]
                for c in range(8):
                    mm(fw, p, p[:], wb, wb[:, c, c4 * 128:(c4 + 1) * 128], hT, hT[:, c, :], c == 0, c == 7)
                u = ub[cc % 2]; a_ = acc[cc % 2]; xa = xact[cc % 2]
                fw.op("act", lambda e, p=p, u=u: e.activation(out=u[:, 3:515], in_=p[:], func=AF.Copy), reads=[p], writes=[u])
                fw.op("dve", lambda e, u=u, cc=cc: e.tensor_copy(out=u[:, 0:3], in_=halo[:, cc, 0:3]), reads=[halo], writes=[u])
                fw.op("dve", lambda e, u=u, cc=cc: e.tensor_copy(out=halo[:, cc, 0:3], in_=u[:, 512:515]), reads=[u], writes=[halo])
                fw.op("act", lambda e, u=u, a_=a_, cc=cc: e.activation(out=a_[:], in_=u[:, 3:515], func=AF.Identity, scale=cw[:, cc, 3:4], bias=cb[:, cc:cc + 1]),
                      reads=[u, cw, cb], writes=[a_])
                for kk in range(3):
                    fw.op("dve", lambda e, u=u, a_=a_, cc=cc, kk=kk: e.scalar_tensor_tensor(out=a_[:], in0=u[:, kk:kk + 512], scalar=cw[:, cc, kk:kk + 1], in1=a_[:],
                                                                                          op0=ALU.mult, op1=ALU.add), reads=[u, cw, a_], writes=[a_])
                if cc < 16:
                    fw.op("act", lambda e, a_=a_, xa=xa: e.activation(out=xa[:], in_=a_[:], func=AF.Silu), reads=[a_], writes=[xa])
                    xs_t = xtm[(cc // 4) % 2]
                    pt = PS[5 + (cc % 2)]
                    for r in range(4):
                        tr(fw, pt, pt[:, r * 128:(r + 1) * 128], xa, xa[:, r * 128:(r + 1) * 128], C_, k.ID, first=(r == 0))
                    fw.op("act", lambda e, pt=pt, xs_t=xs_t, c4=c4: e.activation(out=xs_t[:, :, c4 * 128:(c4 + 1) * 128], in_=pt[:].rearrange("p (r c) -> p r c", r=4), func=AF.Copy),
                          reads=[pt], writes=[xs_t])
                    if c4 == 3:
                        for r in range(4):
                            fw.dma("sp", lambda e, xs_t=xs_t, r=r: e.dma_start(out=k.sxs_d[t0 + r * 128:t0 + (r + 1) * 128, sl * 512:(sl + 1) * 512], in_=xs_t[:, r, :]), reads=[xs_t])
                else:
                    bs = bcst[cc % 2]
                    fw.op("act", lambda e, a_=a_, bs=bs: e.activation(out=bs[:], in_=a_[:], func=AF.Silu), reads=[a_], writes=[bs])
                    fw.dma("sp", lambda e, bs=bs, cc=cc: e.dma_start(out=k.sbc_d[cc - 16, :, t0:t0 + 512], in_=bs[:]), reads=[bs])
        for zs in range(4):
            wb = wsl[nslab % 2]; nslab += 1
            for hf in range(2):
                fw.dma("pool", lambda e, hf=hf: e.dma_start(out=wb[:, hf * 4:(hf + 1) * 4, :],
                                                             in_=w_in[hf * 512:(hf + 1) * 512, zs * 512:(zs + 1) * 512].rearrange("(c p) n -> p c n", p=128)), writes=[wb])
            for r in range(4):
                p = PS[3 + (r % 2)]
                for c in range(8):
                    mm(fw, p, p[:], hT, hT[:, c, r * 128:(r + 1) * 128], wb, wb[:, c, :], c == 0, c == 7)
                zt = zst[r % 2]
                fw.op("act", lambda e, p=p, zt=zt: e.activation(out=zt[:, 0:512], in_=p[:], func=AF.Silu), reads=[p], writes=[zt])
                fw.dma("sp", lambda e, zt=zt, r=r: e.dma_start(out=k.szs_d[t0 + r * 128:t0 + (r + 1) * 128, zs * 512:(zs + 1) * 512], in_=zt[:, 0:512]), reads=[zt])
    fw.barrier()
    fw.release(mkA)
    ones32 = fw.sb("ones32", [32, T])
    fw.op("dve", lambda e: e.memset(ones32[:], 1.0), writes=[ones32])
    acT = fw.sb("acT", [32, T])
    fw.op("dve", lambda e: e.tensor_tensor_scan(out=acT[:], data0=ones32[:], data1=adtT[:], initial=0.0, op0=ALU.mult, op1=ALU.add), reads=[ones32, adtT], writes=[acT])
    fw.dma("sp", lambda e: e.dma_start(out=k.sac_d, in_=acT[:]), reads=[acT])
    fw.barrier()
    for c in range(NT):
        p = PS[c % 2]
        tr(fw, p, p[:, 0:32], dtT, dtT[:, c * 128:(c + 1) * 128], C_, C_[0:32, 0:32])
        tr(fw, p, p[:, 32:64], acT, acT[:, c * 128:(c + 1) * 128], C_, C_[0:32, 0:32], first=False)
        fw.op("act", lambda e, p=p, c=c: e.activation(out=dt_tm[:, c, :], in_=p[:, 0:32], func=AF.Copy), reads=[p], writes=[dt_tm])
        fw.op("act", lambda e, p=p, c=c: e.activation(out=nac_tm[:, c, :], in_=p[:, 32:64], func=AF.Copy, scale=-1.0), reads=[p], writes=[nac_tm])
    fw.barrier()
    fw.release(mk_a)
    mk1 = fw.mark()
    BT = [fw.sb(f"BT{i}", [128, T], BF16) for i in range(2)]
    CT = [fw.sb(f"CT{i}", [128, T], BF16) for i in range(2)]
    abc = [fw.sb(f"abc{i}", [128, T]) for i in range(4)]
    xsf = fw.sb("xsf", [128, NT, 64])
    V = [fw.sb(f"V{i}", [128, NT, 64], BF16) for i in range(4)]
    dec = [fw.sb(f"dec{i}", [128, 512]) for i in range(2)]
    pT = [fw.sb(f"spT{i}", [128, 512], BF16) for i in range(3)]
    ysb = [fw.sb(f"ysb{i}", [128, 4, 64]) for i in range(2)]
    blk = 0; nd = 0; npt = 0
    for g in range(8):
        Bb = BT[g % 2]; Cb = CT[g % 2]
        fw.dma("sp", lambda e: e.dma_start(out=Bb[:], in_=k.sbc_d[g]), writes=[Bb])
        fw.dma("sp", lambda e: e.dma_start(out=Cb[:], in_=k.sbc_d[8 + g]), writes=[Cb])
        for r in range(4):
            hh = g * 4 + r
            fw.dma("sp", lambda e, r=r, hh=hh: e.dma_start(out=abc[r][:], in_=k.sac_d[hh:hh + 1, :].broadcast_to([128, T])), writes=[abc[r]])
            for q4 in range(4):
                fw.dma("sp", lambda e, q4=q4, hh=hh: e.dma_start(out=xsf[:, q4 * 8:(q4 + 1) * 8, :],
                                                                 in_=k.sxs_d[q4 * 1024:(q4 + 1) * 1024, hh * 64:(hh + 1) * 64].rearrange("(c p) d -> p c d", p=128)), writes=[xsf])
            fw.op("dve", lambda e, r=r, hh=hh: e.tensor_tensor(out=V[r][:], in0=xsf[:], in1=dt_tm[:, :, hh:hh + 1].broadcast_to([128, NT, 64]), op=ALU.mult),
                  reads=[xsf, dt_tm], writes=[V[r]])
        for jq in range(8):
            nk = 4 * jq + 4
            for kc in range(nk):
                r0 = max(0, kc - 4 * jq)
                q0 = r0 * 128
                diag = kc >= 4 * jq
                sp_ = PS[blk % 3]; blk += 1
                mm(fw, sp_, sp_[:, q0:512], Bb, Bb[:, kc * 128:(kc + 1) * 128], Cb, Cb[:, jq * 512 + q0:(jq + 1) * 512], True, True)
                for r in range(4):
                    hh = g * 4 + r
                    d_ = dec[nd % 2]; nd += 1
                    pb = pT[npt % 3]; npt += 1
                    qa = jq * 512 + q0
                    if diag:
                        fw.op("dve", lambda e, d_=d_, r=r, qa=qa, hh=hh: e.tensor_scalar(out=d_[:, q0:q0 + 128], in0=abc[r][:, qa:qa + 128], scalar1=nac_tm[:, kc, hh:hh + 1], scalar2=0.0,
                                                                                       op0=ALU.add, op1=ALU.min), reads=[abc[r], nac_tm], writes=[d_])
                        fw.op("act", lambda e, d_=d_: e.activation(out=d_[:, q0:q0 + 128], in_=d_[:, q0:q0 + 128], func=AF.Exp), reads=[d_], writes=[d_])
                        fw.op("dve", lambda e, d_=d_: e.tensor_tensor(out=d_[:, q0:q0 + 128], in0=d_[:, q0:q0 + 128], in1=C_[:, 256:384], op=ALU.mult), reads=[d_, C_], writes=[d_])
                        if q0 + 128 < 512:
                            fw.op("act", lambda e, d_=d_, r=r, qa=qa, hh=hh: e.activation(out=d_[:, q0 + 128:512], in_=abc[r][:, qa + 128:(jq + 1) * 512], func=AF.Exp,
                                                                                     bias=nac_tm[:, kc, hh:hh + 1], scale=1.0), reads=[abc[r], nac_tm, d_], writes=[d_])
                    else:
                        fw.op("act", lambda e, d_=d_, r=r, qa=qa, hh=hh: e.activation(out=d_[:, q0:512], in_=abc[r][:, qa:(jq + 1) * 512], func=AF.Exp,
                                                                                 bias=nac_tm[:, kc, hh:hh + 1], scale=1.0), reads=[abc[r], nac_tm], writes=[d_])
                    fw.op("dve", lambda e, d_=d_, pb=pb, sp_=sp_: e.tensor_tensor(out=pb[:, q0:512], in0=sp_[:, q0:512], in1=d_[:, q0:512], op=ALU.mult), reads=[sp_, d_], writes=[pb])
                    ya = PS[3 + r]
                    for s_ in range(r0, 4):
                        fw.op("pe", lambda e, ya=ya, pb=pb, s_=s_, r=r: e.matmul(ya[:, s_ * 64:(s_ + 1) * 64], pb[:, s_ * 128:(s_ + 1) * 128], V[r][:, kc, :],
                                                                               start=(kc == 0), stop=(kc == 4 * jq + s_)),
                              reads=[pb, V[r]], writes=[ya], accumulate=True)
            for r in range(4):
                hh = g * 4 + r
                ya = PS[3 + r]; yb = ysb[r % 2]
                fw.op("act", lambda e, ya=ya, yb=yb: e.activation(out=yb[:], in_=ya[:, 0:256].rearrange("p (s d) -> p s d", s=4), func=AF.Copy), reads=[ya], writes=[yb])
                fw.dma("sp", lambda e, yb=yb, hh=hh: e.dma_start(out=k.sy_d[jq * 512:(jq + 1) * 512, hh * 64:(hh + 1) * 64].rearrange("(s p) d -> p s d", p=128), in_=yb[:]), reads=[yb])
    fw.barrier()
    fw.release(mk1)
    dbc = fw.sb("dbc", [128, 2048]); gnb = fw.sb("gnb", [128, 2048])
    fw.dma("sp", lambda e: e.dma_start(out=dbc[:], in_=k.ssm_dexp[0:1, :].broadcast_to([128, 2048])), writes=[dbc])
    fw.dma("sp", lambda e: e.dma_start(out=gnb[:], in_=k.ssm_gn[0:1, :].broadcast_to([128, 2048])), writes=[gnb])
    yt = [fw.sb(f"yt{i}", [128, 2048]) for i in range(2)]
    xs2 = [fw.sb(f"xs2{i}", [128, 2048]) for i in range(2)]
    zz = [fw.sb(f"zz{i}", [128, 2048]) for i in range(2)]
    sqv = fw.sb("sqv", [128, 2048])
    gs = fw.sb("gs", [128, 16])
    ynT = [fw.sb(f"ynT{i}", [128, 16, 128], BF16) for i in range(2)]
    for i in range(NT):
        y_ = yt[i % 2]; x_ = xs2[i % 2]; z_ = zz[i % 2]
        fw.dma("sp", lambda e: e.dma_start(out=y_[:], in_=k.sy_d[i * 128:(i + 1) * 128, :]), writes=[y_])
        fw.dma("sp", lambda e: e.dma_start(out=x_[:], in_=k.sxs_d[i * 128:(i + 1) * 128, :]), writes=[x_])
        fw.dma("sp", lambda e: e.dma_start(out=z_[:], in_=k.szs_d[i * 128:(i + 1) * 128, :]), writes=[z_])
        fw.op("dve", lambda e: e.tensor_tensor(out=x_[:], in0=x_[:], in1=dbc[:], op=ALU.mult), reads=[x_, dbc], writes=[x_])
        fw.op("dve", lambda e: e.tensor_tensor(out=y_[:], in0=y_[:], in1=x_[:], op=ALU.add), reads=[y_, x_], writes=[y_])
        fw.op("dve", lambda e: e.tensor_tensor(out=y_[:], in0=y_[:], in1=z_[:], op=ALU.mult), reads=[y_, z_], writes=[y_])
        fw.op("act", lambda e: e.activation(out=sqv[:], in_=y_[:], func=AF.Square), reads=[y_], writes=[sqv])
        fw.op("dve", lambda e: e.tensor_reduce(out=gs[:, 0:8], in_=sqv[:].rearrange("p (g d) -> p g d", g=8), axis=AX.X, op=ALU.add), reads=[sqv], writes=[gs])
        fw.op("act", lambda e: e.activation(out=gs[:, 8:16], in_=gs[:, 0:8], func=AF.Sqrt, bias=k.epsc[:], scale=1.0 / 256), reads=[gs, k.epsc], writes=[gs])
        fw.op("dve", lambda e: e.reciprocal(out=gs[:, 8:16], in_=gs[:, 8:16]), reads=[gs], writes=[gs])
        for g in range(8):
            fw.op("dve", lambda e, g=g: e.scalar_tensor_tensor(out=y_[:, g * 256:(g + 1) * 256], in0=y_[:, g * 256:(g + 1) * 256], scalar=gs[:, 8 + g:9 + g],
                                                               in1=gnb[:, g * 256:(g + 1) * 256], op0=ALU.mult, op1=ALU.mult), reads=[y_, gs, gnb], writes=[y_])
        yT = ynT[i % 2]
        for q4 in range(4):
            p = PS[q4 % 2]
            for c in range(4):
                cc = q4 * 4 + c
                tr(fw, p, p[:, c * 128:(c + 1) * 128], y_, y_[:, cc * 128:(cc + 1) * 128], C_, k.ID, first=(c == 0))
            fw.op("act", lambda e, p=p, q4=q4: e.activation(out=yT[:, q4 * 4:(q4 + 1) * 4, :], in_=p[:].rearrange("p (c t) -> p c t", c=4), func=AF.Copy), reads=[p], writes=[yT])
        fw.dma("sp", lambda e: e.dma_start(out=k.synT_d[:, :, i * 128:(i + 1) * 128].rearrange("c p t -> p c t"), in_=yT[:]), reads=[yT])
    fw.barrier()
    fw.release(mk0)


MOE_B = 256
MOE_NB = 4 * T // MOE_B + 64


def moe_layer_sparse(k, L):
    fw = k.fw; PS = k.PS; C_ = k.C_; MOD = k.MOD; nc = k.nc
    NB = MOE_NB; B = MOE_B
    mk0 = fw.mark()
    S4_all = fw.sb("S4_all", [128, NT, 4], I32); G4_all = fw.sb("G4_all", [128, NT, 4])
    idx_w = fw.sb("idx_w", [128, 128], I32); idx_b = fw.sb("idx_b", [128, 128], I32)
    idx_g = [fw.sb(f"idx_g{i}", [128, 128], I32) for i in range(8)]
    mkD = fw.mark()
    G_all = fw.sb("G_all", [128, NT, 32]); M_all = fw.sb("M_all", [128, NT, 32]); R_all = fw.sb("R_all", [128, NT, 32])
    base = fw.sb("base", [128, 32])
    fw.op("dve", lambda e: e.memset(base[:], 0.0), writes=[base])
    wr = fw.sb("wr", [128, 8, 32])
    fw.dma("sp", lambda e: e.dma_start(out=wr[:], in_=k.w_router[L].rearrange("(c p) n -> p c n", p=128)), writes=[wr])
    brb = fw.sb("brb", [128, 32])
    fw.dma("sp", lambda e: e.dma_start(out=brb[:], in_=k.b_router[L:L + 1, :].broadcast_to([128, 32])), writes=[brb])
    h2f = fw.sb("h2f", [128, 8, 128])
    lg = fw.sb("lg", [128, 32]); m8 = fw.sb("m8", [128, 8]); ex = fw.sb("ex", [128, 32])
    sm = fw.sb("sm", [128, 4])
    for i in range(NT):
        xb = k.xt[i % 2]; hb = k.ht[i % 2]; sb_ = k.st[i % 2]
        fw.dma("sp", lambda e: e.dma_start(out=xb[:], in_=k.xs[i * 128:(i + 1) * 128, :]), writes=[xb])
        k.norm_mod(xb, hb, sb_, 4, 3)
        fw.dma("sp", lambda e: e.dma_start(out=k.h2_d[i * 128:(i + 1) * 128, :], in_=hb[:]), reads=[hb])
        for half, p in ((0, PS[0]), (1, PS[1])):
            for c in range(4):
                cc = half * 4 + c
                tr(fw, p, p[:, c * 128:(c + 1) * 128], hb, hb[:, cc * 128:(cc + 1) * 128], C_, k.ID, first=(c == 0))
            fw.op("act", lambda e, half=half, p=p: e.activation(out=h2f[:, half * 4:(half + 1) * 4, :], in_=p[:].rearrange("p (c t) -> p c t", c=4), func=AF.Copy),
                  reads=[p], writes=[h2f])
        pl = PS[2]
        for c in range(8):
            mm(fw, pl, pl[:, 0:32], h2f, h2f[:, c, :], wr, wr[:, c, :], c == 0, c == 7)
        msk = M_all
        fw.op("dve", lambda e: e.tensor_tensor(out=lg[:], in0=pl[:, 0:32], in1=brb[:], op=ALU.add), reads=[pl, brb], writes=[lg])
        fw.op("dve", lambda e: e.max(out=m8[:], in_=lg[:]), reads=[lg], writes=[m8])
        fw.op("dve", lambda e: e.tensor_scalar(out=M_all[:, i, :], in0=lg[:], scalar1=m8[:, 3:4], scalar2=None, op0=ALU.is_ge), reads=[lg, m8], writes=[M_all])
        fw.op("dve", lambda e: e.tensor_scalar(out=sm[:, 0:1], in0=m8[:, 0:1], scalar1=-1.0, scalar2=None, op0=ALU.mult), reads=[m8], writes=[sm])
        fw.op("act", lambda e: e.activation(out=ex[:], in_=lg[:], func=AF.Exp, bias=sm[:, 0:1], scale=1.0), reads=[lg, sm], writes=[ex])
        fw.op("dve", lambda e: e.tensor_tensor(out=ex[:], in0=ex[:], in1=M_all[:, i, :], op=ALU.mult), reads=[ex, M_all], writes=[ex])
        fw.op("dve", lambda e: e.reduce_sum(out=sm[:, 1:2], in_=ex[:], axis=AX.X), reads=[ex], writes=[sm])
        fw.op("dve", lambda e: e.reciprocal(out=sm[:, 2:3], in_=sm[:, 1:2]), reads=[sm], writes=[sm])
        fw.op("dve", lambda e: e.tensor_scalar(out=G_all[:, i, :], in0=ex[:], scalar1=sm[:, 2:3], scalar2=None, op0=ALU.mult), reads=[ex, sm], writes=[G_all])
        pr = PS[3]; pc = PS[4]
        mm(fw, pr, pr[:, 0:32], C_, C_[:, 384:512], M_all, M_all[:, i, :], True, True)
        mm(fw, pc, pc[:, 0:32], C_, C_[:, 128:256], M_all, M_all[:, i, :], True, True)
        fw.op("dve", lambda e: e.tensor_tensor(out=R_all[:, i, :], in0=pr[:, 0:32], in1=base[:], op=ALU.add), reads=[pr, base], writes=[R_all])
        fw.op("dve", lambda e: e.tensor_tensor(out=base[:], in0=pc[:, 0:32], in1=base[:], op=ALU.add), reads=[pc, base], writes=[base])
    ti = fw.sb("ti", [128, 32], I32); pf = fw.sb("pf", [128, 32]); pend = fw.sb("pend", [128, 32]); pst = fw.sb("pst", [128, 32])
    on32 = fw.sb("on32", [128, 32]); sl_ = fw.sb("sl_", [128, 32]); v_ = fw.sb("v_", [128, 32]); m8s = fw.sb("m8s", [128, 8]); jk = fw.sb("jk", [128, 32])
    fw.op("dve", lambda e: e.memset(on32[:], 1.0), writes=[on32])
    fw.op("dve", lambda e: e.tensor_scalar(out=pf[:], in0=base[:], scalar1=float(2 * B - 1), scalar2=None, op0=ALU.add), reads=[base], writes=[pf])
    fw.op("dve", lambda e: e.tensor_copy(out=ti[:], in_=pf[:]), reads=[pf], writes=[ti])
    sh = (2 * B).bit_length() - 1
    fw.op("dve", lambda e: e.tensor_scalar(out=ti[:], in0=ti[:], scalar1=sh, scalar2=sh, op0=ALU.logical_shift_right, op1=ALU.logical_shift_left), reads=[ti], writes=[ti])
    fw.op("dve", lambda e: e.tensor_copy(out=pf[:], in_=ti[:]), reads=[ti], writes=[pf])
    fw.op("dve", lambda e: e.tensor_tensor_scan(out=pend[:], data0=on32[:], data1=pf[:], initial=0.0, op0=ALU.mult, op1=ALU.add), reads=[on32, pf], writes=[pend])
    fw.op("dve", lambda e: e.tensor_tensor(out=pst[:], in0=pend[:], in1=pf[:], op=ALU.subtract), reads=[pend, pf], writes=[pst])
    for i in range(NT):
        fw.op("dve", lambda e: e.tensor_tensor(out=sl_[:], in0=R_all[:, i, :], in1=pst[:], op=ALU.add), reads=[R_all, pst], writes=[sl_])
        fw.op("dve", lambda e: e.scalar_tensor_tensor(out=v_[:], in0=sl_[:], scalar=1.0, in1=M_all[:, i, :], op0=ALU.add, op1=ALU.mult), reads=[sl_, M_all], writes=[v_])
        fw.op("dve", lambda e: e.max(out=m8s[:], in_=v_[:]), reads=[v_], writes=[m8s])
        fw.op("dve", lambda e: e.tensor_scalar(out=S4_all[:, i, :], in0=m8s[:, 0:4], scalar1=-1.0, scalar2=None, op0=ALU.add), reads=[m8s], writes=[S4_all])
        for kk in range(4):
            fw.op("dve", lambda e, kk=kk: e.scalar_tensor_tensor(out=jk[:], in0=v_[:], scalar=m8s[:, kk:kk + 1], in1=G_all[:, i, :], op0=ALU.is_equal, op1=ALU.mult,
                                                                accum_out=G4_all[:, i, kk:kk + 1]), reads=[v_, m8s, G_all], writes=[jk, G4_all])
    cmp_ = fw.sb("cmp_", [128, 32]); bec = fw.sb("bec", [128, 2]); ber = fw.sb("ber", [1, 136]); crow = fw.sb("crow", [1, 128]); vrow = fw.sb("vrow", [1, 128])
    fw.op("dve", lambda e: e.tensor_scalar(out=cmp_[:], in0=pend[:], scalar1=C_[:, 512:513], scalar2=None, op0=ALU.is_le), reads=[pend, C_], writes=[cmp_])
    fw.op("dve", lambda e: e.reduce_sum(out=bec[:, 0:1], in_=cmp_[:], axis=AX.X), reads=[cmp_], writes=[bec])
    fw.op("dve", lambda e: e.tensor_scalar(out=bec[:, 0:1], in0=bec[:, 0:1], scalar1=31.0, scalar2=None, op0=ALU.min), reads=[bec], writes=[bec])
    fw.op("dve", lambda e: e.tensor_scalar(out=bec[:, 1:2], in0=C_[:, 512:513], scalar1=pend[:, 31:32], scalar2=None, op0=ALU.is_lt), reads=[pend, C_], writes=[bec])
    pt_ = PS[5]
    tr(fw, pt_, pt_[0:1, 0:128], bec, bec[:, 0:1], C_, k.ID)
    tr(fw, pt_, pt_[0:1, 128:256], bec, bec[:, 1:2], C_, k.ID, first=False)
    fw.op("dve", lambda e: e.memset(ber[:], -1.0), writes=[ber])
    fw.op("act", lambda e: e.activation(out=ber[0:1, 4:132], in_=pt_[0:1, 0:128], func=AF.Copy), reads=[pt_], writes=[ber])
    fw.op("act", lambda e: e.activation(out=vrow[:], in_=pt_[0:1, 128:256], func=AF.Copy), reads=[pt_], writes=[vrow])
    fw.op("dve", lambda e: e.tensor_tensor(out=crow[:], in0=ber[0:1, 4:132], in1=ber[0:1, 0:128], op=ALU.not_equal), reads=[ber], writes=[crow])
    fw.op("dve", lambda e: e.tensor_tensor(out=crow[:], in0=crow[:], in1=vrow[:], op=ALU.mult), reads=[crow, vrow], writes=[crow])
    fw.op("dve", lambda e: e.tensor_tensor(out=crow[:], in0=crow[:], in1=C_[0:1, 640:768], op=ALU.mult), reads=[crow, C_], writes=[crow])
    BIG = 1.0e6
    pb1 = PS[6]; pb2 = PS[7]
    mm(fw, pb1, pb1[:, 0:128], C_, C_[0:1, 128:256], ber, ber[0:1, 4:132], True, True)
    mm(fw, pb2, pb2[:, 0:128], C_, C_[0:1, 128:256], crow, crow[0:1, :], True, True)
    cnd = fw.sb("cnd", [128, 128]); tw = fw.sb("tw", [128, 128]); tb = fw.sb("tb", [128, 128])
    fw.op("act", lambda e: e.activation(out=cnd[:], in_=pb2[:, 0:128], func=AF.Copy), reads=[pb2], writes=[cnd])
    fw.op("dve", lambda e: e.tensor_scalar(out=tb[:], in0=pb1[:, 0:128], scalar1=float(L * 32) - BIG, scalar2=None, op0=ALU.add), reads=[pb1], writes=[tb])
    fw.op("dve", lambda e: e.tensor_scalar(out=tw[:], in0=pb1[:, 0:128], scalar1=128.0, scalar2=float(L * 4096) - BIG, op0=ALU.mult, op1=ALU.add), reads=[pb1], writes=[tw])
    fw.op("dve", lambda e: e.tensor_scalar(out=tw[:], in0=tw[:], scalar1=C_[:, 513:514], scalar2=None, op0=ALU.add), reads=[tw, C_], writes=[tw])
    for t_, ix in ((tw, idx_w), (tb, idx_b)):
        fw.op("dve", lambda e, t_=t_: e.tensor_tensor(out=t_[:], in0=t_[:], in1=cnd[:], op=ALU.mult), reads=[t_, cnd], writes=[t_])
        fw.op("dve", lambda e, t_=t_, ix=ix: e.tensor_scalar(out=ix[:], in0=t_[:], scalar1=BIG, scalar2=None, op0=ALU.add), reads=[t_], writes=[ix])
    for c2 in range(8):
        fw.op("dve", lambda e, c2=c2: e.tensor_scalar(out=idx_g[c2][:], in0=tw[:], scalar1=8.0, scalar2=8 * BIG + c2, op0=ALU.mult, op1=ALU.add), reads=[tw], writes=[idx_g[c2]])
    fw.barrier()
    fw.release(mkD)
    MS = ""
    if MS == "E":
        dbt = k.xt[0]
        for j_, src in enumerate((idx_w, idx_b, idx_g[0], idx_g[1])):
            fw.op("dve", lambda e, j_=j_, src=src: e.tensor_copy(out=dbt[:, j_ * 128:(j_ + 1) * 128], in_=src[:]), reads=[src], writes=[dbt])
        fw.op("dve", lambda e: e.tensor_copy(out=dbt[:, 512:640], in_=S4_all[:].rearrange("p a b -> p (a b)")), reads=[S4_all], writes=[dbt])
        fw.op("dve", lambda e: e.tensor_copy(out=dbt[:, 640:768], in_=G4_all[:].rearrange("p a b -> p (a b)")), reads=[G4_all], writes=[dbt])
        fw.dma("sp", lambda e: e.dma_start(out=k.xs[0:128, :], in_=dbt[:]), reads=[dbt])
        fw.barrier()
        fw.release(mk0); return
    fw.store_q = None
    for i in range(NT):
        hb = k.ht[i % 2]
        fw.dma("sp", lambda e: e.dma_start(out=hb[:], in_=k.h2_d[i * 128:(i + 1) * 128, :]), writes=[hb])
        for kk in range(4):
            fw.dma("pool", lambda e, kk=kk: e.indirect_dma_start(out=k.xsort_d, out_offset=bass.IndirectOffsetOnAxis(ap=S4_all[:, i, kk:kk + 1], axis=0),
                                                                 in_=hb[:], in_offset=None), reads=[hb, S4_all])
    fw.barrier()
    if MS == "F":
        fw.release(mk0); return
    mkG = fw.mark()
    wgu = [fw.sb(f"wgu{i}", [128, 8, 2048], BF16) for i in range(2)]
    wdn = [fw.sb(f"wdn{i}", [128, 8, 1024], BF16) for i in range(2)]
    bgr = [fw.sb(f"bgr{i}", [128, 2048], BF16) for i in range(2)]
    bdb = [fw.sb(f"bdb{i}", [128, 1024]) for i in range(2)]
    onesb = fw.sb("onesb", [1, B], BF16)
    fw.op("dve", lambda e: e.memset(onesb[:], 1.0), writes=[onesb])
    xblk = [fw.sb(f"xblk{i}", [128, B // 128, 1024]) for i in range(1)]
    xT = [fw.sb(f"xT{i}", [128, 8, B], BF16) for i in range(2)]
    hact = fw.sb("hact", [128, 8, B], BF16)
    g_s = [fw.sb(f"g_{i}", [128, B]) for i in range(2)]; sg_s = [fw.sb(f"sg_{i}", [128, B]) for i in range(2)]; l_s = [fw.sb(f"l_{i}", [128, B]) for i in range(2)]
    yo = [fw.sb(f"yo{i}", [128, 1024]) for i in range(2)]
    bound_reg = nc.gpsimd.to_reg(2 * 32 * 1024 - 1)
    for bi in range(NB):
        pj = (bi // 2) % 2
        wg = wgu[pj]; wd = wdn[pj]; bg = bgr[pj]; bd = bdb[pj]
        NW = 2 * 32 * 128 - 1
        if bi % 2 == 0:
            for c in range(8):
                fw.dma("pool", lambda e, c=c: e.indirect_dma_start(out=wg[:, c, :], out_offset=None, in_=k.w_gu,
                                                                   in_offset=bass.IndirectOffsetOnAxis(ap=idx_g[c][:, bi:bi + 1], axis=0), bounds_check=bound_reg, oob_is_err=False),
                       reads=[idx_g[c]], writes=[wg])
            for c in range(8):
                fw.dma("pool", lambda e, c=c: e.indirect_dma_start(out=wd[:, c, :], out_offset=None, in_=k.w_dn,
                                                                   in_offset=bass.IndirectOffsetOnAxis(ap=idx_g[c][:, bi:bi + 1], axis=0), bounds_check=bound_reg, oob_is_err=False),
                       reads=[idx_g[c]], writes=[wd])
            fw.dma("pool", lambda e: e.indirect_dma_start(out=bg[:], out_offset=None, in_=k.b_gu, in_offset=bass.IndirectOffsetOnAxis(ap=idx_b[:, bi:bi + 1], axis=0),
                                                          bounds_check=bound_reg, oob_is_err=False), reads=[idx_b], writes=[bg])
            fw.dma("pool", lambda e: e.indirect_dma_start(out=bd[:], out_offset=None, in_=k.b_dn, in_offset=bass.IndirectOffsetOnAxis(ap=idx_b[:, bi:bi + 1], axis=0),
                                                          bounds_check=bound_reg, oob_is_err=False), reads=[idx_b], writes=[bd])
        xb = xblk[0]; xt_ = xT[bi % 2]
        if bi == 0:
            fw.dma("sp", lambda e: e.dma_start(out=xb[:], in_=k.xsort_d[0:B, :].rearrange("(r p) d -> p r d", p=128)), writes=[xb])
        for rt in range(B // 128):
            for half in range(2):
                p = PS[half]
                for c in range(4):
                    cc = half * 4 + c
                    tr(fw, p, p[:, c * 128:(c + 1) * 128], xb, xb[:, rt, bass.ds(cc, 128, 8)], C_, k.ID, first=(c == 0))
                fw.op("act", lambda e, half=half, p=p, rt=rt: e.activation(out=xt_[:, half * 4:(half + 1) * 4, rt * 128:(rt + 1) * 128], in_=p[:].rearrange("p (c t) -> p c t", c=4), func=AF.Copy),
                      reads=[p], writes=[xt_])
        if bi + 1 < NB:
            fw.dma("sp", lambda e: e.dma_start(out=xb[:], in_=k.xsort_d[(bi + 1) * B:(bi + 2) * B, :].rearrange("(r p) d -> p r d", p=128)), writes=[xb])
        for fc in range(8):
            pgl = PS[2 + (fc % 4)]
            g_ = g_s[fc % 2]; sg_ = sg_s[fc % 2]; l_ = l_s[fc % 2]
            for part, off in ((0, 0), (1, 1024)):
                o_ap = pgl[:, part * B:(part + 1) * B]
                for c in range(8):
                    mm(fw, pgl, o_ap, wg, wg[:, c, bass.ds(off + fc, 128, 8)], xt_, xt_[:, c, :], c == 0, False)
                mm(fw, pgl, o_ap, bg, bg[0:1, bass.ds(off + fc, 128, 8)], onesb, onesb[0:1, :], False, True)
            fw.op("dve", lambda e, pgl=pgl, g_=g_: e.tensor_scalar(out=g_[:], in0=pgl[:, 0:B], scalar1=7.0, scalar2=None, op0=ALU.min), reads=[pgl], writes=[g_])
            fw.op("act", lambda e: e.activation(out=sg_[:], in_=g_[:], func=AF.Sigmoid, scale=1.702), reads=[g_], writes=[sg_])
            fw.op("dve", lambda e, pgl=pgl: e.tensor_scalar(out=l_[:], in0=pgl[:, B:2 * B], scalar1=7.0, scalar2=-7.0, op0=ALU.min, op1=ALU.max), reads=[pgl], writes=[l_])
            fw.op("dve", lambda e: e.tensor_tensor(out=g_[:], in0=g_[:], in1=sg_[:], op=ALU.mult), reads=[g_, sg_], writes=[g_])
            fw.op("dve", lambda e, fc=fc: e.scalar_tensor_tensor(out=hact[:, fc, :], in0=l_[:], scalar=1.0, in1=g_[:], op0=ALU.add, op1=ALU.mult), reads=[g_, l_], writes=[hact])
        for rt in range(B // 128):
            yb = yo[rt % 2]
            for half in range(2):
                p = PS[6 + half]
                for fc in range(8):
                    mm(fw, p, p[:], hact, hact[:, fc, rt * 128:(rt + 1) * 128], wd, wd[:, fc, half * 512:(half + 1) * 512], fc == 0, fc == 7)
                fw.op("dve", lambda e, p=p, half=half, yb=yb: e.tensor_tensor(out=yb[:, half * 512:(half + 1) * 512], in0=p[:], in1=bd[:, half * 512:(half + 1) * 512], op=ALU.add),
                      reads=[p, bd], writes=[yb])
            fw.dma("sp", lambda e, yb=yb, rt=rt: e.dma_start(out=k.ysort_d[bi * B + rt * 128:bi * B + (rt + 1) * 128, :], in_=yb[:]), reads=[yb])
    fw.barrier()
    fw.release(mkG)
    if MS == "G":
        fw.release(mk0); return
    fw.store_q = "act"
    yk = [[fw.sb(f"yk{i}_{kk}", [128, 1024]) for kk in range(4)] for i in range(2)]
    for i in range(NT):
        xb = k.xt[i % 2]; ys_ = yk[i % 2]
        fw.dma("sp", lambda e: e.dma_start(out=xb[:], in_=k.xs[i * 128:(i + 1) * 128, :]), writes=[xb])
        for kk in range(4):
            fw.dma("pool", lambda e, kk=kk: e.indirect_dma_start(out=ys_[kk][:], out_offset=None, in_=k.ysort_d,
                                                                 in_offset=bass.IndirectOffsetOnAxis(ap=S4_all[:, i, kk:kk + 1], axis=0)), reads=[S4_all], writes=[ys_[kk]])
        a0 = ys_[0]
        fw.op("dve", lambda e: e.tensor_scalar(out=a0[:], in0=a0[:], scalar1=G4_all[:, i, 0:1], scalar2=None, op0=ALU.mult), reads=[a0, G4_all], writes=[a0])
        for kk in range(1, 4):
            fw.op("dve", lambda e, kk=kk: e.scalar_tensor_tensor(out=a0[:], in0=ys_[kk][:], scalar=G4_all[:, i, kk:kk + 1], in1=a0[:], op0=ALU.mult, op1=ALU.add),
                  reads=[ys_[kk], G4_all, a0], writes=[a0])
        fw.op("dve", lambda e: e.tensor_tensor(out=a0[:], in0=a0[:], in1=MOD[:, 5, :], op=ALU.mult), reads=[a0, MOD], writes=[a0])
        fw.op("dve", lambda e: e.tensor_tensor(out=xb[:], in0=xb[:], in1=a0[:], op=ALU.add), reads=[xb, a0], writes=[xb])
        fw.dma("sp", lambda e: e.dma_start(out=k.xs[i * 128:(i + 1) * 128, :], in_=xb[:]), reads=[xb])
    fw.barrier()
    fw.store_q = "pool"
    fw.release(mk0)


def host_consts():
    cst = np.zeros((128, 1024), np.float32)
    cst[:, 0:128] = np.eye(128, dtype=np.float32)
    cst[:, 128:256] = 1.0
    i = np.arange(128)
    cst[:, 256:384] = (i[None, :] >= i[:, None]).astype(np.float32)
    cst[:, 384:512] = (i[:, None] < i[None, :]).astype(np.float32)
    cst[:, 512] = i * 256.0
    cst[:, 513] = i
    cst[:, 640:768] = (i % 2 == 0).astype(np.float32)[None, :]
    inv = (1.0 / (10000.0 ** (np.arange(0, 64, 2, dtype=np.float32) / 64))).astype(np.float32)
    invf = np.zeros((64, 2), np.float32)
    invf[:32, 0] = inv; invf[32:, 0] = inv
    invf[:32, 1] = -1.0; invf[32:, 1] = 1.0
    return cst, invf


def prep_core(inp, b):
    f = np.ascontiguousarray
    cst, invf = host_consts()
    m = {}
    m["x"] = f(inp["x"][b])
    m["c"] = f(inp["c"][b].reshape(8, 128).T)
    m["pos"] = f(inp["positions"][b].reshape(1, T).astype(np.int32))
    m["w_mod"] = inp["w_mod"]; m["b_mod"] = inp["b_mod"]
    m["g_mix"] = inp["g_mix_norm"]; m["g_ffn"] = inp["g_ffn_norm"]
    w_in = inp["mla_w_in"][0]
    kr = w_in[:, 512:576]
    m["mla_w_in"] = f(np.concatenate([w_in, kr[:, 32:], kr[:, :32]], axis=1))
    m["mla_gq"] = f(inp["mla_g_q"][0].reshape(2, 128).T)
    m["mla_gkv"] = f(inp["mla_g_kv"][0].reshape(2, 128).T)
    wq = inp["mla_w_q_up"][0].reshape(256, 8, 192)
    sw = np.concatenate([wq[:, :, 160:192], wq[:, :, 128:160]], axis=2)
    m["mla_wq"] = f(np.concatenate([wq.reshape(256, 1536), sw.reshape(256, 512)], axis=1))
    wkv = inp["mla_w_kv_up"][0].reshape(256, 8, 256)
    m["mla_wkv"] = f(np.concatenate([wkv[:, :, :128].reshape(256, 1024), wkv[:, :, 128:].reshape(256, 1024)], axis=1))
    m["mla_wo"] = inp["mla_w_out"][0]
    m["w_router"] = inp["moe_w_router"]; m["b_router"] = inp["moe_b_router"]
    m["w_gu"] = inp["moe_w_gate_up"].reshape(2 * 32 * 1024, 2048); m["b_gu"] = inp["moe_b_gate_up"].reshape(64, 2048)
    m["w_dn"] = inp["moe_w_down"].reshape(2 * 32 * 1024, 1024); m["b_dn"] = inp["moe_b_down"].reshape(64, 1024)
    m["g_final"] = f(inp["g_final"].reshape(1, D))
    m["ssm_w_in"] = inp["ssm_w_in"][0]
    m["ssm_cw"] = f(inp["ssm_conv_w"][0].reshape(4, 32, 128).transpose(2, 1, 0))
    m["ssm_cb"] = f(inp["ssm_conv_b"][0].reshape(32, 128).T)
    m["ssm_hv"] = f(np.stack([inp["ssm_dt_bias"][0], inp["ssm_a_log"][0]], axis=1))
    m["ssm_dexp"] = f(np.repeat(inp["ssm_d"][0], 64).reshape(1, 2048))
    m["ssm_gn"] = f(inp["ssm_g_norm"][0].reshape(1, 2048))
    m["ssm_wo"] = inp["ssm_w_out"][0]
    m["cst"] = cst; m["invf"] = invf
    return m

_CACHE = {}


def _program():
    if "k" not in _CACHE:
        k = build()
        k.stop = "all"
        k.compute_mod(0)
        mla_layer(k, 0, True)
        mixer_out_and_moe_router(k, 0, k.mla_wo, 8, k.oT_d)
        moe_layer_sparse(k, 0)
        k.compute_mod(1)
        ssd_layer(k, 1)
        mixer_out_and_moe_router(k, 1, k.ssm_wo, 16, k.synT_d)
        moe_layer_sparse(k, 1)
        final_norm(k)
        k.fw.finish()
        k.fw.close()
        _CACHE["k"] = k
    return _CACHE["k"]


def kernel(**inputs):
    inp = {kk: np.asarray(v) for kk, v in inputs.items()}
    k = _program()
    in_maps = [prep_core(inp, b) for b in range(8)]
    res = run_bass_kernel_spmd(k.nc, in_maps, core_ids=list(range(8)))
    return np.stack([np.asarray(r["out"]) for r in res.results], axis=0).astype(np.float32)
```

```python
import numpy as np
import concourse.bass as bass
import concourse.mybir as mybir
from concourse.bass_utils import run_bass_kernel_spmd

F32 = mybir.dt.float32
F32R = mybir.dt.float32r
BF16 = mybir.dt.bfloat16
I32 = mybir.dt.int32
U32 = mybir.dt.uint32
ALU = mybir.AluOpType
AF = mybir.ActivationFunctionType
AX = mybir.AxisListType

EPOCH = 20000


class Buf:
    __slots__ = ("t", "name", "lastw", "reads", "wfill", "dsem", "dcount", "is_dram")

    def __init__(self, t, name, is_dram=False):
        self.t = t
        self.name = name
        self.lastw = None
        self.reads = []
        self.wfill = []
        self.is_dram = is_dram

    def __getitem__(self, idx):
        return self.t[idx]


class FW:
    def __init__(self, nc):
        self.nc = nc
        self.eng = {"pe": nc.tensor, "dve": nc.vector, "act": nc.scalar,
                    "pool": nc.gpsimd, "sp": nc.sync}
        self.sem = {}
        self.cnt = {}
        self.nsem = 0
        self.waited = {e: {} for e in self.eng}
        self.semobjs = {}
        self.ctx = []
        self.bctx = []
        self.last = {e: None for e in self.eng}
        self.pend = {e: [] for e in self.eng}
        self.store_q = "pool"
        self.dma_toks = []
        self.dma_pool = []
        self.dma_free = []
        for e in ("pe", "dve", "act", "pool"):
            self._new_epoch(e)

    def _mksem(self, name):
        cm = self.nc.semaphore(name)
        s = cm.__enter__()
        self.ctx.append(cm)
        self.nsem += 1
        self.semobjs[id(s)] = s
        return s

    def _new_epoch(self, e):
        self.sem[e] = self._mksem(f"s_{e}_{self.nsem}")
        self.cnt[e] = 0

    def sb(self, name, shape, dt=F32):
        self.uid = getattr(self, "uid", 0) + 1
        cm = self.nc.sbuf_tensor(f"{name}_u{self.uid}", shape, dt)
        t = cm.__enter__()
        self.bctx.append(cm)
        return Buf(t, name)

    def ps(self, name, shape, dt=F32):
        cm = self.nc.psum_tensor(name, shape, dt)
        t = cm.__enter__()
        self.ctx.append(cm)
        return Buf(t, name)

    def dram(self, name, shape, dt=F32, kind="Internal"):
        t = self.nc.dram_tensor(name, shape, dt, kind=kind)
        return Buf(t.ap(), name, is_dram=True)

    def _wait(self, e, tok):
        if tok is None:
            return
        sem, val, _ = tok
        w = self.waited[e]
        if w.get(id(sem), 0) >= val:
            return
        w[id(sem)] = val
        self.eng[e].wait_ge(sem, val)

    def _deps(self, e, reads, writes, accumulate=False, is_dma=False):
        for tok in self.pend[e]:
            self._wait(e, tok)
        self.pend[e] = []
        for b in reads:
            if b is None:
                continue
            if b.lastw is not None:
                self._wait(e, b.lastw)
            for tok in b.wfill:
                self._wait(e, tok)
        for b in writes:
            if b is None:
                continue
            fill = is_dma and b.lastw is not None and b.lastw[2] == "dmaq" and not b.reads
            if b.lastw is not None and not (accumulate and b.lastw[2] == e) and not fill:
                if b.lastw[2] != e or b.lastw[2] in ("sp", "poolq", "actq"):
                    self._wait(e, b.lastw)
                    for tok in b.wfill:
                        self._wait(e, tok)
            for tok in b.reads:
                if tok[2] != e:
                    self._wait(e, tok)

    def _commit(self, tok, reads, writes):
        for b in reads:
            if b is not None:
                b.reads.append(tok)
        for b in writes:
            if b is not None:
                if tok[2] == "dmaq" and b.lastw is not None and b.lastw[2] == "dmaq" and not b.reads:
                    b.wfill.append(b.lastw)
                else:
                    b.wfill = []
                b.lastw = tok
                b.reads = []

    def op(self, e, fn, reads=(), writes=(), accumulate=False):
        self._deps(e, reads, writes, accumulate)
        if self.cnt[e] >= EPOCH:
            self._new_epoch(e)
        inst = fn(self.eng[e])
        self.cnt[e] += 1
        inst.then_inc(self.sem[e], 1)
        tok = (self.sem[e], self.cnt[e], e)
        self.last[e] = tok
        self._commit(tok, reads, writes)
        return tok

    def _dma_sem(self):
        if self.dma_free:
            return self.dma_free.pop()
        s = [self._mksem(f"s_dma_{self.nsem}"), 0]
        self.dma_pool.append(s)
        return s

    def dma(self, q, fn, reads=(), writes=(), semslot=None):
        if q == "sp" and not [w for w in writes if w is not None] and getattr(self, "store_q", None):
            q = self.store_q
        self._deps(q, reads, writes, is_dma=True)
        if semslot is None:
            semslot = self._rot_sem(q)
        if semslot[1] > 0:
            self._wait(q, (semslot[0], semslot[1], "dmaq"))
        inst = fn(self.eng[q])
        semslot[1] += 16
        inst.then_inc(semslot[0], 16)
        tok = (semslot[0], semslot[1], "dmaq")
        self._commit(tok, reads, writes)
        self.dma_toks.append(tok)
        if len(self.dma_toks) > 64:
            self.dma_toks = self.dma_toks[-64:]
        return tok

    NROT = 16

    def _rot_sem(self, q="sp"):
        if not hasattr(self, "_rotq"):
            self._rotq = {}
        if q not in self._rotq:
            self._rotq[q] = {"sems": [[self._mksem(f"s_rot_{q}_{i}"), 0] for i in range(self.NROT)], "i": 0}
        pool = self._rotq[q]
        i = pool["i"]
        pool["i"] = (i + 1) % self.NROT
        return _RotSlot(pool["sems"], i)

    def barrier(self):
        toks = [t for t in self.last.values() if t is not None]
        toks += self.dma_toks
        for pool in getattr(self, "_rotq", {}).values():
            for s in pool["sems"]:
                if s[1] > 0:
                    toks.append((s[0], s[1], "dmaq"))
        for e in self.eng:
            self.pend[e] = list(toks)
        self.dma_toks = []

    def finish(self):
        self.barrier()
        for e in self.eng:
            for tok in self.pend[e]:
                self._wait(e, tok)
            self.pend[e] = []

    def mark(self):
        return len(self.bctx)

    def release(self, m):
        while len(self.bctx) > m:
            self.bctx.pop().__exit__(None, None, None)

    def close(self):
        self.release(0)
        for cm in reversed(self.ctx):
            cm.__exit__(None, None, None)
        self.ctx = []


class _RotSlot(list):
    def __init__(self, sems, i):
        super().__init__(sems[i])
        self.sems = sems
        self.i = i

    def __setitem__(self, k, v):
        super().__setitem__(k, v)
        self.sems[self.i][k] = v


T = 4096
D = 1024
NT = T // 128
EPS = 1e-6
MLA_SCALE = 192 ** -0.5
PI = float(np.pi)
TWO_PI = float(2 * np.pi)


class K:
    pass


def mm(fw, ob, o_ap, lb, l_ap, rb, r_ap, start, stop):
    return fw.op("pe", lambda e: e.matmul(o_ap, l_ap, r_ap, start=start, stop=stop),
                 reads=[lb, rb], writes=[ob], accumulate=not start)


def tr(fw, ob, o_ap, ib, i_ap, ident, id_ap, first=True):
    return fw.op("pe", lambda e: e.transpose(o_ap, i_ap, id_ap), reads=[ib, ident], writes=[ob],
                 accumulate=not first)


def build(stop="all", n_layers=2, debug=False):
    nc = bass.Bass("TRN2", target_bir_lowering=False)
    fw = FW(nc)
    k = K()

    def din(name, shape, dt=F32):
        return nc.dram_tensor(name, shape, dt, kind="ExternalInput").ap()

    x_in = din("x", [T, D])
    c_in = din("c", [128, 8])
    pos_in = din("pos", [1, T], I32)
    w_mod = din("w_mod", [2, D, 6 * D])
    b_mod = din("b_mod", [2, 6 * D])
    g_mix = din("g_mix", [2, D])
    g_ffn = din("g_ffn", [2, D])
    mla_w_in = din("mla_w_in", [D, 640])
    mla_gq = din("mla_gq", [128, 2])
    mla_gkv = din("mla_gkv", [128, 2])
    mla_wq = din("mla_wq", [256, 2048])
    mla_wkv = din("mla_wkv", [256, 2048])
    mla_wo = din("mla_wo", [D, D])
    w_router = din("w_router", [2, D, 32])
    b_router = din("b_router", [2, 32])
    w_gu = din("w_gu", [2 * 32 * 1024, 2 * D])
    b_gu = din("b_gu", [64, 2 * D])
    w_dn = din("w_dn", [2 * 32 * 1024, D])
    b_dn = din("b_dn", [64, D])
    g_final = din("g_final", [1, D])
    ssm_w_in = din("ssm_w_in", [D, 6176])
    ssm_cw = din("ssm_cw", [128, 32, 4])
    ssm_cb = din("ssm_cb", [128, 32])
    ssm_hv = din("ssm_hv", [32, 2])
    ssm_dexp = din("ssm_dexp", [1, 2048])
    ssm_gn = din("ssm_gn", [1, 2048])
    ssm_wo = din("ssm_wo", [2048, D])
    cst = din("cst", [128, 1024])
    invf = din("invf", [64, 2])
    out_d = nc.dram_tensor("out", [T, D], F32, kind="ExternalOutput").ap()
    dbg_d = nc.dram_tensor("dbg", [T, D], F32, kind="ExternalOutput").ap() if debug else None

    def dscr(name, shape, dt=F32):
        return nc.dram_tensor(name, shape, dt, kind="Internal").ap()

    xs = dscr("xs", [T, D])
    qT_d = dscr("qT_d", [8, 192, T], BF16)
    kT_d = dscr("kT_d", [8, 128, T], BF16)
    krT_d = dscr("krT_d", [64, T], BF16)
    v_d = dscr("v_d", [8, T, 128], BF16)
    oT_d = dscr("oT_d", [8, 128, T], BF16)
    yacc = dscr("yacc", [T, D])
    h2_d = dscr("h2_d", [T, D])
    xsort_d = dscr("xsort_d", [4 * T + 64 * 256, D])
    ysort_d = dscr("ysort_d", [4 * T + 64 * 256, D])
    sxs_d = dscr("sxs_d", [T, 2048])
    szs_d = dscr("szs_d", [T, 2048])
    sy_d = dscr("sy_d", [T, 2048])
    sbc_d = dscr("sbc_d", [16, 128, T], BF16)
    sac_d = dscr("sac_d", [32, T])
    synT_d = dscr("synT_d", [16, 128, T], BF16)

    C_ = fw.sb("cst_s", [128, 1024])
    fw.dma("sp", lambda e: e.dma_start(out=C_[:], in_=cst), writes=[C_])
    ident = C_
    ID = C_[:, 0:128]
    ONES = C_[:, 128:256]
    TRI = C_[:, 256:384]
    SL = C_[:, 384:512]
    onesr = fw.sb("onesr", [128, 128], F32R)
    fw.op("act", lambda e: e.activation(out=onesr[:], in_=C_[:, 128:256], func=AF.Copy), reads=[C_], writes=[onesr])
    trib = fw.sb("trib", [128, 128], BF16)
    fw.op("act", lambda e: e.activation(out=trib[:], in_=C_[:, 256:384], func=AF.Copy), reads=[C_], writes=[trib])
    epsc = fw.sb("epsc", [128, 1])
    fw.op("dve", lambda e: e.memset(epsc[:], EPS), writes=[epsc])
    PS = [fw.ps(f"ps{i}", [128, 512]) for i in range(8)]

    cond = fw.sb("cond", [128, 8])
    fw.dma("sp", lambda e: e.dma_start(out=cond[:], in_=c_in), writes=[cond])
    sg = fw.sb("sg", [128, 8])
    fw.op("act", lambda e: e.activation(out=sg[:], in_=cond[:], func=AF.Sigmoid), reads=[cond], writes=[sg])
    fw.op("dve", lambda e: e.tensor_tensor(out=cond[:], in0=cond[:], in1=sg[:], op=ALU.mult), reads=[cond, sg], writes=[cond])
    condb = fw.sb("condb", [128, 8, 128])
    for c in range(8):
        fw.op("dve", lambda e, c=c: e.tensor_scalar(out=condb[:, c, :], in0=C_[:, 128:256], scalar1=cond[:, c:c + 1],
                                                    scalar2=None, op0=ALU.mult), reads=[C_, cond], writes=[condb])
    MOD = fw.sb("MOD", [128, 6, D])

    def compute_mod(L):
        mkm = fw.mark()
        wmt = [fw.sb(f"wmt{i}", [128, 8, 512]) for i in range(2)]
        bmt = [fw.sb(f"bmt{i}", [128, 512]) for i in range(2)]
        gbc = fw.sb("gbc", [128, D])
        for n in range(12):
            wt = wmt[n % 2]
            bt = bmt[n % 2]
            fw.dma("sp", lambda e: e.dma_start(out=wt[:], in_=w_mod[L, :, n * 512:(n + 1) * 512].rearrange("(c p) n -> p c n", p=128)), writes=[wt])
            fw.dma("sp", lambda e: e.dma_start(out=bt[:], in_=b_mod[L:L + 1, n * 512:(n + 1) * 512].broadcast_to([128, 512])), writes=[bt])
            p = PS[n % 2]
            for c in range(8):
                mm(fw, p, p[:], condb, condb[:, c, :], wt, wt[:, c, :], c == 0, c == 7)
            fw.op("dve", lambda e: e.tensor_tensor(out=MOD[:, n // 2, (n % 2) * 512:(n % 2 + 1) * 512], in0=p[:], in1=bt[:], op=ALU.add),
                  reads=[p, bt], writes=[MOD])
        for (slot, g) in ((1, g_mix), (4, g_ffn)):
            fw.dma("sp", lambda e: e.dma_start(out=gbc[:], in_=g[L:L + 1, :].broadcast_to([128, D])), writes=[gbc])
            fw.op("dve", lambda e: e.scalar_tensor_tensor(out=MOD[:, slot, :], in0=MOD[:, slot, :], scalar=1.0, in1=gbc[:],
                                                          op0=ALU.add, op1=ALU.mult), reads=[MOD, gbc], writes=[MOD])
        fw.barrier()
        fw.release(mkm)

    xt = [fw.sb(f"xt{i}", [128, D]) for i in range(2)]
    ht = [fw.sb(f"ht{i}", [128, D]) for i in range(2)]
    junk = fw.sb("junk", [128, D])
    st = [fw.sb(f"st{i}", [128, 4]) for i in range(2)]

    def norm_mod(xb, hb, sb_, a_slot, s_slot):
        fw.op("act", lambda e: e.activation(out=junk[:], in_=xb[:], func=AF.Square, accum_out=sb_[:, 0:1]), reads=[xb], writes=[junk, sb_])
        fw.op("act", lambda e: e.activation(out=sb_[:, 1:2], in_=sb_[:, 0:1], func=AF.Sqrt, bias=epsc[:], scale=1.0 / D), reads=[sb_, epsc], writes=[sb_])
        fw.op("dve", lambda e: e.reciprocal(out=sb_[:, 2:3], in_=sb_[:, 1:2]), reads=[sb_], writes=[sb_])
        fw.op("dve", lambda e: e.scalar_tensor_tensor(out=hb[:], in0=xb[:], scalar=sb_[:, 2:3], in1=MOD[:, a_slot, :], op0=ALU.mult, op1=ALU.mult),
              reads=[xb, sb_, MOD], writes=[hb])
        if s_slot is not None:
            fw.op("dve", lambda e: e.tensor_tensor(out=hb[:], in0=hb[:], in1=MOD[:, s_slot, :], op=ALU.add), reads=[hb, MOD], writes=[hb])

    def transpose_to(hb, dstb, dst_fn, pa, pb):
        for half, p in ((0, pa), (1, pb)):
            for c in range(4):
                cc = half * 4 + c
                tr(fw, p, p[:, c * 128:(c + 1) * 128], hb, hb[:, cc * 128:(cc + 1) * 128], C_, ID, first=(c == 0))
            fw.op("act", lambda e, half=half, p=p: e.activation(out=dst_fn(half), in_=p[:].rearrange("p (c t) -> p c t", c=4), func=AF.Copy),
                  reads=[p], writes=[dstb])

    k.__dict__.update(locals())
    return k


def mla_layer(k, L, src_first):
    fw = k.fw; nc = k.nc; PS = k.PS; C_ = k.C_; MOD = k.MOD
    x_src = k.x_in if src_first else k.xs
    mk0 = fw.mark()
    w_in_s = fw.sb("mla_w_in_s", [128, 8, 640], F32R)
    for h2 in range(2):
        fw.dma("pool", lambda e: e.dma_start(out=w_in_s[:, h2 * 4:(h2 + 1) * 4, :], in_=k.mla_w_in[h2 * 512:(h2 + 1) * 512, :].rearrange("(c p) n -> p c n", p=128)), writes=[w_in_s])
    wq_s = fw.sb("wq_s", [128, 2, 2048], F32R)
    wkv_s = fw.sb("wkv_s", [128, 2, 2048], F32R)
    for c in range(2):
        fw.dma("pool", lambda e: e.dma_start(out=wq_s[:, c, :], in_=k.mla_wq[c * 128:(c + 1) * 128, :]), writes=[wq_s])
        fw.dma("pool", lambda e: e.dma_start(out=wkv_s[:, c, :], in_=k.mla_wkv[c * 128:(c + 1) * 128, :]), writes=[wkv_s])
    gq = fw.sb("gq", [128, 2]); gkv = fw.sb("gkv", [128, 2]); invf = fw.sb("invf_s", [64, 2])
    fw.dma("sp", lambda e: e.dma_start(out=gq[:], in_=k.mla_gq), writes=[gq])
    fw.dma("sp", lambda e: e.dma_start(out=gkv[:], in_=k.mla_gkv), writes=[gkv])
    fw.dma("sp", lambda e: e.dma_start(out=invf[:], in_=k.invf), writes=[invf])
    hT = fw.sb("hT", [128, 8, 512], F32R)
    sq = fw.sb("sq", [128, 2, 512], F32R)
    rstd = fw.sb("rstd", [128, 512])
    latn = fw.sb("latn", [128, 4, 512], F32R)
    posi = fw.sb("posi", [64, 512], I32)
    ang = fw.sb("ang", [64, 512]); kf = fw.sb("kf", [64, 512]); ki = fw.sb("ki", [64, 512], I32)
    rr = fw.sb("rr", [64, 512]); mwrap = fw.sb("mwrap", [64, 512])
    Ct = fw.sb("Ct", [64, 512]); St = fw.sb("St", [64, 512])
    t1 = fw.sb("t1", [64, 512]); t2 = fw.sb("t2", [64, 512])
    stq = [fw.sb(f"stq{i}", [128, 512], BF16) for i in range(2)]
    str_ = [fw.sb(f"str{i}", [64, 512], BF16) for i in range(2)]
    stv = [fw.sb(f"stv{i}", [128, 1024], BF16) for i in range(2)]

    def wrap_sin(dst, src, shift):
        fw.op("dve", lambda e: e.tensor_scalar(out=kf[:], in0=src[:], scalar1=shift, scalar2=1.0 / TWO_PI, op0=ALU.add, op1=ALU.mult), reads=[src], writes=[kf])
        fw.op("dve", lambda e: e.tensor_copy(out=ki[:], in_=kf[:]), reads=[kf], writes=[ki])
        fw.op("dve", lambda e: e.tensor_copy(out=kf[:], in_=ki[:]), reads=[ki], writes=[kf])
        fw.op("dve", lambda e: e.scalar_tensor_tensor(out=rr[:], in0=kf[:], scalar=-TWO_PI, in1=src[:], op0=ALU.mult, op1=ALU.add), reads=[kf, src], writes=[rr])
        if shift != 0.0:
            fw.op("dve", lambda e: e.tensor_scalar(out=rr[:], in0=rr[:], scalar1=shift, scalar2=None, op0=ALU.add), reads=[rr], writes=[rr])
        fw.op("dve", lambda e: e.tensor_scalar(out=mwrap[:], in0=rr[:], scalar1=PI, scalar2=-TWO_PI, op0=ALU.is_gt, op1=ALU.mult), reads=[rr], writes=[mwrap])
        fw.op("dve", lambda e: e.tensor_tensor(out=rr[:], in0=rr[:], in1=mwrap[:], op=ALU.add), reads=[rr, mwrap], writes=[rr])
        fw.op("dve", lambda e: e.tensor_scalar(out=mwrap[:], in0=rr[:], scalar1=-PI, scalar2=TWO_PI, op0=ALU.is_lt, op1=ALU.mult), reads=[rr], writes=[mwrap])
        fw.op("dve", lambda e: e.tensor_tensor(out=rr[:], in0=rr[:], in1=mwrap[:], op=ALU.add), reads=[rr, mwrap], writes=[rr])
        fw.op("dve", lambda e: e.tensor_scalar(out=rr[:], in0=rr[:], scalar1=PI, scalar2=-PI, op0=ALU.min, op1=ALU.max), reads=[rr], writes=[rr])
        fw.op("act", lambda e: e.activation(out=dst[:], in_=rr[:], func=AF.Sin), reads=[rr], writes=[dst])

    for j in range(T // 512):
        t0 = j * 512
        for r in range(4):
            xb = k.xt[r % 2]; hb = k.ht[r % 2]; sb_ = k.st[r % 2]
            fw.dma("sp", lambda e: e.dma_start(out=xb[:], in_=x_src[t0 + r * 128:t0 + (r + 1) * 128, :]), writes=[xb])
            if src_first:
                fw.dma("sp", lambda e: e.dma_start(out=k.xs[t0 + r * 128:t0 + (r + 1) * 128, :], in_=xb[:]), reads=[xb])
            k.norm_mod(xb, hb, sb_, 1, 0)
            k.transpose_to(hb, hT, lambda half, r=r: hT[:, half * 4:(half + 1) * 4, r * 128:(r + 1) * 128], PS[0], PS[1])
        fw.dma("sp", lambda e: e.dma_start(out=posi[:], in_=k.pos_in[0:1, t0:t0 + 512].broadcast_to([64, 512])), writes=[posi])
        fw.op("dve", lambda e: e.tensor_copy(out=ang[:], in_=posi[:]), reads=[posi], writes=[ang])
        fw.op("dve", lambda e: e.tensor_scalar(out=ang[:], in0=ang[:], scalar1=invf[:, 0:1], scalar2=None, op0=ALU.mult), reads=[ang, invf], writes=[ang])
        wrap_sin(St, ang, 0.0)
        wrap_sin(Ct, ang, PI / 2)
        fw.op("dve", lambda e: e.tensor_scalar(out=St[:], in0=St[:], scalar1=invf[:, 1:2], scalar2=None, op0=ALU.mult), reads=[St, invf], writes=[St])
        for oc in range(4):
            p = PS[2 + oc]
            for c in range(8):
                mm(fw, p, p[:], w_in_s, w_in_s[:, c, oc * 128:(oc + 1) * 128], hT, hT[:, c, :], c == 0, c == 7)
        for oc in range(2):
            p = PS[6 + oc]
            for c in range(8):
                mm(fw, p, p[0:64, :], w_in_s, w_in_s[:, c, 512 + oc * 64:512 + (oc + 1) * 64], hT, hT[:, c, :], c == 0, c == 7)
        for grp, gvec in ((0, gq), (1, gkv)):
            for c2 in range(2):
                p = PS[2 + grp * 2 + c2]
                fw.op("act", lambda e, p=p, c2=c2: e.activation(out=sq[:, c2, :], in_=p[:], func=AF.Square), reads=[p], writes=[sq])
            for c2 in range(2):
                mm(fw, PS[0], PS[0][:], k.onesr, k.onesr[:], sq, sq[:, c2, :], c2 == 0, c2 == 1)
            fw.op("act", lambda e: e.activation(out=rstd[:], in_=PS[0][:], func=AF.Sqrt, bias=k.epsc[:], scale=1.0 / 256), reads=[PS[0], k.epsc], writes=[rstd])
            fw.op("dve", lambda e: e.reciprocal(out=rstd[:], in_=rstd[:]), reads=[rstd], writes=[rstd])
            for c2 in range(2):
                p = PS[2 + grp * 2 + c2]
                fw.op("dve", lambda e, p=p, c2=c2, grp=grp, gvec=gvec: e.scalar_tensor_tensor(out=latn[:, grp * 2 + c2, :], in0=p[:], scalar=gvec[:, c2:c2 + 1], in1=rstd[:],
                                                                                     op0=ALU.mult, op1=ALU.mult), reads=[p, gvec, rstd], writes=[latn])
        sr = str_[0]
        fw.op("dve", lambda e: e.tensor_tensor(out=t1[:], in0=PS[6][0:64, :], in1=Ct[:], op=ALU.mult), reads=[PS[6], Ct], writes=[t1])
        fw.op("dve", lambda e: e.tensor_tensor(out=t2[:], in0=PS[7][0:64, :], in1=St[:], op=ALU.mult), reads=[PS[7], St], writes=[t2])
        fw.op("dve", lambda e: e.tensor_tensor(out=sr[:], in0=t1[:], in1=t2[:], op=ALU.add), reads=[t1, t2], writes=[sr])
        fw.dma("sp", lambda e: e.dma_start(out=k.krT_d[:, t0:t0 + 512], in_=sr[:]), reads=[sr])
        for h in range(8):
            pq = PS[2 + (h % 2) * 3]; pr = PS[3 + (h % 2) * 3]; prs = PS[4 + (h % 2) * 3]
            for c in range(2):
                mm(fw, pq, pq[:], wq_s, wq_s[:, c, h * 192:h * 192 + 128], latn, latn[:, c, :], c == 0, c == 1)
            for c in range(2):
                mm(fw, pr, pr[0:64, :], wq_s, wq_s[:, c, h * 192 + 128:h * 192 + 192], latn, latn[:, c, :], c == 0, c == 1)
            for c in range(2):
                mm(fw, prs, prs[0:64, :], wq_s, wq_s[:, c, 1536 + h * 64:1536 + (h + 1) * 64], latn, latn[:, c, :], c == 0, c == 1)
            sq_ = stq[h % 2]; sr = str_[(h + 1) % 2]
            fw.op("act", lambda e, pq=pq, sq_=sq_: e.activation(out=sq_[:], in_=pq[:], func=AF.Copy, scale=MLA_SCALE), reads=[pq], writes=[sq_])
            fw.dma("sp", lambda e, sq_=sq_, h=h: e.dma_start(out=k.qT_d[h, 0:128, t0:t0 + 512], in_=sq_[:]), reads=[sq_])
            fw.op("dve", lambda e, pr=pr: e.scalar_tensor_tensor(out=t1[:], in0=pr[0:64, :], scalar=MLA_SCALE, in1=Ct[:], op0=ALU.mult, op1=ALU.mult), reads=[pr, Ct], writes=[t1])
            fw.op("dve", lambda e, prs=prs: e.scalar_tensor_tensor(out=t2[:], in0=prs[0:64, :], scalar=MLA_SCALE, in1=St[:], op0=ALU.mult, op1=ALU.mult), reads=[prs, St], writes=[t2])
            fw.op("dve", lambda e, sr=sr: e.tensor_tensor(out=sr[:], in0=t1[:], in1=t2[:], op=ALU.add), reads=[t1, t2], writes=[sr])
            fw.dma("sp", lambda e, sr=sr, h=h: e.dma_start(out=k.qT_d[h, 128:192, t0:t0 + 512], in_=sr[:]), reads=[sr])
        for h in range(8):
            pk = PS[2 + (h % 2)]
            for c in range(2):
                mm(fw, pk, pk[:], wkv_s, wkv_s[:, c, h * 128:(h + 1) * 128], latn, latn[:, 2 + c, :], c == 0, c == 1)
            sk = stq[h % 2]
            fw.op("act", lambda e, pk=pk, sk=sk: e.activation(out=sk[:], in_=pk[:], func=AF.Copy), reads=[pk], writes=[sk])
            fw.dma("sp", lambda e, sk=sk, h=h: e.dma_start(out=k.kT_d[h, :, t0:t0 + 512], in_=sk[:]), reads=[sk])
        for r in range(4):
            sv = stv[r % 2]
            for half in range(2):
                p = PS[4 + half]
                for c in range(2):
                    mm(fw, p, p[:], latn, latn[:, 2 + c, r * 128:(r + 1) * 128], wkv_s, wkv_s[:, c, 1024 + half * 512:1024 + (half + 1) * 512], c == 0, c == 1)
                fw.op("act", lambda e, p=p, sv=sv, half=half: e.activation(out=sv[:, half * 512:(half + 1) * 512], in_=p[:], func=AF.Copy), reads=[p], writes=[sv])
            fw.dma("sp", lambda e, sv=sv, r=r: e.dma_start(out=k.v_d[:, t0 + r * 128:t0 + (r + 1) * 128, :].rearrange("h t d -> t h d"),
                                                            in_=sv[:].rearrange("t (h d) -> t h d", h=8)), reads=[sv])
    fw.barrier()
    fw.release(mk0)
    if k.stop == "A":
        return
    krT = fw.sb("krT", [64, T], BF16)
    fw.dma("sp", lambda e: e.dma_start(out=krT[:], in_=k.krT_d), writes=[krT])
    kTs = [fw.sb(f"kTs{i}", [128, T], BF16) for i in range(2)]
    vs = [fw.sb(f"vs{i}", [128, 32, 132], BF16) for i in range(2)]
    for i in range(2):
        fw.op("dve", lambda e, i=i: e.memset(vs[i][:, :, 128:129], 1.0), writes=[vs[i]])
    qn = [fw.sb(f"qn{i}", [128, 512], BF16) for i in range(2)]
    qr = [fw.sb(f"qr{i}", [64, 512], BF16) for i in range(2)]
    pT = [fw.sb(f"pT{i}", [128, 512], BF16) for i in range(3)]
    rs = fw.sb("rs", [128, 4])
    on = [fw.sb(f"on{i}", [128, 128]) for i in range(2)]
    oT = [fw.sb(f"oT{i}", [128, 512], BF16) for i in range(2)]
    blk = 0
    for h in range(8):
        kb = kTs[h % 2]; vb = vs[h % 2]
        fw.dma("sp", lambda e: e.dma_start(out=kb[:], in_=k.kT_d[h]), writes=[kb])
        for q4 in range(4):
            fw.dma("sp", lambda e, q4=q4: e.dma_start(out=vb[:, q4 * 8:(q4 + 1) * 8, 0:128],
                                                      in_=k.v_d[h, q4 * 1024:(q4 + 1) * 1024, :].rearrange("(c p) d -> p c d", p=128)), writes=[vb])
        for jq in range(8):
            qnb = qn[jq % 2]; qrb = qr[jq % 2]
            fw.dma("sp", lambda e: e.dma_start(out=qnb[:], in_=k.qT_d[h, 0:128, jq * 512:(jq + 1) * 512]), writes=[qnb])
            fw.dma("sp", lambda e: e.dma_start(out=qrb[:], in_=k.qT_d[h, 128:192, jq * 512:(jq + 1) * 512]), writes=[qrb])
            nk = 4 * jq + 4
            for kc in range(nk):
                r = max(0, kc - 4 * jq)
                q0 = r * 128
                sp_ = PS[blk % 3]; pb = pT[blk % 3]; blk += 1
                mm(fw, sp_, sp_[:, q0:512], kb, kb[:, kc * 128:(kc + 1) * 128], qnb, qnb[:, q0:512], True, False)
                mm(fw, sp_, sp_[:, q0:512], krT, krT[:, kc * 128:(kc + 1) * 128], qrb, qrb[:, q0:512], False, True)
                fw.op("act", lambda e, sp_=sp_, pb=pb, q0=q0: e.activation(out=pb[:, q0:512], in_=sp_[:, q0:512], func=AF.Exp), reads=[sp_], writes=[pb])
                if kc >= 4 * jq:
                    fw.op("dve", lambda e, pb=pb, q0=q0: e.tensor_tensor(out=pb[:, q0:q0 + 128], in0=pb[:, q0:q0 + 128], in1=k.trib[:], op=ALU.mult),
                          reads=[pb, k.trib], writes=[pb])
                for s_ in range(r, 4):
                    acc = PS[3 + s_]
                    mm(fw, acc, acc[:, 0:129], pb, pb[:, s_ * 128:(s_ + 1) * 128], vb, vb[:, kc, 0:129], kc == 0, kc == 4 * jq + s_)
            ob = oT[jq % 2]
            for s_ in range(4):
                acc = PS[3 + s_]; onb = on[s_ % 2]
                fw.op("dve", lambda e, acc=acc, s_=s_: e.reciprocal(out=rs[:, s_:s_ + 1], in_=acc[:, 128:129]), reads=[acc], writes=[rs])
                fw.op("act", lambda e, acc=acc, onb=onb, s_=s_: e.activation(out=onb[:], in_=acc[:, 0:128], func=AF.Copy, scale=rs[:, s_:s_ + 1]), reads=[acc, rs], writes=[onb])
                tr(fw, PS[7], PS[7][:, s_ * 128:(s_ + 1) * 128], onb, onb[:], C_, k.ID, first=(s_ == 0))
            fw.op("act", lambda e, ob=ob: e.activation(out=ob[:], in_=PS[7][:], func=AF.Copy), reads=[PS[7]], writes=[ob])
            fw.dma("sp", lambda e, ob=ob: e.dma_start(out=k.oT_d[h, :, jq * 512:(jq + 1) * 512], in_=ob[:]), reads=[ob])
    fw.barrier()
    fw.release(mk0)
    if k.stop == "B":
        return


def mixer_out_and_moe_router(k, L, w_out_d, n_kc, oT_src):
    fw = k.fw; PS = k.PS; C_ = k.C_; MOD = k.MOD
    mk0 = fw.mark()
    wo = fw.sb(f"wo_{L}", [128, n_kc, D], BF16)
    for c in range(n_kc):
        fw.dma("pool", lambda e, c=c: e.dma_start(out=wo[:, c, :], in_=w_out_d[c * 128:(c + 1) * 128, :]), writes=[wo])
    oTt = [fw.sb(f"oTt{L}_{i}", [128, n_kc, 512], BF16) for i in range(2)]
    ytmp = fw.sb(f"ytmp{L}", [128, D])
    for j in range(T // 512):
        ob = oTt[j % 2]
        fw.dma("sp", lambda e: e.dma_start(out=ob[:], in_=oT_src[:, :, j * 512:(j + 1) * 512].rearrange("h p t -> p h t")), writes=[ob])
        for r in range(4):
            t0 = j * 512 + r * 128
            xb = k.xt[r % 2]
            fw.dma("sp", lambda e: e.dma_start(out=xb[:], in_=k.xs[t0:t0 + 128, :]), writes=[xb])
            for half in range(2):
                p = PS[half]
                for c in range(n_kc):
                    mm(fw, p, p[:], ob, ob[:, c, r * 128:(r + 1) * 128], wo, wo[:, c, half * 512:(half + 1) * 512], c == 0, c == n_kc - 1)
                fw.op("dve", lambda e, p=p, half=half: e.tensor_tensor(out=ytmp[:, half * 512:(half + 1) * 512], in0=p[:], in1=MOD[:, 2, half * 512:(half + 1) * 512], op=ALU.mult),
                      reads=[p, MOD], writes=[ytmp])
            fw.op("dve", lambda e: e.tensor_tensor(out=xb[:], in0=xb[:], in1=ytmp[:], op=ALU.add), reads=[xb, ytmp], writes=[xb])
            fw.dma("sp", lambda e: e.dma_start(out=k.xs[t0:t0 + 128, :], in_=xb[:]), reads=[xb])
    fw.barrier()
    fw.release(mk0)


def moe_layer(k, L):
    fw = k.fw; PS = k.PS; C_ = k.C_; MOD = k.MOD; nc = k.nc
    mk0 = fw.mark()
    yacc = k.yacc
    h2T = fw.sb("h2T", [128, 8, T], BF16)
    G_all = fw.sb("G_all", [128, NT, 32])
    wr = fw.sb("wr", [128, 8, 32])
    fw.dma("sp", lambda e: e.dma_start(out=wr[:], in_=k.w_router[L].rearrange("(c p) n -> p c n", p=128)), writes=[wr])
    brb = fw.sb("brb", [128, 32])
    fw.dma("sp", lambda e: e.dma_start(out=brb[:], in_=k.b_router[L:L + 1, :].broadcast_to([128, 32])), writes=[brb])
    h2f = fw.sb("h2f", [128, 8, 128])
    lg = fw.sb("lg", [128, 32]); m8 = fw.sb("m8", [128, 8]); msk = fw.sb("msk", [128, 32]); ex = fw.sb("ex", [128, 32])
    sm = fw.sb("sm", [128, 4])
    MS = ""
    for i in range(NT):
        xb = k.xt[i % 2]; hb = k.ht[i % 2]; sb_ = k.st[i % 2]
        fw.dma("sp", lambda e: e.dma_start(out=xb[:], in_=k.xs[i * 128:(i + 1) * 128, :]), writes=[xb])
        if MS == "DL":
            continue
        k.norm_mod(xb, hb, sb_, 4, 3)
        if MS == "D0":
            continue
        for half, p in ((0, PS[0]), (1, PS[1])):
            for c in range(4):
                cc = half * 4 + c
                tr(fw, p, p[:, c * 128:(c + 1) * 128], hb, hb[:, cc * 128:(cc + 1) * 128], C_, k.ID, first=(c == 0))
            fw.op("act", lambda e, half=half, p=p: e.activation(out=h2f[:, half * 4:(half + 1) * 4, :], in_=p[:].rearrange("p (c t) -> p c t", c=4), func=AF.Copy),
                  reads=[p], writes=[h2f])
        fw.op("dve", lambda e: e.tensor_copy(out=h2T[:, :, i * 128:(i + 1) * 128], in_=h2f[:]), reads=[h2f], writes=[h2T])
        if MS == "D1":
            continue
        pl = PS[2]
        for c in range(8):
            mm(fw, pl, pl[:, 0:32], h2f, h2f[:, c, :], wr, wr[:, c, :], c == 0, c == 7)
        fw.op("dve", lambda e: e.tensor_tensor(out=lg[:], in0=pl[:, 0:32], in1=brb[:], op=ALU.add), reads=[pl, brb], writes=[lg])
        if MS == "D2":
            continue
        fw.op("dve", lambda e: e.max(out=m8[:], in_=lg[:]), reads=[lg], writes=[m8])
        fw.op("dve", lambda e: e.tensor_scalar(out=msk[:], in0=lg[:], scalar1=m8[:, 3:4], scalar2=None, op0=ALU.is_ge), reads=[lg, m8], writes=[msk])
        fw.op("dve", lambda e: e.tensor_scalar(out=sm[:, 0:1], in0=m8[:, 0:1], scalar1=-1.0, scalar2=None, op0=ALU.mult), reads=[m8], writes=[sm])
        fw.op("act", lambda e: e.activation(out=ex[:], in_=lg[:], func=AF.Exp, bias=sm[:, 0:1], scale=1.0), reads=[lg, sm], writes=[ex])
        fw.op("dve", lambda e: e.tensor_tensor(out=ex[:], in0=ex[:], in1=msk[:], op=ALU.mult), reads=[ex, msk], writes=[ex])
        fw.op("dve", lambda e: e.reduce_sum(out=sm[:, 1:2], in_=ex[:], axis=AX.X), reads=[ex], writes=[sm])
        fw.op("dve", lambda e: e.reciprocal(out=sm[:, 2:3], in_=sm[:, 1:2]), reads=[sm], writes=[sm])
        fw.op("dve", lambda e: e.tensor_scalar(out=G_all[:, i, :], in0=ex[:], scalar1=sm[:, 2:3], scalar2=None, op0=ALU.mult), reads=[ex, sm], writes=[G_all])
    fw.barrier()
    if MS:
        fw.release(mk0)
        return
    wgu = fw.sb("wgu", [128, 8, 2048], BF16)
    wdn = fw.sb("wdn", [128, 8, 1024], BF16)
    bgu = fw.sb("bgu", [128, 16])
    bdb = fw.sb("bdb", [128, 1024])
    hact = fw.sb("hact", [128, 8, 512], BF16)
    g_ = fw.sb("g_", [128, 512]); sg_ = fw.sb("sg_", [128, 512]); l_ = fw.sb("l_", [128, 512])
    yo = [fw.sb(f"yo{i}", [128, 1024]) for i in range(2)]
    for ex_i in range(32):
        for c in range(8):
            fw.dma("pool", lambda e, c=c: e.dma_start(out=wgu[:, c, :], in_=k.w_gu[L, ex_i, c * 128:(c + 1) * 128, :]), writes=[wgu])
        for c in range(8):
            fw.dma("pool", lambda e, c=c: e.dma_start(out=wdn[:, c, :], in_=k.w_dn[L, ex_i, c * 128:(c + 1) * 128, :]), writes=[wdn])
        with nc.allow_non_contiguous_dma(reason="small bias relayout"):
            fw.dma("sp", lambda e: e.dma_start(out=bgu[:], in_=k.b_gu[L, ex_i, :].rearrange("(c p) -> p c", p=128)), writes=[bgu])
        fw.dma("sp", lambda e: e.dma_start(out=bdb[:], in_=k.b_dn[L, ex_i:ex_i + 1, :].broadcast_to([128, 1024])), writes=[bdb])
        for jt in range(T // 512):
            for fc in range(8):
                pg = PS[(fc % 2) * 2]; pl2 = PS[(fc % 2) * 2 + 1]
                for c in range(8):
                    mm(fw, pg, pg[:], wgu, wgu[:, c, fc * 128:(fc + 1) * 128], h2T, h2T[:, c, jt * 512:(jt + 1) * 512], c == 0, c == 7)
                for c in range(8):
                    mm(fw, pl2, pl2[:], wgu, wgu[:, c, 1024 + fc * 128:1024 + (fc + 1) * 128], h2T, h2T[:, c, jt * 512:(jt + 1) * 512], c == 0, c == 7)
                fw.op("dve", lambda e, pg=pg, fc=fc: e.tensor_scalar(out=g_[:], in0=pg[:], scalar1=bgu[:, fc:fc + 1], scalar2=7.0, op0=ALU.add, op1=ALU.min), reads=[pg, bgu], writes=[g_])
                fw.op("act", lambda e: e.activation(out=sg_[:], in_=g_[:], func=AF.Sigmoid, scale=1.702), reads=[g_], writes=[sg_])
                fw.op("dve", lambda e, pl2=pl2, fc=fc: e.tensor_scalar(out=l_[:], in0=pl2[:], scalar1=bgu[:, 8 + fc:9 + fc], scalar2=7.0, op0=ALU.add, op1=ALU.min), reads=[pl2, bgu], writes=[l_])
                fw.op("dve", lambda e: e.tensor_scalar(out=l_[:], in0=l_[:], scalar1=-7.0, scalar2=1.0, op0=ALU.max, op1=ALU.add), reads=[l_], writes=[l_])
                fw.op("dve", lambda e: e.tensor_tensor(out=g_[:], in0=g_[:], in1=sg_[:], op=ALU.mult), reads=[g_, sg_], writes=[g_])
                fw.op("dve", lambda e, fc=fc: e.tensor_tensor(out=hact[:, fc, :], in0=g_[:], in1=l_[:], op=ALU.mult), reads=[g_, l_], writes=[hact])
            for r in range(4):
                ti = jt * 4 + r
                yb = yo[r % 2]
                for half in range(2):
                    p = PS[4 + half]
                    for fc in range(8):
                        mm(fw, p, p[:], hact, hact[:, fc, r * 128:(r + 1) * 128], wdn, wdn[:, fc, half * 512:(half + 1) * 512], fc == 0, fc == 7)
                    fw.op("dve", lambda e, p=p, half=half, yb=yb: e.tensor_tensor(out=yb[:, half * 512:(half + 1) * 512], in0=p[:], in1=bdb[:, half * 512:(half + 1) * 512], op=ALU.add),
                          reads=[p, bdb], writes=[yb])
                fw.op("dve", lambda e, yb=yb, ti=ti: e.tensor_scalar(out=yb[:], in0=yb[:], scalar1=G_all[:, ti, ex_i:ex_i + 1], scalar2=None, op0=ALU.mult), reads=[yb, G_all], writes=[yb])
                if ex_i == 0:
                    fw.dma("sp", lambda e, yb=yb, ti=ti: e.dma_start(out=yacc[ti * 128:(ti + 1) * 128, :], in_=yb[:]), reads=[yb])
                else:
                    fw.dma("pool", lambda e, yb=yb, ti=ti: e.dma_start(out=yacc[ti * 128:(ti + 1) * 128, :], in_=yb[:], accum_op=ALU.add), reads=[yb])
        fw.barrier()
    for i in range(NT):
        xb = k.xt[i % 2]; yb = yo[i % 2]
        fw.dma("sp", lambda e: e.dma_start(out=xb[:], in_=k.xs[i * 128:(i + 1) * 128, :]), writes=[xb])
        fw.dma("sp", lambda e: e.dma_start(out=yb[:], in_=yacc[i * 128:(i + 1) * 128, :]), writes=[yb])
        fw.op("dve", lambda e: e.tensor_tensor(out=yb[:], in0=yb[:], in1=MOD[:, 5, :], op=ALU.mult), reads=[yb, MOD], writes=[yb])
        fw.op("dve", lambda e: e.tensor_tensor(out=xb[:], in0=xb[:], in1=yb[:], op=ALU.add), reads=[xb, yb], writes=[xb])
        fw.dma("sp", lambda e: e.dma_start(out=k.xs[i * 128:(i + 1) * 128, :], in_=xb[:]), reads=[xb])
    fw.barrier()
    fw.release(mk0)


def final_norm(k):
    fw = k.fw
    gfb = fw.sb("gfb", [128, D])
    fw.dma("sp", lambda e: e.dma_start(out=gfb[:], in_=k.g_final[0:1, :].broadcast_to([128, D])), writes=[gfb])
    fw.op("dve", lambda e: e.tensor_copy(out=k.MOD[:, 1, :], in_=gfb[:]), reads=[gfb], writes=[k.MOD])
    for i in range(NT):
        xb = k.xt[i % 2]; hb = k.ht[i % 2]; sb_ = k.st[i % 2]
        fw.dma("sp", lambda e: e.dma_start(out=xb[:], in_=k.xs[i * 128:(i + 1) * 128, :]), writes=[xb])
        k.norm_mod(xb, hb, sb_, 1, None)
        fw.dma("sp", lambda e: e.dma_start(out=k.out_d[i * 128:(i + 1) * 128, :], in_=hb[:]), reads=[hb])


def ssd_layer(k, L):
    fw = k.fw; nc = k.nc; PS = k.PS; C_ = k.C_; MOD = k.MOD
    mk0 = fw.mark()
    w_in = k.ssm_w_in
    dt_tm = fw.sb("dt_tm", [128, NT, 32]); nac_tm = fw.sb("nac_tm", [128, NT, 32])
    mk_a = fw.mark()
    dtT = fw.sb("dtT", [32, T]); adtT = fw.sb("adtT", [32, T])
    mkA = fw.mark()
    hT = fw.sb("s_hT", [128, 8, 512], F32R)
    wsl = [fw.sb(f"wsl{i}", [128, 8, 512], F32R) for i in range(2)]
    wdt = fw.sb("wdt", [128, 8, 32], F32R)
    fw.dma("pool", lambda e: e.dma_start(out=wdt[:], in_=w_in[:, 6144:6176].rearrange("(c p) n -> p c n", p=128)), writes=[wdt])
    cw = fw.sb("cw", [128, 32, 4]); cb = fw.sb("cb", [128, 32])
    fw.dma("sp", lambda e: e.dma_start(out=cw[:], in_=k.ssm_cw), writes=[cw])
    fw.dma("sp", lambda e: e.dma_start(out=cb[:], in_=k.ssm_cb), writes=[cb])
    hv = fw.sb("hv", [32, 4])
    fw.dma("sp", lambda e: e.dma_start(out=hv[:, 0:2], in_=k.ssm_hv), writes=[hv])
    fw.op("act", lambda e: e.activation(out=hv[:, 2:3], in_=hv[:, 1:2], func=AF.Exp), reads=[hv], writes=[hv])
    fw.op("dve", lambda e: e.tensor_scalar(out=hv[:, 3:4], in0=hv[:, 2:3], scalar1=-1.0, scalar2=None, op0=ALU.mult), reads=[hv], writes=[hv])
    halo = fw.sb("halo", [128, 32, 4])
    fw.op("dve", lambda e: e.memset(halo[:], 0.0), writes=[halo])
    ub = [fw.sb(f"ub{i}", [128, 516]) for i in range(2)]
    acc = [fw.sb(f"cacc{i}", [128, 512]) for i in range(2)]
    xact = [fw.sb(f"xact{i}", [128, 512]) for i in range(2)]
    bcst = [fw.sb(f"bcst{i}", [128, 512], BF16) for i in range(2)]
    xtm = [fw.sb(f"xtm{i}", [128, 4, 512]) for i in range(2)]
    zst = [fw.sb(f"zst{i}", [128, 512]) for i in range(2)]
    et = fw.sb("et", [32, 512])
    nslab = 0
    for j in range(T // 512):
        t0 = j * 512
        for r in range(4):
            xb = k.xt[r % 2]; hb = k.ht[r % 2]; sb_ = k.st[r % 2]
            fw.dma("sp", lambda e: e.dma_start(out=xb[:], in_=k.xs[t0 + r * 128:t0 + (r + 1) * 128, :]), writes=[xb])
            k.norm_mod(xb, hb, sb_, 1, 0)
            k.transpose_to(hb, hT, lambda half, r=r: hT[:, half * 4:(half + 1) * 4, r * 128:(r + 1) * 128], PS[0], PS[1])
        pd = PS[2]
        for c in range(8):
            mm(fw, pd, pd[0:32, :], wdt, wdt[:, c, :], hT, hT[:, c, :], c == 0, c == 7)
        fw.op("act", lambda e: e.activation(out=et[:], in_=pd[0:32, :], func=AF.Exp, bias=hv[:, 0:1], scale=1.0), reads=[pd, hv], writes=[et])
        fw.op("act", lambda e: e.activation(out=dtT[:, t0:t0 + 512], in_=et[:], func=AF.Ln, bias=1.0, scale=1.0), reads=[et], writes=[dtT])
        fw.op("dve", lambda e: e.tensor_scalar(out=adtT[:, t0:t0 + 512], in0=dtT[:, t0:t0 + 512], scalar1=hv[:, 3:4], scalar2=None, op0=ALU.mult), reads=[dtT, hv], writes=[adtT])
        for sl in range(8):
            wb = wsl[nslab % 2]; nslab += 1
            for hf in range(2):
                fw.dma("pool", lambda e, hf=hf: e.dma_start(out=wb[:, hf * 4:(hf + 1) * 4, :],
                                                             in_=w_in[hf * 512:(hf + 1) * 512, 2048 + sl * 512:2048 + (sl + 1) * 512].rearrange("(c p) n -> p c n", p=128)), writes=[wb])
            for c4 in range(4):
                cc = sl * 4 + c4
                p = PS[3 + (cc % 2)]
                for c in range(8):
                    mm(fw, p, p[:], wb, wb[:, c, c4 * 128:(c4 + 1) * 128], hT, hT[:, c, :], c == 0, c == 7)
                u = ub[cc % 2]; a_ = acc[cc % 2]; xa = xact[cc % 2]
                fw.op("act", lambda e, p=p, u=u: e.activation(out=u[:, 3:515], in_=p[:], func=AF.Copy), reads=[p], writes=[u])
                fw.op("dve", lambda e, u=u, cc=cc: e.tensor_copy(out=u[:, 0:3], in_=halo[:, cc, 0:3]), reads=[halo], writes=[u])
                fw.op("dve", lambda e, u=u, cc=cc: e.tensor_copy(out=halo[:, cc, 0:3], in_=u[:, 512:515]), reads=[u], writes=[halo])
                fw.op("act", lambda e, u=u, a_=a_, cc=cc: e.activation(out=a_[:], in_=u[:, 3:515], func=AF.Identity, scale=cw[:, cc, 3:4], bias=cb[:, cc:cc + 1]),
                      reads=[u, cw, cb], writes=[a_])
                for kk in range(3):
                    fw.op("dve", lambda e, u=u, a_=a_, cc=cc, kk=kk: e.scalar_tensor_tensor(out=a_[:], in0=u[:, kk:kk + 512], scalar=cw[:, cc, kk:kk + 1], in1=a_[:],
                                                                                          op0=ALU.mult, op1=ALU.add), reads=[u, cw, a_], writes=[a_])
                if cc < 16:
                    fw.op("act", lambda e, a_=a_, xa=xa: e.activation(out=xa[:], in_=a_[:], func=AF.Silu), reads=[a_], writes=[xa])
                    xs_t = xtm[(cc // 4) % 2]
                    pt = PS[5 + (cc % 2)]
                    for r in range(4):
                        tr(fw, pt, pt[:, r * 128:(r + 1) * 128], xa, xa[:, r * 128:(r + 1) * 128], C_, k.ID, first=(r == 0))
                    fw.op("act", lambda e, pt=pt, xs_t=xs_t, c4=c4: e.activation(out=xs_t[:, :, c4 * 128:(c4 + 1) * 128], in_=pt[:].rearrange("p (r c) -> p r c", r=4), func=AF.Copy),
                          reads=[pt], writes=[xs_t])
                    if c4 == 3:
                        for r in range(4):
                            fw.dma("sp", lambda e, xs_t=xs_t, r=r: e.dma_start(out=k.sxs_d[t0 + r * 128:t0 + (r + 1) * 128, sl * 512:(sl + 1) * 512], in_=xs_t[:, r, :]), reads=[xs_t])
                else:
                    bs = bcst[cc % 2]
                    fw.op("act", lambda e, a_=a_, bs=bs: e.activation(out=bs[:], in_=a_[:], func=AF.Silu), reads=[a_], writes=[bs])
                    fw.dma("sp", lambda e, bs=bs, cc=cc: e.dma_start(out=k.sbc_d[cc - 16, :, t0:t0 + 512], in_=bs[:]), reads=[bs])
        for zs in range(4):
            wb = wsl[nslab % 2]; nslab += 1
            for hf in range(2):
                fw.dma("pool", lambda e, hf=hf: e.dma_start(out=wb[:, hf * 4:(hf + 1) * 4, :],
                                                             in_=w_in[hf * 512:(hf + 1) * 512, zs * 512:(zs + 1) * 512].rearrange("(c p) n -> p c n", p=128)), writes=[wb])
            for r in range(4):
                p = PS[3 + (r % 2)]
                for c in range(8):
                    mm(fw, p, p[:], hT, hT[:, c, r * 128:(r + 1) * 128], wb, wb[:, c, :], c == 0, c == 7)
                zt = zst[r % 2]
                fw.op("act", lambda e, p=p, zt=zt: e.activation(out=zt[:, 0:512], in_=p[:], func=AF.Silu), reads=[p], writes=[zt])
                fw.dma("sp", lambda e, zt=zt, r=r: e.dma_start(out=k.szs_d[t0 + r * 128:t0 + (r + 1) * 128, zs * 512:(zs + 1) * 512], in_=zt[:, 0:512]), reads=[zt])
    fw.barrier()
    fw.release(mkA)
    ones32 = fw.sb("ones32", [32, T])
    fw.op("dve", lambda e: e.memset(ones32[:], 1.0), writes=[ones32])
    acT = fw.sb("acT", [32, T])
    fw.op("dve", lambda e: e.tensor_tensor_scan(out=acT[:], data0=ones32[:], data1=adtT[:], initial=0.0, op0=ALU.mult, op1=ALU.add), reads=[ones32, adtT], writes=[acT])
    fw.dma("sp", lambda e: e.dma_start(out=k.sac_d, in_=acT[:]), reads=[acT])
    fw.barrier()
    for c in range(NT):
        p = PS[c % 2]
        tr(fw, p, p[:, 0:32], dtT, dtT[:, c * 128:(c + 1) * 128], C_, C_[0:32, 0:32])
        tr(fw, p, p[:, 32:64], acT, acT[:, c * 128:(c + 1) * 128], C_, C_[0:32, 0:32], first=False)
        fw.op("act", lambda e, p=p, c=c: e.activation(out=dt_tm[:, c, :], in_=p[:, 0:32], func=AF.Copy), reads=[p], writes=[dt_tm])
        fw.op("act", lambda e, p=p, c=c: e.activation(out=nac_tm[:, c, :], in_=p[:, 32:64], func=AF.Copy, scale=-1.0), reads=[p], writes=[nac_tm])
    fw.barrier()
    fw.release(mk_a)
    mk1 = fw.mark()
    BT = [fw.sb(f"BT{i}", [128, T], BF16) for i in range(2)]
    CT = [fw.sb(f"CT{i}", [128, T], BF16) for i in range(2)]
    abc = [fw.sb(f"abc{i}", [128, T]) for i in range(4)]
    xsf = fw.sb("xsf", [128, NT, 64])
    V = [fw.sb(f"V{i}", [128, NT, 64], BF16) for i in range(4)]
    dec = [fw.sb(f"dec{i}", [128, 512]) for i in range(2)]
    pT = [fw.sb(f"spT{i}", [128, 512], BF16) for i in range(3)]
    ysb = [fw.sb(f"ysb{i}", [128, 4, 64]) for i in range(2)]
    blk = 0; nd = 0; npt = 0
    for g in range(8):
        Bb = BT[g % 2]; Cb = CT[g % 2]
        fw.dma("sp", lambda e: e.dma_start(out=Bb[:], in_=k.sbc_d[g]), writes=[Bb])
        fw.dma("sp", lambda e: e.dma_start(out=Cb[:], in_=k.sbc_d[8 + g]), writes=[Cb])
        for r in range(4):
            hh = g * 4 + r
            fw.dma("sp", lambda e, r=r, hh=hh: e.dma_start(out=abc[r][:], in_=k.sac_d[hh:hh + 1, :].broadcast_to([128, T])), writes=[abc[r]])
            for q4 in range(4):
                fw.dma("sp", lambda e, q4=q4, hh=hh: e.dma_start(out=xsf[:, q4 * 8:(q4 + 1) * 8, :],
                                                                 in_=k.sxs_d[q4 * 1024:(q4 + 1) * 1024, hh * 64:(hh + 1) * 64].rearrange("(c p) d -> p c d", p=128)), writes=[xsf])
            fw.op("dve", lambda e, r=r, hh=hh: e.tensor_tensor(out=V[r][:], in0=xsf[:], in1=dt_tm[:, :, hh:hh + 1].broadcast_to([128, NT, 64]), op=ALU.mult),
                  reads=[xsf, dt_tm], writes=[V[r]])
        for jq in range(8):
            nk = 4 * jq + 4
            for kc in range(nk):
                r0 = max(0, kc - 4 * jq)
                q0 = r0 * 128
                diag = kc >= 4 * jq
                sp_ = PS[blk % 3]; blk += 1
                mm(fw, sp_, sp_[:, q0:512], Bb, Bb[:, kc * 128:(kc + 1) * 128], Cb, Cb[:, jq * 512 + q0:(jq + 1) * 512], True, True)
                for r in range(4):
                    hh = g * 4 + r
                    d_ = dec[nd % 2]; nd += 1
                    pb = pT[npt % 3]; npt += 1
                    qa = jq * 512 + q0
                    if diag:
                        fw.op("dve", lambda e, d_=d_, r=r, qa=qa, hh=hh: e.tensor_scalar(out=d_[:, q0:q0 + 128], in0=abc[r][:, qa:qa + 128], scalar1=nac_tm[:, kc, hh:hh + 1], scalar2=0.0,
                                                                                       op0=ALU.add, op1=ALU.min), reads=[abc[r], nac_tm], writes=[d_])
                        fw.op("act", lambda e, d_=d_: e.activation(out=d_[:, q0:q0 + 128], in_=d_[:, q0:q0 + 128], func=AF.Exp), reads=[d_], writes=[d_])
                        fw.op("dve", lambda e, d_=d_: e.tensor_tensor(out=d_[:, q0:q0 + 128], in0=d_[:, q0:q0 + 128], in1=C_[:, 256:384], op=ALU.mult), reads=[d_, C_], writes=[d_])
                        if q0 + 128 < 512:
                            fw.op("act", lambda e, d_=d_, r=r, qa=qa, hh=hh: e.activation(out=d_[:, q0 + 128:512], in_=abc[r][:, qa + 128:(jq + 1) * 512], func=AF.Exp,
                                                                                     bias=nac_tm[:, kc, hh:hh + 1], scale=1.0), reads=[abc[r], nac_tm, d_], writes=[d_])
                    else:
                        fw.op("act", lambda e, d_=d_, r=r, qa=qa, hh=hh: e.activation(out=d_[:, q0:512], in_=abc[r][:, qa:(jq + 1) * 512], func=AF.Exp,
                                                                                 bias=nac_tm[:, kc, hh:hh + 1], scale=1.0), reads=[abc[r], nac_tm], writes=[d_])
                    fw.op("dve", lambda e, d_=d_, pb=pb, sp_=sp_: e.tensor_tensor(out=pb[:, q0:512], in0=sp_[:, q0:512], in1=d_[:, q0:512], op=ALU.mult), reads=[sp_, d_], writes=[pb])
                    ya = PS[3 + r]
                    for s_ in range(r0, 4):
                        fw.op("pe", lambda e, ya=ya, pb=pb, s_=s_, r=r: e.matmul(ya[:, s_ * 64:(s_ + 1) * 64], pb[:, s_ * 128:(s_ + 1) * 128], V[r][:, kc, :],
                                                                               start=(kc == 0), stop=(kc == 4 * jq + s_)),
                              reads=[pb, V[r]], writes=[ya], accumulate=True)
            for r in range(4):
                hh = g * 4 + r
                ya = PS[3 + r]; yb = ysb[r % 2]
                fw.op("act", lambda e, ya=ya, yb=yb: e.activation(out=yb[:], in_=ya[:, 0:256].rearrange("p (s d) -> p s d", s=4), func=AF.Copy), reads=[ya], writes=[yb])
                fw.dma("sp", lambda e, yb=yb, hh=hh: e.dma_start(out=k.sy_d[jq * 512:(jq + 1) * 512, hh * 64:(hh + 1) * 64].rearrange("(s p) d -> p s d", p=128), in_=yb[:]), reads=[yb])
    fw.barrier()
    fw.release(mk1)
    dbc = fw.sb("dbc", [128, 2048]); gnb = fw.sb("gnb", [128, 2048])
    fw.dma("sp", lambda e: e.dma_start(out=dbc[:], in_=k.ssm_dexp[0:1, :].broadcast_to([128, 2048])), writes=[dbc])
    fw.dma("sp", lambda e: e.dma_start(out=gnb[:], in_=k.ssm_gn[0:1, :].broadcast_to([128, 2048])), writes=[gnb])
    yt = [fw.sb(f"yt{i}", [128, 2048]) for i in range(2)]
    xs2 = [fw.sb(f"xs2{i}", [128, 2048]) for i in range(2)]
    zz = [fw.sb(f"zz{i}", [128, 2048]) for i in range(2)]
    sqv = fw.sb("sqv", [128, 2048])
    gs = fw.sb("gs", [128, 16])
    ynT = [fw.sb(f"ynT{i}", [128, 16, 128], BF16) for i in range(2)]
    for i in range(NT):
        y_ = yt[i % 2]; x_ = xs2[i % 2]; z_ = zz[i % 2]
        fw.dma("sp", lambda e: e.dma_start(out=y_[:], in_=k.sy_d[i * 128:(i + 1) * 128, :]), writes=[y_])
        fw.dma("sp", lambda e: e.dma_start(out=x_[:], in_=k.sxs_d[i * 128:(i + 1) * 128, :]), writes=[x_])
        fw.dma("sp", lambda e: e.dma_start(out=z_[:], in_=k.szs_d[i * 128:(i + 1) * 128, :]), writes=[z_])
        fw.op("dve", lambda e: e.tensor_tensor(out=x_[:], in0=x_[:], in1=dbc[:], op=ALU.mult), reads=[x_, dbc], writes=[x_])
        fw.op("dve", lambda e: e.tensor_tensor(out=y_[:], in0=y_[:], in1=x_[:], op=ALU.add), reads=[y_, x_], writes=[y_])
        fw.op("dve", lambda e: e.tensor_tensor(out=y_[:], in0=y_[:], in1=z_[:], op=ALU.mult), reads=[y_, z_], writes=[y_])
        fw.op("act", lambda e: e.activation(out=sqv[:], in_=y_[:], func=AF.Square), reads=[y_], writes=[sqv])
        fw.op("dve", lambda e: e.tensor_reduce(out=gs[:, 0:8], in_=sqv[:].rearrange("p (g d) -> p g d", g=8), axis=AX.X, op=ALU.add), reads=[sqv], writes=[gs])
        fw.op("act", lambda e: e.activation(out=gs[:, 8:16], in_=gs[:, 0:8], func=AF.Sqrt, bias=k.epsc[:], scale=1.0 / 256), reads=[gs, k.epsc], writes=[gs])
        fw.op("dve", lambda e: e.reciprocal(out=gs[:, 8:16], in_=gs[:, 8:16]), reads=[gs], writes=[gs])
        for g in range(8):
            fw.op("dve", lambda e, g=g: e.scalar_tensor_tensor(out=y_[:, g * 256:(g + 1) * 256], in0=y_[:, g * 256:(g + 1) * 256], scalar=gs[:, 8 + g:9 + g],
                                                               in1=gnb[:, g * 256:(g + 1) * 256], op0=ALU.mult, op1=ALU.mult), reads=[y_, gs, gnb], writes=[y_])
        yT = ynT[i % 2]
        for q4 in range(4):
            p = PS[q4 % 2]
            for c in range(4):
                cc = q4 * 4 + c
                tr(fw, p, p[:, c * 128:(c + 1) * 128], y_, y_[:, cc * 128:(cc + 1) * 128], C_, k.ID, first=(c == 0))
            fw.op("act", lambda e, p=p, q4=q4: e.activation(out=yT[:, q4 * 4:(q4 + 1) * 4, :], in_=p[:].rearrange("p (c t) -> p c t", c=4), func=AF.Copy), reads=[p], writes=[yT])
        fw.dma("sp", lambda e: e.dma_start(out=k.synT_d[:, :, i * 128:(i + 1) * 128].rearrange("c p t -> p c t"), in_=yT[:]), reads=[yT])
    fw.barrier()
    fw.release(mk0)


MOE_B = 256
MOE_NB = 4 * T // MOE_B + 64


def moe_layer_sparse(k, L):
    fw = k.fw; PS = k.PS; C_ = k.C_; MOD = k.MOD; nc = k.nc
    NB = MOE_NB; B = MOE_B
    mk0 = fw.mark()
    S4_all = fw.sb("S4_all", [128, NT, 4], I32); G4_all = fw.sb("G4_all", [128, NT, 4])
    idx_w = fw.sb("idx_w", [128, 128], I32); idx_b = fw.sb("idx_b", [128, 128], I32)
    idx_g = [fw.sb(f"idx_g{i}", [128, 128], I32) for i in range(8)]
    mkD = fw.mark()
    G_all = fw.sb("G_all", [128, NT, 32]); M_all = fw.sb("M_all", [128, NT, 32]); R_all = fw.sb("R_all", [128, NT, 32])
    base = fw.sb("base", [128, 32])
    fw.op("dve", lambda e: e.memset(base[:], 0.0), writes=[base])
    wr = fw.sb("wr", [128, 8, 32])
    fw.dma("sp", lambda e: e.dma_start(out=wr[:], in_=k.w_router[L].rearrange("(c p) n -> p c n", p=128)), writes=[wr])
    brb = fw.sb("brb", [128, 32])
    fw.dma("sp", lambda e: e.dma_start(out=brb[:], in_=k.b_router[L:L + 1, :].broadcast_to([128, 32])), writes=[brb])
    h2f = fw.sb("h2f", [128, 8, 128])
    lg = fw.sb("lg", [128, 32]); m8 = fw.sb("m8", [128, 8]); ex = fw.sb("ex", [128, 32])
    sm = fw.sb("sm", [128, 4])
    for i in range(NT):
        xb = k.xt[i % 2]; hb = k.ht[i % 2]; sb_ = k.st[i % 2]
        fw.dma("sp", lambda e: e.dma_start(out=xb[:], in_=k.xs[i * 128:(i + 1) * 128, :]), writes=[xb])
        k.norm_mod(xb, hb, sb_, 4, 3)
        fw.dma("sp", lambda e: e.dma_start(out=k.h2_d[i * 128:(i + 1) * 128, :], in_=hb[:]), reads=[hb])
        for half, p in ((0, PS[0]), (1, PS[1])):
            for c in range(4):
                cc = half * 4 + c
                tr(fw, p, p[:, c * 128:(c + 1) * 128], hb, hb[:, cc * 128:(cc + 1) * 128], C_, k.ID, first=(c == 0))
            fw.op("act", lambda e, half=half, p=p: e.activation(out=h2f[:, half * 4:(half + 1) * 4, :], in_=p[:].rearrange("p (c t) -> p c t", c=4), func=AF.Copy),
                  reads=[p], writes=[h2f])
        pl = PS[2]
        for c in range(8):
            mm(fw, pl, pl[:, 0:32], h2f, h2f[:, c, :], wr, wr[:, c, :], c == 0, c == 7)
        msk = M_all
        fw.op("dve", lambda e: e.tensor_tensor(out=lg[:], in0=pl[:, 0:32], in1=brb[:], op=ALU.add), reads=[pl, brb], writes=[lg])
        fw.op("dve", lambda e: e.max(out=m8[:], in_=lg[:]), reads=[lg], writes=[m8])
        fw.op("dve", lambda e: e.tensor_scalar(out=M_all[:, i, :], in0=lg[:], scalar1=m8[:, 3:4], scalar2=None, op0=ALU.is_ge), reads=[lg, m8], writes=[M_all])
        fw.op("dve", lambda e: e.tensor_scalar(out=sm[:, 0:1], in0=m8[:, 0:1], scalar1=-1.0, scalar2=None, op0=ALU.mult), reads=[m8], writes=[sm])
        fw.op("act", lambda e: e.activation(out=ex[:], in_=lg[:], func=AF.Exp, bias=sm[:, 0:1], scale=1.0), reads=[lg, sm], writes=[ex])
        fw.op("dve", lambda e: e.tensor_tensor(out=ex[:], in0=ex[:], in1=M_all[:, i, :], op=ALU.mult), reads=[ex, M_all], writes=[ex])
        fw.op("dve", lambda e: e.reduce_sum(out=sm[:, 1:2], in_=ex[:], axis=AX.X), reads=[ex], writes=[sm])
        fw.op("dve", lambda e: e.reciprocal(out=sm[:, 2:3], in_=sm[:, 1:2]), reads=[sm], writes=[sm])
        fw.op("dve", lambda e: e.tensor_scalar(out=G_all[:, i, :], in0=ex[:], scalar1=sm[:, 2:3], scalar2=None, op0=ALU.mult), reads=[ex, sm], writes=[G_all])
        pr = PS[3]; pc = PS[4]
        mm(fw, pr, pr[:, 0:32], C_, C_[:, 384:512], M_all, M_all[:, i, :], True, True)
        mm(fw, pc, pc[:, 0:32], C_, C_[:, 128:256], M_all, M_all[:, i, :], True, True)
        fw.op("dve", lambda e: e.tensor_tensor(out=R_all[:, i, :], in0=pr[:, 0:32], in1=base[:], op=ALU.add), reads=[pr, base], writes=[R_all])
        fw.op("dve", lambda e: e.tensor_tensor(out=base[:], in0=pc[:, 0:32], in1=base[:], op=ALU.add), reads=[pc, base], writes=[base])
    ti = fw.sb("ti", [128, 32], I32); pf = fw.sb("pf", [128, 32]); pend = fw.sb("pend", [128, 32]); pst = fw.sb("pst", [128, 32])
    on32 = fw.sb("on32", [128, 32]); sl_ = fw.sb("sl_", [128, 32]); v_ = fw.sb("v_", [128, 32]); m8s = fw.sb("m8s", [128, 8]); jk = fw.sb("jk", [128, 32])
    fw.op("dve", lambda e: e.memset(on32[:], 1.0), writes=[on32])
    fw.op("dve", lambda e: e.tensor_scalar(out=pf[:], in0=base[:], scalar1=float(2 * B - 1), scalar2=None, op0=ALU.add), reads=[base], writes=[pf])
    fw.op("dve", lambda e: e.tensor_copy(out=ti[:], in_=pf[:]), reads=[pf], writes=[ti])
    sh = (2 * B).bit_length() - 1
    fw.op("dve", lambda e: e.tensor_scalar(out=ti[:], in0=ti[:], scalar1=sh, scalar2=sh, op0=ALU.logical_shift_right, op1=ALU.logical_shift_left), reads=[ti], writes=[ti])
    fw.op("dve", lambda e: e.tensor_copy(out=pf[:], in_=ti[:]), reads=[ti], writes=[pf])
    fw.op("dve", lambda e: e.tensor_tensor_scan(out=pend[:], data0=on32[:], data1=pf[:], initial=0.0, op0=ALU.mult, op1=ALU.add), reads=[on32, pf], writes=[pend])
    fw.op("dve", lambda e: e.tensor_tensor(out=pst[:], in0=pend[:], in1=pf[:], op=ALU.subtract), reads=[pend, pf], writes=[pst])
    for i in range(NT):
        fw.op("dve", lambda e: e.tensor_tensor(out=sl_[:], in0=R_all[:, i, :], in1=pst[:], op=ALU.add), reads=[R_all, pst], writes=[sl_])
        fw.op("dve", lambda e: e.scalar_tensor_tensor(out=v_[:], in0=sl_[:], scalar=1.0, in1=M_all[:, i, :], op0=ALU.add, op1=ALU.mult), reads=[sl_, M_all], writes=[v_])
        fw.op("dve", lambda e: e.max(out=m8s[:], in_=v_[:]), reads=[v_], writes=[m8s])
        fw.op("dve", lambda e: e.tensor_scalar(out=S4_all[:, i, :], in0=m8s[:, 0:4], scalar1=-1.0, scalar2=None, op0=ALU.add), reads=[m8s], writes=[S4_all])
        for kk in range(4):
            fw.op("dve", lambda e, kk=kk: e.scalar_tensor_tensor(out=jk[:], in0=v_[:], scalar=m8s[:, kk:kk + 1], in1=G_all[:, i, :], op0=ALU.is_equal, op1=ALU.mult,
                                                                accum_out=G4_all[:, i, kk:kk + 1]), reads=[v_, m8s, G_all], writes=[jk, G4_all])
    cmp_ = fw.sb("cmp_", [128, 32]); bec = fw.sb("bec", [128, 2]); ber = fw.sb("ber", [1, 136]); crow = fw.sb("crow", [1, 128]); vrow = fw.sb("vrow", [1, 128])
    fw.op("dve", lambda e: e.tensor_scalar(out=cmp_[:], in0=pend[:], scalar1=C_[:, 512:513], scalar2=None, op0=ALU.is_le), reads=[pend, C_], writes=[cmp_])
    fw.op("dve", lambda e: e.reduce_sum(out=bec[:, 0:1], in_=cmp_[:], axis=AX.X), reads=[cmp_], writes=[bec])
    fw.op("dve", lambda e: e.tensor_scalar(out=bec[:, 0:1], in0=bec[:, 0:1], scalar1=31.0, scalar2=None, op0=ALU.min), reads=[bec], writes=[bec])
    fw.op("dve", lambda e: e.tensor_scalar(out=bec[:, 1:2], in0=C_[:, 512:513], scalar1=pend[:, 31:32], scalar2=None, op0=ALU.is_lt), reads=[pend, C_], writes=[bec])
    pt_ = PS[5]
    tr(fw, pt_, pt_[0:1, 0:128], bec, bec[:, 0:1], C_, k.ID)
    tr(fw, pt_, pt_[0:1, 128:256], bec, bec[:, 1:2], C_, k.ID, first=False)
    fw.op("dve", lambda e: e.memset(ber[:], -1.0), writes=[ber])
    fw.op("act", lambda e: e.activation(out=ber[0:1, 4:132], in_=pt_[0:1, 0:128], func=AF.Copy), reads=[pt_], writes=[ber])
    fw.op("act", lambda e: e.activation(out=vrow[:], in_=pt_[0:1, 128:256], func=AF.Copy), reads=[pt_], writes=[vrow])
    fw.op("dve", lambda e: e.tensor_tensor(out=crow[:], in0=ber[0:1, 4:132], in1=ber[0:1, 0:128], op=ALU.not_equal), reads=[ber], writes=[crow])
    fw.op("dve", lambda e: e.tensor_tensor(out=crow[:], in0=crow[:], in1=vrow[:], op=ALU.mult), reads=[crow, vrow], writes=[crow])
    fw.op("dve", lambda e: e.tensor_tensor(out=crow[:], in0=crow[:], in1=C_[0:1, 640:768], op=ALU.mult), reads=[crow, C_], writes=[crow])
    BIG = 1.0e6
    pb1 = PS[6]; pb2 = PS[7]
    mm(fw, pb1, pb1[:, 0:128], C_, C_[0:1, 128:256], ber, ber[0:1, 4:132], True, True)
    mm(fw, pb2, pb2[:, 0:128], C_, C_[0:1, 128:256], crow, crow[0:1, :], True, True)
    cnd = fw.sb("cnd", [128, 128]); tw = fw.sb("tw", [128, 128]); tb = fw.sb("tb", [128, 128])
    fw.op("act", lambda e: e.activation(out=cnd[:], in_=pb2[:, 0:128], func=AF.Copy), reads=[pb2], writes=[cnd])
    fw.op("dve", lambda e: e.tensor_scalar(out=tb[:], in0=pb1[:, 0:128], scalar1=float(L * 32) - BIG, scalar2=None, op0=ALU.add), reads=[pb1], writes=[tb])
    fw.op("dve", lambda e: e.tensor_scalar(out=tw[:], in0=pb1[:, 0:128], scalar1=128.0, scalar2=float(L * 4096) - BIG, op0=ALU.mult, op1=ALU.add), reads=[pb1], writes=[tw])
    fw.op("dve", lambda e: e.tensor_scalar(out=tw[:], in0=tw[:], scalar1=C_[:, 513:514], scalar2=None, op0=ALU.add), reads=[tw, C_], writes=[tw])
    for t_, ix in ((tw, idx_w), (tb, idx_b)):
        fw.op("dve", lambda e, t_=t_: e.tensor_tensor(out=t_[:], in0=t_[:], in1=cnd[:], op=ALU.mult), reads=[t_, cnd], writes=[t_])
        fw.op("dve", lambda e, t_=t_, ix=ix: e.tensor_scalar(out=ix[:], in0=t_[:], scalar1=BIG, scalar2=None, op0=ALU.add), reads=[t_], writes=[ix])
    for c2 in range(8):
        fw.op("dve", lambda e, c2=c2: e.tensor_scalar(out=idx_g[c2][:], in0=tw[:], scalar1=8.0, scalar2=8 * BIG + c2, op0=ALU.mult, op1=ALU.add), reads=[tw], writes=[idx_g[c2]])
    fw.barrier()
    fw.release(mkD)
    MS = ""
    if MS == "E":
        dbt = k.xt[0]
        for j_, src in enumerate((idx_w, idx_b, idx_g[0], idx_g[1])):
            fw.op("dve", lambda e, j_=j_, src=src: e.tensor_copy(out=dbt[:, j_ * 128:(j_ + 1) * 128], in_=src[:]), reads=[src], writes=[dbt])
        fw.op("dve", lambda e: e.tensor_copy(out=dbt[:, 512:640], in_=S4_all[:].rearrange("p a b -> p (a b)")), reads=[S4_all], writes=[dbt])
        fw.op("dve", lambda e: e.tensor_copy(out=dbt[:, 640:768], in_=G4_all[:].rearrange("p a b -> p (a b)")), reads=[G4_all], writes=[dbt])
        fw.dma("sp", lambda e: e.dma_start(out=k.xs[0:128, :], in_=dbt[:]), reads=[dbt])
        fw.barrier()
        fw.release(mk0); return
    fw.store_q = None
    for i in range(NT):
        hb = k.ht[i % 2]
        fw.dma("sp", lambda e: e.dma_start(out=hb[:], in_=k.h2_d[i * 128:(i + 1) * 128, :]), writes=[hb])
        for kk in range(4):
            fw.dma("pool", lambda e, kk=kk: e.indirect_dma_start(out=k.xsort_d, out_offset=bass.IndirectOffsetOnAxis(ap=S4_all[:, i, kk:kk + 1], axis=0),
                                                                 in_=hb[:], in_offset=None), reads=[hb, S4_all])
    fw.barrier()
    if MS == "F":
        fw.release(mk0); return
    mkG = fw.mark()
    wgu = [fw.sb(f"wgu{i}", [128, 8, 2048], BF16) for i in range(2)]
    wdn = [fw.sb(f"wdn{i}", [128, 8, 1024], BF16) for i in range(2)]
    bgr = [fw.sb(f"bgr{i}", [128, 2048], BF16) for i in range(2)]
    bdb = [fw.sb(f"bdb{i}", [128, 1024]) for i in range(2)]
    onesb = fw.sb("onesb", [1, B], BF16)
    fw.op("dve", lambda e: e.memset(onesb[:], 1.0), writes=[onesb])
    xblk = [fw.sb(f"xblk{i}", [128, B // 128, 1024]) for i in range(1)]
    xT = [fw.sb(f"xT{i}", [128, 8, B], BF16) for i in range(2)]
    hact = fw.sb("hact", [128, 8, B], BF16)
    g_s = [fw.sb(f"g_{i}", [128, B]) for i in range(2)]; sg_s = [fw.sb(f"sg_{i}", [128, B]) for i in range(2)]; l_s = [fw.sb(f"l_{i}", [128, B]) for i in range(2)]
    yo = [fw.sb(f"yo{i}", [128, 1024]) for i in range(2)]
    bound_reg = nc.gpsimd.to_reg(2 * 32 * 1024 - 1)
    for bi in range(NB):
        pj = (bi // 2) % 2
        wg = wgu[pj]; wd = wdn[pj]; bg = bgr[pj]; bd = bdb[pj]
        NW = 2 * 32 * 128 - 1
        if bi % 2 == 0:
            for c in range(8):
                fw.dma("pool", lambda e, c=c: e.indirect_dma_start(out=wg[:, c, :], out_offset=None, in_=k.w_gu,
                                                                   in_offset=bass.IndirectOffsetOnAxis(ap=idx_g[c][:, bi:bi + 1], axis=0), bounds_check=bound_reg, oob_is_err=False),
                       reads=[idx_g[c]], writes=[wg])
            for c in range(8):
                fw.dma("pool", lambda e, c=c: e.indirect_dma_start(out=wd[:, c, :], out_offset=None, in_=k.w_dn,
                                                                   in_offset=bass.IndirectOffsetOnAxis(ap=idx_g[c][:, bi:bi + 1], axis=0), bounds_check=bound_reg, oob_is_err=False),
                       reads=[idx_g[c]], writes=[wd])
            fw.dma("pool", lambda e: e.indirect_dma_start(out=bg[:], out_offset=None, in_=k.b_gu, in_offset=bass.IndirectOffsetOnAxis(ap=idx_b[:, bi:bi + 1], axis=0),
                                                          bounds_check=bound_reg, oob_is_err=False), reads=[idx_b], writes=[bg])
            fw.dma("pool", lambda e: e.indirect_dma_start(out=bd[:], out_offset=None, in_=k.b_dn, in_offset=bass.IndirectOffsetOnAxis(ap=idx_b[:, bi:bi + 1], axis=0),
                                                          bounds_check=bound_reg, oob_is_err=False), reads=[idx_b], writes=[bd])
        xb = xblk[0]; xt_ = xT[bi % 2]
        if bi == 0:
            fw.dma("sp", lambda e: e.dma_start(out=xb[:], in_=k.xsort_d[0:B, :].rearrange("(r p) d -> p r d", p=128)), writes=[xb])
        for rt in range(B // 128):
            for half in range(2):
                p = PS[half]
                for c in range(4):
                    cc = half * 4 + c
                    tr(fw, p, p[:, c * 128:(c + 1) * 128], xb, xb[:, rt, bass.ds(cc, 128, 8)], C_, k.ID, first=(c == 0))
                fw.op("act", lambda e, half=half, p=p, rt=rt: e.activation(out=xt_[:, half * 4:(half + 1) * 4, rt * 128:(rt + 1) * 128], in_=p[:].rearrange("p (c t) -> p c t", c=4), func=AF.Copy),
                      reads=[p], writes=[xt_])
        if bi + 1 < NB:
            fw.dma("sp", lambda e: e.dma_start(out=xb[:], in_=k.xsort_d[(bi + 1) * B:(bi + 2) * B, :].rearrange("(r p) d -> p r d", p=128)), writes=[xb])
        for fc in range(8):
            pgl = PS[2 + (fc % 4)]
            g_ = g_s[fc % 2]; sg_ = sg_s[fc % 2]; l_ = l_s[fc % 2]
            for part, off in ((0, 0), (1, 1024)):
                o_ap = pgl[:, part * B:(part + 1) * B]
                for c in range(8):
                    mm(fw, pgl, o_ap, wg, wg[:, c, bass.ds(off + fc, 128, 8)], xt_, xt_[:, c, :], c == 0, False)
                mm(fw, pgl, o_ap, bg, bg[0:1, bass.ds(off + fc, 128, 8)], onesb, onesb[0:1, :], False, True)
            fw.op("dve", lambda e, pgl=pgl, g_=g_: e.tensor_scalar(out=g_[:], in0=pgl[:, 0:B], scalar1=7.0, scalar2=None, op0=ALU.min), reads=[pgl], writes=[g_])
            fw.op("act", lambda e: e.activation(out=sg_[:], in_=g_[:], func=AF.Sigmoid, scale=1.702), reads=[g_], writes=[sg_])
            fw.op("dve", lambda e, pgl=pgl: e.tensor_scalar(out=l_[:], in0=pgl[:, B:2 * B], scalar1=7.0, scalar2=-7.0, op0=ALU.min, op1=ALU.max), reads=[pgl], writes=[l_])
            fw.op("dve", lambda e: e.tensor_tensor(out=g_[:], in0=g_[:], in1=sg_[:], op=ALU.mult), reads=[g_, sg_], writes=[g_])
            fw.op("dve", lambda e, fc=fc: e.scalar_tensor_tensor(out=hact[:, fc, :], in0=l_[:], scalar=1.0, in1=g_[:], op0=ALU.add, op1=ALU.mult), reads=[g_, l_], writes=[hact])
        for rt in range(B // 128):
            yb = yo[rt % 2]
            for half in range(2):
                p = PS[6 + half]
                for fc in range(8):
                    mm(fw, p, p[:], hact, hact[:, fc, rt * 128:(rt + 1) * 128], wd, wd[:, fc, half * 512:(half + 1) * 512], fc == 0, fc == 7)
                fw.op("dve", lambda e, p=p, half=half, yb=yb: e.tensor_tensor(out=yb[:, half * 512:(half + 1) * 512], in0=p[:], in1=bd[:, half * 512:(half + 1) * 512], op=ALU.add),
                      reads=[p, bd], writes=[yb])
            fw.dma("sp", lambda e, yb=yb, rt=rt: e.dma_start(out=k.ysort_d[bi * B + rt * 128:bi * B + (rt + 1) * 128, :], in_=yb[:]), reads=[yb])
    fw.barrier()
    fw.release(mkG)
    if MS == "G":
        fw.release(mk0); return
    fw.store_q = "act"
    yk = [[fw.sb(f"yk{i}_{kk}", [128, 1024]) for kk in range(4)] for i in range(2)]
    for i in range(NT):
        xb = k.xt[i % 2]; ys_ = yk[i % 2]
        fw.dma("sp", lambda e: e.dma_start(out=xb[:], in_=k.xs[i * 128:(i + 1) * 128, :]), writes=[xb])
        for kk in range(4):
            fw.dma("pool", lambda e, kk=kk: e.indirect_dma_start(out=ys_[kk][:], out_offset=None, in_=k.ysort_d,
                                                                 in_offset=bass.IndirectOffsetOnAxis(ap=S4_all[:, i, kk:kk + 1], axis=0)), reads=[S4_all], writes=[ys_[kk]])
        a0 = ys_[0]
        fw.op("dve", lambda e: e.tensor_scalar(out=a0[:], in0=a0[:], scalar1=G4_all[:, i, 0:1], scalar2=None, op0=ALU.mult), reads=[a0, G4_all], writes=[a0])
        for kk in range(1, 4):
            fw.op("dve", lambda e, kk=kk: e.scalar_tensor_tensor(out=a0[:], in0=ys_[kk][:], scalar=G4_all[:, i, kk:kk + 1], in1=a0[:], op0=ALU.mult, op1=ALU.add),
                  reads=[ys_[kk], G4_all, a0], writes=[a0])
        fw.op("dve", lambda e: e.tensor_tensor(out=a0[:], in0=a0[:], in1=MOD[:, 5, :], op=ALU.mult), reads=[a0, MOD], writes=[a0])
        fw.op("dve", lambda e: e.tensor_tensor(out=xb[:], in0=xb[:], in1=a0[:], op=ALU.add), reads=[xb, a0], writes=[xb])
        fw.dma("sp", lambda e: e.dma_start(out=k.xs[i * 128:(i + 1) * 128, :], in_=xb[:]), reads=[xb])
    fw.barrier()
    fw.store_q = "pool"
    fw.release(mk0)


def host_consts():
    cst = np.zeros((128, 1024), np.float32)
    cst[:, 0:128] = np.eye(128, dtype=np.float32)
    cst[:, 128:256] = 1.0
    i = np.arange(128)
    cst[:, 256:384] = (i[None, :] >= i[:, None]).astype(np.float32)
    cst[:, 384:512] = (i[:, None] < i[None, :]).astype(np.float32)
    cst[:, 512] = i * 256.0
    cst[:, 513] = i
    cst[:, 640:768] = (i % 2 == 0).astype(np.float32)[None, :]
    inv = (1.0 / (10000.0 ** (np.arange(0, 64, 2, dtype=np.float32) / 64))).astype(np.float32)
    invf = np.zeros((64, 2), np.float32)
    invf[:32, 0] = inv; invf[32:, 0] = inv
    invf[:32, 1] = -1.0; invf[32:, 1] = 1.0
    return cst, invf


def prep_core(inp, b):
    f = np.ascontiguousarray
    cst, invf = host_consts()
    m = {}
    m["x"] = f(inp["x"][b])
    m["c"] = f(inp["c"][b].reshape(8, 128).T)
    m["pos"] = f(inp["positions"][b].reshape(1, T).astype(np.int32))
    m["w_mod"] = inp["w_mod"]; m["b_mod"] = inp["b_mod"]
    m["g_mix"] = inp["g_mix_norm"]; m["g_ffn"] = inp["g_ffn_norm"]
    w_in = inp["mla_w_in"][0]
    kr = w_in[:, 512:576]
    m["mla_w_in"] = f(np.concatenate([w_in, kr[:, 32:], kr[:, :32]], axis=1))
    m["mla_gq"] = f(inp["mla_g_q"][0].reshape(2, 128).T)
    m["mla_gkv"] = f(inp["mla_g_kv"][0].reshape(2, 128).T)
    wq = inp["mla_w_q_up"][0].reshape(256, 8, 192)
    sw = np.concatenate([wq[:, :, 160:192], wq[:, :, 128:160]], axis=2)
    m["mla_wq"] = f(np.concatenate([wq.reshape(256, 1536), sw.reshape(256, 512)], axis=1))
    wkv = inp["mla_w_kv_up"][0].reshape(256, 8, 256)
    m["mla_wkv"] = f(np.concatenate([wkv[:, :, :128].reshape(256, 1024), wkv[:, :, 128:].reshape(256, 1024)], axis=1))
    m["mla_wo"] = inp["mla_w_out"][0]
    m["w_router"] = inp["moe_w_router"]; m["b_router"] = inp["moe_b_router"]
    m["w_gu"] = inp["moe_w_gate_up"].reshape(2 * 32 * 1024, 2048); m["b_gu"] = inp["moe_b_gate_up"].reshape(64, 2048)
    m["w_dn"] = inp["moe_w_down"].reshape(2 * 32 * 1024, 1024); m["b_dn"] = inp["moe_b_down"].reshape(64, 1024)
    m["g_final"] = f(inp["g_final"].reshape(1, D))
    m["ssm_w_in"] = inp["ssm_w_in"][0]
    m["ssm_cw"] = f(inp["ssm_conv_w"][0].reshape(4, 32, 128).transpose(2, 1, 0))
    m["ssm_cb"] = f(inp["ssm_conv_b"][0].reshape(32, 128).T)
    m["ssm_hv"] = f(np.stack([inp["ssm_dt_bias"][0], inp["ssm_a_log"][0]], axis=1))
    m["ssm_dexp"] = f(np.repeat(inp["ssm_d"][0], 64).reshape(1, 2048))
    m["ssm_gn"] = f(inp["ssm_g_norm"][0].reshape(1, 2048))
    m["ssm_wo"] = inp["ssm_w_out"][0]
    m["cst"] = cst; m["invf"] = invf
    return m

_CACHE = {}


def _program():
    if "k" not in _CACHE:
        k = build()
        k.stop = "all"
        k.compute_mod(0)
        mla_layer(k, 0, True)
        mixer_out_and_moe_router(k, 0, k.mla_wo, 8, k.oT_d)
        moe_layer_sparse(k, 0)
        k.compute_mod(1)
        ssd_layer(k, 1)
        mixer_out_and_moe_router(k, 1, k.ssm_wo, 16, k.synT_d)
        moe_layer_sparse(k, 1)
        final_norm(k)
        k.fw.finish()
        k.fw.close()
        _CACHE["k"] = k
    return _CACHE["k"]


def kernel(**inputs):
    inp = {kk: np.asarray(v) for kk, v in inputs.items()}
    k = _program()
    in_maps = [prep_core(inp, b) for b in range(8)]
    res = run_bass_kernel_spmd(k.nc, in_maps, core_ids=list(range(8)))
    return np.stack([np.asarray(r["out"]) for r in res.results], axis=0).astype(np.float32)
```

```python
import numpy as np
import concourse.bass as bass
import concourse.mybir as mybir
from concourse.bass_utils import run_bass_kernel_spmd

F32 = mybir.dt.float32
F32R = mybir.dt.float32r
BF16 = mybir.dt.bfloat16
I32 = mybir.dt.int32
U32 = mybir.dt.uint32
ALU = mybir.AluOpType
AF = mybir.ActivationFunctionType
AX = mybir.AxisListType

EPOCH = 20000


class Buf:
    __slots__ = ("t", "name", "lastw", "reads", "wfill", "dsem", "dcount", "is_dram")

    def __init__(self, t, name, is_dram=False):
        self.t = t
        self.name = name
        self.lastw = None
        self.reads = []
        self.wfill = []
        self.is_dram = is_dram

    def __getitem__(self, idx):
        return self.t[idx]


class FW:
    def __init__(self, nc):
        self.nc = nc
        self.eng = {"pe": nc.tensor, "dve": nc.vector, "act": nc.scalar,
                    "pool": nc.gpsimd, "sp": nc.sync}
        self.sem = {}
        self.cnt = {}
        self.nsem = 0
        self.waited = {e: {} for e in self.eng}
        self.semobjs = {}
        self.ctx = []
        self.bctx = []
        self.last = {e: None for e in self.eng}
        self.pend = {e: [] for e in self.eng}
        self.store_q = "pool"
        self.dma_toks = []
        self.dma_pool = []
        self.dma_free = []
        for e in ("pe", "dve", "act", "pool"):
            self._new_epoch(e)

    def _mksem(self, name):
        cm = self.nc.semaphore(name)
        s = cm.__enter__()
        self.ctx.append(cm)
        self.nsem += 1
        self.semobjs[id(s)] = s
        return s

    def _new_epoch(self, e):
        self.sem[e] = self._mksem(f"s_{e}_{self.nsem}")
        self.cnt[e] = 0

    def sb(self, name, shape, dt=F32):
        self.uid = getattr(self, "uid", 0) + 1
        cm = self.nc.sbuf_tensor(f"{name}_u{self.uid}", shape, dt)
        t = cm.__enter__()
        self.bctx.append(cm)
        return Buf(t, name)

    def ps(self, name, shape, dt=F32):
        cm = self.nc.psum_tensor(name, shape, dt)
        t = cm.__enter__()
        self.ctx.append(cm)
        return Buf(t, name)

    def dram(self, name, shape, dt=F32, kind="Internal"):
        t = self.nc.dram_tensor(name, shape, dt, kind=kind)
        return Buf(t.ap(), name, is_dram=True)

    def _wait(self, e, tok):
        if tok is None:
            return
        sem, val, _ = tok
        w = self.waited[e]
        if w.get(id(sem), 0) >= val:
            return
        w[id(sem)] = val
        self.eng[e].wait_ge(sem, val)

    def _deps(self, e, reads, writes, accumulate=False, is_dma=False):
        for tok in self.pend[e]:
            self._wait(e, tok)
        self.pend[e] = []
        for b in reads:
            if b is None:
                continue
            if b.lastw is not None:
                self._wait(e, b.lastw)
            for tok in b.wfill:
                self._wait(e, tok)
        for b in writes:
            if b is None:
                continue
            fill = is_dma and b.lastw is not None and b.lastw[2] == "dmaq" and not b.reads
            if b.lastw is not None and not (accumulate and b.lastw[2] == e) and not fill:
                if b.lastw[2] != e or b.lastw[2] in ("sp", "poolq", "actq"):
                    self._wait(e, b.lastw)
                    for tok in b.wfill:
                        self._wait(e, tok)
            for tok in b.reads:
                if tok[2] != e:
                    self._wait(e, tok)

    def _commit(self, tok, reads, writes):
        for b in reads:
            if b is not None:
                b.reads.append(tok)
        for b in writes:
            if b is not None:
                if tok[2] == "dmaq" and b.lastw is not None and b.lastw[2] == "dmaq" and not b.reads:
                    b.wfill.append(b.lastw)
                else:
                    b.wfill = []
                b.lastw = tok
                b.reads = []

    def op(self, e, fn, reads=(), writes=(), accumulate=False):
        self._deps(e, reads, writes, accumulate)
        if self.cnt[e] >= EPOCH:
            self._new_epoch(e)
        inst = fn(self.eng[e])
        self.cnt[e] += 1
        inst.then_inc(self.sem[e], 1)
        tok = (self.sem[e], self.cnt[e], e)
        self.last[e] = tok
        self._commit(tok, reads, writes)
        return tok

    def _dma_sem(self):
        if self.dma_free:
            return self.dma_free.pop()
        s = [self._mksem(f"s_dma_{self.nsem}"), 0]
        self.dma_pool.append(s)
        return s

    def dma(self, q, fn, reads=(), writes=(), semslot=None):
        if q == "sp" and not [w for w in writes if w is not None] and getattr(self, "store_q", None):
            q = self.store_q
        self._deps(q, reads, writes, is_dma=True)
        if semslot is None:
            semslot = self._rot_sem(q)
        if semslot[1] > 0:
            self._wait(q, (semslot[0], semslot[1], "dmaq"))
        inst = fn(self.eng[q])
        semslot[1] += 16
        inst.then_inc(semslot[0], 16)
        tok = (semslot[0], semslot[1], "dmaq")
        self._commit(tok, reads, writes)
        self.dma_toks.append(tok)
        if len(self.dma_toks) > 64:
            self.dma_toks = self.dma_toks[-64:]
        return tok

    NROT = 16

    def _rot_sem(self, q="sp"):
        if not hasattr(self, "_rotq"):
            self._rotq = {}
        if q not in self._rotq:
            self._rotq[q] = {"sems": [[self._mksem(f"s_rot_{q}_{i}"), 0] for i in range(self.NROT)], "i": 0}
        pool = self._rotq[q]
        i = pool["i"]
        pool["i"] = (i + 1) % self.NROT
        return _RotSlot(pool["sems"], i)

    def barrier(self):
        toks = [t for t in self.last.values() if t is not None]
        toks += self.dma_toks
        for pool in getattr(self, "_rotq", {}).values():
            for s in pool["sems"]:
                if s[1] > 0:
                    toks.append((s[0], s[1], "dmaq"))
        for e in self.eng:
            self.pend[e] = list(toks)
        self.dma_toks = []

    def finish(self):
        self.barrier()
        for e in self.eng:
            for tok in self.pend[e]:
                self._wait(e, tok)
            self.pend[e] = []

    def mark(self):
        return len(self.bctx)

    def release(self, m):
        while len(self.bctx) > m:
            self.bctx.pop().__exit__(None, None, None)

    def close(self):
        self.release(0)
        for cm in reversed(self.ctx):
            cm.__exit__(None, None, None)
        self.ctx = []


class _RotSlot(list):
    def __init__(self, sems, i):
        super().__init__(sems[i])
        self.sems = sems
        self.i = i

    def __setitem__(self, k, v):
        super().__setitem__(k, v)
        self.sems[self.i][k] = v


T = 4096
D = 1024
NT = T // 128
EPS = 1e-6
MLA_SCALE = 192 ** -0.5
PI = float(np.pi)
TWO_PI = float(2 * np.pi)


class K:
    pass


def mm(fw, ob, o_ap, lb, l_ap, rb, r_ap, start, stop):
    return fw.op("pe", lambda e: e.matmul(o_ap, l_ap, r_ap, start=start, stop=stop),
                 reads=[lb, rb], writes=[ob], accumulate=not start)


def tr(fw, ob, o_ap, ib, i_ap, ident, id_ap, first=True):
    return fw.op("pe", lambda e: e.transpose(o_ap, i_ap, id_ap), reads=[ib, ident], writes=[ob],
                 accumulate=not first)


def build(stop="all", n_layers=2, debug=False):
    nc = bass.Bass("TRN2", target_bir_lowering=False)
    fw = FW(nc)
    k = K()

    def din(name, shape, dt=F32):
        return nc.dram_tensor(name, shape, dt, kind="ExternalInput").ap()

    x_in = din("x", [T, D])
    c_in = din("c", [128, 8])
    pos_in = din("pos", [1, T], I32)
    w_mod = din("w_mod", [2, D, 6 * D])
    b_mod = din("b_mod", [2, 6 * D])
    g_mix = din("g_mix", [2, D])
    g_ffn = din("g_ffn", [2, D])
    mla_w_in = din("mla_w_in", [D, 640])
    mla_gq = din("mla_gq", [128, 2])
    mla_gkv = din("mla_gkv", [128, 2])
    mla_wq = din("mla_wq", [256, 2048])
    mla_wkv = din("mla_wkv", [256, 2048])
    mla_wo = din("mla_wo", [D, D])
    w_router = din("w_router", [2, D, 32])
    b_router = din("b_router", [2, 32])
    w_gu = din("w_gu", [2 * 32 * 1024, 2 * D])
    b_gu = din("b_gu", [64, 2 * D])
    w_dn = din("w_dn", [2 * 32 * 1024, D])
    b_dn = din("b_dn", [64, D])
    g_final = din("g_final", [1, D])
    ssm_w_in = din("ssm_w_in", [D, 6176])
    ssm_cw = din("ssm_cw", [128, 32, 4])
    ssm_cb = din("ssm_cb", [128, 32])
    ssm_hv = din("ssm_hv", [32, 2])
    ssm_dexp = din("ssm_dexp", [1, 2048])
    ssm_gn = din("ssm_gn", [1, 2048])
    ssm_wo = din("ssm_wo", [2048, D])
    cst = din("cst", [128, 1024])
    invf = din("invf", [64, 2])
    out_d = nc.dram_tensor("out", [T, D], F32, kind="ExternalOutput").ap()
    dbg_d = nc.dram_tensor("dbg", [T, D], F32, kind="ExternalOutput").ap() if debug else None

    def dscr(name, shape, dt=F32):
        return nc.dram_tensor(name, shape, dt, kind="Internal").ap()

    xs = dscr("xs", [T, D])
    qT_d = dscr("qT_d", [8, 192, T], BF16)
    kT_d = dscr("kT_d", [8, 128, T], BF16)
    krT_d = dscr("krT_d", [64, T], BF16)
    v_d = dscr("v_d", [8, T, 128], BF16)
    oT_d = dscr("oT_d", [8, 128, T], BF16)
    yacc = dscr("yacc", [T, D])
    h2_d = dscr("h2_d", [T, D])
    xsort_d = dscr("xsort_d", [4 * T + 64 * 256, D])
    ysort_d = dscr("ysort_d", [4 * T + 64 * 256, D])
    sxs_d = dscr("sxs_d", [T, 2048])
    szs_d = dscr("szs_d", [T, 2048])
    sy_d = dscr("sy_d", [T, 2048])
    sbc_d = dscr("sbc_d", [16, 128, T], BF16)
    sac_d = dscr("sac_d", [32, T])
    synT_d = dscr("synT_d", [16, 128, T], BF16)

    C_ = fw.sb("cst_s", [128, 1024])
    fw.dma("sp", lambda e: e.dma_start(out=C_[:], in_=cst), writes=[C_])
    ident = C_
    ID = C_[:, 0:128]
    ONES = C_[:, 128:256]
    TRI = C_[:, 256:384]
    SL = C_[:, 384:512]
    onesr = fw.sb("onesr", [128, 128], F32R)
    fw.op("act", lambda e: e.activation(out=onesr[:], in_=C_[:, 128:256], func=AF.Copy), reads=[C_], writes=[onesr])
    trib = fw.sb("trib", [128, 128], BF16)
    fw.op("act", lambda e: e.activation(out=trib[:], in_=C_[:, 256:384], func=AF.Copy), reads=[C_], writes=[trib])
    epsc = fw.sb("epsc", [128, 1])
    fw.op("dve", lambda e: e.memset(epsc[:], EPS), writes=[epsc])
    PS = [fw.ps(f"ps{i}", [128, 512]) for i in range(8)]

    cond = fw.sb("cond", [128, 8])
    fw.dma("sp", lambda e: e.dma_start(out=cond[:], in_=c_in), writes=[cond])
    sg = fw.sb("sg", [128, 8])
    fw.op("act", lambda e: e.activation(out=sg[:], in_=cond[:], func=AF.Sigmoid), reads=[cond], writes=[sg])
    fw.op("dve", lambda e: e.tensor_tensor(out=cond[:], in0=cond[:], in1=sg[:], op=ALU.mult), reads=[cond, sg], writes=[cond])
    condb = fw.sb("condb", [128, 8, 128])
    for c in range(8):
        fw.op("dve", lambda e, c=c: e.tensor_scalar(out=condb[:, c, :], in0=C_[:, 128:256], scalar1=cond[:, c:c + 1],
                                                    scalar2=None, op0=ALU.mult), reads=[C_, cond], writes=[condb])
    MOD = fw.sb("MOD", [128, 6, D])

    def compute_mod(L):
        mkm = fw.mark()
        wmt = [fw.sb(f"wmt{i}", [128, 8, 512]) for i in range(2)]
        bmt = [fw.sb(f"bmt{i}", [128, 512]) for i in range(2)]
        gbc = fw.sb("gbc", [128, D])
        for n in range(12):
            wt = wmt[n % 2]
            bt = bmt[n % 2]
            fw.dma("sp", lambda e: e.dma_start(out=wt[:], in_=w_mod[L, :, n * 512:(n + 1) * 512].rearrange("(c p) n -> p c n", p=128)), writes=[wt])
            fw.dma("sp", lambda e: e.dma_start(out=bt[:], in_=b_mod[L:L + 1, n * 512:(n + 1) * 512].broadcast_to([128, 512])), writes=[bt])
            p = PS[n % 2]
            for c in range(8):
                mm(fw, p, p[:], condb, condb[:, c, :], wt, wt[:, c, :], c == 0, c == 7)
            fw.op("dve", lambda e: e.tensor_tensor(out=MOD[:, n // 2, (n % 2) * 512:(n % 2 + 1) * 512], in0=p[:], in1=bt[:], op=ALU.add),
                  reads=[p, bt], writes=[MOD])
        for (slot, g) in ((1, g_mix), (4, g_ffn)):
            fw.dma("sp", lambda e: e.dma_start(out=gbc[:], in_=g[L:L + 1, :].broadcast_to([128, D])), writes=[gbc])
            fw.op("dve", lambda e: e.scalar_tensor_tensor(out=MOD[:, slot, :], in0=MOD[:, slot, :], scalar=1.0, in1=gbc[:],
                                                          op0=ALU.add, op1=ALU.mult), reads=[MOD, gbc], writes=[MOD])
        fw.barrier()
        fw.release(mkm)

    xt = [fw.sb(f"xt{i}", [128, D]) for i in range(2)]
    ht = [fw.sb(f"ht{i}", [128, D]) for i in range(2)]
    junk = fw.sb("junk", [128, D])
    st = [fw.sb(f"st{i}", [128, 4]) for i in range(2)]

    def norm_mod(xb, hb, sb_, a_slot, s_slot):
        fw.op("act", lambda e: e.activation(out=junk[:], in_=xb[:], func=AF.Square, accum_out=sb_[:, 0:1]), reads=[xb], writes=[junk, sb_])
        fw.op("act", lambda e: e.activation(out=sb_[:, 1:2], in_=sb_[:, 0:1], func=AF.Sqrt, bias=epsc[:], scale=1.0 / D), reads=[sb_, epsc], writes=[sb_])
        fw.op("dve", lambda e: e.reciprocal(out=sb_[:, 2:3], in_=sb_[:, 1:2]), reads=[sb_], writes=[sb_])
        fw.op("dve", lambda e: e.scalar_tensor_tensor(out=hb[:], in0=xb[:], scalar=sb_[:, 2:3], in1=MOD[:, a_slot, :], op0=ALU.mult, op1=ALU.mult),
              reads=[xb, sb_, MOD], writes=[hb])
        if s_slot is not None:
            fw.op("dve", lambda e: e.tensor_tensor(out=hb[:], in0=hb[:], in1=MOD[:, s_slot, :], op=ALU.add), reads=[hb, MOD], writes=[hb])

    def transpose_to(hb, dstb, dst_fn, pa, pb):
        for half, p in ((0, pa), (1, pb)):
            for c in range(4):
                cc = half * 4 + c
                tr(fw, p, p[:, c * 128:(c + 1) * 128], hb, hb[:, cc * 128:(cc + 1) * 128], C_, ID, first=(c == 0))
            fw.op("act", lambda e, half=half, p=p: e.activation(out=dst_fn(half), in_=p[:].rearrange("p (c t) -> p c t", c=4), func=AF.Copy),
                  reads=[p], writes=[dstb])

    k.__dict__.update(locals())
    return k


def mla_layer(k, L, src_first):
    fw = k.fw; nc = k.nc; PS = k.PS; C_ = k.C_; MOD = k.MOD
    x_src = k.x_in if src_first else k.xs
    mk0 = fw.mark()
    w_in_s = fw.sb("mla_w_in_s", [128, 8, 640], F32R)
    for h2 in range(2):
        fw.dma("pool", lambda e: e.dma_start(out=w_in_s[:, h2 * 4:(h2 + 1) * 4, :], in_=k.mla_w_in[h2 * 512:(h2 + 1) * 512, :].rearrange("(c p) n -> p c n", p=128)), writes=[w_in_s])
    wq_s = fw.sb("wq_s", [128, 2, 2048], F32R)
    wkv_s = fw.sb("wkv_s", [128, 2, 2048], F32R)
    for c in range(2):
        fw.dma("pool", lambda e: e.dma_start(out=wq_s[:, c, :], in_=k.mla_wq[c * 128:(c + 1) * 128, :]), writes=[wq_s])
        fw.dma("pool", lambda e: e.dma_start(out=wkv_s[:, c, :], in_=k.mla_wkv[c * 128:(c + 1) * 128, :]), writes=[wkv_s])
    gq = fw.sb("gq", [128, 2]); gkv = fw.sb("gkv", [128, 2]); invf = fw.sb("invf_s", [64, 2])
    fw.dma("sp", lambda e: e.dma_start(out=gq[:], in_=k.mla_gq), writes=[gq])
    fw.dma("sp", lambda e: e.dma_start(out=gkv[:], in_=k.mla_gkv), writes=[gkv])
    fw.dma("sp", lambda e: e.dma_start(out=invf[:], in_=k.invf), writes=[invf])
    hT = fw.sb("hT", [128, 8, 512], F32R)
    sq = fw.sb("sq", [128, 2, 512], F32R)
    rstd = fw.sb("rstd", [128, 512])
    latn = fw.sb("latn", [128, 4, 512], F32R)
    posi = fw.sb("posi", [64, 512], I32)
    ang = fw.sb("ang", [64, 512]); kf = fw.sb("kf", [64, 512]); ki = fw.sb("ki", [64, 512], I32)
    rr = fw.sb("rr", [64, 512]); mwrap = fw.sb("mwrap", [64, 512])
    Ct = fw.sb("Ct", [64, 512]); St = fw.sb("St", [64, 512])
    t1 = fw.sb("t1", [64, 512]); t2 = fw.sb("t2", [64, 512])
    stq = [fw.sb(f"stq{i}", [128, 512], BF16) for i in range(2)]
    str_ = [fw.sb(f"str{i}", [64, 512], BF16) for i in range(2)]
    stv = [fw.sb(f"stv{i}", [128, 1024], BF16) for i in range(2)]

    def wrap_sin(dst, src, shift):
        fw.op("dve", lambda e: e.tensor_scalar(out=kf[:], in0=src[:], scalar1=shift, scalar2=1.0 / TWO_PI, op0=ALU.add, op1=ALU.mult), reads=[src], writes=[kf])
        fw.op("dve", lambda e: e.tensor_copy(out=ki[:], in_=kf[:]), reads=[kf], writes=[ki])
        fw.op("dve", lambda e: e.tensor_copy(out=kf[:], in_=ki[:]), reads=[ki], writes=[kf])
        fw.op("dve", lambda e: e.scalar_tensor_tensor(out=rr[:], in0=kf[:], scalar=-TWO_PI, in1=src[:], op0=ALU.mult, op1=ALU.add), reads=[kf, src], writes=[rr])
        if shift != 0.0:
            fw.op("dve", lambda e: e.tensor_scalar(out=rr[:], in0=rr[:], scalar1=shift, scalar2=None, op0=ALU.add), reads=[rr], writes=[rr])
        fw.op("dve", lambda e: e.tensor_scalar(out=mwrap[:], in0=rr[:], scalar1=PI, scalar2=-TWO_PI, op0=ALU.is_gt, op1=ALU.mult), reads=[rr], writes=[mwrap])
        fw.op("dve", lambda e: e.tensor_tensor(out=rr[:], in0=rr[:], in1=mwrap[:], op=ALU.add), reads=[rr, mwrap], writes=[rr])
        fw.op("dve", lambda e: e.tensor_scalar(out=mwrap[:], in0=rr[:], scalar1=-PI, scalar2=TWO_PI, op0=ALU.is_lt, op1=ALU.mult), reads=[rr], writes=[mwrap])
        fw.op("dve", lambda e: e.tensor_tensor(out=rr[:], in0=rr[:], in1=mwrap[:], op=ALU.add), reads=[rr, mwrap], writes=[rr])
        fw.op("dve", lambda e: e.tensor_scalar(out=rr[:], in0=rr[:], scalar1=PI, scalar2=-PI, op0=ALU.min, op1=ALU.max), reads=[rr], writes=[rr])
        fw.op("act", lambda e: e.activation(out=dst[:], in_=rr[:], func=AF.Sin), reads=[rr], writes=[dst])

    for j in range(T // 512):
        t0 = j * 512
        for r in range(4):
            xb = k.xt[r % 2]; hb = k.ht[r % 2]; sb_ = k.st[r % 2]
            fw.dma("sp", lambda e: e.dma_start(out=xb[:], in_=x_src[t0 + r * 128:t0 + (r + 1) * 128, :]), writes=[xb])
            if src_first:
                fw.dma("sp", lambda e: e.dma_start(out=k.xs[t0 + r * 128:t0 + (r + 1) * 128, :], in_=xb[:]), reads=[xb])
            k.norm_mod(xb, hb, sb_, 1, 0)
            k.transpose_to(hb, hT, lambda half, r=r: hT[:, half * 4:(half + 1) * 4, r * 128:(r + 1) * 128], PS[0], PS[1])
        fw.dma("sp", lambda e: e.dma_start(out=posi[:], in_=k.pos_in[0:1, t0:t0 + 512].broadcast_to([64, 512])), writes=[posi])
        fw.op("dve", lambda e: e.tensor_copy(out=ang[:], in_=posi[:]), reads=[posi], writes=[ang])
        fw.op("dve", lambda e: e.tensor_scalar(out=ang[:], in0=ang[:], scalar1=invf[:, 0:1], scalar2=None, op0=ALU.mult), reads=[ang, invf], writes=[ang])
        wrap_sin(St, ang, 0.0)
        wrap_sin(Ct, ang, PI / 2)
        fw.op("dve", lambda e: e.tensor_scalar(out=St[:], in0=St[:], scalar1=invf[:, 1:2], scalar2=None, op0=ALU.mult), reads=[St, invf], writes=[St])
        for oc in range(4):
            p = PS[2 + oc]
            for c in range(8):
                mm(fw, p, p[:], w_in_s, w_in_s[:, c, oc * 128:(oc + 1) * 128], hT, hT[:, c, :], c == 0, c == 7)
        for oc in range(2):
            p = PS[6 + oc]
            for c in range(8):
                mm(fw, p, p[0:64, :], w_in_s, w_in_s[:, c, 512 + oc * 64:512 + (oc + 1) * 64], hT, hT[:, c, :], c == 0, c == 7)
        for grp, gvec in ((0, gq), (1, gkv)):
            for c2 in range(2):
                p = PS[2 + grp * 2 + c2]
                fw.op("act", lambda e, p=p, c2=c2: e.activation(out=sq[:, c2, :], in_=p[:], func=AF.Square), reads=[p], writes=[sq])
            for c2 in range(2):
                mm(fw, PS[0], PS[0][:], k.onesr, k.onesr[:], sq, sq[:, c2, :], c2 == 0, c2 == 1)
            fw.op("act", lambda e: e.activation(out=rstd[:], in_=PS[0][:], func=AF.Sqrt, bias=k.epsc[:], scale=1.0 / 256), reads=[PS[0], k.epsc], writes=[rstd])
            fw.op("dve", lambda e: e.reciprocal(out=rstd[:], in_=rstd[:]), reads=[rstd], writes=[rstd])
            for c2 in range(2):
                p = PS[2 + grp * 2 + c2]
                fw.op("dve", lambda e, p=p, c2=c2, grp=grp, gvec=gvec: e.scalar_tensor_tensor(out=latn[:, grp * 2 + c2, :], in0=p[:], scalar=gvec[:, c2:c2 + 1], in1=rstd[:],
                                                                                     op0=ALU.mult, op1=ALU.mult), reads=[p, gvec, rstd], writes=[latn])
        sr = str_[0]
        fw.op("dve", lambda e: e.tensor_tensor(out=t1[:], in0=PS[6][0:64, :], in1=Ct[:], op=ALU.mult), reads=[PS[6], Ct], writes=[t1])
        fw.op("dve", lambda e: e.tensor_tensor(out=t2[:], in0=PS[7][0:64, :], in1=St[:], op=ALU.mult), reads=[PS[7], St], writes=[t2])
        fw.op("dve", lambda e: e.tensor_tensor(out=sr[:], in0=t1[:], in1=t2[:], op=ALU.add), reads=[t1, t2], writes=[sr])
        fw.dma("sp", lambda e: e.dma_start(out=k.krT_d[:, t0:t0 + 512], in_=sr[:]), reads=[sr])
        for h in range(8):
            pq = PS[2 + (h % 2) * 3]; pr = PS[3 + (h % 2) * 3]; prs = PS[4 + (h % 2) * 3]
            for c in range(2):
                mm(fw, pq, pq[:], wq_s, wq_s[:, c, h * 192:h * 192 + 128], latn, latn[:, c, :], c == 0, c == 1)
            for c in range(2):
                mm(fw, pr, pr[0:64, :], wq_s, wq_s[:, c, h * 192 + 128:h * 192 + 192], latn, latn[:, c, :], c == 0, c == 1)
            for c in range(2):
                mm(fw, prs, prs[0:64, :], wq_s, wq_s[:, c, 1536 + h * 64:1536 + (h + 1) * 64], latn, latn[:, c, :], c == 0, c == 1)
            sq_ = stq[h % 2]; sr = str_[(h + 1) % 2]
            fw.op("act", lambda e, pq=pq, sq_=sq_: e.activation(out=sq_[:], in_=pq[:], func=AF.Copy, scale=MLA_SCALE), reads=[pq], writes=[sq_])
            fw.dma("sp", lambda e, sq_=sq_, h=h: e.dma_start(out=k.qT_d[h, 0:128, t0:t0 + 512], in_=sq_[:]), reads=[sq_])
            fw.op("dve", lambda e, pr=pr: e.scalar_tensor_tensor(out=t1[:], in0=pr[0:64, :], scalar=MLA_SCALE, in1=Ct[:], op0=ALU.mult, op1=ALU.mult), reads=[pr, Ct], writes=[t1])
            fw.op("dve", lambda e, prs=prs: e.scalar_tensor_tensor(out=t2[:], in0=prs[0:64, :], scalar=MLA_SCALE, in1=St[:], op0=ALU.mult, op1=ALU.mult), reads=[prs, St], writes=[t2])
            fw.op("dve", lambda e, sr=sr: e.tensor_tensor(out=sr[:], in0=t1[:], in1=t2[:], op=ALU.add), reads=[t1, t2], writes=[sr])
            fw.dma("sp", lambda e, sr=sr, h=h: e.dma_start(out=k.qT_d[h, 128:192, t0:t0 + 512], in_=sr[:]), reads=[sr])
        for h in range(8):
            pk = PS[2 + (h % 2)]
            for c in range(2):
                mm(fw, pk, pk[:], wkv_s, wkv_s[:, c, h * 128:(h + 1) * 128], latn, latn[:, 2 + c, :], c == 0, c == 1)
            sk = stq[h % 2]
            fw.op("act", lambda e, pk=pk, sk=sk: e.activation(out=sk[:], in_=pk[:], func=AF.Copy), reads=[pk], writes=[sk])
            fw.dma("sp", lambda e, sk=sk, h=h: e.dma_start(out=k.kT_d[h, :, t0:t0 + 512], in_=sk[:]), reads=[sk])
        for r in range(4):
            sv = stv[r % 2]
            for half in range(2):
                p = PS[4 + half]
                for c in range(2):
                    mm(fw, p, p[:], latn, latn[:, 2 + c, r * 128:(r + 1) * 128], wkv_s, wkv_s[:, c, 1024 + half * 512:1024 + (half + 1) * 512], c == 0, c == 1)
                fw.op("act", lambda e, p=p, sv=sv, half=half: e.activation(out=sv[:, half * 512:(half + 1) * 512], in_=p[:], func=AF.Copy), reads=[p], writes=[sv])
            fw.dma("sp", lambda e, sv=sv, r=r: e.dma_start(out=k.v_d[:, t0 + r * 128:t0 + (r + 1) * 128, :].rearrange("h t d -> t h d"),
                                                            in_=sv[:].rearrange("t (h d) -> t h d", h=8)), reads=[sv])
    fw.barrier()
    fw.release(mk0)
    if k.stop == "A":
        return
    krT = fw.sb("krT", [64, T], BF16)
    fw.dma("sp", lambda e: e.dma_start(out=krT[:], in_=k.krT_d), writes=[krT])
    kTs = [fw.sb(f"kTs{i}", [128, T], BF16) for i in range(2)]
    vs = [fw.sb(f"vs{i}", [128, 32, 132], BF16) for i in range(2)]
    for i in range(2):
        fw.op("dve", lambda e, i=i: e.memset(vs[i][:, :, 128:129], 1.0), writes=[vs[i]])
    qn = [fw.sb(f"qn{i}", [128, 512], BF16) for i in range(2)]
    qr = [fw.sb(f"qr{i}", [64, 512], BF16) for i in range(2)]
    pT = [fw.sb(f"pT{i}", [128, 512], BF16) for i in range(3)]
    rs = fw.sb("rs", [128, 4])
    on = [fw.sb(f"on{i}", [128, 128]) for i in range(2)]
    oT = [fw.sb(f"oT{i}", [128, 512], BF16) for i in range(2)]
    blk = 0
    for h in range(8):
        kb = kTs[h % 2]; vb = vs[h % 2]
        fw.dma("sp", lambda e: e.dma_start(out=kb[:], in_=k.kT_d[h]), writes=[kb])
        for q4 in range(4):
            fw.dma("sp", lambda e, q4=q4: e.dma_start(out=vb[:, q4 * 8:(q4 + 1) * 8, 0:128],
                                                      in_=k.v_d[h, q4 * 1024:(q4 + 1) * 1024, :].rearrange("(c p) d -> p c d", p=128)), writes=[vb])
        for jq in range(8):
            qnb = qn[jq % 2]; qrb = qr[jq % 2]
            fw.dma("sp", lambda e: e.dma_start(out=qnb[:], in_=k.qT_d[h, 0:128, jq * 512:(jq + 1) * 512]), writes=[qnb])
            fw.dma("sp", lambda e: e.dma_start(out=qrb[:], in_=k.qT_d[h, 128:192, jq * 512:(jq + 1) * 512]), writes=[qrb])
            nk = 4 * jq + 4
            for kc in range(nk):
                r = max(0, kc - 4 * jq)
                q0 = r * 128
                sp_ = PS[blk % 3]; pb = pT[blk % 3]; blk += 1
                mm(fw, sp_, sp_[:, q0:512], kb, kb[:, kc * 128:(kc + 1) * 128], qnb, qnb[:, q0:512], True, False)
                mm(fw, sp_, sp_[:, q0:512], krT, krT[:, kc * 128:(kc + 1) * 128], qrb, qrb[:, q0:512], False, True)
                fw.op("act", lambda e, sp_=sp_, pb=pb, q0=q0: e.activation(out=pb[:, q0:512], in_=sp_[:, q0:512], func=AF.Exp), reads=[sp_], writes=[pb])
                if kc >= 4 * jq:
                    fw.op("dve", lambda e, pb=pb, q0=q0: e.tensor_tensor(out=pb[:, q0:q0 + 128], in0=pb[:, q0:q0 + 128], in1=k.trib[:], op=ALU.mult),
                          reads=[pb, k.trib], writes=[pb])
                for s_ in range(r, 4):
                    acc = PS[3 + s_]
                    mm(fw, acc, acc[:, 0:129], pb, pb[:, s_ * 128:(s_ + 1) * 128], vb, vb[:, kc, 0:129], kc == 0, kc == 4 * jq + s_)
            ob = oT[jq % 2]
            for s_ in range(4):
                acc = PS[3 + s_]; onb = on[s_ % 2]
                fw.op("dve", lambda e, acc=acc, s_=s_: e.reciprocal(out=rs[:, s_:s_ + 1], in_=acc[:, 128:129]), reads=[acc], writes=[rs])
                fw.op("act", lambda e, acc=acc, onb=onb, s_=s_: e.activation(out=onb[:], in_=acc[:, 0:128], func=AF.Copy, scale=rs[:, s_:s_ + 1]), reads=[acc, rs], writes=[onb])
                tr(fw, PS[7], PS[7][:, s_ * 128:(s_ + 1) * 128], onb, onb[:], C_, k.ID, first=(s_ == 0))
            fw.op("act", lambda e, ob=ob: e.activation(out=ob[:], in_=PS[7][:], func=AF.Copy), reads=[PS[7]], writes=[ob])
            fw.dma("sp", lambda e, ob=ob: e.dma_start(out=k.oT_d[h, :, jq * 512:(jq + 1) * 512], in_=ob[:]), reads=[ob])
    fw.barrier()
    fw.release(mk0)
    if k.stop == "B":
        return


def mixer_out_and_moe_router(k, L, w_out_d, n_kc, oT_src):
    fw = k.fw; PS = k.PS; C_ = k.C_; MOD = k.MOD
    mk0 = fw.mark()
    wo = fw.sb(f"wo_{L}", [128, n_kc, D], BF16)
    for c in range(n_kc):
        fw.dma("pool", lambda e, c=c: e.dma_start(out=wo[:, c, :], in_=w_out_d[c * 128:(c + 1) * 128, :]), writes=[wo])
    oTt = [fw.sb(f"oTt{L}_{i}", [128, n_kc, 512], BF16) for i in range(2)]
    ytmp = fw.sb(f"ytmp{L}", [128, D])
    for j in range(T // 512):
        ob = oTt[j % 2]
        fw.dma("sp", lambda e: e.dma_start(out=ob[:], in_=oT_src[:, :, j * 512:(j + 1) * 512].rearrange("h p t -> p h t")), writes=[ob])
        for r in range(4):
            t0 = j * 512 + r * 128
            xb = k.xt[r % 2]
            fw.dma("sp", lambda e: e.dma_start(out=xb[:], in_=k.xs[t0:t0 + 128, :]), writes=[xb])
            for half in range(2):
                p = PS[half]
                for c in range(n_kc):
                    mm(fw, p, p[:], ob, ob[:, c, r * 128:(r + 1) * 128], wo, wo[:, c, half * 512:(half + 1) * 512], c == 0, c == n_kc - 1)
                fw.op("dve", lambda e, p=p, half=half: e.tensor_tensor(out=ytmp[:, half * 512:(half + 1) * 512], in0=p[:], in1=MOD[:, 2, half * 512:(half + 1) * 512], op=ALU.mult),
                      reads=[p, MOD], writes=[ytmp])
            fw.op("dve", lambda e: e.tensor_tensor(out=xb[:], in0=xb[:], in1=ytmp[:], op=ALU.add), reads=[xb, ytmp], writes=[xb])
            fw.dma("sp", lambda e: e.dma_start(out=k.xs[t0:t0 + 128, :], in_=xb[:]), reads=[xb])
    fw.barrier()
    fw.release(mk0)


def moe_layer(k, L):
    fw = k.fw; PS = k.PS; C_ = k.C_; MOD = k.MOD; nc = k.nc
    mk0 = fw.mark()
    yacc = k.yacc
    h2T = fw.sb("h2T", [128, 8, T], BF16)
    G_all = fw.sb("G_all", [128, NT, 32])
    wr = fw.sb("wr", [128, 8, 32])
    fw.dma("sp", lambda e: e.dma_start(out=wr[:], in_=k.w_router[L].rearrange("(c p) n -> p c n", p=128)), writes=[wr])
    brb = fw.sb("brb", [128, 32])
    fw.dma("sp", lambda e: e.dma_start(out=brb[:], in_=k.b_router[L:L + 1, :].broadcast_to([128, 32])), writes=[brb])
    h2f = fw.sb("h2f", [128, 8, 128])
    lg = fw.sb("lg", [128, 32]); m8 = fw.sb("m8", [128, 8]); msk = fw.sb("msk", [128, 32]); ex = fw.sb("ex", [128, 32])
    sm = fw.sb("sm", [128, 4])
    MS = ""
    for i in range(NT):
        xb = k.xt[i % 2]; hb = k.ht[i % 2]; sb_ = k.st[i % 2]
        fw.dma("sp", lambda e: e.dma_start(out=xb[:], in_=k.xs[i * 128:(i + 1) * 128, :]), writes=[xb])
        if MS == "DL":
            continue
        k.norm_mod(xb, hb, sb_, 4, 3)
        if MS == "D0":
            continue
        for half, p in ((0, PS[0]), (1, PS[1])):
            for c in range(4):
                cc = half * 4 + c
                tr(fw, p, p[:, c * 128:(c + 1) * 128], hb, hb[:, cc * 128:(cc + 1) * 128], C_, k.ID, first=(c == 0))
            fw.op("act", lambda e, half=half, p=p: e.activation(out=h2f[:, half * 4:(half + 1) * 4, :], in_=p[:].rearrange("p (c t) -> p c t", c=4), func=AF.Copy),
                  reads=[p], writes=[h2f])
        fw.op("dve", lambda e: e.tensor_copy(out=h2T[:, :, i * 128:(i + 1) * 128], in_=h2f[:]), reads=[h2f], writes=[h2T])
        if MS == "D1":
            continue
        pl = PS[2]
        for c in range(8):
            mm(fw, pl, pl[:, 0:32], h2f, h2f[:, c, :], wr, wr[:, c, :], c == 0, c == 7)
        fw.op("dve", lambda e: e.tensor_tensor(out=lg[:], in0=pl[:, 0:32], in1=brb[:], op=ALU.add), reads=[pl, brb], writes=[lg])
        if MS == "D2":
            continue
        fw.op("dve", lambda e: e.max(out=m8[:], in_=lg[:]), reads=[lg], writes=[m8])
        fw.op("dve", lambda e: e.tensor_scalar(out=msk[:], in0=lg[:], scalar1=m8[:, 3:4], scalar2=None, op0=ALU.is_ge), reads=[lg, m8], writes=[msk])
        fw.op("dve", lambda e: e.tensor_scalar(out=sm[:, 0:1], in0=m8[:, 0:1], scalar1=-1.0, scalar2=None, op0=ALU.mult), reads=[m8], writes=[sm])
        fw.op("act", lambda e: e.activation(out=ex[:], in_=lg[:], func=AF.Exp, bias=sm[:, 0:1], scale=1.0), reads=[lg, sm], writes=[ex])
        fw.op("dve", lambda e: e.tensor_tensor(out=ex[:], in0=ex[:], in1=msk[:], op=ALU.mult), reads=[ex, msk], writes=[ex])
        fw.op("dve", lambda e: e.reduce_sum(out=sm[:, 1:2], in_=ex[:], axis=AX.X), reads=[ex], writes=[sm])
        fw.op("dve", lambda e: e.reciprocal(out=sm[:, 2:3], in_=sm[:, 1:2]), reads=[sm], writes=[sm])
        fw.op("dve", lambda e: e.tensor_scalar(out=G_all[:, i, :], in0=ex[:], scalar1=sm[:, 2:3], scalar2=None, op0=ALU.mult), reads=[ex, sm], writes=[G_all])
    fw.barrier()
    if MS:
        fw.release(mk0)
        return
    wgu = fw.sb("wgu", [128, 8, 2048], BF16)
    wdn = fw.sb("wdn", [128, 8, 1024], BF16)
    bgu = fw.sb("bgu", [128, 16])
    bdb = fw.sb("bdb", [128, 1024])
    hact = fw.sb("hact", [128, 8, 512], BF16)
    g_ = fw.sb("g_", [128, 512]); sg_ = fw.sb("sg_", [128, 512]); l_ = fw.sb("l_", [128, 512])
    yo = [fw.sb(f"yo{i}", [128, 1024]) for i in range(2)]
    for ex_i in range(32):
        for c in range(8):
            fw.dma("pool", lambda e, c=c: e.dma_start(out=wgu[:, c, :], in_=k.w_gu[L, ex_i, c * 128:(c + 1) * 128, :]), writes=[wgu])
        for c in range(8):
            fw.dma("pool", lambda e, c=c: e.dma_start(out=wdn[:, c, :], in_=k.w_dn[L, ex_i, c * 128:(c + 1) * 128, :]), writes=[wdn])
        with nc.allow_non_contiguous_dma(reason="small bias relayout"):
            fw.dma("sp", lambda e: e.dma_start(out=bgu[:], in_=k.b_gu[L, ex_i, :].rearrange("(c p) -> p c", p=128)), writes=[bgu])
        fw.dma("sp", lambda e: e.dma_start(out=bdb[:], in_=k.b_dn[L, ex_i:ex_i + 1, :].broadcast_to([128, 1024])), writes=[bdb])
        for jt in range(T // 512):
            for fc in range(8):
                pg = PS[(fc % 2) * 2]; pl2 = PS[(fc % 2) * 2 + 1]
                for c in range(8):
                    mm(fw, pg, pg[:], wgu, wgu[:, c, fc * 128:(fc + 1) * 128], h2T, h2T[:, c, jt * 512:(jt + 1) * 512], c == 0, c == 7)
                for c in range(8):
                    mm(fw, pl2, pl2[:], wgu, wgu[:, c, 1024 + fc * 128:1024 + (fc + 1) * 128], h2T, h2T[:, c, jt * 512:(jt + 1) * 512], c == 0, c == 7)
                fw.op("dve", lambda e, pg=pg, fc=fc: e.tensor_scalar(out=g_[:], in0=pg[:], scalar1=bgu[:, fc:fc + 1], scalar2=7.0, op0=ALU.add, op1=ALU.min), reads=[pg, bgu], writes=[g_])
                fw.op("act", lambda e: e.activation(out=sg_[:], in_=g_[:], func=AF.Sigmoid, scale=1.702), reads=[g_], writes=[sg_])
                fw.op("dve", lambda e, pl2=pl2, fc=fc: e.tensor_scalar(out=l_[:], in0=pl2[:], scalar1=bgu[:, 8 + fc:9 + fc], scalar2=7.0, op0=ALU.add, op1=ALU.min), reads=[pl2, bgu], writes=[l_])
                fw.op("dve", lambda e: e.tensor_scalar(out=l_[:], in0=l_[:], scalar1=-7.0, scalar2=1.0, op0=ALU.max, op1=ALU.add), reads=[l_], writes=[l_])
                fw.op("dve", lambda e: e.tensor_tensor(out=g_[:], in0=g_[:], in1=sg_[:], op=ALU.mult), reads=[g_, sg_], writes=[g_])
                fw.op("dve", lambda e, fc=fc: e.tensor_tensor(out=hact[:, fc, :], in0=g_[:], in1=l_[:], op=ALU.mult), reads=[g_, l_], writes=[hact])
            for r in range(4):
                ti = jt * 4 + r
                yb = yo[r % 2]
                for half in range(2):
                    p = PS[4 + half]
                    for fc in range(8):
                        mm(fw, p, p[:], hact, hact[:, fc, r * 128:(r + 1) * 128], wdn, wdn[:, fc, half * 512:(half + 1) * 512], fc == 0, fc == 7)
                    fw.op("dve", lambda e, p=p, half=half, yb=yb: e.tensor_tensor(out=yb[:, half * 512:(half + 1) * 512], in0=p[:], in1=bdb[:, half * 512:(half + 1) * 512], op=ALU.add),
                          reads=[p, bdb], writes=[yb])
                fw.op("dve", lambda e, yb=yb, ti=ti: e.tensor_scalar(out=yb[:], in0=yb[:], scalar1=G_all[:, ti, ex_i:ex_i + 1], scalar2=None, op0=ALU.mult), reads=[yb, G_all], writes=[yb])
                if ex_i == 0:
                    fw.dma("sp", lambda e, yb=yb, ti=ti: e.dma_start(out=yacc[ti * 128:(ti + 1) * 128, :], in_=yb[:]), reads=[yb])
                else:
                    fw.dma("pool", lambda e, yb=yb, ti=ti: e.dma_start(out=yacc[ti * 128:(ti + 1) * 128, :], in_=yb[:], accum_op=ALU.add), reads=[yb])
        fw.barrier()
    for i in range(NT):
        xb = k.xt[i % 2]; yb = yo[i % 2]
        fw.dma("sp", lambda e: e.dma_start(out=xb[:], in_=k.xs[i * 128:(i + 1) * 128, :]), writes=[xb])
        fw.dma("sp", lambda e: e.dma_start(out=yb[:], in_=yacc[i * 128:(i + 1) * 128, :]), writes=[yb])
        fw.op("dve", lambda e: e.tensor_tensor(out=yb[:], in0=yb[:], in1=MOD[:, 5, :], op=ALU.mult), reads=[yb, MOD], writes=[yb])
        fw.op("dve", lambda e: e.tensor_tensor(out=xb[:], in0=xb[:], in1=yb[:], op=ALU.add), reads=[xb, yb], writes=[xb])
        fw.dma("sp", lambda e: e.dma_start(out=k.xs[i * 128:(i + 1) * 128, :], in_=xb[:]), reads=[xb])
    fw.barrier()
    fw.release(mk0)


def final_norm(k):
    fw = k.fw
    gfb = fw.sb("gfb", [128, D])
    fw.dma("sp", lambda e: e.dma_start(out=gfb[:], in_=k.g_final[0:1, :].broadcast_to([128, D])), writes=[gfb])
    fw.op("dve", lambda e: e.tensor_copy(out=k.MOD[:, 1, :], in_=gfb[:]), reads=[gfb], writes=[k.MOD])
    for i in range(NT):
        xb = k.xt[i % 2]; hb = k.ht[i % 2]; sb_ = k.st[i % 2]
        fw.dma("sp", lambda e: e.dma_start(out=xb[:], in_=k.xs[i * 128:(i + 1) * 128, :]), writes=[xb])
        k.norm_mod(xb, hb, sb_, 1, None)
        fw.dma("sp", lambda e: e.dma_start(out=k.out_d[i * 128:(i + 1) * 128, :], in_=hb[:]), reads=[hb])


def ssd_layer(k, L):
    fw = k.fw; nc = k.nc; PS = k.PS; C_ = k.C_; MOD = k.MOD
    mk0 = fw.mark()
    w_in = k.ssm_w_in
    fw.store_q = None
    dt_tm = fw.sb("dt_tm", [128, NT, 32]); nac_tm = fw.sb("nac_tm", [128, NT, 32])
    mk_a = fw.mark()
    dtT = fw.sb("dtT", [32, T]); adtT = fw.sb("adtT", [32, T])
    mkA = fw.mark()
    hT = fw.sb("s_hT", [128, 8, 512], F32R)
    wsl = [fw.sb(f"wsl{i}", [128, 8, 512], F32R) for i in range(2)]
    wdt = fw.sb("wdt", [128, 8, 32], F32R)
    fw.dma("pool", lambda e: e.dma_start(out=wdt[:], in_=w_in[:, 6144:6176].rearrange("(c p) n -> p c n", p=128)), writes=[wdt])
    cw = fw.sb("cw", [128, 32, 4]); cb = fw.sb("cb", [128, 32])
    fw.dma("sp", lambda e: e.dma_start(out=cw[:], in_=k.ssm_cw), writes=[cw])
    fw.dma("sp", lambda e: e.dma_start(out=cb[:], in_=k.ssm_cb), writes=[cb])
    hv = fw.sb("hv", [32, 4])
    fw.dma("sp", lambda e: e.dma_start(out=hv[:, 0:2], in_=k.ssm_hv), writes=[hv])
    fw.op("act", lambda e: e.activation(out=hv[:, 2:3], in_=hv[:, 1:2], func=AF.Exp), reads=[hv], writes=[hv])
    fw.op("dve", lambda e: e.tensor_scalar(out=hv[:, 3:4], in0=hv[:, 2:3], scalar1=-1.0, scalar2=None, op0=ALU.mult), reads=[hv], writes=[hv])
    halo = fw.sb("halo", [128, 32, 4])
    fw.op("dve", lambda e: e.memset(halo[:], 0.0), writes=[halo])
    ub = [fw.sb(f"ub{i}", [128, 516]) for i in range(2)]
    acc = [fw.sb(f"cacc{i}", [128, 512]) for i in range(2)]
    xact = [fw.sb(f"xact{i}", [128, 512]) for i in range(2)]
    bcst = [fw.sb(f"bcst{i}", [128, 512], BF16) for i in range(2)]
    xtm = [fw.sb(f"xtm{i}", [128, 4, 512]) for i in range(2)]
    zst = [fw.sb(f"zst{i}", [128, 512]) for i in range(2)]
    et = fw.sb("et", [32, 512])
    nslab = 0
    for j in range(T // 512):
        t0 = j * 512
        for r in range(4):
            xb = k.xt[r % 2]; hb = k.ht[r % 2]; sb_ = k.st[r % 2]
            fw.dma("pool", lambda e: e.dma_start(out=xb[:], in_=k.xs[t0 + r * 128:t0 + (r + 1) * 128, :]), writes=[xb])
            k.norm_mod(xb, hb, sb_, 1, 0)
            k.transpose_to(hb, hT, lambda half, r=r: hT[:, half * 4:(half + 1) * 4, r * 128:(r + 1) * 128], PS[0], PS[1])
        pd = PS[2]
        for c in range(8):
            mm(fw, pd, pd[0:32, :], wdt, wdt[:, c, :], hT, hT[:, c, :], c == 0, c == 7)
        fw.op("act", lambda e: e.activation(out=et[:], in_=pd[0:32, :], func=AF.Exp, bias=hv[:, 0:1], scale=1.0), reads=[pd, hv], writes=[et])
        fw.op("act", lambda e: e.activation(out=dtT[:, t0:t0 + 512], in_=et[:], func=AF.Ln, bias=1.0, scale=1.0), reads=[et], writes=[dtT])
        fw.op("dve", lambda e: e.tensor_scalar(out=adtT[:, t0:t0 + 512], in0=dtT[:, t0:t0 + 512], scalar1=hv[:, 3:4], scalar2=None, op0=ALU.mult), reads=[dtT, hv], writes=[adtT])
        for sl in range(8):
            wb = wsl[nslab % 2]; nslab += 1
            for hf in range(2):
                fw.dma("pool", lambda e, hf=hf: e.dma_start(out=wb[:, hf * 4:(hf + 1) * 4, :],
                                                             in_=w_in[hf * 512:(hf + 1) * 512, 2048 + sl * 512:2048 + (sl + 1) * 512].rearrange("(c p) n -> p c n", p=128)), writes=[wb])
            for c4 in range(4):
                cc = sl * 4 + c4
                p = PS[3 + (cc % 2)]
                for c in range(8):
                    mm(fw, p, p[:], wb, wb[:, c, c4 * 128:(c4 + 1) * 128], hT, hT[:, c, :], c == 0, c == 7)
                u = ub[cc % 2]; a_ = acc[cc % 2]; xa = xact[cc % 2]
                fw.op("act", lambda e, p=p, u=u: e.activation(out=u[:, 3:515], in_=p[:], func=AF.Copy), reads=[p], writes=[u])
                fw.op("dve", lambda e, u=u, cc=cc: e.tensor_copy(out=u[:, 0:3], in_=halo[:, cc, 0:3]), reads=[halo], writes=[u])
                fw.op("dve", lambda e, u=u, cc=cc: e.tensor_copy(out=halo[:, cc, 0:3], in_=u[:, 512:515]), reads=[u], writes=[halo])
                fw.op("act", lambda e, u=u, a_=a_, cc=cc: e.activation(out=a_[:], in_=u[:, 3:515], func=AF.Identity, scale=cw[:, cc, 3:4], bias=cb[:, cc:cc + 1]),
                      reads=[u, cw, cb], writes=[a_])
                for kk in range(3):
                    fw.op("dve", lambda e, u=u, a_=a_, cc=cc, kk=kk: e.scalar_tensor_tensor(out=a_[:], in0=u[:, kk:kk + 512], scalar=cw[:, cc, kk:kk + 1], in1=a_[:],
                                                                                          op0=ALU.mult, op1=ALU.add), reads=[u, cw, a_], writes=[a_])
                if cc < 16:
                    fw.op("act", lambda e, a_=a_, xa=xa: e.activation(out=xa[:], in_=a_[:], func=AF.Silu), reads=[a_], writes=[xa])
                    xs_t = xtm[(cc // 4) % 2]
                    pt = PS[5 + (cc % 2)]
                    for r in range(4):
                        tr(fw, pt, pt[:, r * 128:(r + 1) * 128], xa, xa[:, r * 128:(r + 1) * 128], C_, k.ID, first=(r == 0))
                    fw.op("act", lambda e, pt=pt, xs_t=xs_t, c4=c4: e.activation(out=xs_t[:, :, c4 * 128:(c4 + 1) * 128], in_=pt[:].rearrange("p (r c) -> p r c", r=4), func=AF.Copy),
                          reads=[pt], writes=[xs_t])
                    if c4 == 3:
                        for r in range(4):
                            fw.dma("sp", lambda e, xs_t=xs_t, r=r: e.dma_start(out=k.sxs_d[t0 + r * 128:t0 + (r + 1) * 128, sl * 512:(sl + 1) * 512], in_=xs_t[:, r, :]), reads=[xs_t])
                else:
                    bs = bcst[cc % 2]
                    fw.op("act", lambda e, a_=a_, bs=bs: e.activation(out=bs[:], in_=a_[:], func=AF.Silu), reads=[a_], writes=[bs])
                    fw.dma("sp", lambda e, bs=bs, cc=cc: e.dma_start(out=k.sbc_d[cc - 16, :, t0:t0 + 512], in_=bs[:]), reads=[bs])
        for zs in range(4):
            wb = wsl[nslab % 2]; nslab += 1
            for hf in range(2):
                fw.dma("pool", lambda e, hf=hf: e.dma_start(out=wb[:, hf * 4:(hf + 1) * 4, :],
                                                             in_=w_in[hf * 512:(hf + 1) * 512, zs * 512:(zs + 1) * 512].rearrange("(c p) n -> p c n", p=128)), writes=[wb])
            for r in range(4):
                p = PS[3 + (r % 2)]
                for c in range(8):
                    mm(fw, p, p[:], hT, hT[:, c, r * 128:(r + 1) * 128], wb, wb[:, c, :], c == 0, c == 7)
                zt = zst[r % 2]
                fw.op("act", lambda e, p=p, zt=zt: e.activation(out=zt[:, 0:512], in_=p[:], func=AF.Silu), reads=[p], writes=[zt])
                fw.dma("sp", lambda e, zt=zt, r=r: e.dma_start(out=k.szs_d[t0 + r * 128:t0 + (r + 1) * 128, zs * 512:(zs + 1) * 512], in_=zt[:, 0:512]), reads=[zt])
    fw.barrier()
    fw.store_q = "pool"
    fw.release(mkA)
    ones32 = fw.sb("ones32", [32, T])
    fw.op("dve", lambda e: e.memset(ones32[:], 1.0), writes=[ones32])
    acT = fw.sb("acT", [32, T])
    fw.op("dve", lambda e: e.tensor_tensor_scan(out=acT[:], data0=ones32[:], data1=adtT[:], initial=0.0, op0=ALU.mult, op1=ALU.add), reads=[ones32, adtT], writes=[acT])
    fw.dma("sp", lambda e: e.dma_start(out=k.sac_d, in_=acT[:]), reads=[acT])
    fw.barrier()
    for c in range(NT):
        p = PS[c % 2]
        tr(fw, p, p[:, 0:32], dtT, dtT[:, c * 128:(c + 1) * 128], C_, C_[0:32, 0:32])
        tr(fw, p, p[:, 32:64], acT, acT[:, c * 128:(c + 1) * 128], C_, C_[0:32, 0:32], first=False)
        fw.op("act", lambda e, p=p, c=c: e.activation(out=dt_tm[:, c, :], in_=p[:, 0:32], func=AF.Copy), reads=[p], writes=[dt_tm])
        fw.op("act", lambda e, p=p, c=c: e.activation(out=nac_tm[:, c, :], in_=p[:, 32:64], func=AF.Copy, scale=-1.0), reads=[p], writes=[nac_tm])
    fw.barrier()
    fw.release(mk_a)
    mk1 = fw.mark()
    BT = [fw.sb(f"BT{i}", [128, T], BF16) for i in range(2)]
    CT = [fw.sb(f"CT{i}", [128, T], BF16) for i in range(2)]
    abc = [fw.sb(f"abc{i}", [128, T]) for i in range(4)]
    xsf = fw.sb("xsf", [128, NT, 64])
    V = [fw.sb(f"V{i}", [128, NT, 64], BF16) for i in range(4)]
    dec = [fw.sb(f"dec{i}", [128, 512]) for i in range(4)]
    pT = [fw.sb(f"spT{i}", [128, 512], BF16) for i in range(6)]
    ysb = [fw.sb(f"ysb{i}", [128, 4, 64]) for i in range(2)]
    blk = 0; nd = 0; npt = 0
    for g in range(8):
        Bb = BT[g % 2]; Cb = CT[g % 2]
        fw.dma("sp", lambda e: e.dma_start(out=Bb[:], in_=k.sbc_d[g]), writes=[Bb])
        fw.dma("sp", lambda e: e.dma_start(out=Cb[:], in_=k.sbc_d[8 + g]), writes=[Cb])
        for r in range(4):
            hh = g * 4 + r
            fw.dma("sp", lambda e, r=r, hh=hh: e.dma_start(out=abc[r][:], in_=k.sac_d[hh:hh + 1, :].broadcast_to([128, T])), writes=[abc[r]])
            for q4 in range(4):
                fw.dma("sp", lambda e, q4=q4, hh=hh: e.dma_start(out=xsf[:, q4 * 8:(q4 + 1) * 8, :],
                                                                 in_=k.sxs_d[q4 * 1024:(q4 + 1) * 1024, hh * 64:(hh + 1) * 64].rearrange("(c p) d -> p c d", p=128)), writes=[xsf])
            fw.op("dve", lambda e, r=r, hh=hh: e.tensor_tensor(out=V[r][:], in0=xsf[:], in1=dt_tm[:, :, hh:hh + 1].broadcast_to([128, NT, 64]), op=ALU.mult),
                  reads=[xsf, dt_tm], writes=[V[r]])
        for jq in range(8):
            nk = 4 * jq + 4
            for kc in range(nk):
                r0 = max(0, kc - 4 * jq)
                q0 = r0 * 128
                diag = kc >= 4 * jq
                sp_ = PS[blk % 3]; blk += 1
                mm(fw, sp_, sp_[:, q0:512], Bb, Bb[:, kc * 128:(kc + 1) * 128], Cb, Cb[:, jq * 512 + q0:(jq + 1) * 512], True, True)
                for r in range(4):
                    hh = g * 4 + r
                    d_ = dec[nd % 4]; nd += 1
                    pb = pT[npt % 6]; npt += 1
                    qa = jq * 512 + q0
                    if diag:
                        fw.op("dve", lambda e, d_=d_, r=r, qa=qa, hh=hh: e.tensor_scalar(out=d_[:, q0:q0 + 128], in0=abc[r][:, qa:qa + 128], scalar1=nac_tm[:, kc, hh:hh + 1], scalar2=0.0,
                                                                                       op0=ALU.add, op1=ALU.min), reads=[abc[r], nac_tm], writes=[d_])
                        fw.op("act", lambda e, d_=d_: e.activation(out=d_[:, q0:q0 + 128], in_=d_[:, q0:q0 + 128], func=AF.Exp), reads=[d_], writes=[d_])
                        fw.op("dve", lambda e, d_=d_: e.tensor_tensor(out=d_[:, q0:q0 + 128], in0=d_[:, q0:q0 + 128], in1=C_[:, 256:384], op=ALU.mult), reads=[d_, C_], writes=[d_])
                        if q0 + 128 < 512:
                            fw.op("act", lambda e, d_=d_, r=r, qa=qa, hh=hh: e.activation(out=d_[:, q0 + 128:512], in_=abc[r][:, qa + 128:(jq + 1) * 512], func=AF.Exp,
                                                                                     bias=nac_tm[:, kc, hh:hh + 1], scale=1.0), reads=[abc[r], nac_tm, d_], writes=[d_])
                    else:
                        fw.op("act", lambda e, d_=d_, r=r, qa=qa, hh=hh: e.activation(out=d_[:, q0:512], in_=abc[r][:, qa:(jq + 1) * 512], func=AF.Exp,
                                                                                 bias=nac_tm[:, kc, hh:hh + 1], scale=1.0), reads=[abc[r], nac_tm], writes=[d_])
                    fw.op("dve", lambda e, d_=d_, pb=pb, sp_=sp_: e.tensor_tensor(out=pb[:, q0:512], in0=sp_[:, q0:512], in1=d_[:, q0:512], op=ALU.mult), reads=[sp_, d_], writes=[pb])
                    ya = PS[3 + r]
                    for s_ in range(r0, 4):
                        fw.op("pe", lambda e, ya=ya, pb=pb, s_=s_, r=r: e.matmul(ya[:, s_ * 64:(s_ + 1) * 64], pb[:, s_ * 128:(s_ + 1) * 128], V[r][:, kc, :],
                                                                               start=(kc == 0), stop=(kc == 4 * jq + s_)),
                              reads=[pb, V[r]], writes=[ya], accumulate=True)
            for r in range(4):
                hh = g * 4 + r
                ya = PS[3 + r]; yb = ysb[r % 2]
                fw.op("act", lambda e, ya=ya, yb=yb: e.activation(out=yb[:], in_=ya[:, 0:256].rearrange("p (s d) -> p s d", s=4), func=AF.Copy), reads=[ya], writes=[yb])
                fw.dma("sp", lambda e, yb=yb, hh=hh: e.dma_start(out=k.sy_d[jq * 512:(jq + 1) * 512, hh * 64:(hh + 1) * 64].rearrange("(s p) d -> p s d", p=128), in_=yb[:]), reads=[yb])
    fw.barrier()
    fw.release(mk1)
    dbc = fw.sb("dbc", [128, 2048]); gnb = fw.sb("gnb", [128, 2048])
    fw.dma("sp", lambda e: e.dma_start(out=dbc[:], in_=k.ssm_dexp[0:1, :].broadcast_to([128, 2048])), writes=[dbc])
    fw.dma("sp", lambda e: e.dma_start(out=gnb[:], in_=k.ssm_gn[0:1, :].broadcast_to([128, 2048])), writes=[gnb])
    yt = [fw.sb(f"yt{i}", [128, 2048]) for i in range(2)]
    xs2 = [fw.sb(f"xs2{i}", [128, 2048]) for i in range(2)]
    zz = [fw.sb(f"zz{i}", [128, 2048]) for i in range(2)]
    sqv = fw.sb("sqv", [128, 2048])
    gs = fw.sb("gs", [128, 16])
    ynT = [fw.sb(f"ynT{i}", [128, 16, 128], BF16) for i in range(2)]
    for i in range(NT):
        y_ = yt[i % 2]; x_ = xs2[i % 2]; z_ = zz[i % 2]
        fw.dma("sp", lambda e: e.dma_start(out=y_[:], in_=k.sy_d[i * 128:(i + 1) * 128, :]), writes=[y_])
        fw.dma("sp", lambda e: e.dma_start(out=x_[:], in_=k.sxs_d[i * 128:(i + 1) * 128, :]), writes=[x_])
        fw.dma("sp", lambda e: e.dma_start(out=z_[:], in_=k.szs_d[i * 128:(i + 1) * 128, :]), writes=[z_])
        fw.op("dve", lambda e: e.tensor_tensor(out=x_[:], in0=x_[:], in1=dbc[:], op=ALU.mult), reads=[x_, dbc], writes=[x_])
        fw.op("dve", lambda e: e.tensor_tensor(out=y_[:], in0=y_[:], in1=x_[:], op=ALU.add), reads=[y_, x_], writes=[y_])
        fw.op("dve", lambda e: e.tensor_tensor(out=y_[:], in0=y_[:], in1=z_[:], op=ALU.mult), reads=[y_, z_], writes=[y_])
        fw.op("act", lambda e: e.activation(out=sqv[:], in_=y_[:], func=AF.Square), reads=[y_], writes=[sqv])
        fw.op("dve", lambda e: e.tensor_reduce(out=gs[:, 0:8], in_=sqv[:].rearrange("p (g d) -> p g d", g=8), axis=AX.X, op=ALU.add), reads=[sqv], writes=[gs])
        fw.op("act", lambda e: e.activation(out=gs[:, 8:16], in_=gs[:, 0:8], func=AF.Sqrt, bias=k.epsc[:], scale=1.0 / 256), reads=[gs, k.epsc], writes=[gs])
        fw.op("dve", lambda e: e.reciprocal(out=gs[:, 8:16], in_=gs[:, 8:16]), reads=[gs], writes=[gs])
        for g in range(8):
            fw.op("dve", lambda e, g=g: e.scalar_tensor_tensor(out=y_[:, g * 256:(g + 1) * 256], in0=y_[:, g * 256:(g + 1) * 256], scalar=gs[:, 8 + g:9 + g],
                                                               in1=gnb[:, g * 256:(g + 1) * 256], op0=ALU.mult, op1=ALU.mult), reads=[y_, gs, gnb], writes=[y_])
        yT = ynT[i % 2]
        for q4 in range(4):
            p = PS[q4 % 2]
            for c in range(4):
                cc = q4 * 4 + c
                tr(fw, p, p[:, c * 128:(c + 1) * 128], y_, y_[:, cc * 128:(cc + 1) * 128], C_, k.ID, first=(c == 0))
            fw.op("act", lambda e, p=p, q4=q4: e.activation(out=yT[:, q4 * 4:(q4 + 1) * 4, :], in_=p[:].rearrange("p (c t) -> p c t", c=4), func=AF.Copy), reads=[p], writes=[yT])
        fw.dma("sp", lambda e: e.dma_start(out=k.synT_d[:, :, i * 128:(i + 1) * 128].rearrange("c p t -> p c t"), in_=yT[:]), reads=[yT])
    fw.barrier()
    fw.release(mk0)


MOE_B = 256
MOE_NB = 4 * T // MOE_B + 64


def moe_layer_sparse(k, L):
    fw = k.fw; PS = k.PS; C_ = k.C_; MOD = k.MOD; nc = k.nc
    NB = MOE_NB; B = MOE_B
    mk0 = fw.mark()
    S4_all = fw.sb("S4_all", [128, NT, 4], I32); G4_all = fw.sb("G4_all", [128, NT, 4])
    idx_w = fw.sb("idx_w", [128, 128], I32); idx_b = fw.sb("idx_b", [128, 128], I32)
    idx_g = [fw.sb(f"idx_g{i}", [128, 128], I32) for i in range(8)]
    mkD = fw.mark()
    G_all = fw.sb("G_all", [128, NT, 32]); M_all = fw.sb("M_all", [128, NT, 32]); R_all = fw.sb("R_all", [128, NT, 32])
    base = fw.sb("base", [128, 32])
    fw.op("dve", lambda e: e.memset(base[:], 0.0), writes=[base])
    wr = fw.sb("wr", [128, 8, 32])
    fw.dma("sp", lambda e: e.dma_start(out=wr[:], in_=k.w_router[L].rearrange("(c p) n -> p c n", p=128)), writes=[wr])
    brb = fw.sb("brb", [128, 32])
    fw.dma("sp", lambda e: e.dma_start(out=brb[:], in_=k.b_router[L:L + 1, :].broadcast_to([128, 32])), writes=[brb])
    h2f = fw.sb("h2f", [128, 8, 128])
    lg = fw.sb("lg", [128, 32]); m8 = fw.sb("m8", [128, 8]); ex = fw.sb("ex", [128, 32])
    sm = fw.sb("sm", [128, 4])
    for i in range(NT):
        xb = k.xt[i % 2]; hb = k.ht[i % 2]; sb_ = k.st[i % 2]
        fw.dma("sp", lambda e: e.dma_start(out=xb[:], in_=k.xs[i * 128:(i + 1) * 128, :]), writes=[xb])
        k.norm_mod(xb, hb, sb_, 4, 3)
        fw.dma("sp", lambda e: e.dma_start(out=k.h2_d[i * 128:(i + 1) * 128, :], in_=hb[:]), reads=[hb])
        for half, p in ((0, PS[0]), (1, PS[1])):
            for c in range(4):
                cc = half * 4 + c
                tr(fw, p, p[:, c * 128:(c + 1) * 128], hb, hb[:, cc * 128:(cc + 1) * 128], C_, k.ID, first=(c == 0))
            fw.op("act", lambda e, half=half, p=p: e.activation(out=h2f[:, half * 4:(half + 1) * 4, :], in_=p[:].rearrange("p (c t) -> p c t", c=4), func=AF.Copy),
                  reads=[p], writes=[h2f])
        pl = PS[2]
        for c in range(8):
            mm(fw, pl, pl[:, 0:32], h2f, h2f[:, c, :], wr, wr[:, c, :], c == 0, c == 7)
        msk = M_all
        fw.op("dve", lambda e: e.tensor_tensor(out=lg[:], in0=pl[:, 0:32], in1=brb[:], op=ALU.add), reads=[pl, brb], writes=[lg])
        fw.op("dve", lambda e: e.max(out=m8[:], in_=lg[:]), reads=[lg], writes=[m8])
        fw.op("dve", lambda e: e.tensor_scalar(out=M_all[:, i, :], in0=lg[:], scalar1=m8[:, 3:4], scalar2=None, op0=ALU.is_ge), reads=[lg, m8], writes=[M_all])
        fw.op("dve", lambda e: e.tensor_scalar(out=sm[:, 0:1], in0=m8[:, 0:1], scalar1=-1.0, scalar2=None, op0=ALU.mult), reads=[m8], writes=[sm])
        fw.op("act", lambda e: e.activation(out=ex[:], in_=lg[:], func=AF.Exp, bias=sm[:, 0:1], scale=1.0), reads=[lg, sm], writes=[ex])
        fw.op("dve", lambda e: e.tensor_tensor(out=ex[:], in0=ex[:], in1=M_all[:, i, :], op=ALU.mult), reads=[ex, M_all], writes=[ex])
        fw.op("dve", lambda e: e.reduce_sum(out=sm[:, 1:2], in_=ex[:], axis=AX.X), reads=[ex], writes=[sm])
        fw.op("dve", lambda e: e.reciprocal(out=sm[:, 2:3], in_=sm[:, 1:2]), reads=[sm], writes=[sm])
        fw.op("dve", lambda e: e.tensor_scalar(out=G_all[:, i, :], in0=ex[:], scalar1=sm[:, 2:3], scalar2=None, op0=ALU.mult), reads=[ex, sm], writes=[G_all])
        pr = PS[3]; pc = PS[4]
        mm(fw, pr, pr[:, 0:32], C_, C_[:, 384:512], M_all, M_all[:, i, :], True, True)
        mm(fw, pc, pc[:, 0:32], C_, C_[:, 128:256], M_all, M_all[:, i, :], True, True)
        fw.op("dve", lambda e: e.tensor_tensor(out=R_all[:, i, :], in0=pr[:, 0:32], in1=base[:], op=ALU.add), reads=[pr, base], writes=[R_all])
        fw.op("dve", lambda e: e.tensor_tensor(out=base[:], in0=pc[:, 0:32], in1=base[:], op=ALU.add), reads=[pc, base], writes=[base])
    ti = fw.sb("ti", [128, 32], I32); pf = fw.sb("pf", [128, 32]); pend = fw.sb("pend", [128, 32]); pst = fw.sb("pst", [128, 32])
    on32 = fw.sb("on32", [128, 32]); sl_ = fw.sb("sl_", [128, 32]); v_ = fw.sb("v_", [128, 32]); m8s = fw.sb("m8s", [128, 8]); jk = fw.sb("jk", [128, 32])
    fw.op("dve", lambda e: e.memset(on32[:], 1.0), writes=[on32])
    fw.op("dve", lambda e: e.tensor_scalar(out=pf[:], in0=base[:], scalar1=float(2 * B - 1), scalar2=None, op0=ALU.add), reads=[base], writes=[pf])
    fw.op("dve", lambda e: e.tensor_copy(out=ti[:], in_=pf[:]), reads=[pf], writes=[ti])
    sh = (2 * B).bit_length() - 1
    fw.op("dve", lambda e: e.tensor_scalar(out=ti[:], in0=ti[:], scalar1=sh, scalar2=sh, op0=ALU.logical_shift_right, op1=ALU.logical_shift_left), reads=[ti], writes=[ti])
    fw.op("dve", lambda e: e.tensor_copy(out=pf[:], in_=ti[:]), reads=[ti], writes=[pf])
    fw.op("dve", lambda e: e.tensor_tensor_scan(out=pend[:], data0=on32[:], data1=pf[:], initial=0.0, op0=ALU.mult, op1=ALU.add), reads=[on32, pf], writes=[pend])
    fw.op("dve", lambda e: e.tensor_tensor(out=pst[:], in0=pend[:], in1=pf[:], op=ALU.subtract), reads=[pend, pf], writes=[pst])
    for i in range(NT):
        fw.op("dve", lambda e: e.tensor_tensor(out=sl_[:], in0=R_all[:, i, :], in1=pst[:], op=ALU.add), reads=[R_all, pst], writes=[sl_])
        fw.op("dve", lambda e: e.scalar_tensor_tensor(out=v_[:], in0=sl_[:], scalar=1.0, in1=M_all[:, i, :], op0=ALU.add, op1=ALU.mult), reads=[sl_, M_all], writes=[v_])
        fw.op("dve", lambda e: e.max(out=m8s[:], in_=v_[:]), reads=[v_], writes=[m8s])
        fw.op("dve", lambda e: e.tensor_scalar(out=S4_all[:, i, :], in0=m8s[:, 0:4], scalar1=-1.0, scalar2=None, op0=ALU.add), reads=[m8s], writes=[S4_all])
        for kk in range(4):
            fw.op("dve", lambda e, kk=kk: e.scalar_tensor_tensor(out=jk[:], in0=v_[:], scalar=m8s[:, kk:kk + 1], in1=G_all[:, i, :], op0=ALU.is_equal, op1=ALU.mult,
                                                                accum_out=G4_all[:, i, kk:kk + 1]), reads=[v_, m8s, G_all], writes=[jk, G4_all])
    cmp_ = fw.sb("cmp_", [128, 32]); bec = fw.sb("bec", [128, 2]); ber = fw.sb("ber", [1, 136]); crow = fw.sb("crow", [1, 128]); vrow = fw.sb("vrow", [1, 128])
    fw.op("dve", lambda e: e.tensor_scalar(out=cmp_[:], in0=pend[:], scalar1=C_[:, 512:513], scalar2=None, op0=ALU.is_le), reads=[pend, C_], writes=[cmp_])
    fw.op("dve", lambda e: e.reduce_sum(out=bec[:, 0:1], in_=cmp_[:], axis=AX.X), reads=[cmp_], writes=[bec])
    fw.op("dve", lambda e: e.tensor_scalar(out=bec[:, 0:1], in0=bec[:, 0:1], scalar1=31.0, scalar2=None, op0=ALU.min), reads=[bec], writes=[bec])
    fw.op("dve", lambda e: e.tensor_scalar(out=bec[:, 1:2], in0=C_[:, 512:513], scalar1=pend[:, 31:32], scalar2=None, op0=ALU.is_lt), reads=[pend, C_], writes=[bec])
    pt_ = PS[5]
    tr(fw, pt_, pt_[0:1, 0:128], bec, bec[:, 0:1], C_, k.ID)
    tr(fw, pt_, pt_[0:1, 128:256], bec, bec[:, 1:2], C_, k.ID, first=False)
    fw.op("dve", lambda e: e.memset(ber[:], -1.0), writes=[ber])
    fw.op("act", lambda e: e.activation(out=ber[0:1, 4:132], in_=pt_[0:1, 0:128], func=AF.Copy), reads=[pt_], writes=[ber])
    fw.op("act", lambda e: e.activation(out=vrow[:], in_=pt_[0:1, 128:256], func=AF.Copy), reads=[pt_], writes=[vrow])
    fw.op("dve", lambda e: e.tensor_tensor(out=crow[:], in0=ber[0:1, 4:132], in1=ber[0:1, 0:128], op=ALU.not_equal), reads=[ber], writes=[crow])
    fw.op("dve", lambda e: e.tensor_tensor(out=crow[:], in0=crow[:], in1=vrow[:], op=ALU.mult), reads=[crow, vrow], writes=[crow])
    fw.op("dve", lambda e: e.tensor_tensor(out=crow[:], in0=crow[:], in1=C_[0:1, 640:768], op=ALU.mult), reads=[crow, C_], writes=[crow])
    BIG = 1.0e6
    pb1 = PS[6]; pb2 = PS[7]
    mm(fw, pb1, pb1[:, 0:128], C_, C_[0:1, 128:256], ber, ber[0:1, 4:132], True, True)
    mm(fw, pb2, pb2[:, 0:128], C_, C_[0:1, 128:256], crow, crow[0:1, :], True, True)
    cnd = fw.sb("cnd", [128, 128]); tw = fw.sb("tw", [128, 128]); tb = fw.sb("tb", [128, 128])
    fw.op("act", lambda e: e.activation(out=cnd[:], in_=pb2[:, 0:128], func=AF.Copy), reads=[pb2], writes=[cnd])
    fw.op("dve", lambda e: e.tensor_scalar(out=tb[:], in0=pb1[:, 0:128], scalar1=float(L * 32) - BIG, scalar2=None, op0=ALU.add), reads=[pb1], writes=[tb])
    fw.op("dve", lambda e: e.tensor_scalar(out=tw[:], in0=pb1[:, 0:128], scalar1=128.0, scalar2=float(L * 4096) - BIG, op0=ALU.mult, op1=ALU.add), reads=[pb1], writes=[tw])
    fw.op("dve", lambda e: e.tensor_scalar(out=tw[:], in0=tw[:], scalar1=C_[:, 513:514], scalar2=None, op0=ALU.add), reads=[tw, C_], writes=[tw])
    for t_, ix in ((tw, idx_w), (tb, idx_b)):
        fw.op("dve", lambda e, t_=t_: e.tensor_tensor(out=t_[:], in0=t_[:], in1=cnd[:], op=ALU.mult), reads=[t_, cnd], writes=[t_])
        fw.op("dve", lambda e, t_=t_, ix=ix: e.tensor_scalar(out=ix[:], in0=t_[:], scalar1=BIG, scalar2=None, op0=ALU.add), reads=[t_], writes=[ix])
    for c2 in range(8):
        fw.op("dve", lambda e, c2=c2: e.tensor_scalar(out=idx_g[c2][:], in0=tw[:], scalar1=8.0, scalar2=8 * BIG + c2, op0=ALU.mult, op1=ALU.add), reads=[tw], writes=[idx_g[c2]])
    fw.barrier()
    fw.release(mkD)
    MS = ""
    if MS == "E":
        dbt = k.xt[0]
        for j_, src in enumerate((idx_w, idx_b, idx_g[0], idx_g[1])):
            fw.op("dve", lambda e, j_=j_, src=src: e.tensor_copy(out=dbt[:, j_ * 128:(j_ + 1) * 128], in_=src[:]), reads=[src], writes=[dbt])
        fw.op("dve", lambda e: e.tensor_copy(out=dbt[:, 512:640], in_=S4_all[:].rearrange("p a b -> p (a b)")), reads=[S4_all], writes=[dbt])
        fw.op("dve", lambda e: e.tensor_copy(out=dbt[:, 640:768], in_=G4_all[:].rearrange("p a b -> p (a b)")), reads=[G4_all], writes=[dbt])
        fw.dma("sp", lambda e: e.dma_start(out=k.xs[0:128, :], in_=dbt[:]), reads=[dbt])
        fw.barrier()
        fw.release(mk0); return
    fw.store_q = None
    for i in range(NT):
        hb = k.ht[i % 2]
        fw.dma("sp", lambda e: e.dma_start(out=hb[:], in_=k.h2_d[i * 128:(i + 1) * 128, :]), writes=[hb])
        for kk in range(4):
            fw.dma("pool", lambda e, kk=kk: e.indirect_dma_start(out=k.xsort_d, out_offset=bass.IndirectOffsetOnAxis(ap=S4_all[:, i, kk:kk + 1], axis=0),
                                                                 in_=hb[:], in_offset=None), reads=[hb, S4_all])
    fw.barrier()
    if MS == "F":
        fw.release(mk0); return
    mkG = fw.mark()
    wgu = [fw.sb(f"wgu{i}", [128, 8, 2048], BF16) for i in range(2)]
    wdn = [fw.sb(f"wdn{i}", [128, 8, 1024], BF16) for i in range(2)]
    bgr = [fw.sb(f"bgr{i}", [128, 2048], BF16) for i in range(2)]
    bdb = [fw.sb(f"bdb{i}", [128, 1024]) for i in range(2)]
    onesb = fw.sb("onesb", [1, B], BF16)
    fw.op("dve", lambda e: e.memset(onesb[:], 1.0), writes=[onesb])
    xblk = [fw.sb(f"xblk{i}", [128, B // 128, 1024]) for i in range(1)]
    xT = [fw.sb(f"xT{i}", [128, 8, B], BF16) for i in range(2)]
    hact = fw.sb("hact", [128, 8, B], BF16)
    g_s = [fw.sb(f"g_{i}", [128, B]) for i in range(2)]; sg_s = [fw.sb(f"sg_{i}", [128, B]) for i in range(2)]; l_s = [fw.sb(f"l_{i}", [128, B]) for i in range(2)]
    yo = [fw.sb(f"yo{i}", [128, 1024]) for i in range(2)]
    bound_reg = nc.gpsimd.to_reg(2 * 32 * 1024 - 1)
    for bi in range(NB):
        pj = (bi // 2) % 2
        wg = wgu[pj]; wd = wdn[pj]; bg = bgr[pj]; bd = bdb[pj]
        NW = 2 * 32 * 128 - 1
        if bi % 2 == 0:
            for c in range(8):
                fw.dma("pool", lambda e, c=c: e.indirect_dma_start(out=wg[:, c, :], out_offset=None, in_=k.w_gu,
                                                                   in_offset=bass.IndirectOffsetOnAxis(ap=idx_g[c][:, bi:bi + 1], axis=0), bounds_check=bound_reg, oob_is_err=False),
                       reads=[idx_g[c]], writes=[wg])
            for c in range(8):
                fw.dma("pool", lambda e, c=c: e.indirect_dma_start(out=wd[:, c, :], out_offset=None, in_=k.w_dn,
                                                                   in_offset=bass.IndirectOffsetOnAxis(ap=idx_g[c][:, bi:bi + 1], axis=0), bounds_check=bound_reg, oob_is_err=False),
                       reads=[idx_g[c]], writes=[wd])
            fw.dma("pool", lambda e: e.indirect_dma_start(out=bg[:], out_offset=None, in_=k.b_gu, in_offset=bass.IndirectOffsetOnAxis(ap=idx_b[:, bi:bi + 1], axis=0),
                                                          bounds_check=bound_reg, oob_is_err=False), reads=[idx_b], writes=[bg])
            fw.dma("pool", lambda e: e.indirect_dma_start(out=bd[:], out_offset=None, in_=k.b_dn, in_offset=bass.IndirectOffsetOnAxis(ap=idx_b[:, bi:bi + 1], axis=0),
                                                          bounds_check=bound_reg, oob_is_err=False), reads=[idx_b], writes=[bd])
        xb = xblk[0]; xt_ = xT[bi % 2]
        if bi == 0:
            fw.dma("sp", lambda e: e.dma_start(out=xb[:], in_=k.xsort_d[0:B, :].rearrange("(r p) d -> p r d", p=128)), writes=[xb])
        for rt in range(B // 128):
            for half in range(2):
                p = PS[half]
                for c in range(4):
                    cc = half * 4 + c
                    tr(fw, p, p[:, c * 128:(c + 1) * 128], xb, xb[:, rt, bass.ds(cc, 128, 8)], C_, k.ID, first=(c == 0))
                fw.op("act", lambda e, half=half, p=p, rt=rt: e.activation(out=xt_[:, half * 4:(half + 1) * 4, rt * 128:(rt + 1) * 128], in_=p[:].rearrange("p (c t) -> p c t", c=4), func=AF.Copy),
                      reads=[p], writes=[xt_])
        if bi + 1 < NB:
            fw.dma("sp", lambda e: e.dma_start(out=xb[:], in_=k.xsort_d[(bi + 1) * B:(bi + 2) * B, :].rearrange("(r p) d -> p r d", p=128)), writes=[xb])
        for fc in range(8):
            pgl = PS[2 + (fc % 4)]
            g_ = g_s[fc % 2]; sg_ = sg_s[fc % 2]; l_ = l_s[fc % 2]
            for part, off in ((0, 0), (1, 1024)):
                o_ap = pgl[:, part * B:(part + 1) * B]
                for c in range(8):
                    mm(fw, pgl, o_ap, wg, wg[:, c, bass.ds(off + fc, 128, 8)], xt_, xt_[:, c, :], c == 0, False)
                mm(fw, pgl, o_ap, bg, bg[0:1, bass.ds(off + fc, 128, 8)], onesb, onesb[0:1, :], False, True)
            fw.op("dve", lambda e, pgl=pgl, g_=g_: e.tensor_scalar(out=g_[:], in0=pgl[:, 0:B], scalar1=7.0, scalar2=None, op0=ALU.min), reads=[pgl], writes=[g_])
            fw.op("act", lambda e: e.activation(out=sg_[:], in_=g_[:], func=AF.Sigmoid, scale=1.702), reads=[g_], writes=[sg_])
            fw.op("dve", lambda e, pgl=pgl: e.tensor_scalar(out=l_[:], in0=pgl[:, B:2 * B], scalar1=7.0, scalar2=-7.0, op0=ALU.min, op1=ALU.max), reads=[pgl], writes=[l_])
            fw.op("dve", lambda e: e.tensor_tensor(out=g_[:], in0=g_[:], in1=sg_[:], op=ALU.mult), reads=[g_, sg_], writes=[g_])
            fw.op("dve", lambda e, fc=fc: e.scalar_tensor_tensor(out=hact[:, fc, :], in0=l_[:], scalar=1.0, in1=g_[:], op0=ALU.add, op1=ALU.mult), reads=[g_, l_], writes=[hact])
        for rt in range(B // 128):
            yb = yo[rt % 2]
            for half in range(2):
                p = PS[6 + half]
                for fc in range(8):
                    mm(fw, p, p[:], hact, hact[:, fc, rt * 128:(rt + 1) * 128], wd, wd[:, fc, half * 512:(half + 1) * 512], fc == 0, fc == 7)
                fw.op("dve", lambda e, p=p, half=half, yb=yb: e.tensor_tensor(out=yb[:, half * 512:(half + 1) * 512], in0=p[:], in1=bd[:, half * 512:(half + 1) * 512], op=ALU.add),
                      reads=[p, bd], writes=[yb])
            fw.dma("sp", lambda e, yb=yb, rt=rt: e.dma_start(out=k.ysort_d[bi * B + rt * 128:bi * B + (rt + 1) * 128, :], in_=yb[:]), reads=[yb])
    fw.barrier()
    fw.release(mkG)
    if MS == "G":
        fw.release(mk0); return
    fw.store_q = "act"
    yk = [[fw.sb(f"yk{i}_{kk}", [128, 1024]) for kk in range(4)] for i in range(2)]
    for i in range(NT):
        xb = k.xt[i % 2]; ys_ = yk[i % 2]
        fw.dma("sp", lambda e: e.dma_start(out=xb[:], in_=k.xs[i * 128:(i + 1) * 128, :]), writes=[xb])
        for kk in range(4):
            fw.dma("pool", lambda e, kk=kk: e.indirect_dma_start(out=ys_[kk][:], out_offset=None, in_=k.ysort_d,
                                                                 in_offset=bass.IndirectOffsetOnAxis(ap=S4_all[:, i, kk:kk + 1], axis=0)), reads=[S4_all], writes=[ys_[kk]])
        a0 = ys_[0]
        fw.op("dve", lambda e: e.tensor_scalar(out=a0[:], in0=a0[:], scalar1=G4_all[:, i, 0:1], scalar2=None, op0=ALU.mult), reads=[a0, G4_all], writes=[a0])
        for kk in range(1, 4):
            fw.op("dve", lambda e, kk=kk: e.scalar_tensor_tensor(out=a0[:], in0=ys_[kk][:], scalar=G4_all[:, i, kk:kk + 1], in1=a0[:], op0=ALU.mult, op1=ALU.add),
                  reads=[ys_[kk], G4_all, a0], writes=[a0])
        fw.op("dve", lambda e: e.tensor_tensor(out=a0[:], in0=a0[:], in1=MOD[:, 5, :], op=ALU.mult), reads=[a0, MOD], writes=[a0])
        fw.op("dve", lambda e: e.tensor_tensor(out=xb[:], in0=xb[:], in1=a0[:], op=ALU.add), reads=[xb, a0], writes=[xb])
        fw.dma("sp", lambda e: e.dma_start(out=k.xs[i * 128:(i + 1) * 128, :], in_=xb[:]), reads=[xb])
    fw.barrier()
    fw.store_q = "pool"
    fw.release(mk0)


def host_consts():
    cst = np.zeros((128, 1024), np.float32)
    cst[:, 0:128] = np.eye(128, dtype=np.float32)
    cst[:, 128:256] = 1.0
    i = np.arange(128)
    cst[:, 256:384] = (i[None, :] >= i[:, None]).astype(np.float32)
    cst[:, 384:512] = (i[:, None] < i[None, :]).astype(np.float32)
    cst[:, 512] = i * 256.0
    cst[:, 513] = i
    cst[:, 640:768] = (i % 2 == 0).astype(np.float32)[None, :]
    inv = (1.0 / (10000.0 ** (np.arange(0, 64, 2, dtype=np.float32) / 64))).astype(np.float32)
    invf = np.zeros((64, 2), np.float32)
    invf[:32, 0] = inv; invf[32:, 0] = inv
    invf[:32, 1] = -1.0; invf[32:, 1] = 1.0
    return cst, invf


def prep_core(inp, b):
    f = np.ascontiguousarray
    cst, invf = host_consts()
    m = {}
    m["x"] = f(inp["x"][b])
    m["c"] = f(inp["c"][b].reshape(8, 128).T)
    m["pos"] = f(inp["positions"][b].reshape(1, T).astype(np.int32))
    m["w_mod"] = inp["w_mod"]; m["b_mod"] = inp["b_mod"]
    m["g_mix"] = inp["g_mix_norm"]; m["g_ffn"] = inp["g_ffn_norm"]
    w_in = inp["mla_w_in"][0]
    kr = w_in[:, 512:576]
    m["mla_w_in"] = f(np.concatenate([w_in, kr[:, 32:], kr[:, :32]], axis=1))
    m["mla_gq"] = f(inp["mla_g_q"][0].reshape(2, 128).T)
    m["mla_gkv"] = f(inp["mla_g_kv"][0].reshape(2, 128).T)
    wq = inp["mla_w_q_up"][0].reshape(256, 8, 192)
    sw = np.concatenate([wq[:, :, 160:192], wq[:, :, 128:160]], axis=2)
    m["mla_wq"] = f(np.concatenate([wq.reshape(256, 1536), sw.reshape(256, 512)], axis=1))
    wkv = inp["mla_w_kv_up"][0].reshape(256, 8, 256)
    m["mla_wkv"] = f(np.concatenate([wkv[:, :, :128].reshape(256, 1024), wkv[:, :, 128:].reshape(256, 1024)], axis=1))
    m["mla_wo"] = inp["mla_w_out"][0]
    m["w_router"] = inp["moe_w_router"]; m["b_router"] = inp["moe_b_router"]
    m["w_gu"] = inp["moe_w_gate_up"].reshape(2 * 32 * 1024, 2048); m["b_gu"] = inp["moe_b_gate_up"].reshape(64, 2048)
    m["w_dn"] = inp["moe_w_down"].reshape(2 * 32 * 1024, 1024); m["b_dn"] = inp["moe_b_down"].reshape(64, 1024)
    m["g_final"] = f(inp["g_final"].reshape(1, D))
    m["ssm_w_in"] = inp["ssm_w_in"][0]
    m["ssm_cw"] = f(inp["ssm_conv_w"][0].reshape(4, 32, 128).transpose(2, 1, 0))
    m["ssm_cb"] = f(inp["ssm_conv_b"][0].reshape(32, 128).T)
    m["ssm_hv"] = f(np.stack([inp["ssm_dt_bias"][0], inp["ssm_a_log"][0]], axis=1))
    m["ssm_dexp"] = f(np.repeat(inp["ssm_d"][0], 64).reshape(1, 2048))
    m["ssm_gn"] = f(inp["ssm_g_norm"][0].reshape(1, 2048))
    m["ssm_wo"] = inp["ssm_w_out"][0]
    m["cst"] = cst; m["invf"] = invf
    return m

_CACHE = {}


def _program():
    if "k" not in _CACHE:
        k = build()
        k.stop = "all"
        k.compute_mod(0)
        mla_layer(k, 0, True)
        mixer_out_and_moe_router(k, 0, k.mla_wo, 8, k.oT_d)
        moe_layer_sparse(k, 0)
        k.compute_mod(1)
        ssd_layer(k, 1)
        mixer_out_and_moe_router(k, 1, k.ssm_wo, 16, k.synT_d)
        moe_layer_sparse(k, 1)
        final_norm(k)
        k.fw.finish()
        k.fw.close()
        _CACHE["k"] = k
    return _CACHE["k"]


def kernel(**inputs):
    inp = {kk: np.asarray(v) for kk, v in inputs.items()}
    k = _program()
    in_maps = [prep_core(inp, b) for b in range(8)]
    res = run_bass_kernel_spmd(k.nc, in_maps, core_ids=list(range(8)))
    return np.stack([np.asarray(r["out"]) for r in res.results], axis=0).astype(np.float32)
```

```python
import numpy as np
import concourse.bass as bass
import concourse.mybir as mybir
from concourse.bass_utils import run_bass_kernel_spmd

F32 = mybir.dt.float32
F32R = mybir.dt.float32r
BF16 = mybir.dt.bfloat16
I32 = mybir.dt.int32
U32 = mybir.dt.uint32
ALU = mybir.AluOpType
AF = mybir.ActivationFunctionType
AX = mybir.AxisListType

EPOCH = 20000


class Buf:
    __slots__ = ("t", "name", "lastw", "reads", "wfill", "dsem", "dcount", "is_dram")

    def __init__(self, t, name, is_dram=False):
        self.t = t
        self.name = name
        self.lastw = None
        self.reads = []
        self.wfill = []
        self.is_dram = is_dram

    def __getitem__(self, idx):
        return self.t[idx]


class FW:
    def __init__(self, nc):
        self.nc = nc
        self.eng = {"pe": nc.tensor, "dve": nc.vector, "act": nc.scalar,
                    "pool": nc.gpsimd, "sp": nc.sync}
        self.sem = {}
        self.cnt = {}
        self.nsem = 0
        self.waited = {e: {} for e in self.eng}
        self.semobjs = {}
        self.ctx = []
        self.bctx = []
        self.last = {e: None for e in self.eng}
        self.pend = {e: [] for e in self.eng}
        self.store_q = "pool"
        self.dma_toks = []
        self.dma_pool = []
        self.dma_free = []
        for e in ("pe", "dve", "act", "pool"):
            self._new_epoch(e)

    def _mksem(self, name):
        cm = self.nc.semaphore(name)
        s = cm.__enter__()
        self.ctx.append(cm)
        self.nsem += 1
        self.semobjs[id(s)] = s
        return s

    def _new_epoch(self, e):
        self.sem[e] = self._mksem(f"s_{e}_{self.nsem}")
        self.cnt[e] = 0

    def sb(self, name, shape, dt=F32):
        self.uid = getattr(self, "uid", 0) + 1
        cm = self.nc.sbuf_tensor(f"{name}_u{self.uid}", shape, dt)
        t = cm.__enter__()
        self.bctx.append(cm)
        return Buf(t, name)

    def ps(self, name, shape, dt=F32):
        cm = self.nc.psum_tensor(name, shape, dt)
        t = cm.__enter__()
        self.ctx.append(cm)
        return Buf(t, name)

    def dram(self, name, shape, dt=F32, kind="Internal"):
        t = self.nc.dram_tensor(name, shape, dt, kind=kind)
        return Buf(t.ap(), name, is_dram=True)

    def _wait(self, e, tok):
        if tok is None:
            return
        sem, val, _ = tok
        w = self.waited[e]
        if w.get(id(sem), 0) >= val:
            return
        w[id(sem)] = val
        self.eng[e].wait_ge(sem, val)

    def _deps(self, e, reads, writes, accumulate=False, is_dma=False):
        for tok in self.pend[e]:
            self._wait(e, tok)
        self.pend[e] = []
        for b in reads:
            if b is None:
                continue
            if b.lastw is not None:
                self._wait(e, b.lastw)
            for tok in b.wfill:
                self._wait(e, tok)
        for b in writes:
            if b is None:
                continue
            fill = is_dma and b.lastw is not None and b.lastw[2] == "dmaq" and not b.reads
            if b.lastw is not None and not (accumulate and b.lastw[2] == e) and not fill:
                if b.lastw[2] != e or b.lastw[2] in ("sp", "poolq", "actq"):
                    self._wait(e, b.lastw)
                    for tok in b.wfill:
                        self._wait(e, tok)
            for tok in b.reads:
                if tok[2] != e:
                    self._wait(e, tok)

    def _commit(self, tok, reads, writes):
        for b in reads:
            if b is not None:
                b.reads.append(tok)
        for b in writes:
            if b is not None:
                if tok[2] == "dmaq" and b.lastw is not None and b.lastw[2] == "dmaq" and not b.reads:
                    b.wfill.append(b.lastw)
                else:
                    b.wfill = []
                b.lastw = tok
                b.reads = []

    def op(self, e, fn, reads=(), writes=(), accumulate=False):
        self._deps(e, reads, writes, accumulate)
        if self.cnt[e] >= EPOCH:
            self._new_epoch(e)
        inst = fn(self.eng[e])
        self.cnt[e] += 1
        inst.then_inc(self.sem[e], 1)
        tok = (self.sem[e], self.cnt[e], e)
        self.last[e] = tok
        self._commit(tok, reads, writes)
        return tok

    def _dma_sem(self):
        if self.dma_free:
            return self.dma_free.pop()
        s = [self._mksem(f"s_dma_{self.nsem}"), 0]
        self.dma_pool.append(s)
        return s

    def dma(self, q, fn, reads=(), writes=(), semslot=None):
        if q == "sp" and not [w for w in writes if w is not None] and getattr(self, "store_q", None):
            q = self.store_q
        self._deps(q, reads, writes, is_dma=True)
        if semslot is None:
            semslot = self._rot_sem(q)
        if semslot[1] > 0:
            self._wait(q, (semslot[0], semslot[1], "dmaq"))
        inst = fn(self.eng[q])
        semslot[1] += 16
        inst.then_inc(semslot[0], 16)
        tok = (semslot[0], semslot[1], "dmaq")
        self._commit(tok, reads, writes)
        self.dma_toks.append(tok)
        if len(self.dma_toks) > 64:
            self.dma_toks = self.dma_toks[-64:]
        return tok

    NROT = 16

    def _rot_sem(self, q="sp"):
        if not hasattr(self, "_rotq"):
            self._rotq = {}
        if q not in self._rotq:
            self._rotq[q] = {"sems": [[self._mksem(f"s_rot_{q}_{i}"), 0] for i in range(self.NROT)], "i": 0}
        pool = self._rotq[q]
        i = pool["i"]
        pool["i"] = (i + 1) % self.NROT
        return _RotSlot(pool["sems"], i)

    def barrier(self):
        toks = [t for t in self.last.values() if t is not None]
        toks += self.dma_toks
        for pool in getattr(self, "_rotq", {}).values():
            for s in pool["sems"]:
                if s[1] > 0:
                    toks.append((s[0], s[1], "dmaq"))
        for e in self.eng:
            self.pend[e] = list(toks)
        self.dma_toks = []

    def finish(self):
        self.barrier()
        for e in self.eng:
            for tok in self.pend[e]:
                self._wait(e, tok)
            self.pend[e] = []

    def mark(self):
        return len(self.bctx)

    def release(self, m):
        while len(self.bctx) > m:
            self.bctx.pop().__exit__(None, None, None)

    def close(self):
        self.release(0)
        for cm in reversed(self.ctx):
            cm.__exit__(None, None, None)
        self.ctx = []


class _RotSlot(list):
    def __init__(self, sems, i):
        super().__init__(sems[i])
        self.sems = sems
        self.i = i

    def __setitem__(self, k, v):
        super().__setitem__(k, v)
        self.sems[self.i][k] = v


T = 4096
D = 1024
NT = T // 128
EPS = 1e-6
MLA_SCALE = 192 ** -0.5
PI = float(np.pi)
TWO_PI = float(2 * np.pi)


class K:
    pass


def mm(fw, ob, o_ap, lb, l_ap, rb, r_ap, start, stop):
    return fw.op("pe", lambda e: e.matmul(o_ap, l_ap, r_ap, start=start, stop=stop),
                 reads=[lb, rb], writes=[ob], accumulate=not start)


def tr(fw, ob, o_ap, ib, i_ap, ident, id_ap, first=True):
    return fw.op("pe", lambda e: e.transpose(o_ap, i_ap, id_ap), reads=[ib, ident], writes=[ob],
                 accumulate=not first)


def build(stop="all", n_layers=2, debug=False):
    nc = bass.Bass("TRN2", target_bir_lowering=False)
    fw = FW(nc)
    k = K()

    def din(name, shape, dt=F32):
        return nc.dram_tensor(name, shape, dt, kind="ExternalInput").ap()

    x_in = din("x", [T, D])
    c_in = din("c", [128, 8])
    pos_in = din("pos", [1, T], I32)
    w_mod = din("w_mod", [2, D, 6 * D])
    b_mod = din("b_mod", [2, 6 * D])
    g_mix = din("g_mix", [2, D])
    g_ffn = din("g_ffn", [2, D])
    mla_w_in = din("mla_w_in", [D, 640])
    mla_gq = din("mla_gq", [128, 2])
    mla_gkv = din("mla_gkv", [128, 2])
    mla_wq = din("mla_wq", [256, 2048])
    mla_wkv = din("mla_wkv", [256, 2048])
    mla_wo = din("mla_wo", [D, D])
    w_router = din("w_router", [2, D, 32])
    b_router = din("b_router", [2, 32])
    w_gu = din("w_gu", [2 * 32 * 1024, 2 * D])
    b_gu = din("b_gu", [64, 2 * D])
    w_dn = din("w_dn", [2 * 32 * 1024, D])
    b_dn = din("b_dn", [64, D])
    g_final = din("g_final", [1, D])
    ssm_w_in = din("ssm_w_in", [D, 6176])
    ssm_cw = din("ssm_cw", [128, 32, 4])
    ssm_cb = din("ssm_cb", [128, 32])
    ssm_hv = din("ssm_hv", [32, 2])
    ssm_dexp = din("ssm_dexp", [1, 2048])
    ssm_gn = din("ssm_gn", [1, 2048])
    ssm_wo = din("ssm_wo", [2048, D])
    cst = din("cst", [128, 1024])
    invf = din("invf", [64, 2])
    out_d = nc.dram_tensor("out", [T, D], F32, kind="ExternalOutput").ap()
    dbg_d = nc.dram_tensor("dbg", [T, D], F32, kind="ExternalOutput").ap() if debug else None

    def dscr(name, shape, dt=F32):
        return nc.dram_tensor(name, shape, dt, kind="Internal").ap()

    xs = dscr("xs", [T, D])
    qT_d = dscr("qT_d", [8, 192, T], BF16)
    kT_d = dscr("kT_d", [8, 128, T], BF16)
    krT_d = dscr("krT_d", [64, T], BF16)
    v_d = dscr("v_d", [8, T, 128], BF16)
    oT_d = dscr("oT_d", [8, 128, T], BF16)
    yacc = dscr("yacc", [T, D])
    h2_d = dscr("h2_d", [T, D])
    xsort_d = dscr("xsort_d", [4 * T + 64 * 256, D])
    ysort_d = dscr("ysort_d", [4 * T + 64 * 256, D])
    sxs_d = dscr("sxs_d", [T, 2048])
    szs_d = dscr("szs_d", [T, 2048])
    sy_d = dscr("sy_d", [T, 2048])
    sbc_d = dscr("sbc_d", [16, 128, T], BF16)
    sac_d = dscr("sac_d", [32, T])
    synT_d = dscr("synT_d", [16, 128, T], BF16)

    C_ = fw.sb("cst_s", [128, 1024])
    fw.dma("sp", lambda e: e.dma_start(out=C_[:], in_=cst), writes=[C_])
    ident = C_
    ID = C_[:, 0:128]
    ONES = C_[:, 128:256]
    TRI = C_[:, 256:384]
    SL = C_[:, 384:512]
    onesr = fw.sb("onesr", [128, 128], F32R)
    fw.op("act", lambda e: e.activation(out=onesr[:], in_=C_[:, 128:256], func=AF.Copy), reads=[C_], writes=[onesr])
    trib = fw.sb("trib", [128, 128], BF16)
    fw.op("act", lambda e: e.activation(out=trib[:], in_=C_[:, 256:384], func=AF.Copy), reads=[C_], writes=[trib])
    epsc = fw.sb("epsc", [128, 1])
    fw.op("dve", lambda e: e.memset(epsc[:], EPS), writes=[epsc])
    PS = [fw.ps(f"ps{i}", [128, 512]) for i in range(8)]

    cond = fw.sb("cond", [128, 8])
    fw.dma("sp", lambda e: e.dma_start(out=cond[:], in_=c_in), writes=[cond])
    sg = fw.sb("sg", [128, 8])
    fw.op("act", lambda e: e.activation(out=sg[:], in_=cond[:], func=AF.Sigmoid), reads=[cond], writes=[sg])
    fw.op("dve", lambda e: e.tensor_tensor(out=cond[:], in0=cond[:], in1=sg[:], op=ALU.mult), reads=[cond, sg], writes=[cond])
    condb = fw.sb("condb", [128, 8, 128])
    for c in range(8):
        fw.op("dve", lambda e, c=c: e.tensor_scalar(out=condb[:, c, :], in0=C_[:, 128:256], scalar1=cond[:, c:c + 1],
                                                    scalar2=None, op0=ALU.mult), reads=[C_, cond], writes=[condb])
    MOD = fw.sb("MOD", [128, 6, D])

    def compute_mod(L):
        mkm = fw.mark()
        wmt = [fw.sb(f"wmt{i}", [128, 8, 512]) for i in range(2)]
        bmt = [fw.sb(f"bmt{i}", [128, 512]) for i in range(2)]
        gbc = fw.sb("gbc", [128, D])
        for n in range(12):
            wt = wmt[n % 2]
            bt = bmt[n % 2]
            fw.dma("sp", lambda e: e.dma_start(out=wt[:], in_=w_mod[L, :, n * 512:(n + 1) * 512].rearrange("(c p) n -> p c n", p=128)), writes=[wt])
            fw.dma("sp", lambda e: e.dma_start(out=bt[:], in_=b_mod[L:L + 1, n * 512:(n + 1) * 512].broadcast_to([128, 512])), writes=[bt])
            p = PS[n % 2]
            for c in range(8):
                mm(fw, p, p[:], condb, condb[:, c, :], wt, wt[:, c, :], c == 0, c == 7)
            fw.op("dve", lambda e: e.tensor_tensor(out=MOD[:, n // 2, (n % 2) * 512:(n % 2 + 1) * 512], in0=p[:], in1=bt[:], op=ALU.add),
                  reads=[p, bt], writes=[MOD])
        for (slot, g) in ((1, g_mix), (4, g_ffn)):
            fw.dma("sp", lambda e: e.dma_start(out=gbc[:], in_=g[L:L + 1, :].broadcast_to([128, D])), writes=[gbc])
            fw.op("dve", lambda e: e.scalar_tensor_tensor(out=MOD[:, slot, :], in0=MOD[:, slot, :], scalar=1.0, in1=gbc[:],
                                                          op0=ALU.add, op1=ALU.mult), reads=[MOD, gbc], writes=[MOD])
        fw.barrier()
        fw.release(mkm)

    xt = [fw.sb(f"xt{i}", [128, D]) for i in range(2)]
    ht = [fw.sb(f"ht{i}", [128, D]) for i in range(2)]
    junk = fw.sb("junk", [128, D])
    st = [fw.sb(f"st{i}", [128, 4]) for i in range(2)]

    def norm_mod(xb, hb, sb_, a_slot, s_slot):
        fw.op("act", lambda e: e.activation(out=junk[:], in_=xb[:], func=AF.Square, accum_out=sb_[:, 0:1]), reads=[xb], writes=[junk, sb_])
        fw.op("act", lambda e: e.activation(out=sb_[:, 1:2], in_=sb_[:, 0:1], func=AF.Sqrt, bias=epsc[:], scale=1.0 / D), reads=[sb_, epsc], writes=[sb_])
        fw.op("dve", lambda e: e.reciprocal(out=sb_[:, 2:3], in_=sb_[:, 1:2]), reads=[sb_], writes=[sb_])
        fw.op("dve", lambda e: e.scalar_tensor_tensor(out=hb[:], in0=xb[:], scalar=sb_[:, 2:3], in1=MOD[:, a_slot, :], op0=ALU.mult, op1=ALU.mult),
              reads=[xb, sb_, MOD], writes=[hb])
        if s_slot is not None:
            fw.op("dve", lambda e: e.tensor_tensor(out=hb[:], in0=hb[:], in1=MOD[:, s_slot, :], op=ALU.add), reads=[hb, MOD], writes=[hb])

    def transpose_to(hb, dstb, dst_fn, pa, pb):
        for half, p in ((0, pa), (1, pb)):
            for c in range(4):
                cc = half * 4 + c
                tr(fw, p, p[:, c * 128:(c + 1) * 128], hb, hb[:, cc * 128:(cc + 1) * 128], C_, ID, first=(c == 0))
            fw.op("act", lambda e, half=half, p=p: e.activation(out=dst_fn(half), in_=p[:].rearrange("p (c t) -> p c t", c=4), func=AF.Copy),
                  reads=[p], writes=[dstb])

    k.__dict__.update(locals())
    return k


def mla_layer(k, L, src_first):
    fw = k.fw; nc = k.nc; PS = k.PS; C_ = k.C_; MOD = k.MOD
    x_src = k.x_in if src_first else k.xs
    mk0 = fw.mark()
    w_in_s = fw.sb("mla_w_in_s", [128, 8, 640], F32R)
    for h2 in range(2):
        fw.dma("pool", lambda e: e.dma_start(out=w_in_s[:, h2 * 4:(h2 + 1) * 4, :], in_=k.mla_w_in[h2 * 512:(h2 + 1) * 512, :].rearrange("(c p) n -> p c n", p=128)), writes=[w_in_s])
    wq_s = fw.sb("wq_s", [128, 2, 2048], F32R)
    wkv_s = fw.sb("wkv_s", [128, 2, 2048], F32R)
    for c in range(2):
        fw.dma("pool", lambda e: e.dma_start(out=wq_s[:, c, :], in_=k.mla_wq[c * 128:(c + 1) * 128, :]), writes=[wq_s])
        fw.dma("pool", lambda e: e.dma_start(out=wkv_s[:, c, :], in_=k.mla_wkv[c * 128:(c + 1) * 128, :]), writes=[wkv_s])
    gq = fw.sb("gq", [128, 2]); gkv = fw.sb("gkv", [128, 2]); invf = fw.sb("invf_s", [64, 2])
    fw.dma("sp", lambda e: e.dma_start(out=gq[:], in_=k.mla_gq), writes=[gq])
    fw.dma("sp", lambda e: e.dma_start(out=gkv[:], in_=k.mla_gkv), writes=[gkv])
    fw.dma("sp", lambda e: e.dma_start(out=invf[:], in_=k.invf), writes=[invf])
    hT = fw.sb("hT", [128, 8, 512], F32R)
    sq = fw.sb("sq", [128, 2, 512], F32R)
    rstd = fw.sb("rstd", [128, 512])
    latn = fw.sb("latn", [128, 4, 512], F32R)
    posi = fw.sb("posi", [64, 512], I32)
    ang = fw.sb("ang", [64, 512]); kf = fw.sb("kf", [64, 512]); ki = fw.sb("ki", [64, 512], I32)
    rr = fw.sb("rr", [64, 512]); mwrap = fw.sb("mwrap", [64, 512])
    Ct = fw.sb("Ct", [64, 512]); St = fw.sb("St", [64, 512])
    t1 = fw.sb("t1", [64, 512]); t2 = fw.sb("t2", [64, 512])
    stq = [fw.sb(f"stq{i}", [128, 512], BF16) for i in range(2)]
    str_ = [fw.sb(f"str{i}", [64, 512], BF16) for i in range(2)]
    stv = [fw.sb(f"stv{i}", [128, 1024], BF16) for i in range(2)]

    def wrap_sin(dst, src, shift):
        fw.op("dve", lambda e: e.tensor_scalar(out=kf[:], in0=src[:], scalar1=shift, scalar2=1.0 / TWO_PI, op0=ALU.add, op1=ALU.mult), reads=[src], writes=[kf])
        fw.op("dve", lambda e: e.tensor_copy(out=ki[:], in_=kf[:]), reads=[kf], writes=[ki])
        fw.op("dve", lambda e: e.tensor_copy(out=kf[:], in_=ki[:]), reads=[ki], writes=[kf])
        fw.op("dve", lambda e: e.scalar_tensor_tensor(out=rr[:], in0=kf[:], scalar=-TWO_PI, in1=src[:], op0=ALU.mult, op1=ALU.add), reads=[kf, src], writes=[rr])
        if shift != 0.0:
            fw.op("dve", lambda e: e.tensor_scalar(out=rr[:], in0=rr[:], scalar1=shift, scalar2=None, op0=ALU.add), reads=[rr], writes=[rr])
        fw.op("dve", lambda e: e.tensor_scalar(out=mwrap[:], in0=rr[:], scalar1=PI, scalar2=-TWO_PI, op0=ALU.is_gt, op1=ALU.mult), reads=[rr], writes=[mwrap])
        fw.op("dve", lambda e: e.tensor_tensor(out=rr[:], in0=rr[:], in1=mwrap[:], op=ALU.add), reads=[rr, mwrap], writes=[rr])
        fw.op("dve", lambda e: e.tensor_scalar(out=mwrap[:], in0=rr[:], scalar1=-PI, scalar2=TWO_PI, op0=ALU.is_lt, op1=ALU.mult), reads=[rr], writes=[mwrap])
        fw.op("dve", lambda e: e.tensor_tensor(out=rr[:], in0=rr[:], in1=mwrap[:], op=ALU.add), reads=[rr, mwrap], writes=[rr])
        fw.op("dve", lambda e: e.tensor_scalar(out=rr[:], in0=rr[:], scalar1=PI, scalar2=-PI, op0=ALU.min, op1=ALU.max), reads=[rr], writes=[rr])
        fw.op("act", lambda e: e.activation(out=dst[:], in_=rr[:], func=AF.Sin), reads=[rr], writes=[dst])

    for j in range(T // 512):
        t0 = j * 512
        for r in range(4):
            xb = k.xt[r % 2]; hb = k.ht[r % 2]; sb_ = k.st[r % 2]
            fw.dma("sp", lambda e: e.dma_start(out=xb[:], in_=x_src[t0 + r * 128:t0 + (r + 1) * 128, :]), writes=[xb])
            if src_first:
                fw.dma("sp", lambda e: e.dma_start(out=k.xs[t0 + r * 128:t0 + (r + 1) * 128, :], in_=xb[:]), reads=[xb])
            k.norm_mod(xb, hb, sb_, 1, 0)
            k.transpose_to(hb, hT, lambda half, r=r: hT[:, half * 4:(half + 1) * 4, r * 128:(r + 1) * 128], PS[0], PS[1])
        fw.dma("sp", lambda e: e.dma_start(out=posi[:], in_=k.pos_in[0:1, t0:t0 + 512].broadcast_to([64, 512])), writes=[posi])
        fw.op("dve", lambda e: e.tensor_copy(out=ang[:], in_=posi[:]), reads=[posi], writes=[ang])
        fw.op("dve", lambda e: e.tensor_scalar(out=ang[:], in0=ang[:], scalar1=invf[:, 0:1], scalar2=None, op0=ALU.mult), reads=[ang, invf], writes=[ang])
        wrap_sin(St, ang, 0.0)
        wrap_sin(Ct, ang, PI / 2)
        fw.op("dve", lambda e: e.tensor_scalar(out=St[:], in0=St[:], scalar1=invf[:, 1:2], scalar2=None, op0=ALU.mult), reads=[St, invf], writes=[St])
        for oc in range(4):
            p = PS[2 + oc]
            for c in range(8):
                mm(fw, p, p[:], w_in_s, w_in_s[:, c, oc * 128:(oc + 1) * 128], hT, hT[:, c, :], c == 0, c == 7)
        for oc in range(2):
            p = PS[6 + oc]
            for c in range(8):
                mm(fw, p, p[0:64, :], w_in_s, w_in_s[:, c, 512 + oc * 64:512 + (oc + 1) * 64], hT, hT[:, c, :], c == 0, c == 7)
        for grp, gvec in ((0, gq), (1, gkv)):
            for c2 in range(2):
                p = PS[2 + grp * 2 + c2]
                fw.op("act", lambda e, p=p, c2=c2: e.activation(out=sq[:, c2, :], in_=p[:], func=AF.Square), reads=[p], writes=[sq])
            for c2 in range(2):
                mm(fw, PS[0], PS[0][:], k.onesr, k.onesr[:], sq, sq[:, c2, :], c2 == 0, c2 == 1)
            fw.op("act", lambda e: e.activation(out=rstd[:], in_=PS[0][:], func=AF.Sqrt, bias=k.epsc[:], scale=1.0 / 256), reads=[PS[0], k.epsc], writes=[rstd])
            fw.op("dve", lambda e: e.reciprocal(out=rstd[:], in_=rstd[:]), reads=[rstd], writes=[rstd])
            for c2 in range(2):
                p = PS[2 + grp * 2 + c2]
                fw.op("dve", lambda e, p=p, c2=c2, grp=grp, gvec=gvec: e.scalar_tensor_tensor(out=latn[:, grp * 2 + c2, :], in0=p[:], scalar=gvec[:, c2:c2 + 1], in1=rstd[:],
                                                                                     op0=ALU.mult, op1=ALU.mult), reads=[p, gvec, rstd], writes=[latn])
        sr = str_[0]
        fw.op("dve", lambda e: e.tensor_tensor(out=t1[:], in0=PS[6][0:64, :], in1=Ct[:], op=ALU.mult), reads=[PS[6], Ct], writes=[t1])
        fw.op("dve", lambda e: e.tensor_tensor(out=t2[:], in0=PS[7][0:64, :], in1=St[:], op=ALU.mult), reads=[PS[7], St], writes=[t2])
        fw.op("dve", lambda e: e.tensor_tensor(out=sr[:], in0=t1[:], in1=t2[:], op=ALU.add), reads=[t1, t2], writes=[sr])
        fw.dma("sp", lambda e: e.dma_start(out=k.krT_d[:, t0:t0 + 512], in_=sr[:]), reads=[sr])
        for h in range(8):
            pq = PS[2 + (h % 2) * 3]; pr = PS[3 + (h % 2) * 3]; prs = PS[4 + (h % 2) * 3]
            for c in range(2):
                mm(fw, pq, pq[:], wq_s, wq_s[:, c, h * 192:h * 192 + 128], latn, latn[:, c, :], c == 0, c == 1)
            for c in range(2):
                mm(fw, pr, pr[0:64, :], wq_s, wq_s[:, c, h * 192 + 128:h * 192 + 192], latn, latn[:, c, :], c == 0, c == 1)
            for c in range(2):
                mm(fw, prs, prs[0:64, :], wq_s, wq_s[:, c, 1536 + h * 64:1536 + (h + 1) * 64], latn, latn[:, c, :], c == 0, c == 1)
            sq_ = stq[h % 2]; sr = str_[(h + 1) % 2]
            fw.op("act", lambda e, pq=pq, sq_=sq_: e.activation(out=sq_[:], in_=pq[:], func=AF.Copy, scale=MLA_SCALE), reads=[pq], writes=[sq_])
            fw.dma("sp", lambda e, sq_=sq_, h=h: e.dma_start(out=k.qT_d[h, 0:128, t0:t0 + 512], in_=sq_[:]), reads=[sq_])
            fw.op("dve", lambda e, pr=pr: e.scalar_tensor_tensor(out=t1[:], in0=pr[0:64, :], scalar=MLA_SCALE, in1=Ct[:], op0=ALU.mult, op1=ALU.mult), reads=[pr, Ct], writes=[t1])
            fw.op("dve", lambda e, prs=prs: e.scalar_tensor_tensor(out=t2[:], in0=prs[0:64, :], scalar=MLA_SCALE, in1=St[:], op0=ALU.mult, op1=ALU.mult), reads=[prs, St], writes=[t2])
            fw.op("dve", lambda e, sr=sr: e.tensor_tensor(out=sr[:], in0=t1[:], in1=t2[:], op=ALU.add), reads=[t1, t2], writes=[sr])
            fw.dma("sp", lambda e, sr=sr, h=h: e.dma_start(out=k.qT_d[h, 128:192, t0:t0 + 512], in_=sr[:]), reads=[sr])
        for h in range(8):
            pk = PS[2 + (h % 2)]
            for c in range(2):
                mm(fw, pk, pk[:], wkv_s, wkv_s[:, c, h * 128:(h + 1) * 128], latn, latn[:, 2 + c, :], c == 0, c == 1)
            sk = stq[h % 2]
            fw.op("act", lambda e, pk=pk, sk=sk: e.activation(out=sk[:], in_=pk[:], func=AF.Copy), reads=[pk], writes=[sk])
            fw.dma("sp", lambda e, sk=sk, h=h: e.dma_start(out=k.kT_d[h, :, t0:t0 + 512], in_=sk[:]), reads=[sk])
        for r in range(4):
            sv = stv[r % 2]
            for half in range(2):
                p = PS[4 + half]
                for c in range(2):
                    mm(fw, p, p[:], latn, latn[:, 2 + c, r * 128:(r + 1) * 128], wkv_s, wkv_s[:, c, 1024 + half * 512:1024 + (half + 1) * 512], c == 0, c == 1)
                fw.op("act", lambda e, p=p, sv=sv, half=half: e.activation(out=sv[:, half * 512:(half + 1) * 512], in_=p[:], func=AF.Copy), reads=[p], writes=[sv])
            fw.dma("sp", lambda e, sv=sv, r=r: e.dma_start(out=k.v_d[:, t0 + r * 128:t0 + (r + 1) * 128, :].rearrange("h t d -> t h d"),
                                                            in_=sv[:].rearrange("t (h d) -> t h d", h=8)), reads=[sv])
    fw.barrier()
    fw.release(mk0)
    if k.stop == "A":
        return
    krT = fw.sb("krT", [64, T], BF16)
    fw.dma("sp", lambda e: e.dma_start(out=krT[:], in_=k.krT_d), writes=[krT])
    kTs = [fw.sb(f"kTs{i}", [128, T], BF16) for i in range(2)]
    vs = [fw.sb(f"vs{i}", [128, 32, 132], BF16) for i in range(2)]
    for i in range(2):
        fw.op("dve", lambda e, i=i: e.memset(vs[i][:, :, 128:129], 1.0), writes=[vs[i]])
    qn = [fw.sb(f"qn{i}", [128, 512], BF16) for i in range(2)]
    qr = [fw.sb(f"qr{i}", [64, 512], BF16) for i in range(2)]
    pT = [fw.sb(f"pT{i}", [128, 512], BF16) for i in range(3)]
    rs = fw.sb("rs", [128, 4])
    on = [fw.sb(f"on{i}", [128, 128]) for i in range(2)]
    oT = [fw.sb(f"oT{i}", [128, 512], BF16) for i in range(2)]
    blk = 0
    for h in range(8):
        kb = kTs[h % 2]; vb = vs[h % 2]
        fw.dma("sp", lambda e: e.dma_start(out=kb[:], in_=k.kT_d[h]), writes=[kb])
        for q4 in range(4):
            fw.dma("sp", lambda e, q4=q4: e.dma_start(out=vb[:, q4 * 8:(q4 + 1) * 8, 0:128],
                                                      in_=k.v_d[h, q4 * 1024:(q4 + 1) * 1024, :].rearrange("(c p) d -> p c d", p=128)), writes=[vb])
        for jq in range(8):
            qnb = qn[jq % 2]; qrb = qr[jq % 2]
            fw.dma("sp", lambda e: e.dma_start(out=qnb[:], in_=k.qT_d[h, 0:128, jq * 512:(jq + 1) * 512]), writes=[qnb])
            fw.dma("sp", lambda e: e.dma_start(out=qrb[:], in_=k.qT_d[h, 128:192, jq * 512:(jq + 1) * 512]), writes=[qrb])
            nk = 4 * jq + 4

            def emit_qk(kc_):
                q0_ = max(0, kc_ - 4 * jq) * 128
                s__ = PS[(blk + kc_) % 3]
                mm(fw, s__, s__[:, q0_:512], kb, kb[:, kc_ * 128:(kc_ + 1) * 128], qnb, qnb[:, q0_:512], True, False)
                mm(fw, s__, s__[:, q0_:512], krT, krT[:, kc_ * 128:(kc_ + 1) * 128], qrb, qrb[:, q0_:512], False, True)

            emit_qk(0)
            for kc in range(nk):
                r = max(0, kc - 4 * jq)
                q0 = r * 128
                sp_ = PS[(blk + kc) % 3]; pb = pT[(blk + kc) % 3]
                if kc + 1 < nk:
                    emit_qk(kc + 1)
                fw.op("act", lambda e, sp_=sp_, pb=pb, q0=q0: e.activation(out=pb[:, q0:512], in_=sp_[:, q0:512], func=AF.Exp), reads=[sp_], writes=[pb])
                if kc >= 4 * jq:
                    fw.op("dve", lambda e, pb=pb, q0=q0: e.tensor_tensor(out=pb[:, q0:q0 + 128], in0=pb[:, q0:q0 + 128], in1=k.trib[:], op=ALU.mult),
                          reads=[pb, k.trib], writes=[pb])
                for s_ in range(r, 4):
                    acc = PS[3 + s_]
                    mm(fw, acc, acc[:, 0:129], pb, pb[:, s_ * 128:(s_ + 1) * 128], vb, vb[:, kc, 0:129], kc == 0, kc == 4 * jq + s_)
            blk += nk
            ob = oT[jq % 2]
            for s_ in range(4):
                acc = PS[3 + s_]; onb = on[s_ % 2]
                fw.op("dve", lambda e, acc=acc, s_=s_: e.reciprocal(out=rs[:, s_:s_ + 1], in_=acc[:, 128:129]), reads=[acc], writes=[rs])
                fw.op("act", lambda e, acc=acc, onb=onb, s_=s_: e.activation(out=onb[:], in_=acc[:, 0:128], func=AF.Copy, scale=rs[:, s_:s_ + 1]), reads=[acc, rs], writes=[onb])
                tr(fw, PS[7], PS[7][:, s_ * 128:(s_ + 1) * 128], onb, onb[:], C_, k.ID, first=(s_ == 0))
            fw.op("act", lambda e, ob=ob: e.activation(out=ob[:], in_=PS[7][:], func=AF.Copy), reads=[PS[7]], writes=[ob])
            fw.dma("sp", lambda e, ob=ob: e.dma_start(out=k.oT_d[h, :, jq * 512:(jq + 1) * 512], in_=ob[:]), reads=[ob])
    fw.barrier()
    fw.release(mk0)
    if k.stop == "B":
        return


def mixer_out_and_moe_router(k, L, w_out_d, n_kc, oT_src):
    fw = k.fw; PS = k.PS; C_ = k.C_; MOD = k.MOD
    mk0 = fw.mark()
    wo = fw.sb(f"wo_{L}", [128, n_kc, D], BF16)
    for c in range(n_kc):
        fw.dma("pool", lambda e, c=c: e.dma_start(out=wo[:, c, :], in_=w_out_d[c * 128:(c + 1) * 128, :]), writes=[wo])
    oTt = [fw.sb(f"oTt{L}_{i}", [128, n_kc, 512], BF16) for i in range(2)]
    ytmp = fw.sb(f"ytmp{L}", [128, D])
    for j in range(T // 512):
        ob = oTt[j % 2]
        fw.dma("sp", lambda e: e.dma_start(out=ob[:], in_=oT_src[:, :, j * 512:(j + 1) * 512].rearrange("h p t -> p h t")), writes=[ob])
        for r in range(4):
            t0 = j * 512 + r * 128
            xb = k.xt[r % 2]
            fw.dma("sp", lambda e: e.dma_start(out=xb[:], in_=k.xs[t0:t0 + 128, :]), writes=[xb])
            for half in range(2):
                p = PS[half]
                for c in range(n_kc):
                    mm(fw, p, p[:], ob, ob[:, c, r * 128:(r + 1) * 128], wo, wo[:, c, half * 512:(half + 1) * 512], c == 0, c == n_kc - 1)
                fw.op("dve", lambda e, p=p, half=half: e.tensor_tensor(out=ytmp[:, half * 512:(half + 1) * 512], in0=p[:], in1=MOD[:, 2, half * 512:(half + 1) * 512], op=ALU.mult),
                      reads=[p, MOD], writes=[ytmp])
            fw.op("dve", lambda e: e.tensor_tensor(out=xb[:], in0=xb[:], in1=ytmp[:], op=ALU.add), reads=[xb, ytmp], writes=[xb])
            fw.dma("sp", lambda e: e.dma_start(out=k.xs[t0:t0 + 128, :], in_=xb[:]), reads=[xb])
    fw.barrier()
    fw.release(mk0)


def moe_layer(k, L):
    fw = k.fw; PS = k.PS; C_ = k.C_; MOD = k.MOD; nc = k.nc
    mk0 = fw.mark()
    yacc = k.yacc
    h2T = fw.sb("h2T", [128, 8, T], BF16)
    G_all = fw.sb("G_all", [128, NT, 32])
    wr = fw.sb("wr", [128, 8, 32])
    fw.dma("sp", lambda e: e.dma_start(out=wr[:], in_=k.w_router[L].rearrange("(c p) n -> p c n", p=128)), writes=[wr])
    brb = fw.sb("brb", [128, 32])
    fw.dma("sp", lambda e: e.dma_start(out=brb[:], in_=k.b_router[L:L + 1, :].broadcast_to([128, 32])), writes=[brb])
    h2f = fw.sb("h2f", [128, 8, 128])
    lg = fw.sb("lg", [128, 32]); m8 = fw.sb("m8", [128, 8]); msk = fw.sb("msk", [128, 32]); ex = fw.sb("ex", [128, 32])
    sm = fw.sb("sm", [128, 4])
    MS = ""
    for i in range(NT):
        xb = k.xt[i % 2]; hb = k.ht[i % 2]; sb_ = k.st[i % 2]
        fw.dma("sp", lambda e: e.dma_start(out=xb[:], in_=k.xs[i * 128:(i + 1) * 128, :]), writes=[xb])
        if MS == "DL":
            continue
        k.norm_mod(xb, hb, sb_, 4, 3)
        if MS == "D0":
            continue
        for half, p in ((0, PS[0]), (1, PS[1])):
            for c in range(4):
                cc = half * 4 + c
                tr(fw, p, p[:, c * 128:(c + 1) * 128], hb, hb[:, cc * 128:(cc + 1) * 128], C_, k.ID, first=(c == 0))
            fw.op("act", lambda e, half=half, p=p: e.activation(out=h2f[:, half * 4:(half + 1) * 4, :], in_=p[:].rearrange("p (c t) -> p c t", c=4), func=AF.Copy),
                  reads=[p], writes=[h2f])
        fw.op("dve", lambda e: e.tensor_copy(out=h2T[:, :, i * 128:(i + 1) * 128], in_=h2f[:]), reads=[h2f], writes=[h2T])
        if MS == "D1":
            continue
        pl = PS[2]
        for c in range(8):
            mm(fw, pl, pl[:, 0:32], h2f, h2f[:, c, :], wr, wr[:, c, :], c == 0, c == 7)
        fw.op("dve", lambda e: e.tensor_tensor(out=lg[:], in0=pl[:, 0:32], in1=brb[:], op=ALU.add), reads=[pl, brb], writes=[lg])
        if MS == "D2":
            continue
        fw.op("dve", lambda e: e.max(out=m8[:], in_=lg[:]), reads=[lg], writes=[m8])
        fw.op("dve", lambda e: e.tensor_scalar(out=msk[:], in0=lg[:], scalar1=m8[:, 3:4], scalar2=None, op0=ALU.is_ge), reads=[lg, m8], writes=[msk])
        fw.op("dve", lambda e: e.tensor_scalar(out=sm[:, 0:1], in0=m8[:, 0:1], scalar1=-1.0, scalar2=None, op0=ALU.mult), reads=[m8], writes=[sm])
        fw.op("act", lambda e: e.activation(out=ex[:], in_=lg[:], func=AF.Exp, bias=sm[:, 0:1], scale=1.0), reads=[lg, sm], writes=[ex])
        fw.op("dve", lambda e: e.tensor_tensor(out=ex[:], in0=ex[:], in1=msk[:], op=ALU.mult), reads=[ex, msk], writes=[ex])
        fw.op("dve", lambda e: e.reduce_sum(out=sm[:, 1:2], in_=ex[:], axis=AX.X), reads=[ex], writes=[sm])
        fw.op("dve", lambda e: e.reciprocal(out=sm[:, 2:3], in_=sm[:, 1:2]), reads=[sm], writes=[sm])
        fw.op("dve", lambda e: e.tensor_scalar(out=G_all[:, i, :], in0=ex[:], scalar1=sm[:, 2:3], scalar2=None, op0=ALU.mult), reads=[ex, sm], writes=[G_all])
    fw.barrier()
    if MS:
        fw.release(mk0)
        return
    wgu = fw.sb("wgu", [128, 8, 2048], BF16)
    wdn = fw.sb("wdn", [128, 8, 1024], BF16)
    bgu = fw.sb("bgu", [128, 16])
    bdb = fw.sb("bdb", [128, 1024])
    hact = fw.sb("hact", [128, 8, 512], BF16)
    g_ = fw.sb("g_", [128, 512]); sg_ = fw.sb("sg_", [128, 512]); l_ = fw.sb("l_", [128, 512])
    yo = [fw.sb(f"yo{i}", [128, 1024]) for i in range(2)]
    for ex_i in range(32):
        for c in range(8):
            fw.dma("pool", lambda e, c=c: e.dma_start(out=wgu[:, c, :], in_=k.w_gu[L, ex_i, c * 128:(c + 1) * 128, :]), writes=[wgu])
        for c in range(8):
            fw.dma("pool", lambda e, c=c: e.dma_start(out=wdn[:, c, :], in_=k.w_dn[L, ex_i, c * 128:(c + 1) * 128, :]), writes=[wdn])
        with nc.allow_non_contiguous_dma(reason="small bias relayout"):
            fw.dma("sp", lambda e: e.dma_start(out=bgu[:], in_=k.b_gu[L, ex_i, :].rearrange("(c p) -> p c", p=128)), writes=[bgu])
        fw.dma("sp", lambda e: e.dma_start(out=bdb[:], in_=k.b_dn[L, ex_i:ex_i + 1, :].broadcast_to([128, 1024])), writes=[bdb])
        for jt in range(T // 512):
            for fc in range(8):
                pg = PS[(fc % 2) * 2]; pl2 = PS[(fc % 2) * 2 + 1]
                for c in range(8):
                    mm(fw, pg, pg[:], wgu, wgu[:, c, fc * 128:(fc + 1) * 128], h2T, h2T[:, c, jt * 512:(jt + 1) * 512], c == 0, c == 7)
                for c in range(8):
                    mm(fw, pl2, pl2[:], wgu, wgu[:, c, 1024 + fc * 128:1024 + (fc + 1) * 128], h2T, h2T[:, c, jt * 512:(jt + 1) * 512], c == 0, c == 7)
                fw.op("dve", lambda e, pg=pg, fc=fc: e.tensor_scalar(out=g_[:], in0=pg[:], scalar1=bgu[:, fc:fc + 1], scalar2=7.0, op0=ALU.add, op1=ALU.min), reads=[pg, bgu], writes=[g_])
                fw.op("act", lambda e: e.activation(out=sg_[:], in_=g_[:], func=AF.Sigmoid, scale=1.702), reads=[g_], writes=[sg_])
                fw.op("dve", lambda e, pl2=pl2, fc=fc: e.tensor_scalar(out=l_[:], in0=pl2[:], scalar1=bgu[:, 8 + fc:9 + fc], scalar2=7.0, op0=ALU.add, op1=ALU.min), reads=[pl2, bgu], writes=[l_])
                fw.op("dve", lambda e: e.tensor_scalar(out=l_[:], in0=l_[:], scalar1=-7.0, scalar2=1.0, op0=ALU.max, op1=ALU.add), reads=[l_], writes=[l_])
                fw.op("dve", lambda e: e.tensor_tensor(out=g_[:], in0=g_[:], in1=sg_[:], op=ALU.mult), reads=[g_, sg_], writes=[g_])
                fw.op("dve", lambda e, fc=fc: e.tensor_tensor(out=hact[:, fc, :], in0=g_[:], in1=l_[:], op=ALU.mult), reads=[g_, l_], writes=[hact])
            for r in range(4):
                ti = jt * 4 + r
                yb = yo[r % 2]
                for half in range(2):
                    p = PS[4 + half]
                    for fc in range(8):
                        mm(fw, p, p[:], hact, hact[:, fc, r * 128:(r + 1) * 128], wdn, wdn[:, fc, half * 512:(half + 1) * 512], fc == 0, fc == 7)
                    fw.op("dve", lambda e, p=p, half=half, yb=yb: e.tensor_tensor(out=yb[:, half * 512:(half + 1) * 512], in0=p[:], in1=bdb[:, half * 512:(half + 1) * 512], op=ALU.add),
                          reads=[p, bdb], writes=[yb])
                fw.op("dve", lambda e, yb=yb, ti=ti: e.tensor_scalar(out=yb[:], in0=yb[:], scalar1=G_all[:, ti, ex_i:ex_i + 1], scalar2=None, op0=ALU.mult), reads=[yb, G_all], writes=[yb])
                if ex_i == 0:
                    fw.dma("sp", lambda e, yb=yb, ti=ti: e.dma_start(out=yacc[ti * 128:(ti + 1) * 128, :], in_=yb[:]), reads=[yb])
                else:
                    fw.dma("pool", lambda e, yb=yb, ti=ti: e.dma_start(out=yacc[ti * 128:(ti + 1) * 128, :], in_=yb[:], accum_op=ALU.add), reads=[yb])
        fw.barrier()
    for i in range(NT):
        xb = k.xt[i % 2]; yb = yo[i % 2]
        fw.dma("sp", lambda e: e.dma_start(out=xb[:], in_=k.xs[i * 128:(i + 1) * 128, :]), writes=[xb])
        fw.dma("sp", lambda e: e.dma_start(out=yb[:], in_=yacc[i * 128:(i + 1) * 128, :]), writes=[yb])
        fw.op("dve", lambda e: e.tensor_tensor(out=yb[:], in0=yb[:], in1=MOD[:, 5, :], op=ALU.mult), reads=[yb, MOD], writes=[yb])
        fw.op("dve", lambda e: e.tensor_tensor(out=xb[:], in0=xb[:], in1=yb[:], op=ALU.add), reads=[xb, yb], writes=[xb])
        fw.dma("sp", lambda e: e.dma_start(out=k.xs[i * 128:(i + 1) * 128, :], in_=xb[:]), reads=[xb])
    fw.barrier()
    fw.release(mk0)


def final_norm(k):
    fw = k.fw
    gfb = fw.sb("gfb", [128, D])
    fw.dma("sp", lambda e: e.dma_start(out=gfb[:], in_=k.g_final[0:1, :].broadcast_to([128, D])), writes=[gfb])
    fw.op("dve", lambda e: e.tensor_copy(out=k.MOD[:, 1, :], in_=gfb[:]), reads=[gfb], writes=[k.MOD])
    for i in range(NT):
        xb = k.xt[i % 2]; hb = k.ht[i % 2]; sb_ = k.st[i % 2]
        fw.dma("sp", lambda e: e.dma_start(out=xb[:], in_=k.xs[i * 128:(i + 1) * 128, :]), writes=[xb])
        k.norm_mod(xb, hb, sb_, 1, None)
        fw.dma("sp", lambda e: e.dma_start(out=k.out_d[i * 128:(i + 1) * 128, :], in_=hb[:]), reads=[hb])


def ssd_layer(k, L):
    fw = k.fw; nc = k.nc; PS = k.PS; C_ = k.C_; MOD = k.MOD
    mk0 = fw.mark()
    w_in = k.ssm_w_in
    fw.store_q = None
    dt_tm = fw.sb("dt_tm", [128, NT, 32]); nac_tm = fw.sb("nac_tm", [128, NT, 32])
    mk_a = fw.mark()
    dtT = fw.sb("dtT", [32, T]); adtT = fw.sb("adtT", [32, T])
    mkA = fw.mark()
    hT = fw.sb("s_hT", [128, 8, 512], F32R)
    wsl = [fw.sb(f"wsl{i}", [128, 8, 512], F32R) for i in range(2)]
    wdt = fw.sb("wdt", [128, 8, 32], F32R)
    fw.dma("pool", lambda e: e.dma_start(out=wdt[:], in_=w_in[:, 6144:6176].rearrange("(c p) n -> p c n", p=128)), writes=[wdt])
    cw = fw.sb("cw", [128, 32, 4]); cb = fw.sb("cb", [128, 32])
    fw.dma("sp", lambda e: e.dma_start(out=cw[:], in_=k.ssm_cw), writes=[cw])
    fw.dma("sp", lambda e: e.dma_start(out=cb[:], in_=k.ssm_cb), writes=[cb])
    hv = fw.sb("hv", [32, 4])
    fw.dma("sp", lambda e: e.dma_start(out=hv[:, 0:2], in_=k.ssm_hv), writes=[hv])
    fw.op("act", lambda e: e.activation(out=hv[:, 2:3], in_=hv[:, 1:2], func=AF.Exp), reads=[hv], writes=[hv])
    fw.op("dve", lambda e: e.tensor_scalar(out=hv[:, 3:4], in0=hv[:, 2:3], scalar1=-1.0, scalar2=None, op0=ALU.mult), reads=[hv], writes=[hv])
    halo = fw.sb("halo", [128, 32, 4])
    fw.op("dve", lambda e: e.memset(halo[:], 0.0), writes=[halo])
    ub = [fw.sb(f"ub{i}", [128, 516]) for i in range(2)]
    acc = [fw.sb(f"cacc{i}", [128, 512]) for i in range(2)]
    xact = [fw.sb(f"xact{i}", [128, 512]) for i in range(2)]
    bcst = [fw.sb(f"bcst{i}", [128, 512], BF16) for i in range(2)]
    xtm = [fw.sb(f"xtm{i}", [128, 4, 512]) for i in range(2)]
    zst = [fw.sb(f"zst{i}", [128, 512]) for i in range(2)]
    et = fw.sb("et", [32, 512])
    nslab = 0
    for j in range(T // 512):
        t0 = j * 512
        for r in range(4):
            xb = k.xt[r % 2]; hb = k.ht[r % 2]; sb_ = k.st[r % 2]
            fw.dma("pool", lambda e: e.dma_start(out=xb[:], in_=k.xs[t0 + r * 128:t0 + (r + 1) * 128, :]), writes=[xb])
            k.norm_mod(xb, hb, sb_, 1, 0)
            k.transpose_to(hb, hT, lambda half, r=r: hT[:, half * 4:(half + 1) * 4, r * 128:(r + 1) * 128], PS[0], PS[1])
        pd = PS[2]
        for c in range(8):
            mm(fw, pd, pd[0:32, :], wdt, wdt[:, c, :], hT, hT[:, c, :], c == 0, c == 7)
        fw.op("act", lambda e: e.activation(out=et[:], in_=pd[0:32, :], func=AF.Exp, bias=hv[:, 0:1], scale=1.0), reads=[pd, hv], writes=[et])
        fw.op("act", lambda e: e.activation(out=dtT[:, t0:t0 + 512], in_=et[:], func=AF.Ln, bias=1.0, scale=1.0), reads=[et], writes=[dtT])
        fw.op("dve", lambda e: e.tensor_scalar(out=adtT[:, t0:t0 + 512], in0=dtT[:, t0:t0 + 512], scalar1=hv[:, 3:4], scalar2=None, op0=ALU.mult), reads=[dtT, hv], writes=[adtT])
        for sl in range(8):
            wb = wsl[nslab % 2]; nslab += 1
            for hf in range(2):
                fw.dma("pool", lambda e, hf=hf: e.dma_start(out=wb[:, hf * 4:(hf + 1) * 4, :],
                                                             in_=w_in[hf * 512:(hf + 1) * 512, 2048 + sl * 512:2048 + (sl + 1) * 512].rearrange("(c p) n -> p c n", p=128)), writes=[wb])
            for c4 in range(4):
                cc = sl * 4 + c4
                p = PS[3 + (cc % 2)]
                for c in range(8):
                    mm(fw, p, p[:], wb, wb[:, c, c4 * 128:(c4 + 1) * 128], hT, hT[:, c, :], c == 0, c == 7)
                u = ub[cc % 2]; a_ = acc[cc % 2]; xa = xact[cc % 2]
                fw.op("act", lambda e, p=p, u=u: e.activation(out=u[:, 3:515], in_=p[:], func=AF.Copy), reads=[p], writes=[u])
                fw.op("dve", lambda e, u=u, cc=cc: e.tensor_copy(out=u[:, 0:3], in_=halo[:, cc, 0:3]), reads=[halo], writes=[u])
                fw.op("dve", lambda e, u=u, cc=cc: e.tensor_copy(out=halo[:, cc, 0:3], in_=u[:, 512:515]), reads=[u], writes=[halo])
                fw.op("act", lambda e, u=u, a_=a_, cc=cc: e.activation(out=a_[:], in_=u[:, 3:515], func=AF.Identity, scale=cw[:, cc, 3:4], bias=cb[:, cc:cc + 1]),
                      reads=[u, cw, cb], writes=[a_])
                for kk in range(3):
                    fw.op("dve", lambda e, u=u, a_=a_, cc=cc, kk=kk: e.scalar_tensor_tensor(out=a_[:], in0=u[:, kk:kk + 512], scalar=cw[:, cc, kk:kk + 1], in1=a_[:],
                                                                                          op0=ALU.mult, op1=ALU.add), reads=[u, cw, a_], writes=[a_])
                if cc < 16:
                    fw.op("act", lambda e, a_=a_, xa=xa: e.activation(out=xa[:], in_=a_[:], func=AF.Silu), reads=[a_], writes=[xa])
                    xs_t = xtm[(cc // 4) % 2]
                    pt = PS[5 + (cc % 2)]
                    for r in range(4):
                        tr(fw, pt, pt[:, r * 128:(r + 1) * 128], xa, xa[:, r * 128:(r + 1) * 128], C_, k.ID, first=(r == 0))
                    fw.op("act", lambda e, pt=pt, xs_t=xs_t, c4=c4: e.activation(out=xs_t[:, :, c4 * 128:(c4 + 1) * 128], in_=pt[:].rearrange("p (r c) -> p r c", r=4), func=AF.Copy),
                          reads=[pt], writes=[xs_t])
                    if c4 == 3:
                        for r in range(4):
                            fw.dma("sp", lambda e, xs_t=xs_t, r=r: e.dma_start(out=k.sxs_d[t0 + r * 128:t0 + (r + 1) * 128, sl * 512:(sl + 1) * 512], in_=xs_t[:, r, :]), reads=[xs_t])
                else:
                    bs = bcst[cc % 2]
                    fw.op("act", lambda e, a_=a_, bs=bs: e.activation(out=bs[:], in_=a_[:], func=AF.Silu), reads=[a_], writes=[bs])
                    fw.dma("sp", lambda e, bs=bs, cc=cc: e.dma_start(out=k.sbc_d[cc - 16, :, t0:t0 + 512], in_=bs[:]), reads=[bs])
        for zs in range(4):
            wb = wsl[nslab % 2]; nslab += 1
            for hf in range(2):
                fw.dma("pool", lambda e, hf=hf: e.dma_start(out=wb[:, hf * 4:(hf + 1) * 4, :],
                                                             in_=w_in[hf * 512:(hf + 1) * 512, zs * 512:(zs + 1) * 512].rearrange("(c p) n -> p c n", p=128)), writes=[wb])
            for r in range(4):
                p = PS[3 + (r % 2)]
                for c in range(8):
                    mm(fw, p, p[:], hT, hT[:, c, r * 128:(r + 1) * 128], wb, wb[:, c, :], c == 0, c == 7)
                zt = zst[r % 2]
                fw.op("act", lambda e, p=p, zt=zt: e.activation(out=zt[:, 0:512], in_=p[:], func=AF.Silu), reads=[p], writes=[zt])
                fw.dma("sp", lambda e, zt=zt, r=r: e.dma_start(out=k.szs_d[t0 + r * 128:t0 + (r + 1) * 128, zs * 512:(zs + 1) * 512], in_=zt[:, 0:512]), reads=[zt])
    fw.barrier()
    fw.store_q = "pool"
    fw.release(mkA)
    ones32 = fw.sb("ones32", [32, T])
    fw.op("dve", lambda e: e.memset(ones32[:], 1.0), writes=[ones32])
    acT = fw.sb("acT", [32, T])
    fw.op("dve", lambda e: e.tensor_tensor_scan(out=acT[:], data0=ones32[:], data1=adtT[:], initial=0.0, op0=ALU.mult, op1=ALU.add), reads=[ones32, adtT], writes=[acT])
    fw.dma("sp", lambda e: e.dma_start(out=k.sac_d, in_=acT[:]), reads=[acT])
    fw.barrier()
    for c in range(NT):
        p = PS[c % 2]
        tr(fw, p, p[:, 0:32], dtT, dtT[:, c * 128:(c + 1) * 128], C_, C_[0:32, 0:32])
        tr(fw, p, p[:, 32:64], acT, acT[:, c * 128:(c + 1) * 128], C_, C_[0:32, 0:32], first=False)
        fw.op("act", lambda e, p=p, c=c: e.activation(out=dt_tm[:, c, :], in_=p[:, 0:32], func=AF.Copy), reads=[p], writes=[dt_tm])
        fw.op("act", lambda e, p=p, c=c: e.activation(out=nac_tm[:, c, :], in_=p[:, 32:64], func=AF.Copy, scale=-1.0), reads=[p], writes=[nac_tm])
    fw.barrier()
    fw.release(mk_a)
    mk1 = fw.mark()
    BT = [fw.sb(f"BT{i}", [128, T], BF16) for i in range(2)]
    CT = [fw.sb(f"CT{i}", [128, T], BF16) for i in range(2)]
    abc = [fw.sb(f"abc{i}", [128, T]) for i in range(4)]
    xsf = fw.sb("xsf", [128, NT, 64])
    V = [fw.sb(f"V{i}", [128, NT, 64], BF16) for i in range(4)]
    dec = [fw.sb(f"dec{i}", [128, 512]) for i in range(4)]
    pT = [fw.sb(f"spT{i}", [128, 512], BF16) for i in range(6)]
    ysb = [fw.sb(f"ysb{i}", [128, 4, 64]) for i in range(2)]
    blk = 0; nd = 0; npt = 0
    for g in range(8):
        Bb = BT[g % 2]; Cb = CT[g % 2]
        fw.dma("sp", lambda e: e.dma_start(out=Bb[:], in_=k.sbc_d[g]), writes=[Bb])
        fw.dma("sp", lambda e: e.dma_start(out=Cb[:], in_=k.sbc_d[8 + g]), writes=[Cb])
        for r in range(4):
            hh = g * 4 + r
            fw.dma("sp", lambda e, r=r, hh=hh: e.dma_start(out=abc[r][:], in_=k.sac_d[hh:hh + 1, :].broadcast_to([128, T])), writes=[abc[r]])
            for q4 in range(4):
                fw.dma("sp", lambda e, q4=q4, hh=hh: e.dma_start(out=xsf[:, q4 * 8:(q4 + 1) * 8, :],
                                                                 in_=k.sxs_d[q4 * 1024:(q4 + 1) * 1024, hh * 64:(hh + 1) * 64].rearrange("(c p) d -> p c d", p=128)), writes=[xsf])
            fw.op("dve", lambda e, r=r, hh=hh: e.tensor_tensor(out=V[r][:], in0=xsf[:], in1=dt_tm[:, :, hh:hh + 1].broadcast_to([128, NT, 64]), op=ALU.mult),
                  reads=[xsf, dt_tm], writes=[V[r]])
        for jq in range(8):
            nk = 4 * jq + 4

            def emit_st(kc_):
                q0_ = max(0, kc_ - 4 * jq) * 128
                s__ = PS[(blk + kc_) % 3]
                mm(fw, s__, s__[:, q0_:512], Bb, Bb[:, kc_ * 128:(kc_ + 1) * 128], Cb, Cb[:, jq * 512 + q0_:(jq + 1) * 512], True, True)

            emit_st(0)
            for kc in range(nk):
                r0 = max(0, kc - 4 * jq)
                q0 = r0 * 128
                diag = kc >= 4 * jq
                sp_ = PS[(blk + kc) % 3]
                if kc + 1 < nk:
                    emit_st(kc + 1)
                for r in range(4):
                    hh = g * 4 + r
                    d_ = dec[nd % 4]; nd += 1
                    pb = pT[npt % 6]; npt += 1
                    qa = jq * 512 + q0
                    if diag:
                        fw.op("dve", lambda e, d_=d_, r=r, qa=qa, hh=hh: e.tensor_scalar(out=d_[:, q0:q0 + 128], in0=abc[r][:, qa:qa + 128], scalar1=nac_tm[:, kc, hh:hh + 1], scalar2=0.0,
                                                                                       op0=ALU.add, op1=ALU.min), reads=[abc[r], nac_tm], writes=[d_])
                        fw.op("act", lambda e, d_=d_: e.activation(out=d_[:, q0:q0 + 128], in_=d_[:, q0:q0 + 128], func=AF.Exp), reads=[d_], writes=[d_])
                        fw.op("dve", lambda e, d_=d_: e.tensor_tensor(out=d_[:, q0:q0 + 128], in0=d_[:, q0:q0 + 128], in1=C_[:, 256:384], op=ALU.mult), reads=[d_, C_], writes=[d_])
                        if q0 + 128 < 512:
                            fw.op("act", lambda e, d_=d_, r=r, qa=qa, hh=hh: e.activation(out=d_[:, q0 + 128:512], in_=abc[r][:, qa + 128:(jq + 1) * 512], func=AF.Exp,
                                                                                     bias=nac_tm[:, kc, hh:hh + 1], scale=1.0), reads=[abc[r], nac_tm, d_], writes=[d_])
                    else:
                        fw.op("act", lambda e, d_=d_, r=r, qa=qa, hh=hh: e.activation(out=d_[:, q0:512], in_=abc[r][:, qa:(jq + 1) * 512], func=AF.Exp,
                                                                                 bias=nac_tm[:, kc, hh:hh + 1], scale=1.0), reads=[abc[r], nac_tm], writes=[d_])
                    fw.op("dve", lambda e, d_=d_, pb=pb, sp_=sp_: e.tensor_tensor(out=pb[:, q0:512], in0=sp_[:, q0:512], in1=d_[:, q0:512], op=ALU.mult), reads=[sp_, d_], writes=[pb])
                    ya = PS[3 + r]
                    for s_ in range(r0, 4):
                        fw.op("pe", lambda e, ya=ya, pb=pb, s_=s_, r=r: e.matmul(ya[:, s_ * 64:(s_ + 1) * 64], pb[:, s_ * 128:(s_ + 1) * 128], V[r][:, kc, :],
                                                                               start=(kc == 0), stop=(kc == 4 * jq + s_)),
                              reads=[pb, V[r]], writes=[ya], accumulate=True)
            blk += nk
            for r in range(4):
                hh = g * 4 + r
                ya = PS[3 + r]; yb = ysb[r % 2]
                fw.op("act", lambda e, ya=ya, yb=yb: e.activation(out=yb[:], in_=ya[:, 0:256].rearrange("p (s d) -> p s d", s=4), func=AF.Copy), reads=[ya], writes=[yb])
                fw.dma("sp", lambda e, yb=yb, hh=hh: e.dma_start(out=k.sy_d[jq * 512:(jq + 1) * 512, hh * 64:(hh + 1) * 64].rearrange("(s p) d -> p s d", p=128), in_=yb[:]), reads=[yb])
    fw.barrier()
    fw.release(mk1)
    dbc = fw.sb("dbc", [128, 2048]); gnb = fw.sb("gnb", [128, 2048])
    fw.dma("sp", lambda e: e.dma_start(out=dbc[:], in_=k.ssm_dexp[0:1, :].broadcast_to([128, 2048])), writes=[dbc])
    fw.dma("sp", lambda e: e.dma_start(out=gnb[:], in_=k.ssm_gn[0:1, :].broadcast_to([128, 2048])), writes=[gnb])
    yt = [fw.sb(f"yt{i}", [128, 2048]) for i in range(2)]
    xs2 = [fw.sb(f"xs2{i}", [128, 2048]) for i in range(2)]
    zz = [fw.sb(f"zz{i}", [128, 2048]) for i in range(2)]
    sqv = fw.sb("sqv", [128, 2048])
    gs = fw.sb("gs", [128, 16])
    ynT = [fw.sb(f"ynT{i}", [128, 16, 128], BF16) for i in range(2)]
    for i in range(NT):
        y_ = yt[i % 2]; x_ = xs2[i % 2]; z_ = zz[i % 2]
        fw.dma("sp", lambda e: e.dma_start(out=y_[:], in_=k.sy_d[i * 128:(i + 1) * 128, :]), writes=[y_])
        fw.dma("sp", lambda e: e.dma_start(out=x_[:], in_=k.sxs_d[i * 128:(i + 1) * 128, :]), writes=[x_])
        fw.dma("sp", lambda e: e.dma_start(out=z_[:], in_=k.szs_d[i * 128:(i + 1) * 128, :]), writes=[z_])
        fw.op("dve", lambda e: e.tensor_tensor(out=x_[:], in0=x_[:], in1=dbc[:], op=ALU.mult), reads=[x_, dbc], writes=[x_])
        fw.op("dve", lambda e: e.tensor_tensor(out=y_[:], in0=y_[:], in1=x_[:], op=ALU.add), reads=[y_, x_], writes=[y_])
        fw.op("dve", lambda e: e.tensor_tensor(out=y_[:], in0=y_[:], in1=z_[:], op=ALU.mult), reads=[y_, z_], writes=[y_])
        fw.op("act", lambda e: e.activation(out=sqv[:], in_=y_[:], func=AF.Square), reads=[y_], writes=[sqv])
        fw.op("dve", lambda e: e.tensor_reduce(out=gs[:, 0:8], in_=sqv[:].rearrange("p (g d) -> p g d", g=8), axis=AX.X, op=ALU.add), reads=[sqv], writes=[gs])
        fw.op("act", lambda e: e.activation(out=gs[:, 8:16], in_=gs[:, 0:8], func=AF.Sqrt, bias=k.epsc[:], scale=1.0 / 256), reads=[gs, k.epsc], writes=[gs])
        fw.op("dve", lambda e: e.reciprocal(out=gs[:, 8:16], in_=gs[:, 8:16]), reads=[gs], writes=[gs])
        for g in range(8):
            fw.op("dve", lambda e, g=g: e.scalar_tensor_tensor(out=y_[:, g * 256:(g + 1) * 256], in0=y_[:, g * 256:(g + 1) * 256], scalar=gs[:, 8 + g:9 + g],
                                                               in1=gnb[:, g * 256:(g + 1) * 256], op0=ALU.mult, op1=ALU.mult), reads=[y_, gs, gnb], writes=[y_])
        yT = ynT[i % 2]
        for q4 in range(4):
            p = PS[q4 % 2]
            for c in range(4):
                cc = q4 * 4 + c
                tr(fw, p, p[:, c * 128:(c + 1) * 128], y_, y_[:, cc * 128:(cc + 1) * 128], C_, k.ID, first=(c == 0))
            fw.op("act", lambda e, p=p, q4=q4: e.activation(out=yT[:, q4 * 4:(q4 + 1) * 4, :], in_=p[:].rearrange("p (c t) -> p c t", c=4), func=AF.Copy), reads=[p], writes=[yT])
        fw.dma("sp", lambda e: e.dma_start(out=k.synT_d[:, :, i * 128:(i + 1) * 128].rearrange("c p t -> p c t"), in_=yT[:]), reads=[yT])
    fw.barrier()
    fw.release(mk0)


MOE_B = 256
MOE_NB = 4 * T // MOE_B + 64


def moe_layer_sparse(k, L):
    fw = k.fw; PS = k.PS; C_ = k.C_; MOD = k.MOD; nc = k.nc
    NB = MOE_NB; B = MOE_B
    mk0 = fw.mark()
    S4_all = fw.sb("S4_all", [128, NT, 4], I32); G4_all = fw.sb("G4_all", [128, NT, 4])
    idx_w = fw.sb("idx_w", [128, 128], I32); idx_b = fw.sb("idx_b", [128, 128], I32)
    idx_g = [fw.sb(f"idx_g{i}", [128, 128], I32) for i in range(8)]
    mkD = fw.mark()
    G_all = fw.sb("G_all", [128, NT, 32]); M_all = fw.sb("M_all", [128, NT, 32]); R_all = fw.sb("R_all", [128, NT, 32])
    base = fw.sb("base", [128, 32])
    fw.op("dve", lambda e: e.memset(base[:], 0.0), writes=[base])
    wr = fw.sb("wr", [128, 8, 32])
    fw.dma("sp", lambda e: e.dma_start(out=wr[:], in_=k.w_router[L].rearrange("(c p) n -> p c n", p=128)), writes=[wr])
    brb = fw.sb("brb", [128, 32])
    fw.dma("sp", lambda e: e.dma_start(out=brb[:], in_=k.b_router[L:L + 1, :].broadcast_to([128, 32])), writes=[brb])
    h2f = fw.sb("h2f", [128, 8, 128])
    lg = fw.sb("lg", [128, 32]); m8 = fw.sb("m8", [128, 8]); ex = fw.sb("ex", [128, 32])
    sm = fw.sb("sm", [128, 4])
    for i in range(NT):
        xb = k.xt[i % 2]; hb = k.ht[i % 2]; sb_ = k.st[i % 2]
        fw.dma("sp", lambda e: e.dma_start(out=xb[:], in_=k.xs[i * 128:(i + 1) * 128, :]), writes=[xb])
        k.norm_mod(xb, hb, sb_, 4, 3)
        fw.dma("sp", lambda e: e.dma_start(out=k.h2_d[i * 128:(i + 1) * 128, :], in_=hb[:]), reads=[hb])
        for half, p in ((0, PS[0]), (1, PS[1])):
            for c in range(4):
                cc = half * 4 + c
                tr(fw, p, p[:, c * 128:(c + 1) * 128], hb, hb[:, cc * 128:(cc + 1) * 128], C_, k.ID, first=(c == 0))
            fw.op("act", lambda e, half=half, p=p: e.activation(out=h2f[:, half * 4:(half + 1) * 4, :], in_=p[:].rearrange("p (c t) -> p c t", c=4), func=AF.Copy),
                  reads=[p], writes=[h2f])
        pl = PS[2]
        for c in range(8):
            mm(fw, pl, pl[:, 0:32], h2f, h2f[:, c, :], wr, wr[:, c, :], c == 0, c == 7)
        msk = M_all
        fw.op("dve", lambda e: e.tensor_tensor(out=lg[:], in0=pl[:, 0:32], in1=brb[:], op=ALU.add), reads=[pl, brb], writes=[lg])
        fw.op("dve", lambda e: e.max(out=m8[:], in_=lg[:]), reads=[lg], writes=[m8])
        fw.op("dve", lambda e: e.tensor_scalar(out=M_all[:, i, :], in0=lg[:], scalar1=m8[:, 3:4], scalar2=None, op0=ALU.is_ge), reads=[lg, m8], writes=[M_all])
        fw.op("dve", lambda e: e.tensor_scalar(out=sm[:, 0:1], in0=m8[:, 0:1], scalar1=-1.0, scalar2=None, op0=ALU.mult), reads=[m8], writes=[sm])
        fw.op("act", lambda e: e.activation(out=ex[:], in_=lg[:], func=AF.Exp, bias=sm[:, 0:1], scale=1.0), reads=[lg, sm], writes=[ex])
        fw.op("dve", lambda e: e.tensor_tensor(out=ex[:], in0=ex[:], in1=M_all[:, i, :], op=ALU.mult), reads=[ex, M_all], writes=[ex])
        fw.op("dve", lambda e: e.reduce_sum(out=sm[:, 1:2], in_=ex[:], axis=AX.X), reads=[ex], writes=[sm])
        fw.op("dve", lambda e: e.reciprocal(out=sm[:, 2:3], in_=sm[:, 1:2]), reads=[sm], writes=[sm])
        fw.op("dve", lambda e: e.tensor_scalar(out=G_all[:, i, :], in0=ex[:], scalar1=sm[:, 2:3], scalar2=None, op0=ALU.mult), reads=[ex, sm], writes=[G_all])
        pr = PS[3]; pc = PS[4]
        mm(fw, pr, pr[:, 0:32], C_, C_[:, 384:512], M_all, M_all[:, i, :], True, True)
        mm(fw, pc, pc[:, 0:32], C_, C_[:, 128:256], M_all, M_all[:, i, :], True, True)
        fw.op("dve", lambda e: e.tensor_tensor(out=R_all[:, i, :], in0=pr[:, 0:32], in1=base[:], op=ALU.add), reads=[pr, base], writes=[R_all])
        fw.op("dve", lambda e: e.tensor_tensor(out=base[:], in0=pc[:, 0:32], in1=base[:], op=ALU.add), reads=[pc, base], writes=[base])
    ti = fw.sb("ti", [128, 32], I32); pf = fw.sb("pf", [128, 32]); pend = fw.sb("pend", [128, 32]); pst = fw.sb("pst", [128, 32])
    on32 = fw.sb("on32", [128, 32]); sl_ = fw.sb("sl_", [128, 32]); v_ = fw.sb("v_", [128, 32]); m8s = fw.sb("m8s", [128, 8]); jk = fw.sb("jk", [128, 32])
    fw.op("dve", lambda e: e.memset(on32[:], 1.0), writes=[on32])
    fw.op("dve", lambda e: e.tensor_scalar(out=pf[:], in0=base[:], scalar1=float(2 * B - 1), scalar2=None, op0=ALU.add), reads=[base], writes=[pf])
    fw.op("dve", lambda e: e.tensor_copy(out=ti[:], in_=pf[:]), reads=[pf], writes=[ti])
    sh = (2 * B).bit_length() - 1
    fw.op("dve", lambda e: e.tensor_scalar(out=ti[:], in0=ti[:], scalar1=sh, scalar2=sh, op0=ALU.logical_shift_right, op1=ALU.logical_shift_left), reads=[ti], writes=[ti])
    fw.op("dve", lambda e: e.tensor_copy(out=pf[:], in_=ti[:]), reads=[ti], writes=[pf])
    fw.op("dve", lambda e: e.tensor_tensor_scan(out=pend[:], data0=on32[:], data1=pf[:], initial=0.0, op0=ALU.mult, op1=ALU.add), reads=[on32, pf], writes=[pend])
    fw.op("dve", lambda e: e.tensor_tensor(out=pst[:], in0=pend[:], in1=pf[:], op=ALU.subtract), reads=[pend, pf], writes=[pst])
    for i in range(NT):
        fw.op("dve", lambda e: e.tensor_tensor(out=sl_[:], in0=R_all[:, i, :], in1=pst[:], op=ALU.add), reads=[R_all, pst], writes=[sl_])
        fw.op("dve", lambda e: e.scalar_tensor_tensor(out=v_[:], in0=sl_[:], scalar=1.0, in1=M_all[:, i, :], op0=ALU.add, op1=ALU.mult), reads=[sl_, M_all], writes=[v_])
        fw.op("dve", lambda e: e.max(out=m8s[:], in_=v_[:]), reads=[v_], writes=[m8s])
        fw.op("dve", lambda e: e.tensor_scalar(out=S4_all[:, i, :], in0=m8s[:, 0:4], scalar1=-1.0, scalar2=None, op0=ALU.add), reads=[m8s], writes=[S4_all])
        for kk in range(4):
            fw.op("dve", lambda e, kk=kk: e.scalar_tensor_tensor(out=jk[:], in0=v_[:], scalar=m8s[:, kk:kk + 1], in1=G_all[:, i, :], op0=ALU.is_equal, op1=ALU.mult,
                                                                accum_out=G4_all[:, i, kk:kk + 1]), reads=[v_, m8s, G_all], writes=[jk, G4_all])
    cmp_ = fw.sb("cmp_", [128, 32]); bec = fw.sb("bec", [128, 2]); ber = fw.sb("ber", [1, 136]); crow = fw.sb("crow", [1, 128]); vrow = fw.sb("vrow", [1, 128])
    fw.op("dve", lambda e: e.tensor_scalar(out=cmp_[:], in0=pend[:], scalar1=C_[:, 512:513], scalar2=None, op0=ALU.is_le), reads=[pend, C_], writes=[cmp_])
    fw.op("dve", lambda e: e.reduce_sum(out=bec[:, 0:1], in_=cmp_[:], axis=AX.X), reads=[cmp_], writes=[bec])
    fw.op("dve", lambda e: e.tensor_scalar(out=bec[:, 0:1], in0=bec[:, 0:1], scalar1=31.0, scalar2=None, op0=ALU.min), reads=[bec], writes=[bec])
    fw.op("dve", lambda e: e.tensor_scalar(out=bec[:, 1:2], in0=C_[:, 512:513], scalar1=pend[:, 31:32], scalar2=None, op0=ALU.is_lt), reads=[pend, C_], writes=[bec])
    pt_ = PS[5]
    tr(fw, pt_, pt_[0:1, 0:128], bec, bec[:, 0:1], C_, k.ID)
    tr(fw, pt_, pt_[0:1, 128:256], bec, bec[:, 1:2], C_, k.ID, first=False)
    fw.op("dve", lambda e: e.memset(ber[:], -1.0), writes=[ber])
    fw.op("act", lambda e: e.activation(out=ber[0:1, 4:132], in_=pt_[0:1, 0:128], func=AF.Copy), reads=[pt_], writes=[ber])
    fw.op("act", lambda e: e.activation(out=vrow[:], in_=pt_[0:1, 128:256], func=AF.Copy), reads=[pt_], writes=[vrow])
    fw.op("dve", lambda e: e.tensor_tensor(out=crow[:], in0=ber[0:1, 4:132], in1=ber[0:1, 0:128], op=ALU.not_equal), reads=[ber], writes=[crow])
    fw.op("dve", lambda e: e.tensor_tensor(out=crow[:], in0=crow[:], in1=vrow[:], op=ALU.mult), reads=[crow, vrow], writes=[crow])
    fw.op("dve", lambda e: e.tensor_tensor(out=crow[:], in0=crow[:], in1=C_[0:1, 640:768], op=ALU.mult), reads=[crow, C_], writes=[crow])
    BIG = 1.0e6
    pb1 = PS[6]; pb2 = PS[7]
    mm(fw, pb1, pb1[:, 0:128], C_, C_[0:1, 128:256], ber, ber[0:1, 4:132], True, True)
    mm(fw, pb2, pb2[:, 0:128], C_, C_[0:1, 128:256], crow, crow[0:1, :], True, True)
    cnd = fw.sb("cnd", [128, 128]); tw = fw.sb("tw", [128, 128]); tb = fw.sb("tb", [128, 128])
    fw.op("act", lambda e: e.activation(out=cnd[:], in_=pb2[:, 0:128], func=AF.Copy), reads=[pb2], writes=[cnd])
    fw.op("dve", lambda e: e.tensor_scalar(out=tb[:], in0=pb1[:, 0:128], scalar1=float(L * 32) - BIG, scalar2=None, op0=ALU.add), reads=[pb1], writes=[tb])
    fw.op("dve", lambda e: e.tensor_scalar(out=tw[:], in0=pb1[:, 0:128], scalar1=128.0, scalar2=float(L * 4096) - BIG, op0=ALU.mult, op1=ALU.add), reads=[pb1], writes=[tw])
    fw.op("dve", lambda e: e.tensor_scalar(out=tw[:], in0=tw[:], scalar1=C_[:, 513:514], scalar2=None, op0=ALU.add), reads=[tw, C_], writes=[tw])
    for t_, ix in ((tw, idx_w), (tb, idx_b)):
        fw.op("dve", lambda e, t_=t_: e.tensor_tensor(out=t_[:], in0=t_[:], in1=cnd[:], op=ALU.mult), reads=[t_, cnd], writes=[t_])
        fw.op("dve", lambda e, t_=t_, ix=ix: e.tensor_scalar(out=ix[:], in0=t_[:], scalar1=BIG, scalar2=None, op0=ALU.add), reads=[t_], writes=[ix])
    for c2 in range(8):
        fw.op("dve", lambda e, c2=c2: e.tensor_scalar(out=idx_g[c2][:], in0=tw[:], scalar1=8.0, scalar2=8 * BIG + c2, op0=ALU.mult, op1=ALU.add), reads=[tw], writes=[idx_g[c2]])
    fw.barrier()
    fw.release(mkD)
    MS = ""
    if MS == "E":
        dbt = k.xt[0]
        for j_, src in enumerate((idx_w, idx_b, idx_g[0], idx_g[1])):
            fw.op("dve", lambda e, j_=j_, src=src: e.tensor_copy(out=dbt[:, j_ * 128:(j_ + 1) * 128], in_=src[:]), reads=[src], writes=[dbt])
        fw.op("dve", lambda e: e.tensor_copy(out=dbt[:, 512:640], in_=S4_all[:].rearrange("p a b -> p (a b)")), reads=[S4_all], writes=[dbt])
        fw.op("dve", lambda e: e.tensor_copy(out=dbt[:, 640:768], in_=G4_all[:].rearrange("p a b -> p (a b)")), reads=[G4_all], writes=[dbt])
        fw.dma("sp", lambda e: e.dma_start(out=k.xs[0:128, :], in_=dbt[:]), reads=[dbt])
        fw.barrier()
        fw.release(mk0); return
    fw.store_q = None
    for i in range(NT):
        hb = k.ht[i % 2]
        fw.dma("sp", lambda e: e.dma_start(out=hb[:], in_=k.h2_d[i * 128:(i + 1) * 128, :]), writes=[hb])
        for kk in range(4):
            fw.dma("pool", lambda e, kk=kk: e.indirect_dma_start(out=k.xsort_d, out_offset=bass.IndirectOffsetOnAxis(ap=S4_all[:, i, kk:kk + 1], axis=0),
                                                                 in_=hb[:], in_offset=None), reads=[hb, S4_all])
    fw.barrier()
    if MS == "F":
        fw.release(mk0); return
    mkG = fw.mark()
    wgu = [fw.sb(f"wgu{i}", [128, 8, 2048], BF16) for i in range(2)]
    wdn = [fw.sb(f"wdn{i}", [128, 8, 1024], BF16) for i in range(2)]
    bgr = [fw.sb(f"bgr{i}", [128, 2048], BF16) for i in range(2)]
    bdb = [fw.sb(f"bdb{i}", [128, 1024]) for i in range(2)]
    onesb = fw.sb("onesb", [1, B], BF16)
    fw.op("dve", lambda e: e.memset(onesb[:], 1.0), writes=[onesb])
    xblk = [fw.sb(f"xblk{i}", [128, B // 128, 1024]) for i in range(1)]
    xT = [fw.sb(f"xT{i}", [128, 8, B], BF16) for i in range(2)]
    hact = fw.sb("hact", [128, 8, B], BF16)
    g_s = [fw.sb(f"g_{i}", [128, B]) for i in range(2)]; sg_s = [fw.sb(f"sg_{i}", [128, B]) for i in range(2)]; l_s = [fw.sb(f"l_{i}", [128, B]) for i in range(2)]
    yo = [fw.sb(f"yo{i}", [128, 1024]) for i in range(2)]
    bound_reg = nc.gpsimd.to_reg(2 * 32 * 1024 - 1)
    for bi in range(NB):
        pj = (bi // 2) % 2
        wg = wgu[pj]; wd = wdn[pj]; bg = bgr[pj]; bd = bdb[pj]
        NW = 2 * 32 * 128 - 1
        if bi % 2 == 0:
            for c in range(8):
                fw.dma("pool", lambda e, c=c: e.indirect_dma_start(out=wg[:, c, :], out_offset=None, in_=k.w_gu,
                                                                   in_offset=bass.IndirectOffsetOnAxis(ap=idx_g[c][:, bi:bi + 1], axis=0), bounds_check=bound_reg, oob_is_err=False),
                       reads=[idx_g[c]], writes=[wg])
            for c in range(8):
                fw.dma("pool", lambda e, c=c: e.indirect_dma_start(out=wd[:, c, :], out_offset=None, in_=k.w_dn,
                                                                   in_offset=bass.IndirectOffsetOnAxis(ap=idx_g[c][:, bi:bi + 1], axis=0), bounds_check=bound_reg, oob_is_err=False),
                       reads=[idx_g[c]], writes=[wd])
            fw.dma("pool", lambda e: e.indirect_dma_start(out=bg[:], out_offset=None, in_=k.b_gu, in_offset=bass.IndirectOffsetOnAxis(ap=idx_b[:, bi:bi + 1], axis=0),
                                                          bounds_check=bound_reg, oob_is_err=False), reads=[idx_b], writes=[bg])
            fw.dma("pool", lambda e: e.indirect_dma_start(out=bd[:], out_offset=None, in_=k.b_dn, in_offset=bass.IndirectOffsetOnAxis(ap=idx_b[:, bi:bi + 1], axis=0),
                                                          bounds_check=bound_reg, oob_is_err=False), reads=[idx_b], writes=[bd])
        xb = xblk[0]; xt_ = xT[bi % 2]
        if bi == 0:
            fw.dma("sp", lambda e: e.dma_start(out=xb[:], in_=k.xsort_d[0:B, :].rearrange("(r p) d -> p r d", p=128)), writes=[xb])
        for rt in range(B // 128):
            for half in range(2):
                p = PS[half]
                for c in range(4):
                    cc = half * 4 + c
                    tr(fw, p, p[:, c * 128:(c + 1) * 128], xb, xb[:, rt, bass.ds(cc, 128, 8)], C_, k.ID, first=(c == 0))
                fw.op("act", lambda e, half=half, p=p, rt=rt: e.activation(out=xt_[:, half * 4:(half + 1) * 4, rt * 128:(rt + 1) * 128], in_=p[:].rearrange("p (c t) -> p c t", c=4), func=AF.Copy),
                      reads=[p], writes=[xt_])
        if bi + 1 < NB:
            fw.dma("sp", lambda e: e.dma_start(out=xb[:], in_=k.xsort_d[(bi + 1) * B:(bi + 2) * B, :].rearrange("(r p) d -> p r d", p=128)), writes=[xb])
        for fc in range(8):
            pgl = PS[2 + (fc % 4)]
            g_ = g_s[fc % 2]; sg_ = sg_s[fc % 2]; l_ = l_s[fc % 2]
            for part, off in ((0, 0), (1, 1024)):
                o_ap = pgl[:, part * B:(part + 1) * B]
                for c in range(8):
                    mm(fw, pgl, o_ap, wg, wg[:, c, bass.ds(off + fc, 128, 8)], xt_, xt_[:, c, :], c == 0, False)
                mm(fw, pgl, o_ap, bg, bg[0:1, bass.ds(off + fc, 128, 8)], onesb, onesb[0:1, :], False, True)
            fw.op("dve", lambda e, pgl=pgl, g_=g_: e.tensor_scalar(out=g_[:], in0=pgl[:, 0:B], scalar1=7.0, scalar2=None, op0=ALU.min), reads=[pgl], writes=[g_])
            fw.op("act", lambda e: e.activation(out=sg_[:], in_=g_[:], func=AF.Sigmoid, scale=1.702), reads=[g_], writes=[sg_])
            fw.op("dve", lambda e, pgl=pgl: e.tensor_scalar(out=l_[:], in0=pgl[:, B:2 * B], scalar1=7.0, scalar2=-7.0, op0=ALU.min, op1=ALU.max), reads=[pgl], writes=[l_])
            fw.op("dve", lambda e: e.tensor_tensor(out=g_[:], in0=g_[:], in1=sg_[:], op=ALU.mult), reads=[g_, sg_], writes=[g_])
            fw.op("dve", lambda e, fc=fc: e.scalar_tensor_tensor(out=hact[:, fc, :], in0=l_[:], scalar=1.0, in1=g_[:], op0=ALU.add, op1=ALU.mult), reads=[g_, l_], writes=[hact])
        for rt in range(B // 128):
            yb = yo[rt % 2]
            for half in range(2):
                p = PS[6 + half]
                for fc in range(8):
                    mm(fw, p, p[:], hact, hact[:, fc, rt * 128:(rt + 1) * 128], wd, wd[:, fc, half * 512:(half + 1) * 512], fc == 0, fc == 7)
                fw.op("dve", lambda e, p=p, half=half, yb=yb: e.tensor_tensor(out=yb[:, half * 512:(half + 1) * 512], in0=p[:], in1=bd[:, half * 512:(half + 1) * 512], op=ALU.add),
                      reads=[p, bd], writes=[yb])
            fw.dma("sp", lambda e, yb=yb, rt=rt: e.dma_start(out=k.ysort_d[bi * B + rt * 128:bi * B + (rt + 1) * 128, :], in_=yb[:]), reads=[yb])
    fw.barrier()
    fw.release(mkG)
    if MS == "G":
        fw.release(mk0); return
    fw.store_q = "act"
    yk = [[fw.sb(f"yk{i}_{kk}", [128, 1024]) for kk in range(4)] for i in range(2)]
    for i in range(NT):
        xb = k.xt[i % 2]; ys_ = yk[i % 2]
        fw.dma("sp", lambda e: e.dma_start(out=xb[:], in_=k.xs[i * 128:(i + 1) * 128, :]), writes=[xb])
        for kk in range(4):
            fw.dma("pool", lambda e, kk=kk: e.indirect_dma_start(out=ys_[kk][:], out_offset=None, in_=k.ysort_d,
                                                                 in_offset=bass.IndirectOffsetOnAxis(ap=S4_all[:, i, kk:kk + 1], axis=0)), reads=[S4_all], writes=[ys_[kk]])
        a0 = ys_[0]
        fw.op("dve", lambda e: e.tensor_scalar(out=a0[:], in0=a0[:], scalar1=G4_all[:, i, 0:1], scalar2=None, op0=ALU.mult), reads=[a0, G4_all], writes=[a0])
        for kk in range(1, 4):
            fw.op("dve", lambda e, kk=kk: e.scalar_tensor_tensor(out=a0[:], in0=ys_[kk][:], scalar=G4_all[:, i, kk:kk + 1], in1=a0[:], op0=ALU.mult, op1=ALU.add),
                  reads=[ys_[kk], G4_all, a0], writes=[a0])
        fw.op("dve", lambda e: e.tensor_tensor(out=a0[:], in0=a0[:], in1=MOD[:, 5, :], op=ALU.mult), reads=[a0, MOD], writes=[a0])
        fw.op("dve", lambda e: e.tensor_tensor(out=xb[:], in0=xb[:], in1=a0[:], op=ALU.add), reads=[xb, a0], writes=[xb])
        fw.dma("sp", lambda e: e.dma_start(out=k.xs[i * 128:(i + 1) * 128, :], in_=xb[:]), reads=[xb])
    fw.barrier()
    fw.store_q = "pool"
    fw.release(mk0)


def host_consts():
    cst = np.zeros((128, 1024), np.float32)
    cst[:, 0:128] = np.eye(128, dtype=np.float32)
    cst[:, 128:256] = 1.0
    i = np.arange(128)
    cst[:, 256:384] = (i[None, :] >= i[:, None]).astype(np.float32)
    cst[:, 384:512] = (i[:, None] < i[None, :]).astype(np.float32)
    cst[:, 512] = i * 256.0
    cst[:, 513] = i
    cst[:, 640:768] = (i % 2 == 0).astype(np.float32)[None, :]
    inv = (1.0 / (10000.0 ** (np.arange(0, 64, 2, dtype=np.float32) / 64))).astype(np.float32)
    invf = np.zeros((64, 2), np.float32)
    invf[:32, 0] = inv; invf[32:, 0] = inv
    invf[:32, 1] = -1.0; invf[32:, 1] = 1.0
    return cst, invf


def prep_core(inp, b):
    f = np.ascontiguousarray
    cst, invf = host_consts()
    m = {}
    m["x"] = f(inp["x"][b])
    m["c"] = f(inp["c"][b].reshape(8, 128).T)
    m["pos"] = f(inp["positions"][b].reshape(1, T).astype(np.int32))
    m["w_mod"] = inp["w_mod"]; m["b_mod"] = inp["b_mod"]
    m["g_mix"] = inp["g_mix_norm"]; m["g_ffn"] = inp["g_ffn_norm"]
    w_in = inp["mla_w_in"][0]
    kr = w_in[:, 512:576]
    m["mla_w_in"] = f(np.concatenate([w_in, kr[:, 32:], kr[:, :32]], axis=1))
    m["mla_gq"] = f(inp["mla_g_q"][0].reshape(2, 128).T)
    m["mla_gkv"] = f(inp["mla_g_kv"][0].reshape(2, 128).T)
    wq = inp["mla_w_q_up"][0].reshape(256, 8, 192)
    sw = np.concatenate([wq[:, :, 160:192], wq[:, :, 128:160]], axis=2)
    m["mla_wq"] = f(np.concatenate([wq.reshape(256, 1536), sw.reshape(256, 512)], axis=1))
    wkv = inp["mla_w_kv_up"][0].reshape(256, 8, 256)
    m["mla_wkv"] = f(np.concatenate([wkv[:, :, :128].reshape(256, 1024), wkv[:, :, 128:].reshape(256, 1024)], axis=1))
    m["mla_wo"] = inp["mla_w_out"][0]
    m["w_router"] = inp["moe_w_router"]; m["b_router"] = inp["moe_b_router"]
    m["w_gu"] = inp["moe_w_gate_up"].reshape(2 * 32 * 1024, 2048); m["b_gu"] = inp["moe_b_gate_up"].reshape(64, 2048)
    m["w_dn"] = inp["moe_w_down"].reshape(2 * 32 * 1024, 1024); m["b_dn"] = inp["moe_b_down"].reshape(64, 1024)
    m["g_final"] = f(inp["g_final"].reshape(1, D))
    m["ssm_w_in"] = inp["ssm_w_in"][0]
    m["ssm_cw"] = f(inp["ssm_conv_w"][0].reshape(4, 32, 128).transpose(2, 1, 0))
    m["ssm_cb"] = f(inp["ssm_conv_b"][0].reshape(32, 128).T)
    m["ssm_hv"] = f(np.stack([inp["ssm_dt_bias"][0], inp["ssm_a_log"][0]], axis=1))
    m["ssm_dexp"] = f(np.repeat(inp["ssm_d"][0], 64).reshape(1, 2048))
    m["ssm_gn"] = f(inp["ssm_g_norm"][0].reshape(1, 2048))
    m["ssm_wo"] = inp["ssm_w_out"][0]
    m["cst"] = cst; m["invf"] = invf
    return m

_CACHE = {}


def _program():
    if "k" not in _CACHE:
        k = build()
        k.stop = "all"
        k.compute_mod(0)
        mla_layer(k, 0, True)
        mixer_out_and_moe_router(k, 0, k.mla_wo, 8, k.oT_d)
        moe_layer_sparse(k, 0)
        k.compute_mod(1)
        ssd_layer(k, 1)
        mixer_out_and_moe_router(k, 1, k.ssm_wo, 16, k.synT_d)
        moe_layer_sparse(k, 1)
        final_norm(k)
        k.fw.finish()
        k.fw.close()
        _CACHE["k"] = k
    return _CACHE["k"]


def kernel(**inputs):
    inp = {kk: np.asarray(v) for kk, v in inputs.items()}
    k = _program()
    in_maps = [prep_core(inp, b) for b in range(8)]
    res = run_bass_kernel_spmd(k.nc, in_maps, core_ids=list(range(8)))
    return np.stack([np.asarray(r["out"]) for r in res.results], axis=0).astype(np.float32)
```

```python
import numpy as np
import concourse.bass as bass
import concourse.mybir as mybir
from concourse.bass_utils import run_bass_kernel_spmd

F32 = mybir.dt.float32
F32R = mybir.dt.float32r
BF16 = mybir.dt.bfloat16
I32 = mybir.dt.int32
U32 = mybir.dt.uint32
ALU = mybir.AluOpType
AF = mybir.ActivationFunctionType
AX = mybir.AxisListType

EPOCH = 20000


class Buf:
    __slots__ = ("t", "name", "lastw", "reads", "wfill", "dsem", "dcount", "is_dram")

    def __init__(self, t, name, is_dram=False):
        self.t = t
        self.name = name
        self.lastw = None
        self.reads = []
        self.wfill = []
        self.is_dram = is_dram

    def __getitem__(self, idx):
        return self.t[idx]


class FW:
    def __init__(self, nc):
        self.nc = nc
        self.eng = {"pe": nc.tensor, "dve": nc.vector, "act": nc.scalar,
                    "pool": nc.gpsimd, "sp": nc.sync}
        self.sem = {}
        self.cnt = {}
        self.nsem = 0
        self.waited = {e: {} for e in self.eng}
        self.semobjs = {}
        self.ctx = []
        self.bctx = []
        self.last = {e: None for e in self.eng}
        self.pend = {e: [] for e in self.eng}
        self.store_q = "pool"
        self.dma_toks = []
        self.dma_pool = []
        self.dma_free = []
        for e in ("pe", "dve", "act", "pool"):
            self._new_epoch(e)

    def _mksem(self, name):
        cm = self.nc.semaphore(name)
        s = cm.__enter__()
        self.ctx.append(cm)
        self.nsem += 1
        self.semobjs[id(s)] = s
        return s

    def _new_epoch(self, e):
        self.sem[e] = self._mksem(f"s_{e}_{self.nsem}")
        self.cnt[e] = 0

    def sb(self, name, shape, dt=F32):
        self.uid = getattr(self, "uid", 0) + 1
        cm = self.nc.sbuf_tensor(f"{name}_u{self.uid}", shape, dt)
        t = cm.__enter__()
        self.bctx.append(cm)
        return Buf(t, name)

    def ps(self, name, shape, dt=F32):
        cm = self.nc.psum_tensor(name, shape, dt)
        t = cm.__enter__()
        self.ctx.append(cm)
        return Buf(t, name)

    def dram(self, name, shape, dt=F32, kind="Internal"):
        t = self.nc.dram_tensor(name, shape, dt, kind=kind)
        return Buf(t.ap(), name, is_dram=True)

    def _wait(self, e, tok):
        if tok is None:
            return
        sem, val, _ = tok
        w = self.waited[e]
        if w.get(id(sem), 0) >= val:
            return
        w[id(sem)] = val
        self.eng[e].wait_ge(sem, val)

    def _deps(self, e, reads, writes, accumulate=False, is_dma=False):
        for tok in self.pend[e]:
            self._wait(e, tok)
        self.pend[e] = []
        for b in reads:
            if b is None:
                continue
            if b.lastw is not None:
                self._wait(e, b.lastw)
            for tok in b.wfill:
                self._wait(e, tok)
        for b in writes:
            if b is None:
                continue
            fill = is_dma and b.lastw is not None and b.lastw[2] == "dmaq" and not b.reads
            if b.lastw is not None and not (accumulate and b.lastw[2] == e) and not fill:
                if b.lastw[2] != e or b.lastw[2] in ("sp", "poolq", "actq"):
                    self._wait(e, b.lastw)
                    for tok in b.wfill:
                        self._wait(e, tok)
            for tok in b.reads:
                if tok[2] != e:
                    self._wait(e, tok)

    def _commit(self, tok, reads, writes):
        for b in reads:
            if b is not None:
                b.reads.append(tok)
        for b in writes:
            if b is not None:
                if tok[2] == "dmaq" and b.lastw is not None and b.lastw[2] == "dmaq" and not b.reads:
                    b.wfill.append(b.lastw)
                else:
                    b.wfill = []
                b.lastw = tok
                b.reads = []

    def op(self, e, fn, reads=(), writes=(), accumulate=False):
        self._deps(e, reads, writes, accumulate)
        if self.cnt[e] >= EPOCH:
            self._new_epoch(e)
        inst = fn(self.eng[e])
        self.cnt[e] += 1
        inst.then_inc(self.sem[e], 1)
        tok = (self.sem[e], self.cnt[e], e)
        self.last[e] = tok
        self._commit(tok, reads, writes)
        return tok

    def _dma_sem(self):
        if self.dma_free:
            return self.dma_free.pop()
        s = [self._mksem(f"s_dma_{self.nsem}"), 0]
        self.dma_pool.append(s)
        return s

    def dma(self, q, fn, reads=(), writes=(), semslot=None):
        if q == "sp" and not [w for w in writes if w is not None] and getattr(self, "store_q", None):
            q = self.store_q
        self._deps(q, reads, writes, is_dma=True)
        if semslot is None:
            semslot = self._rot_sem(q)
        if semslot[1] > 0:
            self._wait(q, (semslot[0], semslot[1], "dmaq"))
        inst = fn(self.eng[q])
        semslot[1] += 16
        inst.then_inc(semslot[0], 16)
        tok = (semslot[0], semslot[1], "dmaq")
        self._commit(tok, reads, writes)
        self.dma_toks.append(tok)
        if len(self.dma_toks) > 64:
            self.dma_toks = self.dma_toks[-64:]
        return tok

    NROT = 16

    def _rot_sem(self, q="sp"):
        if not hasattr(self, "_rotq"):
            self._rotq = {}
        if q not in self._rotq:
            self._rotq[q] = {"sems": [[self._mksem(f"s_rot_{q}_{i}"), 0] for i in range(self.NROT)], "i": 0}
        pool = self._rotq[q]
        i = pool["i"]
        pool["i"] = (i + 1) % self.NROT
        return _RotSlot(pool["sems"], i)

    def barrier(self):
        toks = [t for t in self.last.values() if t is not None]
        toks += self.dma_toks
        for pool in getattr(self, "_rotq", {}).values():
            for s in pool["sems"]:
                if s[1] > 0:
                    toks.append((s[0], s[1], "dmaq"))
        for e in self.eng:
            self.pend[e] = list(toks)
        self.dma_toks = []

    def finish(self):
        self.barrier()
        for e in self.eng:
            for tok in self.pend[e]:
                self._wait(e, tok)
            self.pend[e] = []

    def mark(self):
        return len(self.bctx)

    def release(self, m):
        while len(self.bctx) > m:
            self.bctx.pop().__exit__(None, None, None)

    def close(self):
        self.release(0)
        for cm in reversed(self.ctx):
            cm.__exit__(None, None, None)
        self.ctx = []


class _RotSlot(list):
    def __init__(self, sems, i):
        super().__init__(sems[i])
        self.sems = sems
        self.i = i

    def __setitem__(self, k, v):
        super().__setitem__(k, v)
        self.sems[self.i][k] = v


T = 4096
D = 1024
NT = T // 128
EPS = 1e-6
MLA_SCALE = 192 ** -0.5
PI = float(np.pi)
TWO_PI = float(2 * np.pi)


class K:
    pass


def mm(fw, ob, o_ap, lb, l_ap, rb, r_ap, start, stop):
    return fw.op("pe", lambda e: e.matmul(o_ap, l_ap, r_ap, start=start, stop=stop),
                 reads=[lb, rb], writes=[ob], accumulate=not start)


def tr(fw, ob, o_ap, ib, i_ap, ident, id_ap, first=True):
    return fw.op("pe", lambda e: e.transpose(o_ap, i_ap, id_ap), reads=[ib, ident], writes=[ob],
                 accumulate=not first)


def build(stop="all", n_layers=2, debug=False):
    nc = bass.Bass("TRN2", target_bir_lowering=False)
    fw = FW(nc)
    k = K()

    def din(name, shape, dt=F32):
        return nc.dram_tensor(name, shape, dt, kind="ExternalInput").ap()

    x_in = din("x", [T, D])
    c_in = din("c", [128, 8])
    pos_in = din("pos", [1, T], I32)
    w_mod = din("w_mod", [2, D, 6 * D])
    b_mod = din("b_mod", [2, 6 * D])
    g_mix = din("g_mix", [2, D])
    g_ffn = din("g_ffn", [2, D])
    mla_w_in = din("mla_w_in", [D, 640])
    mla_gq = din("mla_gq", [128, 2])
    mla_gkv = din("mla_gkv", [128, 2])
    mla_wq = din("mla_wq", [256, 2048])
    mla_wkv = din("mla_wkv", [256, 2048])
    mla_wo = din("mla_wo", [D, D])
    w_router = din("w_router", [2, D, 32])
    b_router = din("b_router", [2, 32])
    w_gu = din("w_gu", [2 * 32 * 1024, 2 * D])
    b_gu = din("b_gu", [64, 2 * D])
    w_dn = din("w_dn", [2 * 32 * 1024, D])
    b_dn = din("b_dn", [64, D])
    g_final = din("g_final", [1, D])
    ssm_w_in = din("ssm_w_in", [D, 6176])
    ssm_cw = din("ssm_cw", [128, 32, 4])
    ssm_cb = din("ssm_cb", [128, 32])
    ssm_hv = din("ssm_hv", [32, 2])
    ssm_dexp = din("ssm_dexp", [1, 2048])
    ssm_gn = din("ssm_gn", [1, 2048])
    ssm_wo = din("ssm_wo", [2048, D])
    cst = din("cst", [128, 1024])
    invf = din("invf", [64, 2])
    out_d = nc.dram_tensor("out", [T, D], F32, kind="ExternalOutput").ap()
    dbg_d = nc.dram_tensor("dbg", [T, D], F32, kind="ExternalOutput").ap() if debug else None

    def dscr(name, shape, dt=F32):
        return nc.dram_tensor(name, shape, dt, kind="Internal").ap()

    xs = dscr("xs", [T, D])
    qT_d = dscr("qT_d", [8, 192, T], BF16)
    kT_d = dscr("kT_d", [8, 128, T], BF16)
    krT_d = dscr("krT_d", [64, T], BF16)
    v_d = dscr("v_d", [8, T, 128], BF16)
    oT_d = dscr("oT_d", [8, 128, T], BF16)
    yacc = dscr("yacc", [T, D])
    h2_d = dscr("h2_d", [T, D])
    xsort_d = dscr("xsort_d", [4 * T + 64 * 256, D])
    ysort_d = dscr("ysort_d", [4 * T + 64 * 256, D])
    sxs_d = dscr("sxs_d", [T, 2048])
    szs_d = dscr("szs_d", [T, 2048])
    sy_d = dscr("sy_d", [T, 2048])
    sbc_d = dscr("sbc_d", [16, 128, T], BF16)
    sac_d = dscr("sac_d", [32, T])
    synT_d = dscr("synT_d", [16, 128, T], BF16)

    C_ = fw.sb("cst_s", [128, 1024])
    fw.dma("sp", lambda e: e.dma_start(out=C_[:], in_=cst), writes=[C_])
    ident = C_
    ID = C_[:, 0:128]
    ONES = C_[:, 128:256]
    TRI = C_[:, 256:384]
    SL = C_[:, 384:512]
    onesr = fw.sb("onesr", [128, 128], F32R)
    fw.op("act", lambda e: e.activation(out=onesr[:], in_=C_[:, 128:256], func=AF.Copy), reads=[C_], writes=[onesr])
    trib = fw.sb("trib", [128, 128], BF16)
    fw.op("act", lambda e: e.activation(out=trib[:], in_=C_[:, 256:384], func=AF.Copy), reads=[C_], writes=[trib])
    epsc = fw.sb("epsc", [128, 1])
    fw.op("dve", lambda e: e.memset(epsc[:], EPS), writes=[epsc])
    PS = [fw.ps(f"ps{i}", [128, 512]) for i in range(8)]

    cond = fw.sb("cond", [128, 8])
    fw.dma("sp", lambda e: e.dma_start(out=cond[:], in_=c_in), writes=[cond])
    sg = fw.sb("sg", [128, 8])
    fw.op("act", lambda e: e.activation(out=sg[:], in_=cond[:], func=AF.Sigmoid), reads=[cond], writes=[sg])
    fw.op("dve", lambda e: e.tensor_tensor(out=cond[:], in0=cond[:], in1=sg[:], op=ALU.mult), reads=[cond, sg], writes=[cond])
    condb = fw.sb("condb", [128, 8, 128])
    for c in range(8):
        fw.op("dve", lambda e, c=c: e.tensor_scalar(out=condb[:, c, :], in0=C_[:, 128:256], scalar1=cond[:, c:c + 1],
                                                    scalar2=None, op0=ALU.mult), reads=[C_, cond], writes=[condb])
    MOD = fw.sb("MOD", [128, 6, D])

    def compute_mod(L):
        mkm = fw.mark()
        wmt = [fw.sb(f"wmt{i}", [128, 8, 512]) for i in range(2)]
        bmt = [fw.sb(f"bmt{i}", [128, 512]) for i in range(2)]
        gbc = fw.sb("gbc", [128, D])
        for n in range(12):
            wt = wmt[n % 2]
            bt = bmt[n % 2]
            fw.dma("sp", lambda e: e.dma_start(out=wt[:], in_=w_mod[L, :, n * 512:(n + 1) * 512].rearrange("(c p) n -> p c n", p=128)), writes=[wt])
            fw.dma("sp", lambda e: e.dma_start(out=bt[:], in_=b_mod[L:L + 1, n * 512:(n + 1) * 512].broadcast_to([128, 512])), writes=[bt])
            p = PS[n % 2]
            for c in range(8):
                mm(fw, p, p[:], condb, condb[:, c, :], wt, wt[:, c, :], c == 0, c == 7)
            fw.op("dve", lambda e: e.tensor_tensor(out=MOD[:, n // 2, (n % 2) * 512:(n % 2 + 1) * 512], in0=p[:], in1=bt[:], op=ALU.add),
                  reads=[p, bt], writes=[MOD])
        for (slot, g) in ((1, g_mix), (4, g_ffn)):
            fw.dma("sp", lambda e: e.dma_start(out=gbc[:], in_=g[L:L + 1, :].broadcast_to([128, D])), writes=[gbc])
            fw.op("dve", lambda e: e.scalar_tensor_tensor(out=MOD[:, slot, :], in0=MOD[:, slot, :], scalar=1.0, in1=gbc[:],
                                                          op0=ALU.add, op1=ALU.mult), reads=[MOD, gbc], writes=[MOD])
        fw.barrier()
        fw.release(mkm)

    xt = [fw.sb(f"xt{i}", [128, D]) for i in range(2)]
    ht = [fw.sb(f"ht{i}", [128, D]) for i in range(2)]
    junk = fw.sb("junk", [128, D])
    st = [fw.sb(f"st{i}", [128, 4]) for i in range(2)]

    def norm_mod(xb, hb, sb_, a_slot, s_slot):
        fw.op("act", lambda e: e.activation(out=junk[:], in_=xb[:], func=AF.Square, accum_out=sb_[:, 0:1]), reads=[xb], writes=[junk, sb_])
        fw.op("act", lambda e: e.activation(out=sb_[:, 1:2], in_=sb_[:, 0:1], func=AF.Sqrt, bias=epsc[:], scale=1.0 / D), reads=[sb_, epsc], writes=[sb_])
        fw.op("dve", lambda e: e.reciprocal(out=sb_[:, 2:3], in_=sb_[:, 1:2]), reads=[sb_], writes=[sb_])
        fw.op("dve", lambda e: e.scalar_tensor_tensor(out=hb[:], in0=xb[:], scalar=sb_[:, 2:3], in1=MOD[:, a_slot, :], op0=ALU.mult, op1=ALU.mult),
              reads=[xb, sb_, MOD], writes=[hb])
        if s_slot is not None:
            fw.op("dve", lambda e: e.tensor_tensor(out=hb[:], in0=hb[:], in1=MOD[:, s_slot, :], op=ALU.add), reads=[hb, MOD], writes=[hb])

    def transpose_to(hb, dstb, dst_fn, pa, pb):
        for half, p in ((0, pa), (1, pb)):
            for c in range(4):
                cc = half * 4 + c
                tr(fw, p, p[:, c * 128:(c + 1) * 128], hb, hb[:, cc * 128:(cc + 1) * 128], C_, ID, first=(c == 0))
            fw.op("act", lambda e, half=half, p=p: e.activation(out=dst_fn(half), in_=p[:].rearrange("p (c t) -> p c t", c=4), func=AF.Copy),
                  reads=[p], writes=[dstb])

    k.__dict__.update(locals())
    return k


def mla_layer(k, L, src_first):
    fw = k.fw; nc = k.nc; PS = k.PS; C_ = k.C_; MOD = k.MOD
    x_src = k.x_in if src_first else k.xs
    mk0 = fw.mark()
    w_in_s = fw.sb("mla_w_in_s", [128, 8, 640], F32R)
    for h2 in range(2):
        fw.dma("pool", lambda e: e.dma_start(out=w_in_s[:, h2 * 4:(h2 + 1) * 4, :], in_=k.mla_w_in[h2 * 512:(h2 + 1) * 512, :].rearrange("(c p) n -> p c n", p=128)), writes=[w_in_s])
    wq_s = fw.sb("wq_s", [128, 2, 2048], F32R)
    wkv_s = fw.sb("wkv_s", [128, 2, 2048], F32R)
    for c in range(2):
        fw.dma("pool", lambda e: e.dma_start(out=wq_s[:, c, :], in_=k.mla_wq[c * 128:(c + 1) * 128, :]), writes=[wq_s])
        fw.dma("pool", lambda e: e.dma_start(out=wkv_s[:, c, :], in_=k.mla_wkv[c * 128:(c + 1) * 128, :]), writes=[wkv_s])
    gq = fw.sb("gq", [128, 2]); gkv = fw.sb("gkv", [128, 2]); invf = fw.sb("invf_s", [64, 2])
    fw.dma("sp", lambda e: e.dma_start(out=gq[:], in_=k.mla_gq), writes=[gq])
    fw.dma("sp", lambda e: e.dma_start(out=gkv[:], in_=k.mla_gkv), writes=[gkv])
    fw.dma("sp", lambda e: e.dma_start(out=invf[:], in_=k.invf), writes=[invf])
    hT = fw.sb("hT", [128, 8, 512], F32R)
    sq = fw.sb("sq", [128, 2, 512], F32R)
    rstd = fw.sb("rstd", [128, 512])
    latn = fw.sb("latn", [128, 4, 512], F32R)
    posi = fw.sb("posi", [64, 512], I32)
    ang = fw.sb("ang", [64, 512]); kf = fw.sb("kf", [64, 512]); ki = fw.sb("ki", [64, 512], I32)
    rr = fw.sb("rr", [64, 512]); mwrap = fw.sb("mwrap", [64, 512])
    Ct = fw.sb("Ct", [64, 512]); St = fw.sb("St", [64, 512])
    t1 = fw.sb("t1", [64, 512]); t2 = fw.sb("t2", [64, 512])
    stq = [fw.sb(f"stq{i}", [128, 512], BF16) for i in range(2)]
    str_ = [fw.sb(f"str{i}", [64, 512], BF16) for i in range(2)]
    stv = [fw.sb(f"stv{i}", [128, 1024], BF16) for i in range(2)]

    def wrap_sin(dst, src, shift):
        fw.op("dve", lambda e: e.tensor_scalar(out=kf[:], in0=src[:], scalar1=shift, scalar2=1.0 / TWO_PI, op0=ALU.add, op1=ALU.mult), reads=[src], writes=[kf])
        fw.op("dve", lambda e: e.tensor_copy(out=ki[:], in_=kf[:]), reads=[kf], writes=[ki])
        fw.op("dve", lambda e: e.tensor_copy(out=kf[:], in_=ki[:]), reads=[ki], writes=[kf])
        fw.op("dve", lambda e: e.scalar_tensor_tensor(out=rr[:], in0=kf[:], scalar=-TWO_PI, in1=src[:], op0=ALU.mult, op1=ALU.add), reads=[kf, src], writes=[rr])
        if shift != 0.0:
            fw.op("dve", lambda e: e.tensor_scalar(out=rr[:], in0=rr[:], scalar1=shift, scalar2=None, op0=ALU.add), reads=[rr], writes=[rr])
        fw.op("dve", lambda e: e.tensor_scalar(out=mwrap[:], in0=rr[:], scalar1=PI, scalar2=-TWO_PI, op0=ALU.is_gt, op1=ALU.mult), reads=[rr], writes=[mwrap])
        fw.op("dve", lambda e: e.tensor_tensor(out=rr[:], in0=rr[:], in1=mwrap[:], op=ALU.add), reads=[rr, mwrap], writes=[rr])
        fw.op("dve", lambda e: e.tensor_scalar(out=mwrap[:], in0=rr[:], scalar1=-PI, scalar2=TWO_PI, op0=ALU.is_lt, op1=ALU.mult), reads=[rr], writes=[mwrap])
        fw.op("dve", lambda e: e.tensor_tensor(out=rr[:], in0=rr[:], in1=mwrap[:], op=ALU.add), reads=[rr, mwrap], writes=[rr])
        fw.op("dve", lambda e: e.tensor_scalar(out=rr[:], in0=rr[:], scalar1=PI, scalar2=-PI, op0=ALU.min, op1=ALU.max), reads=[rr], writes=[rr])
        fw.op("act", lambda e: e.activation(out=dst[:], in_=rr[:], func=AF.Sin), reads=[rr], writes=[dst])

    for j in range(T // 512):
        t0 = j * 512
        for r in range(4):
            xb = k.xt[r % 2]; hb = k.ht[r % 2]; sb_ = k.st[r % 2]
            fw.dma("sp", lambda e: e.dma_start(out=xb[:], in_=x_src[t0 + r * 128:t0 + (r + 1) * 128, :]), writes=[xb])
            if src_first:
                fw.dma("sp", lambda e: e.dma_start(out=k.xs[t0 + r * 128:t0 + (r + 1) * 128, :], in_=xb[:]), reads=[xb])
            k.norm_mod(xb, hb, sb_, 1, 0)
            k.transpose_to(hb, hT, lambda half, r=r: hT[:, half * 4:(half + 1) * 4, r * 128:(r + 1) * 128], PS[0], PS[1])
        fw.dma("sp", lambda e: e.dma_start(out=posi[:], in_=k.pos_in[0:1, t0:t0 + 512].broadcast_to([64, 512])), writes=[posi])
        fw.op("dve", lambda e: e.tensor_copy(out=ang[:], in_=posi[:]), reads=[posi], writes=[ang])
        fw.op("dve", lambda e: e.tensor_scalar(out=ang[:], in0=ang[:], scalar1=invf[:, 0:1], scalar2=None, op0=ALU.mult), reads=[ang, invf], writes=[ang])
        wrap_sin(St, ang, 0.0)
        wrap_sin(Ct, ang, PI / 2)
        fw.op("dve", lambda e: e.tensor_scalar(out=St[:], in0=St[:], scalar1=invf[:, 1:2], scalar2=None, op0=ALU.mult), reads=[St, invf], writes=[St])
        for oc in range(4):
            p = PS[2 + oc]
            for c in range(8):
                mm(fw, p, p[:], w_in_s, w_in_s[:, c, oc * 128:(oc + 1) * 128], hT, hT[:, c, :], c == 0, c == 7)
        for oc in range(2):
            p = PS[6 + oc]
            for c in range(8):
                mm(fw, p, p[0:64, :], w_in_s, w_in_s[:, c, 512 + oc * 64:512 + (oc + 1) * 64], hT, hT[:, c, :], c == 0, c == 7)
        for grp, gvec in ((0, gq), (1, gkv)):
            for c2 in range(2):
                p = PS[2 + grp * 2 + c2]
                fw.op("act", lambda e, p=p, c2=c2: e.activation(out=sq[:, c2, :], in_=p[:], func=AF.Square), reads=[p], writes=[sq])
            for c2 in range(2):
                mm(fw, PS[0], PS[0][:], k.onesr, k.onesr[:], sq, sq[:, c2, :], c2 == 0, c2 == 1)
            fw.op("act", lambda e: e.activation(out=rstd[:], in_=PS[0][:], func=AF.Sqrt, bias=k.epsc[:], scale=1.0 / 256), reads=[PS[0], k.epsc], writes=[rstd])
            fw.op("dve", lambda e: e.reciprocal(out=rstd[:], in_=rstd[:]), reads=[rstd], writes=[rstd])
            for c2 in range(2):
                p = PS[2 + grp * 2 + c2]
                fw.op("dve", lambda e, p=p, c2=c2, grp=grp, gvec=gvec: e.scalar_tensor_tensor(out=latn[:, grp * 2 + c2, :], in0=p[:], scalar=gvec[:, c2:c2 + 1], in1=rstd[:],
                                                                                     op0=ALU.mult, op1=ALU.mult), reads=[p, gvec, rstd], writes=[latn])
        sr = str_[0]
        fw.op("dve", lambda e: e.tensor_tensor(out=t1[:], in0=PS[6][0:64, :], in1=Ct[:], op=ALU.mult), reads=[PS[6], Ct], writes=[t1])
        fw.op("dve", lambda e: e.tensor_tensor(out=t2[:], in0=PS[7][0:64, :], in1=St[:], op=ALU.mult), reads=[PS[7], St], writes=[t2])
        fw.op("dve", lambda e: e.tensor_tensor(out=sr[:], in0=t1[:], in1=t2[:], op=ALU.add), reads=[t1, t2], writes=[sr])
        fw.dma("sp", lambda e: e.dma_start(out=k.krT_d[:, t0:t0 + 512], in_=sr[:]), reads=[sr])
        for h in range(8):
            pq = PS[2 + (h % 2) * 3]; pr = PS[3 + (h % 2) * 3]; prs = PS[4 + (h % 2) * 3]
            for c in range(2):
                mm(fw, pq, pq[:], wq_s, wq_s[:, c, h * 192:h * 192 + 128], latn, latn[:, c, :], c == 0, c == 1)
            for c in range(2):
                mm(fw, pr, pr[0:64, :], wq_s, wq_s[:, c, h * 192 + 128:h * 192 + 192], latn, latn[:, c, :], c == 0, c == 1)
            for c in range(2):
                mm(fw, prs, prs[0:64, :], wq_s, wq_s[:, c, 1536 + h * 64:1536 + (h + 1) * 64], latn, latn[:, c, :], c == 0, c == 1)
            sq_ = stq[h % 2]; sr = str_[(h + 1) % 2]
            fw.op("act", lambda e, pq=pq, sq_=sq_: e.activation(out=sq_[:], in_=pq[:], func=AF.Copy, scale=MLA_SCALE), reads=[pq], writes=[sq_])
            fw.dma("sp", lambda e, sq_=sq_, h=h: e.dma_start(out=k.qT_d[h, 0:128, t0:t0 + 512], in_=sq_[:]), reads=[sq_])
            fw.op("dve", lambda e, pr=pr: e.scalar_tensor_tensor(out=t1[:], in0=pr[0:64, :], scalar=MLA_SCALE, in1=Ct[:], op0=ALU.mult, op1=ALU.mult), reads=[pr, Ct], writes=[t1])
            fw.op("dve", lambda e, prs=prs: e.scalar_tensor_tensor(out=t2[:], in0=prs[0:64, :], scalar=MLA_SCALE, in1=St[:], op0=ALU.mult, op1=ALU.mult), reads=[prs, St], writes=[t2])
            fw.op("dve", lambda e, sr=sr: e.tensor_tensor(out=sr[:], in0=t1[:], in1=t2[:], op=ALU.add), reads=[t1, t2], writes=[sr])
            fw.dma("sp", lambda e, sr=sr, h=h: e.dma_start(out=k.qT_d[h, 128:192, t0:t0 + 512], in_=sr[:]), reads=[sr])
        for h in range(8):
            pk = PS[2 + (h % 2)]
            for c in range(2):
                mm(fw, pk, pk[:], wkv_s, wkv_s[:, c, h * 128:(h + 1) * 128], latn, latn[:, 2 + c, :], c == 0, c == 1)
            sk = stq[h % 2]
            fw.op("act", lambda e, pk=pk, sk=sk: e.activation(out=sk[:], in_=pk[:], func=AF.Copy), reads=[pk], writes=[sk])
            fw.dma("sp", lambda e, sk=sk, h=h: e.dma_start(out=k.kT_d[h, :, t0:t0 + 512], in_=sk[:]), reads=[sk])
        for r in range(4):
            sv = stv[r % 2]
            for half in range(2):
                p = PS[4 + half]
                for c in range(2):
                    mm(fw, p, p[:], latn, latn[:, 2 + c, r * 128:(r + 1) * 128], wkv_s, wkv_s[:, c, 1024 + half * 512:1024 + (half + 1) * 512], c == 0, c == 1)
                fw.op("act", lambda e, p=p, sv=sv, half=half: e.activation(out=sv[:, half * 512:(half + 1) * 512], in_=p[:], func=AF.Copy), reads=[p], writes=[sv])
            fw.dma("sp", lambda e, sv=sv, r=r: e.dma_start(out=k.v_d[:, t0 + r * 128:t0 + (r + 1) * 128, :].rearrange("h t d -> t h d"),
                                                            in_=sv[:].rearrange("t (h d) -> t h d", h=8)), reads=[sv])
    fw.barrier()
    fw.release(mk0)
    if k.stop == "A":
        return
    krT = fw.sb("krT", [64, T], BF16)
    fw.dma("sp", lambda e: e.dma_start(out=krT[:], in_=k.krT_d), writes=[krT])
    kTs = [fw.sb(f"kTs{i}", [128, T], BF16) for i in range(2)]
    vs = [fw.sb(f"vs{i}", [128, 32, 132], BF16) for i in range(2)]
    for i in range(2):
        fw.op("dve", lambda e, i=i: e.memset(vs[i][:, :, 128:129], 1.0), writes=[vs[i]])
    qn = [fw.sb(f"qn{i}", [128, 512], BF16) for i in range(2)]
    qr = [fw.sb(f"qr{i}", [64, 512], BF16) for i in range(2)]
    pT = [fw.sb(f"pT{i}", [128, 512], BF16) for i in range(3)]
    rs = fw.sb("rs", [128, 4])
    on = [fw.sb(f"on{i}", [128, 128]) for i in range(2)]
    oT = [fw.sb(f"oT{i}", [128, 512], BF16) for i in range(2)]
    blk = 0
    for h in range(8):
        kb = kTs[h % 2]; vb = vs[h % 2]
        fw.dma("sp", lambda e: e.dma_start(out=kb[:], in_=k.kT_d[h]), writes=[kb])
        for q4 in range(4):
            fw.dma("sp", lambda e, q4=q4: e.dma_start(out=vb[:, q4 * 8:(q4 + 1) * 8, 0:128],
                                                      in_=k.v_d[h, q4 * 1024:(q4 + 1) * 1024, :].rearrange("(c p) d -> p c d", p=128)), writes=[vb])
        for jq in range(8):
            qnb = qn[jq % 2]; qrb = qr[jq % 2]
            fw.dma("sp", lambda e: e.dma_start(out=qnb[:], in_=k.qT_d[h, 0:128, jq * 512:(jq + 1) * 512]), writes=[qnb])
            fw.dma("sp", lambda e: e.dma_start(out=qrb[:], in_=k.qT_d[h, 128:192, jq * 512:(jq + 1) * 512]), writes=[qrb])
            nk = 4 * jq + 4

            def emit_qk(kc_):
                q0_ = max(0, kc_ - 4 * jq) * 128
                s__ = PS[(blk + kc_) % 3]
                mm(fw, s__, s__[:, q0_:512], kb, kb[:, kc_ * 128:(kc_ + 1) * 128], qnb, qnb[:, q0_:512], True, False)
                mm(fw, s__, s__[:, q0_:512], krT, krT[:, kc_ * 128:(kc_ + 1) * 128], qrb, qrb[:, q0_:512], False, True)

            emit_qk(0)
            for kc in range(nk):
                r = max(0, kc - 4 * jq)
                q0 = r * 128
                sp_ = PS[(blk + kc) % 3]; pb = pT[(blk + kc) % 3]
                if kc + 1 < nk:
                    emit_qk(kc + 1)
                fw.op("act", lambda e, sp_=sp_, pb=pb, q0=q0: e.activation(out=pb[:, q0:512], in_=sp_[:, q0:512], func=AF.Exp), reads=[sp_], writes=[pb])
                if kc >= 4 * jq:
                    fw.op("dve", lambda e, pb=pb, q0=q0: e.tensor_tensor(out=pb[:, q0:q0 + 128], in0=pb[:, q0:q0 + 128], in1=k.trib[:], op=ALU.mult),
                          reads=[pb, k.trib], writes=[pb])
                for s_ in range(r, 4):
                    acc = PS[3 + s_]
                    mm(fw, acc, acc[:, 0:129], pb, pb[:, s_ * 128:(s_ + 1) * 128], vb, vb[:, kc, 0:129], kc == 0, kc == 4 * jq + s_)
            blk += nk
            ob = oT[jq % 2]
            for s_ in range(4):
                acc = PS[3 + s_]; onb = on[s_ % 2]
                fw.op("dve", lambda e, acc=acc, s_=s_: e.reciprocal(out=rs[:, s_:s_ + 1], in_=acc[:, 128:129]), reads=[acc], writes=[rs])
                fw.op("act", lambda e, acc=acc, onb=onb, s_=s_: e.activation(out=onb[:], in_=acc[:, 0:128], func=AF.Copy, scale=rs[:, s_:s_ + 1]), reads=[acc, rs], writes=[onb])
                tr(fw, PS[7], PS[7][:, s_ * 128:(s_ + 1) * 128], onb, onb[:], C_, k.ID, first=(s_ == 0))
            fw.op("act", lambda e, ob=ob: e.activation(out=ob[:], in_=PS[7][:], func=AF.Copy), reads=[PS[7]], writes=[ob])
            fw.dma("sp", lambda e, ob=ob: e.dma_start(out=k.oT_d[h, :, jq * 512:(jq + 1) * 512], in_=ob[:]), reads=[ob])
    fw.barrier()
    fw.release(mk0)
    if k.stop == "B":
        return


def mixer_out_and_moe_router(k, L, w_out_d, n_kc, oT_src):
    fw = k.fw; PS = k.PS; C_ = k.C_; MOD = k.MOD
    mk0 = fw.mark()
    wo = fw.sb(f"wo_{L}", [128, n_kc, D], BF16)
    for c in range(n_kc):
        fw.dma("pool", lambda e, c=c: e.dma_start(out=wo[:, c, :], in_=w_out_d[c * 128:(c + 1) * 128, :]), writes=[wo])
    oTt = [fw.sb(f"oTt{L}_{i}", [128, n_kc, 512], BF16) for i in range(2)]
    ytmp = fw.sb(f"ytmp{L}", [128, D])
    for j in range(T // 512):
        ob = oTt[j % 2]
        fw.dma("sp", lambda e: e.dma_start(out=ob[:], in_=oT_src[:, :, j * 512:(j + 1) * 512].rearrange("h p t -> p h t")), writes=[ob])
        for r in range(4):
            t0 = j * 512 + r * 128
            xb = k.xt[r % 2]
            fw.dma("sp", lambda e: e.dma_start(out=xb[:], in_=k.xs[t0:t0 + 128, :]), writes=[xb])
            for half in range(2):
                p = PS[half]
                for c in range(n_kc):
                    mm(fw, p, p[:], ob, ob[:, c, r * 128:(r + 1) * 128], wo, wo[:, c, half * 512:(half + 1) * 512], c == 0, c == n_kc - 1)
                fw.op("dve", lambda e, p=p, half=half: e.tensor_tensor(out=ytmp[:, half * 512:(half + 1) * 512], in0=p[:], in1=MOD[:, 2, half * 512:(half + 1) * 512], op=ALU.mult),
                      reads=[p, MOD], writes=[ytmp])
            fw.op("dve", lambda e: e.tensor_tensor(out=xb[:], in0=xb[:], in1=ytmp[:], op=ALU.add), reads=[xb, ytmp], writes=[xb])
            fw.dma("sp", lambda e: e.dma_start(out=k.xs[t0:t0 + 128, :], in_=xb[:]), reads=[xb])
    fw.barrier()
    fw.release(mk0)


def moe_layer(k, L):
    fw = k.fw; PS = k.PS; C_ = k.C_; MOD = k.MOD; nc = k.nc
    mk0 = fw.mark()
    yacc = k.yacc
    h2T = fw.sb("h2T", [128, 8, T], BF16)
    G_all = fw.sb("G_all", [128, NT, 32])
    wr = fw.sb("wr", [128, 8, 32])
    fw.dma("sp", lambda e: e.dma_start(out=wr[:], in_=k.w_router[L].rearrange("(c p) n -> p c n", p=128)), writes=[wr])
    brb = fw.sb("brb", [128, 32])
    fw.dma("sp", lambda e: e.dma_start(out=brb[:], in_=k.b_router[L:L + 1, :].broadcast_to([128, 32])), writes=[brb])
    h2f = fw.sb("h2f", [128, 8, 128])
    lg = fw.sb("lg", [128, 32]); m8 = fw.sb("m8", [128, 8]); msk = fw.sb("msk", [128, 32]); ex = fw.sb("ex", [128, 32])
    sm = fw.sb("sm", [128, 4])
    MS = ""
    for i in range(NT):
        xb = k.xt[i % 2]; hb = k.ht[i % 2]; sb_ = k.st[i % 2]
        fw.dma("sp", lambda e: e.dma_start(out=xb[:], in_=k.xs[i * 128:(i + 1) * 128, :]), writes=[xb])
        if MS == "DL":
            continue
        k.norm_mod(xb, hb, sb_, 4, 3)
        if MS == "D0":
            continue
        for half, p in ((0, PS[0]), (1, PS[1])):
            for c in range(4):
                cc = half * 4 + c
                tr(fw, p, p[:, c * 128:(c + 1) * 128], hb, hb[:, cc * 128:(cc + 1) * 128], C_, k.ID, first=(c == 0))
            fw.op("act", lambda e, half=half, p=p: e.activation(out=h2f[:, half * 4:(half + 1) * 4, :], in_=p[:].rearrange("p (c t) -> p c t", c=4), func=AF.Copy),
                  reads=[p], writes=[h2f])
        fw.op("dve", lambda e: e.tensor_copy(out=h2T[:, :, i * 128:(i + 1) * 128], in_=h2f[:]), reads=[h2f], writes=[h2T])
        if MS == "D1":
            continue
        pl = PS[2]
        for c in range(8):
            mm(fw, pl, pl[:, 0:32], h2f, h2f[:, c, :], wr, wr[:, c, :], c == 0, c == 7)
        fw.op("dve", lambda e: e.tensor_tensor(out=lg[:], in0=pl[:, 0:32], in1=brb[:], op=ALU.add), reads=[pl, brb], writes=[lg])
        if MS == "D2":
            continue
        fw.op("dve", lambda e: e.max(out=m8[:], in_=lg[:]), reads=[lg], writes=[m8])
        fw.op("dve", lambda e: e.tensor_scalar(out=msk[:], in0=lg[:], scalar1=m8[:, 3:4], scalar2=None, op0=ALU.is_ge), reads=[lg, m8], writes=[msk])
        fw.op("dve", lambda e: e.tensor_scalar(out=sm[:, 0:1], in0=m8[:, 0:1], scalar1=-1.0, scalar2=None, op0=ALU.mult), reads=[m8], writes=[sm])
        fw.op("act", lambda e: e.activation(out=ex[:], in_=lg[:], func=AF.Exp, bias=sm[:, 0:1], scale=1.0), reads=[lg, sm], writes=[ex])
        fw.op("dve", lambda e: e.tensor_tensor(out=ex[:], in0=ex[:], in1=msk[:], op=ALU.mult), reads=[ex, msk], writes=[ex])
        fw.op("dve", lambda e: e.reduce_sum(out=sm[:, 1:2], in_=ex[:], axis=AX.X), reads=[ex], writes=[sm])
        fw.op("dve", lambda e: e.reciprocal(out=sm[:, 2:3], in_=sm[:, 1:2]), reads=[sm], writes=[sm])
        fw.op("dve", lambda e: e.tensor_scalar(out=G_all[:, i, :], in0=ex[:], scalar1=sm[:, 2:3], scalar2=None, op0=ALU.mult), reads=[ex, sm], writes=[G_all])
    fw.barrier()
    if MS:
        fw.release(mk0)
        return
    wgu = fw.sb("wgu", [128, 8, 2048], BF16)
    wdn = fw.sb("wdn", [128, 8, 1024], BF16)
    bgu = fw.sb("bgu", [128, 16])
    bdb = fw.sb("bdb", [128, 1024])
    hact = fw.sb("hact", [128, 8, 512], BF16)
    g_ = fw.sb("g_", [128, 512]); sg_ = fw.sb("sg_", [128, 512]); l_ = fw.sb("l_", [128, 512])
    yo = [fw.sb(f"yo{i}", [128, 1024]) for i in range(2)]
    for ex_i in range(32):
        for c in range(8):
            fw.dma("pool", lambda e, c=c: e.dma_start(out=wgu[:, c, :], in_=k.w_gu[L, ex_i, c * 128:(c + 1) * 128, :]), writes=[wgu])
        for c in range(8):
            fw.dma("pool", lambda e, c=c: e.dma_start(out=wdn[:, c, :], in_=k.w_dn[L, ex_i, c * 128:(c + 1) * 128, :]), writes=[wdn])
        with nc.allow_non_contiguous_dma(reason="small bias relayout"):
            fw.dma("sp", lambda e: e.dma_start(out=bgu[:], in_=k.b_gu[L, ex_i, :].rearrange("(c p) -> p c", p=128)), writes=[bgu])
        fw.dma("sp", lambda e: e.dma_start(out=bdb[:], in_=k.b_dn[L, ex_i:ex_i + 1, :].broadcast_to([128, 1024])), writes=[bdb])
        for jt in range(T // 512):
            for fc in range(8):
                pg = PS[(fc % 2) * 2]; pl2 = PS[(fc % 2) * 2 + 1]
                for c in range(8):
                    mm(fw, pg, pg[:], wgu, wgu[:, c, fc * 128:(fc + 1) * 128], h2T, h2T[:, c, jt * 512:(jt + 1) * 512], c == 0, c == 7)
                for c in range(8):
                    mm(fw, pl2, pl2[:], wgu, wgu[:, c, 1024 + fc * 128:1024 + (fc + 1) * 128], h2T, h2T[:, c, jt * 512:(jt + 1) * 512], c == 0, c == 7)
                fw.op("dve", lambda e, pg=pg, fc=fc: e.tensor_scalar(out=g_[:], in0=pg[:], scalar1=bgu[:, fc:fc + 1], scalar2=7.0, op0=ALU.add, op1=ALU.min), reads=[pg, bgu], writes=[g_])
                fw.op("act", lambda e: e.activation(out=sg_[:], in_=g_[:], func=AF.Sigmoid, scale=1.702), reads=[g_], writes=[sg_])
                fw.op("dve", lambda e, pl2=pl2, fc=fc: e.tensor_scalar(out=l_[:], in0=pl2[:], scalar1=bgu[:, 8 + fc:9 + fc], scalar2=7.0, op0=ALU.add, op1=ALU.min), reads=[pl2, bgu], writes=[l_])
                fw.op("dve", lambda e: e.tensor_scalar(out=l_[:], in0=l_[:], scalar1=-7.0, scalar2=1.0, op0=ALU.max, op1=ALU.add), reads=[l_], writes=[l_])
                fw.op("dve", lambda e: e.tensor_tensor(out=g_[:], in0=g_[:], in1=sg_[:], op=ALU.mult), reads=[g_, sg_], writes=[g_])
                fw.op("dve", lambda e, fc=fc: e.tensor_tensor(out=hact[:, fc, :], in0=g_[:], in1=l_[:], op=ALU.mult), reads=[g_, l_], writes=[hact])
            for r in range(4):
                ti = jt * 4 + r
                yb = yo[r % 2]
                for half in range(2):
                    p = PS[4 + half]
                    for fc in range(8):
                        mm(fw, p, p[:], hact, hact[:, fc, r * 128:(r + 1) * 128], wdn, wdn[:, fc, half * 512:(half + 1) * 512], fc == 0, fc == 7)
                    fw.op("dve", lambda e, p=p, half=half, yb=yb: e.tensor_tensor(out=yb[:, half * 512:(half + 1) * 512], in0=p[:], in1=bdb[:, half * 512:(half + 1) * 512], op=ALU.add),
                          reads=[p, bdb], writes=[yb])
                fw.op("dve", lambda e, yb=yb, ti=ti: e.tensor_scalar(out=yb[:], in0=yb[:], scalar1=G_all[:, ti, ex_i:ex_i + 1], scalar2=None, op0=ALU.mult), reads=[yb, G_all], writes=[yb])
                if ex_i == 0:
                    fw.dma("sp", lambda e, yb=yb, ti=ti: e.dma_start(out=yacc[ti * 128:(ti + 1) * 128, :], in_=yb[:]), reads=[yb])
                else:
                    fw.dma("pool", lambda e, yb=yb, ti=ti: e.dma_start(out=yacc[ti * 128:(ti + 1) * 128, :], in_=yb[:], accum_op=ALU.add), reads=[yb])
        fw.barrier()
    for i in range(NT):
        xb = k.xt[i % 2]; yb = yo[i % 2]
        fw.dma("sp", lambda e: e.dma_start(out=xb[:], in_=k.xs[i * 128:(i + 1) * 128, :]), writes=[xb])
        fw.dma("sp", lambda e: e.dma_start(out=yb[:], in_=yacc[i * 128:(i + 1) * 128, :]), writes=[yb])
        fw.op("dve", lambda e: e.tensor_tensor(out=yb[:], in0=yb[:], in1=MOD[:, 5, :], op=ALU.mult), reads=[yb, MOD], writes=[yb])
        fw.op("dve", lambda e: e.tensor_tensor(out=xb[:], in0=xb[:], in1=yb[:], op=ALU.add), reads=[xb, yb], writes=[xb])
        fw.dma("sp", lambda e: e.dma_start(out=k.xs[i * 128:(i + 1) * 128, :], in_=xb[:]), reads=[xb])
    fw.barrier()
    fw.release(mk0)


def final_norm(k):
    fw = k.fw
    gfb = fw.sb("gfb", [128, D])
    fw.dma("sp", lambda e: e.dma_start(out=gfb[:], in_=k.g_final[0:1, :].broadcast_to([128, D])), writes=[gfb])
    fw.op("dve", lambda e: e.tensor_copy(out=k.MOD[:, 1, :], in_=gfb[:]), reads=[gfb], writes=[k.MOD])
    for i in range(NT):
        xb = k.xt[i % 2]; hb = k.ht[i % 2]; sb_ = k.st[i % 2]
        fw.dma("sp", lambda e: e.dma_start(out=xb[:], in_=k.xs[i * 128:(i + 1) * 128, :]), writes=[xb])
        k.norm_mod(xb, hb, sb_, 1, None)
        fw.dma("sp", lambda e: e.dma_start(out=k.out_d[i * 128:(i + 1) * 128, :], in_=hb[:]), reads=[hb])


def ssd_layer(k, L):
    fw = k.fw; nc = k.nc; PS = k.PS; C_ = k.C_; MOD = k.MOD
    mk0 = fw.mark()
    w_in = k.ssm_w_in
    fw.store_q = None
    dt_tm = fw.sb("dt_tm", [128, NT, 32]); nac_tm = fw.sb("nac_tm", [128, NT, 32])
    mk_a = fw.mark()
    dtT = fw.sb("dtT", [32, T]); adtT = fw.sb("adtT", [32, T])
    mkA = fw.mark()
    hT = fw.sb("s_hT", [128, 8, 512], F32R)
    wsl = [fw.sb(f"wsl{i}", [128, 8, 512], F32R) for i in range(2)]
    wdt = fw.sb("wdt", [128, 8, 32], F32R)
    fw.dma("pool", lambda e: e.dma_start(out=wdt[:], in_=w_in[:, 6144:6176].rearrange("(c p) n -> p c n", p=128)), writes=[wdt])
    cw = fw.sb("cw", [128, 32, 4]); cb = fw.sb("cb", [128, 32])
    fw.dma("sp", lambda e: e.dma_start(out=cw[:], in_=k.ssm_cw), writes=[cw])
    fw.dma("sp", lambda e: e.dma_start(out=cb[:], in_=k.ssm_cb), writes=[cb])
    hv = fw.sb("hv", [32, 4])
    fw.dma("sp", lambda e: e.dma_start(out=hv[:, 0:2], in_=k.ssm_hv), writes=[hv])
    fw.op("act", lambda e: e.activation(out=hv[:, 2:3], in_=hv[:, 1:2], func=AF.Exp), reads=[hv], writes=[hv])
    fw.op("dve", lambda e: e.tensor_scalar(out=hv[:, 3:4], in0=hv[:, 2:3], scalar1=-1.0, scalar2=None, op0=ALU.mult), reads=[hv], writes=[hv])
    halo = fw.sb("halo", [128, 32, 4])
    fw.op("dve", lambda e: e.memset(halo[:], 0.0), writes=[halo])
    ub = [fw.sb(f"ub{i}", [128, 516]) for i in range(2)]
    acc = [fw.sb(f"cacc{i}", [128, 512]) for i in range(2)]
    xact = [fw.sb(f"xact{i}", [128, 512]) for i in range(2)]
    bcst = [fw.sb(f"bcst{i}", [128, 512], BF16) for i in range(2)]
    xtm = [fw.sb(f"xtm{i}", [128, 4, 512]) for i in range(2)]
    zst = [fw.sb(f"zst{i}", [128, 512]) for i in range(2)]
    et = fw.sb("et", [32, 512])
    nslab = 0
    for j in range(T // 512):
        t0 = j * 512
        for r in range(4):
            xb = k.xt[r % 2]; hb = k.ht[r % 2]; sb_ = k.st[r % 2]
            fw.dma("pool", lambda e: e.dma_start(out=xb[:], in_=k.xs[t0 + r * 128:t0 + (r + 1) * 128, :]), writes=[xb])
            k.norm_mod(xb, hb, sb_, 1, 0)
            k.transpose_to(hb, hT, lambda half, r=r: hT[:, half * 4:(half + 1) * 4, r * 128:(r + 1) * 128], PS[0], PS[1])
        pd = PS[2]
        for c in range(8):
            mm(fw, pd, pd[0:32, :], wdt, wdt[:, c, :], hT, hT[:, c, :], c == 0, c == 7)
        fw.op("act", lambda e: e.activation(out=et[:], in_=pd[0:32, :], func=AF.Exp, bias=hv[:, 0:1], scale=1.0), reads=[pd, hv], writes=[et])
        fw.op("act", lambda e: e.activation(out=dtT[:, t0:t0 + 512], in_=et[:], func=AF.Ln, bias=1.0, scale=1.0), reads=[et], writes=[dtT])
        fw.op("dve", lambda e: e.tensor_scalar(out=adtT[:, t0:t0 + 512], in0=dtT[:, t0:t0 + 512], scalar1=hv[:, 3:4], scalar2=None, op0=ALU.mult), reads=[dtT, hv], writes=[adtT])
        for sl in range(8):
            wb = wsl[nslab % 2]; nslab += 1
            for hf in range(2):
                fw.dma("pool", lambda e, hf=hf: e.dma_start(out=wb[:, hf * 4:(hf + 1) * 4, :],
                                                             in_=w_in[hf * 512:(hf + 1) * 512, 2048 + sl * 512:2048 + (sl + 1) * 512].rearrange("(c p) n -> p c n", p=128)), writes=[wb])
            for c4 in range(4):
                cc = sl * 4 + c4
                p = PS[3 + (cc % 2)]
                for c in range(8):
                    mm(fw, p, p[:], wb, wb[:, c, c4 * 128:(c4 + 1) * 128], hT, hT[:, c, :], c == 0, c == 7)
                u = ub[cc % 2]; a_ = acc[cc % 2]; xa = xact[cc % 2]
                fw.op("act", lambda e, p=p, u=u: e.activation(out=u[:, 3:515], in_=p[:], func=AF.Copy), reads=[p], writes=[u])
                fw.op("dve", lambda e, u=u, cc=cc: e.tensor_copy(out=u[:, 0:3], in_=halo[:, cc, 0:3]), reads=[halo], writes=[u])
                fw.op("dve", lambda e, u=u, cc=cc: e.tensor_copy(out=halo[:, cc, 0:3], in_=u[:, 512:515]), reads=[u], writes=[halo])
                fw.op("act", lambda e, u=u, a_=a_, cc=cc: e.activation(out=a_[:], in_=u[:, 3:515], func=AF.Identity, scale=cw[:, cc, 3:4], bias=cb[:, cc:cc + 1]),
                      reads=[u, cw, cb], writes=[a_])
                for kk in range(3):
                    fw.op("dve", lambda e, u=u, a_=a_, cc=cc, kk=kk: e.scalar_tensor_tensor(out=a_[:], in0=u[:, kk:kk + 512], scalar=cw[:, cc, kk:kk + 1], in1=a_[:],
                                                                                          op0=ALU.mult, op1=ALU.add), reads=[u, cw, a_], writes=[a_])
                if cc < 16:
                    fw.op("act", lambda e, a_=a_, xa=xa: e.activation(out=xa[:], in_=a_[:], func=AF.Silu), reads=[a_], writes=[xa])
                    xs_t = xtm[(cc // 4) % 2]
                    pt = PS[5 + (cc % 2)]
                    for r in range(4):
                        tr(fw, pt, pt[:, r * 128:(r + 1) * 128], xa, xa[:, r * 128:(r + 1) * 128], C_, k.ID, first=(r == 0))
                    fw.op("act", lambda e, pt=pt, xs_t=xs_t, c4=c4: e.activation(out=xs_t[:, :, c4 * 128:(c4 + 1) * 128], in_=pt[:].rearrange("p (r c) -> p r c", r=4), func=AF.Copy),
                          reads=[pt], writes=[xs_t])
                    if c4 == 3:
                        for r in range(4):
                            fw.dma("sp", lambda e, xs_t=xs_t, r=r: e.dma_start(out=k.sxs_d[t0 + r * 128:t0 + (r + 1) * 128, sl * 512:(sl + 1) * 512], in_=xs_t[:, r, :]), reads=[xs_t])
                else:
                    bs = bcst[cc % 2]
                    fw.op("act", lambda e, a_=a_, bs=bs: e.activation(out=bs[:], in_=a_[:], func=AF.Silu), reads=[a_], writes=[bs])
                    fw.dma("sp", lambda e, bs=bs, cc=cc: e.dma_start(out=k.sbc_d[cc - 16, :, t0:t0 + 512], in_=bs[:]), reads=[bs])
        for zs in range(4):
            wb = wsl[nslab % 2]; nslab += 1
            for hf in range(2):
                fw.dma("pool", lambda e, hf=hf: e.dma_start(out=wb[:, hf * 4:(hf + 1) * 4, :],
                                                             in_=w_in[hf * 512:(hf + 1) * 512, zs * 512:(zs + 1) * 512].rearrange("(c p) n -> p c n", p=128)), writes=[wb])
            for r in range(4):
                p = PS[3 + (r % 2)]
                for c in range(8):
                    mm(fw, p, p[:], hT, hT[:, c, r * 128:(r + 1) * 128], wb, wb[:, c, :], c == 0, c == 7)
                zt = zst[r % 2]
                fw.op("act", lambda e, p=p, zt=zt: e.activation(out=zt[:, 0:512], in_=p[:], func=AF.Silu), reads=[p], writes=[zt])
                fw.dma("sp", lambda e, zt=zt, r=r: e.dma_start(out=k.szs_d[t0 + r * 128:t0 + (r + 1) * 128, zs * 512:(zs + 1) * 512], in_=zt[:, 0:512]), reads=[zt])
    fw.barrier()
    fw.store_q = "pool"
    fw.release(mkA)
    ones32 = fw.sb("ones32", [32, T])
    fw.op("dve", lambda e: e.memset(ones32[:], 1.0), writes=[ones32])
    acT = fw.sb("acT", [32, T])
    fw.op("dve", lambda e: e.tensor_tensor_scan(out=acT[:], data0=ones32[:], data1=adtT[:], initial=0.0, op0=ALU.mult, op1=ALU.add), reads=[ones32, adtT], writes=[acT])
    fw.dma("sp", lambda e: e.dma_start(out=k.sac_d, in_=acT[:]), reads=[acT])
    fw.barrier()
    for c in range(NT):
        p = PS[c % 2]
        tr(fw, p, p[:, 0:32], dtT, dtT[:, c * 128:(c + 1) * 128], C_, C_[0:32, 0:32])
        tr(fw, p, p[:, 32:64], acT, acT[:, c * 128:(c + 1) * 128], C_, C_[0:32, 0:32], first=False)
        fw.op("act", lambda e, p=p, c=c: e.activation(out=dt_tm[:, c, :], in_=p[:, 0:32], func=AF.Copy), reads=[p], writes=[dt_tm])
        fw.op("act", lambda e, p=p, c=c: e.activation(out=nac_tm[:, c, :], in_=p[:, 32:64], func=AF.Copy, scale=-1.0), reads=[p], writes=[nac_tm])
    fw.barrier()
    fw.release(mk_a)
    mk1 = fw.mark()
    BT = [fw.sb(f"BT{i}", [128, T], BF16) for i in range(2)]
    CT = [fw.sb(f"CT{i}", [128, T], BF16) for i in range(2)]
    abc = [fw.sb(f"abc{i}", [128, T]) for i in range(4)]
    xsf = fw.sb("xsf", [128, NT, 64])
    V = [fw.sb(f"V{i}", [128, NT, 64], BF16) for i in range(4)]
    dec = [fw.sb(f"dec{i}", [128, 512]) for i in range(4)]
    pT = [fw.sb(f"spT{i}", [128, 512], BF16) for i in range(6)]
    ysb = [fw.sb(f"ysb{i}", [128, 4, 64]) for i in range(2)]
    blk = 0; nd = 0; npt = 0
    for g in range(8):
        Bb = BT[g % 2]; Cb = CT[g % 2]
        fw.dma("sp", lambda e: e.dma_start(out=Bb[:], in_=k.sbc_d[g]), writes=[Bb])
        fw.dma("sp", lambda e: e.dma_start(out=Cb[:], in_=k.sbc_d[8 + g]), writes=[Cb])
        for r in range(4):
            hh = g * 4 + r
            fw.dma("sp", lambda e, r=r, hh=hh: e.dma_start(out=abc[r][:], in_=k.sac_d[hh:hh + 1, :].broadcast_to([128, T])), writes=[abc[r]])
            for q4 in range(4):
                fw.dma("sp", lambda e, q4=q4, hh=hh: e.dma_start(out=xsf[:, q4 * 8:(q4 + 1) * 8, :],
                                                                 in_=k.sxs_d[q4 * 1024:(q4 + 1) * 1024, hh * 64:(hh + 1) * 64].rearrange("(c p) d -> p c d", p=128)), writes=[xsf])
            fw.op("dve", lambda e, r=r, hh=hh: e.tensor_tensor(out=V[r][:], in0=xsf[:], in1=dt_tm[:, :, hh:hh + 1].broadcast_to([128, NT, 64]), op=ALU.mult),
                  reads=[xsf, dt_tm], writes=[V[r]])
        for jq in range(8):
            nk = 4 * jq + 4

            def emit_st(kc_):
                q0_ = max(0, kc_ - 4 * jq) * 128
                s__ = PS[(blk + kc_) % 3]
                mm(fw, s__, s__[:, q0_:512], Bb, Bb[:, kc_ * 128:(kc_ + 1) * 128], Cb, Cb[:, jq * 512 + q0_:(jq + 1) * 512], True, True)

            emit_st(0)
            for kc in range(nk):
                r0 = max(0, kc - 4 * jq)
                q0 = r0 * 128
                diag = kc >= 4 * jq
                sp_ = PS[(blk + kc) % 3]
                if kc + 1 < nk:
                    emit_st(kc + 1)
                for r in range(4):
                    hh = g * 4 + r
                    d_ = dec[nd % 4]; nd += 1
                    pb = pT[npt % 6]; npt += 1
                    qa = jq * 512 + q0
                    if diag:
                        fw.op("dve", lambda e, d_=d_, r=r, qa=qa, hh=hh: e.tensor_scalar(out=d_[:, q0:q0 + 128], in0=abc[r][:, qa:qa + 128], scalar1=nac_tm[:, kc, hh:hh + 1], scalar2=0.0,
                                                                                       op0=ALU.add, op1=ALU.min), reads=[abc[r], nac_tm], writes=[d_])
                        fw.op("act", lambda e, d_=d_: e.activation(out=d_[:, q0:q0 + 128], in_=d_[:, q0:q0 + 128], func=AF.Exp), reads=[d_], writes=[d_])
                        fw.op("dve", lambda e, d_=d_: e.tensor_tensor(out=d_[:, q0:q0 + 128], in0=d_[:, q0:q0 + 128], in1=C_[:, 256:384], op=ALU.mult), reads=[d_, C_], writes=[d_])
                        if q0 + 128 < 512:
                            fw.op("act", lambda e, d_=d_, r=r, qa=qa, hh=hh: e.activation(out=d_[:, q0 + 128:512], in_=abc[r][:, qa + 128:(jq + 1) * 512], func=AF.Exp,
                                                                                     bias=nac_tm[:, kc, hh:hh + 1], scale=1.0), reads=[abc[r], nac_tm, d_], writes=[d_])
                    else:
                        fw.op("act", lambda e, d_=d_, r=r, qa=qa, hh=hh: e.activation(out=d_[:, q0:512], in_=abc[r][:, qa:(jq + 1) * 512], func=AF.Exp,
                                                                                 bias=nac_tm[:, kc, hh:hh + 1], scale=1.0), reads=[abc[r], nac_tm], writes=[d_])
                    fw.op("dve", lambda e, d_=d_, pb=pb, sp_=sp_: e.tensor_tensor(out=pb[:, q0:512], in0=sp_[:, q0:512], in1=d_[:, q0:512], op=ALU.mult), reads=[sp_, d_], writes=[pb])
                    ya = PS[3 + r]
                    for s_ in range(r0, 4):
                        fw.op("pe", lambda e, ya=ya, pb=pb, s_=s_, r=r: e.matmul(ya[:, s_ * 64:(s_ + 1) * 64], pb[:, s_ * 128:(s_ + 1) * 128], V[r][:, kc, :],
                                                                               start=(kc == 0), stop=(kc == 4 * jq + s_)),
                              reads=[pb, V[r]], writes=[ya], accumulate=True)
            blk += nk
            for r in range(4):
                hh = g * 4 + r
                ya = PS[3 + r]; yb = ysb[r % 2]
                fw.op("act", lambda e, ya=ya, yb=yb: e.activation(out=yb[:], in_=ya[:, 0:256].rearrange("p (s d) -> p s d", s=4), func=AF.Copy), reads=[ya], writes=[yb])
                fw.dma("sp", lambda e, yb=yb, hh=hh: e.dma_start(out=k.sy_d[jq * 512:(jq + 1) * 512, hh * 64:(hh + 1) * 64].rearrange("(s p) d -> p s d", p=128), in_=yb[:]), reads=[yb])
    fw.barrier()
    fw.release(mk1)
    dbc = fw.sb("dbc", [128, 2048]); gnb = fw.sb("gnb", [128, 2048])
    fw.dma("sp", lambda e: e.dma_start(out=dbc[:], in_=k.ssm_dexp[0:1, :].broadcast_to([128, 2048])), writes=[dbc])
    fw.dma("sp", lambda e: e.dma_start(out=gnb[:], in_=k.ssm_gn[0:1, :].broadcast_to([128, 2048])), writes=[gnb])
    yt = [fw.sb(f"yt{i}", [128, 2048]) for i in range(2)]
    xs2 = [fw.sb(f"xs2{i}", [128, 2048]) for i in range(2)]
    zz = [fw.sb(f"zz{i}", [128, 2048]) for i in range(2)]
    sqv = fw.sb("sqv", [128, 2048])
    gs = fw.sb("gs", [128, 16])
    ynT = [fw.sb(f"ynT{i}", [128, 16, 128], BF16) for i in range(2)]
    for i in range(NT):
        y_ = yt[i % 2]; x_ = xs2[i % 2]; z_ = zz[i % 2]
        fw.dma("sp", lambda e: e.dma_start(out=y_[:], in_=k.sy_d[i * 128:(i + 1) * 128, :]), writes=[y_])
        fw.dma("sp", lambda e: e.dma_start(out=x_[:], in_=k.sxs_d[i * 128:(i + 1) * 128, :]), writes=[x_])
        fw.dma("sp", lambda e: e.dma_start(out=z_[:], in_=k.szs_d[i * 128:(i + 1) * 128, :]), writes=[z_])
        fw.op("dve", lambda e: e.tensor_tensor(out=x_[:], in0=x_[:], in1=dbc[:], op=ALU.mult), reads=[x_, dbc], writes=[x_])
        fw.op("dve", lambda e: e.tensor_tensor(out=y_[:], in0=y_[:], in1=x_[:], op=ALU.add), reads=[y_, x_], writes=[y_])
        fw.op("dve", lambda e: e.tensor_tensor(out=y_[:], in0=y_[:], in1=z_[:], op=ALU.mult), reads=[y_, z_], writes=[y_])
        fw.op("act", lambda e: e.activation(out=sqv[:], in_=y_[:], func=AF.Square), reads=[y_], writes=[sqv])
        fw.op("dve", lambda e: e.tensor_reduce(out=gs[:, 0:8], in_=sqv[:].rearrange("p (g d) -> p g d", g=8), axis=AX.X, op=ALU.add), reads=[sqv], writes=[gs])
        fw.op("act", lambda e: e.activation(out=gs[:, 8:16], in_=gs[:, 0:8], func=AF.Sqrt, bias=k.epsc[:], scale=1.0 / 256), reads=[gs, k.epsc], writes=[gs])
        fw.op("dve", lambda e: e.reciprocal(out=gs[:, 8:16], in_=gs[:, 8:16]), reads=[gs], writes=[gs])
        for g in range(8):
            fw.op("dve", lambda e, g=g: e.scalar_tensor_tensor(out=y_[:, g * 256:(g + 1) * 256], in0=y_[:, g * 256:(g + 1) * 256], scalar=gs[:, 8 + g:9 + g],
                                                               in1=gnb[:, g * 256:(g + 1) * 256], op0=ALU.mult, op1=ALU.mult), reads=[y_, gs, gnb], writes=[y_])
        yT = ynT[i % 2]
        for q4 in range(4):
            p = PS[q4 % 2]
            for c in range(4):
                cc = q4 * 4 + c
                tr(fw, p, p[:, c * 128:(c + 1) * 128], y_, y_[:, cc * 128:(cc + 1) * 128], C_, k.ID, first=(c == 0))
            fw.op("act", lambda e, p=p, q4=q4: e.activation(out=yT[:, q4 * 4:(q4 + 1) * 4, :], in_=p[:].rearrange("p (c t) -> p c t", c=4), func=AF.Copy), reads=[p], writes=[yT])
        fw.dma("sp", lambda e: e.dma_start(out=k.synT_d[:, :, i * 128:(i + 1) * 128].rearrange("c p t -> p c t"), in_=yT[:]), reads=[yT])
    fw.barrier()
    fw.release(mk0)


MOE_B = 256
MOE_NB = 4 * T // MOE_B + 64


def moe_layer_sparse(k, L):
    fw = k.fw; PS = k.PS; C_ = k.C_; MOD = k.MOD; nc = k.nc
    NB = MOE_NB; B = MOE_B
    mk0 = fw.mark()
    S4_all = fw.sb("S4_all", [128, NT, 4], I32); G4_all = fw.sb("G4_all", [128, NT, 4])
    idx_w = fw.sb("idx_w", [128, 128], I32); idx_b = fw.sb("idx_b", [128, 128], I32)
    idx_g = [fw.sb(f"idx_g{i}", [128, 128], I32) for i in range(8)]
    mkD = fw.mark()
    G_all = fw.sb("G_all", [128, NT, 32]); M_all = fw.sb("M_all", [128, NT, 32]); R_all = fw.sb("R_all", [128, NT, 32])
    base = fw.sb("base", [128, 32])
    fw.op("dve", lambda e: e.memset(base[:], 0.0), writes=[base])
    wr = fw.sb("wr", [128, 8, 32])
    fw.dma("sp", lambda e: e.dma_start(out=wr[:], in_=k.w_router[L].rearrange("(c p) n -> p c n", p=128)), writes=[wr])
    brb = fw.sb("brb", [128, 32])
    fw.dma("sp", lambda e: e.dma_start(out=brb[:], in_=k.b_router[L:L + 1, :].broadcast_to([128, 32])), writes=[brb])
    h2f = fw.sb("h2f", [128, 8, 128])
    lg = fw.sb("lg", [128, 32]); m8 = fw.sb("m8", [128, 8]); ex = fw.sb("ex", [128, 32])
    sm = fw.sb("sm", [128, 4])
    for i in range(NT):
        xb = k.xt[i % 2]; hb = k.ht[i % 2]; sb_ = k.st[i % 2]
        fw.dma("sp", lambda e: e.dma_start(out=xb[:], in_=k.xs[i * 128:(i + 1) * 128, :]), writes=[xb])
        k.norm_mod(xb, hb, sb_, 4, 3)
        fw.dma("sp", lambda e: e.dma_start(out=k.h2_d[i * 128:(i + 1) * 128, :], in_=hb[:]), reads=[hb])
        for half, p in ((0, PS[0]), (1, PS[1])):
            for c in range(4):
                cc = half * 4 + c
                tr(fw, p, p[:, c * 128:(c + 1) * 128], hb, hb[:, cc * 128:(cc + 1) * 128], C_, k.ID, first=(c == 0))
            fw.op("act", lambda e, half=half, p=p: e.activation(out=h2f[:, half * 4:(half + 1) * 4, :], in_=p[:].rearrange("p (c t) -> p c t", c=4), func=AF.Copy),
                  reads=[p], writes=[h2f])
        pl = PS[2]
        for c in range(8):
            mm(fw, pl, pl[:, 0:32], h2f, h2f[:, c, :], wr, wr[:, c, :], c == 0, c == 7)
        msk = M_all
        fw.op("dve", lambda e: e.tensor_tensor(out=lg[:], in0=pl[:, 0:32], in1=brb[:], op=ALU.add), reads=[pl, brb], writes=[lg])
        fw.op("dve", lambda e: e.max(out=m8[:], in_=lg[:]), reads=[lg], writes=[m8])
        fw.op("dve", lambda e: e.tensor_scalar(out=M_all[:, i, :], in0=lg[:], scalar1=m8[:, 3:4], scalar2=None, op0=ALU.is_ge), reads=[lg, m8], writes=[M_all])
        fw.op("dve", lambda e: e.tensor_scalar(out=sm[:, 0:1], in0=m8[:, 0:1], scalar1=-1.0, scalar2=None, op0=ALU.mult), reads=[m8], writes=[sm])
        fw.op("act", lambda e: e.activation(out=ex[:], in_=lg[:], func=AF.Exp, bias=sm[:, 0:1], scale=1.0), reads=[lg, sm], writes=[ex])
        fw.op("dve", lambda e: e.tensor_tensor(out=ex[:], in0=ex[:], in1=M_all[:, i, :], op=ALU.mult), reads=[ex, M_all], writes=[ex])
        fw.op("dve", lambda e: e.reduce_sum(out=sm[:, 1:2], in_=ex[:], axis=AX.X), reads=[ex], writes=[sm])
        fw.op("dve", lambda e: e.reciprocal(out=sm[:, 2:3], in_=sm[:, 1:2]), reads=[sm], writes=[sm])
        fw.op("dve", lambda e: e.tensor_scalar(out=G_all[:, i, :], in0=ex[:], scalar1=sm[:, 2:3], scalar2=None, op0=ALU.mult), reads=[ex, sm], writes=[G_all])
        pr = PS[3]; pc = PS[4]
        mm(fw, pr, pr[:, 0:32], C_, C_[:, 384:512], M_all, M_all[:, i, :], True, True)
        mm(fw, pc, pc[:, 0:32], C_, C_[:, 128:256], M_all, M_all[:, i, :], True, True)
        fw.op("dve", lambda e: e.tensor_tensor(out=R_all[:, i, :], in0=pr[:, 0:32], in1=base[:], op=ALU.add), reads=[pr, base], writes=[R_all])
        fw.op("dve", lambda e: e.tensor_tensor(out=base[:], in0=pc[:, 0:32], in1=base[:], op=ALU.add), reads=[pc, base], writes=[base])
    ti = fw.sb("ti", [128, 32], I32); pf = fw.sb("pf", [128, 32]); pend = fw.sb("pend", [128, 32]); pst = fw.sb("pst", [128, 32])
    on32 = fw.sb("on32", [128, 32]); sl_ = fw.sb("sl_", [128, 32]); v_ = fw.sb("v_", [128, 32]); m8s = fw.sb("m8s", [128, 8]); jk = fw.sb("jk", [128, 32])
    fw.op("dve", lambda e: e.memset(on32[:], 1.0), writes=[on32])
    fw.op("dve", lambda e: e.tensor_scalar(out=pf[:], in0=base[:], scalar1=float(2 * B - 1), scalar2=None, op0=ALU.add), reads=[base], writes=[pf])
    fw.op("dve", lambda e: e.tensor_copy(out=ti[:], in_=pf[:]), reads=[pf], writes=[ti])
    sh = (2 * B).bit_length() - 1
    fw.op("dve", lambda e: e.tensor_scalar(out=ti[:], in0=ti[:], scalar1=sh, scalar2=sh, op0=ALU.logical_shift_right, op1=ALU.logical_shift_left), reads=[ti], writes=[ti])
    fw.op("dve", lambda e: e.tensor_copy(out=pf[:], in_=ti[:]), reads=[ti], writes=[pf])
    fw.op("dve", lambda e: e.tensor_tensor_scan(out=pend[:], data0=on32[:], data1=pf[:], initial=0.0, op0=ALU.mult, op1=ALU.add), reads=[on32, pf], writes=[pend])
    fw.op("dve", lambda e: e.tensor_tensor(out=pst[:], in0=pend[:], in1=pf[:], op=ALU.subtract), reads=[pend, pf], writes=[pst])
    for i in range(NT):
        fw.op("dve", lambda e: e.tensor_tensor(out=sl_[:], in0=R_all[:, i, :], in1=pst[:], op=ALU.add), reads=[R_all, pst], writes=[sl_])
        fw.op("dve", lambda e: e.scalar_tensor_tensor(out=v_[:], in0=sl_[:], scalar=1.0, in1=M_all[:, i, :], op0=ALU.add, op1=ALU.mult), reads=[sl_, M_all], writes=[v_])
        fw.op("dve", lambda e: e.max(out=m8s[:], in_=v_[:]), reads=[v_], writes=[m8s])
        fw.op("dve", lambda e: e.tensor_scalar(out=S4_all[:, i, :], in0=m8s[:, 0:4], scalar1=-1.0, scalar2=None, op0=ALU.add), reads=[m8s], writes=[S4_all])
        for kk in range(4):
            fw.op("dve", lambda e, kk=kk: e.scalar_tensor_tensor(out=jk[:], in0=v_[:], scalar=m8s[:, kk:kk + 1], in1=G_all[:, i, :], op0=ALU.is_equal, op1=ALU.mult,
                                                                accum_out=G4_all[:, i, kk:kk + 1]), reads=[v_, m8s, G_all], writes=[jk, G4_all])
    cmp_ = fw.sb("cmp_", [128, 32]); bec = fw.sb("bec", [128, 2]); ber = fw.sb("ber", [1, 136]); crow = fw.sb("crow", [1, 128]); vrow = fw.sb("vrow", [1, 128])
    fw.op("dve", lambda e: e.tensor_scalar(out=cmp_[:], in0=pend[:], scalar1=C_[:, 512:513], scalar2=None, op0=ALU.is_le), reads=[pend, C_], writes=[cmp_])
    fw.op("dve", lambda e: e.reduce_sum(out=bec[:, 0:1], in_=cmp_[:], axis=AX.X), reads=[cmp_], writes=[bec])
    fw.op("dve", lambda e: e.tensor_scalar(out=bec[:, 0:1], in0=bec[:, 0:1], scalar1=31.0, scalar2=None, op0=ALU.min), reads=[bec], writes=[bec])
    fw.op("dve", lambda e: e.tensor_scalar(out=bec[:, 1:2], in0=C_[:, 512:513], scalar1=pend[:, 31:32], scalar2=None, op0=ALU.is_lt), reads=[pend, C_], writes=[bec])
    pt_ = PS[5]
    tr(fw, pt_, pt_[0:1, 0:128], bec, bec[:, 0:1], C_, k.ID)
    tr(fw, pt_, pt_[0:1, 128:256], bec, bec[:, 1:2], C_, k.ID, first=False)
    fw.op("dve", lambda e: e.memset(ber[:], -1.0), writes=[ber])
    fw.op("act", lambda e: e.activation(out=ber[0:1, 4:132], in_=pt_[0:1, 0:128], func=AF.Copy), reads=[pt_], writes=[ber])
    fw.op("act", lambda e: e.activation(out=vrow[:], in_=pt_[0:1, 128:256], func=AF.Copy), reads=[pt_], writes=[vrow])
    fw.op("dve", lambda e: e.tensor_tensor(out=crow[:], in0=ber[0:1, 4:132], in1=ber[0:1, 0:128], op=ALU.not_equal), reads=[ber], writes=[crow])
    fw.op("dve", lambda e: e.tensor_tensor(out=crow[:], in0=crow[:], in1=vrow[:], op=ALU.mult), reads=[crow, vrow], writes=[crow])
    fw.op("dve", lambda e: e.tensor_tensor(out=crow[:], in0=crow[:], in1=C_[0:1, 640:768], op=ALU.mult), reads=[crow, C_], writes=[crow])
    BIG = 1.0e6
    pb1 = PS[6]; pb2 = PS[7]
    mm(fw, pb1, pb1[:, 0:128], C_, C_[0:1, 128:256], ber, ber[0:1, 4:132], True, True)
    mm(fw, pb2, pb2[:, 0:128], C_, C_[0:1, 128:256], crow, crow[0:1, :], True, True)
    cnd = fw.sb("cnd", [128, 128]); tw = fw.sb("tw", [128, 128]); tb = fw.sb("tb", [128, 128])
    fw.op("act", lambda e: e.activation(out=cnd[:], in_=pb2[:, 0:128], func=AF.Copy), reads=[pb2], writes=[cnd])
    fw.op("dve", lambda e: e.tensor_scalar(out=tb[:], in0=pb1[:, 0:128], scalar1=float(L * 32) - BIG, scalar2=None, op0=ALU.add), reads=[pb1], writes=[tb])
    fw.op("dve", lambda e: e.tensor_scalar(out=tw[:], in0=pb1[:, 0:128], scalar1=128.0, scalar2=float(L * 4096) - BIG, op0=ALU.mult, op1=ALU.add), reads=[pb1], writes=[tw])
    fw.op("dve", lambda e: e.tensor_scalar(out=tw[:], in0=tw[:], scalar1=C_[:, 513:514], scalar2=None, op0=ALU.add), reads=[tw, C_], writes=[tw])
    for t_, ix in ((tw, idx_w), (tb, idx_b)):
        fw.op("dve", lambda e, t_=t_: e.tensor_tensor(out=t_[:], in0=t_[:], in1=cnd[:], op=ALU.mult), reads=[t_, cnd], writes=[t_])
        fw.op("dve", lambda e, t_=t_, ix=ix: e.tensor_scalar(out=ix[:], in0=t_[:], scalar1=BIG, scalar2=None, op0=ALU.add), reads=[t_], writes=[ix])
    for c2 in range(8):
        fw.op("dve", lambda e, c2=c2: e.tensor_scalar(out=idx_g[c2][:], in0=tw[:], scalar1=8.0, scalar2=8 * BIG + c2, op0=ALU.mult, op1=ALU.add), reads=[tw], writes=[idx_g[c2]])
    fw.barrier()
    fw.release(mkD)
    MS = ""
    if MS == "E":
        dbt = k.xt[0]
        for j_, src in enumerate((idx_w, idx_b, idx_g[0], idx_g[1])):
            fw.op("dve", lambda e, j_=j_, src=src: e.tensor_copy(out=dbt[:, j_ * 128:(j_ + 1) * 128], in_=src[:]), reads=[src], writes=[dbt])
        fw.op("dve", lambda e: e.tensor_copy(out=dbt[:, 512:640], in_=S4_all[:].rearrange("p a b -> p (a b)")), reads=[S4_all], writes=[dbt])
        fw.op("dve", lambda e: e.tensor_copy(out=dbt[:, 640:768], in_=G4_all[:].rearrange("p a b -> p (a b)")), reads=[G4_all], writes=[dbt])
        fw.dma("sp", lambda e: e.dma_start(out=k.xs[0:128, :], in_=dbt[:]), reads=[dbt])
        fw.barrier()
        fw.release(mk0); return
    fw.store_q = None
    for i in range(NT):
        hb = k.ht[i % 2]
        fw.dma("sp", lambda e: e.dma_start(out=hb[:], in_=k.h2_d[i * 128:(i + 1) * 128, :]), writes=[hb])
        for kk in range(4):
            fw.dma("pool", lambda e, kk=kk: e.indirect_dma_start(out=k.xsort_d, out_offset=bass.IndirectOffsetOnAxis(ap=S4_all[:, i, kk:kk + 1], axis=0),
                                                                 in_=hb[:], in_offset=None), reads=[hb, S4_all])
    fw.barrier()
    if MS == "F":
        fw.release(mk0); return
    mkG = fw.mark()
    wgu = [fw.sb(f"wgu{i}", [128, 8, 2048], BF16) for i in range(2)]
    wdn = [fw.sb(f"wdn{i}", [128, 8, 1024], BF16) for i in range(2)]
    bgr = [fw.sb(f"bgr{i}", [128, 2048], BF16) for i in range(2)]
    bdb = [fw.sb(f"bdb{i}", [128, 1024]) for i in range(2)]
    onesb = fw.sb("onesb", [1, B], BF16)
    fw.op("dve", lambda e: e.memset(onesb[:], 1.0), writes=[onesb])
    xblk = [fw.sb(f"xblk{i}", [128, B // 128, 1024]) for i in range(1)]
    xT = [fw.sb(f"xT{i}", [128, 8, B], BF16) for i in range(2)]
    hact = fw.sb("hact", [128, 8, B], BF16)
    g_s = [fw.sb(f"g_{i}", [128, B]) for i in range(2)]; sg_s = [fw.sb(f"sg_{i}", [128, B]) for i in range(2)]; l_s = [fw.sb(f"l_{i}", [128, B]) for i in range(2)]
    yo = [fw.sb(f"yo{i}", [128, 1024]) for i in range(2)]
    bound_reg = nc.gpsimd.to_reg(2 * 32 * 1024 - 1)
    for bi in range(NB):
        pj = (bi // 2) % 2
        wg = wgu[pj]; wd = wdn[pj]; bg = bgr[pj]; bd = bdb[pj]
        NW = 2 * 32 * 128 - 1
        if bi % 2 == 0:
            for c in range(8):
                fw.dma("pool", lambda e, c=c: e.indirect_dma_start(out=wg[:, c, :], out_offset=None, in_=k.w_gu,
                                                                   in_offset=bass.IndirectOffsetOnAxis(ap=idx_g[c][:, bi:bi + 1], axis=0), bounds_check=bound_reg, oob_is_err=False),
                       reads=[idx_g[c]], writes=[wg])
            for c in range(8):
                fw.dma("pool", lambda e, c=c: e.indirect_dma_start(out=wd[:, c, :], out_offset=None, in_=k.w_dn,
                                                                   in_offset=bass.IndirectOffsetOnAxis(ap=idx_g[c][:, bi:bi + 1], axis=0), bounds_check=bound_reg, oob_is_err=False),
                       reads=[idx_g[c]], writes=[wd])
            fw.dma("pool", lambda e: e.indirect_dma_start(out=bg[:], out_offset=None, in_=k.b_gu, in_offset=bass.IndirectOffsetOnAxis(ap=idx_b[:, bi:bi + 1], axis=0),
                                                          bounds_check=bound_reg, oob_is_err=False), reads=[idx_b], writes=[bg])
            fw.dma("pool", lambda e: e.indirect_dma_start(out=bd[:], out_offset=None, in_=k.b_dn, in_offset=bass.IndirectOffsetOnAxis(ap=idx_b[:, bi:bi + 1], axis=0),
                                                          bounds_check=bound_reg, oob_is_err=False), reads=[idx_b], writes=[bd])
        xb = xblk[0]; xt_ = xT[bi % 2]

        def emit_xT(bj):
            xtj = xT[bj % 2]
            for rt in range(B // 128):
                for half in range(2):
                    p = PS[half]
                    for c in range(4):
                        cc = half * 4 + c
                        tr(fw, p, p[:, c * 128:(c + 1) * 128], xb, xb[:, rt, bass.ds(cc, 128, 8)], C_, k.ID, first=(c == 0))
                    fw.op("act", lambda e, half=half, p=p, rt=rt: e.activation(out=xtj[:, half * 4:(half + 1) * 4, rt * 128:(rt + 1) * 128], in_=p[:].rearrange("p (c t) -> p c t", c=4), func=AF.Copy),
                          reads=[p], writes=[xtj])
            if bj + 1 < NB:
                fw.dma("sp", lambda e: e.dma_start(out=xb[:], in_=k.xsort_d[(bj + 1) * B:(bj + 2) * B, :].rearrange("(r p) d -> p r d", p=128)), writes=[xb])

        if bi == 0:
            fw.dma("sp", lambda e: e.dma_start(out=xb[:], in_=k.xsort_d[0:B, :].rearrange("(r p) d -> p r d", p=128)), writes=[xb])
            emit_xT(0)
        for fc in range(8):
            pgl = PS[2 + (fc % 4)]
            g_ = g_s[fc % 2]; sg_ = sg_s[fc % 2]; l_ = l_s[fc % 2]
            for part, off in ((0, 0), (1, 1024)):
                o_ap = pgl[:, part * B:(part + 1) * B]
                for c in range(8):
                    mm(fw, pgl, o_ap, wg, wg[:, c, bass.ds(off + fc, 128, 8)], xt_, xt_[:, c, :], c == 0, False)
                mm(fw, pgl, o_ap, bg, bg[0:1, bass.ds(off + fc, 128, 8)], onesb, onesb[0:1, :], False, True)
            fw.op("dve", lambda e, pgl=pgl, g_=g_: e.tensor_scalar(out=g_[:], in0=pgl[:, 0:B], scalar1=7.0, scalar2=None, op0=ALU.min), reads=[pgl], writes=[g_])
            fw.op("act", lambda e: e.activation(out=sg_[:], in_=g_[:], func=AF.Sigmoid, scale=1.702), reads=[g_], writes=[sg_])
            fw.op("dve", lambda e, pgl=pgl: e.tensor_scalar(out=l_[:], in0=pgl[:, B:2 * B], scalar1=7.0, scalar2=-7.0, op0=ALU.min, op1=ALU.max), reads=[pgl], writes=[l_])
            fw.op("dve", lambda e: e.tensor_tensor(out=g_[:], in0=g_[:], in1=sg_[:], op=ALU.mult), reads=[g_, sg_], writes=[g_])
            fw.op("dve", lambda e, fc=fc: e.scalar_tensor_tensor(out=hact[:, fc, :], in0=l_[:], scalar=1.0, in1=g_[:], op0=ALU.add, op1=ALU.mult), reads=[g_, l_], writes=[hact])
        if bi + 1 < NB:
            emit_xT(bi + 1)
        for rt in range(B // 128):
            yb = yo[rt % 2]
            for half in range(2):
                p = PS[6 + half]
                for fc in range(8):
                    mm(fw, p, p[:], hact, hact[:, fc, rt * 128:(rt + 1) * 128], wd, wd[:, fc, half * 512:(half + 1) * 512], fc == 0, fc == 7)
                fw.op("dve", lambda e, p=p, half=half, yb=yb: e.tensor_tensor(out=yb[:, half * 512:(half + 1) * 512], in0=p[:], in1=bd[:, half * 512:(half + 1) * 512], op=ALU.add),
                      reads=[p, bd], writes=[yb])
            fw.dma("sp", lambda e, yb=yb, rt=rt: e.dma_start(out=k.ysort_d[bi * B + rt * 128:bi * B + (rt + 1) * 128, :], in_=yb[:]), reads=[yb])
    fw.barrier()
    fw.release(mkG)
    if MS == "G":
        fw.release(mk0); return
    fw.store_q = "act"
    yk = [[fw.sb(f"yk{i}_{kk}", [128, 1024]) for kk in range(4)] for i in range(2)]
    for i in range(NT):
        xb = k.xt[i % 2]; ys_ = yk[i % 2]
        fw.dma("sp", lambda e: e.dma_start(out=xb[:], in_=k.xs[i * 128:(i + 1) * 128, :]), writes=[xb])
        for kk in range(4):
            fw.dma("pool", lambda e, kk=kk: e.indirect_dma_start(out=ys_[kk][:], out_offset=None, in_=k.ysort_d,
                                                                 in_offset=bass.IndirectOffsetOnAxis(ap=S4_all[:, i, kk:kk + 1], axis=0)), reads=[S4_all], writes=[ys_[kk]])
        a0 = ys_[0]
        fw.op("dve", lambda e: e.tensor_scalar(out=a0[:], in0=a0[:], scalar1=G4_all[:, i, 0:1], scalar2=None, op0=ALU.mult), reads=[a0, G4_all], writes=[a0])
        for kk in range(1, 4):
            fw.op("dve", lambda e, kk=kk: e.scalar_tensor_tensor(out=a0[:], in0=ys_[kk][:], scalar=G4_all[:, i, kk:kk + 1], in1=a0[:], op0=ALU.mult, op1=ALU.add),
                  reads=[ys_[kk], G4_all, a0], writes=[a0])
        fw.op("dve", lambda e: e.tensor_tensor(out=a0[:], in0=a0[:], in1=MOD[:, 5, :], op=ALU.mult), reads=[a0, MOD], writes=[a0])
        fw.op("dve", lambda e: e.tensor_tensor(out=xb[:], in0=xb[:], in1=a0[:], op=ALU.add), reads=[xb, a0], writes=[xb])
        fw.dma("sp", lambda e: e.dma_start(out=k.xs[i * 128:(i + 1) * 128, :], in_=xb[:]), reads=[xb])
    fw.barrier()
    fw.store_q = "pool"
    fw.release(mk0)


def host_consts():
    cst = np.zeros((128, 1024), np.float32)
    cst[:, 0:128] = np.eye(128, dtype=np.float32)
    cst[:, 128:256] = 1.0
    i = np.arange(128)
    cst[:, 256:384] = (i[None, :] >= i[:, None]).astype(np.float32)
    cst[:, 384:512] = (i[:, None] < i[None, :]).astype(np.float32)
    cst[:, 512] = i * 256.0
    cst[:, 513] = i
    cst[:, 640:768] = (i % 2 == 0).astype(np.float32)[None, :]
    inv = (1.0 / (10000.0 ** (np.arange(0, 64, 2, dtype=np.float32) / 64))).astype(np.float32)
    invf = np.zeros((64, 2), np.float32)
    invf[:32, 0] = inv; invf[32:, 0] = inv
    invf[:32, 1] = -1.0; invf[32:, 1] = 1.0
    return cst, invf


def prep_core(inp, b):
    f = np.ascontiguousarray
    cst, invf = host_consts()
    m = {}
    m["x"] = f(inp["x"][b])
    m["c"] = f(inp["c"][b].reshape(8, 128).T)
    m["pos"] = f(inp["positions"][b].reshape(1, T).astype(np.int32))
    m["w_mod"] = inp["w_mod"]; m["b_mod"] = inp["b_mod"]
    m["g_mix"] = inp["g_mix_norm"]; m["g_ffn"] = inp["g_ffn_norm"]
    w_in = inp["mla_w_in"][0]
    kr = w_in[:, 512:576]
    m["mla_w_in"] = f(np.concatenate([w_in, kr[:, 32:], kr[:, :32]], axis=1))
    m["mla_gq"] = f(inp["mla_g_q"][0].reshape(2, 128).T)
    m["mla_gkv"] = f(inp["mla_g_kv"][0].reshape(2, 128).T)
    wq = inp["mla_w_q_up"][0].reshape(256, 8, 192)
    sw = np.concatenate([wq[:, :, 160:192], wq[:, :, 128:160]], axis=2)
    m["mla_wq"] = f(np.concatenate([wq.reshape(256, 1536), sw.reshape(256, 512)], axis=1))
    wkv = inp["mla_w_kv_up"][0].reshape(256, 8, 256)
    m["mla_wkv"] = f(np.concatenate([wkv[:, :, :128].reshape(256, 1024), wkv[:, :, 128:].reshape(256, 1024)], axis=1))
    m["mla_wo"] = inp["mla_w_out"][0]
    m["w_router"] = inp["moe_w_router"]; m["b_router"] = inp["moe_b_router"]
    m["w_gu"] = inp["moe_w_gate_up"].reshape(2 * 32 * 1024, 2048); m["b_gu"] = inp["moe_b_gate_up"].reshape(64, 2048)
    m["w_dn"] = inp["moe_w_down"].reshape(2 * 32 * 1024, 1024); m["b_dn"] = inp["moe_b_down"].reshape(64, 1024)
    m["g_final"] = f(inp["g_final"].reshape(1, D))
    m["ssm_w_in"] = inp["ssm_w_in"][0]
    m["ssm_cw"] = f(inp["ssm_conv_w"][0].reshape(4, 32, 128).transpose(2, 1, 0))
    m["ssm_cb"] = f(inp["ssm_conv_b"][0].reshape(32, 128).T)
    m["ssm_hv"] = f(np.stack([inp["ssm_dt_bias"][0], inp["ssm_a_log"][0]], axis=1))
    m["ssm_dexp"] = f(np.repeat(inp["ssm_d"][0], 64).reshape(1, 2048))
    m["ssm_gn"] = f(inp["ssm_g_norm"][0].reshape(1, 2048))
    m["ssm_wo"] = inp["ssm_w_out"][0]
    m["cst"] = cst; m["invf"] = invf
    return m

_CACHE = {}


def _program():
    if "k" not in _CACHE:
        k = build()
        k.stop = "all"
        k.compute_mod(0)
        mla_layer(k, 0, True)
        mixer_out_and_moe_router(k, 0, k.mla_wo, 8, k.oT_d)
        moe_layer_sparse(k, 0)
        k.compute_mod(1)
        ssd_layer(k, 1)
        mixer_out_and_moe_router(k, 1, k.ssm_wo, 16, k.synT_d)
        moe_layer_sparse(k, 1)
        final_norm(k)
        k.fw.finish()
        k.fw.close()
        _CACHE["k"] = k
    return _CACHE["k"]


def kernel(**inputs):
    inp = {kk: np.asarray(v) for kk, v in inputs.items()}
    k = _program()
    in_maps = [prep_core(inp, b) for b in range(8)]
    res = run_bass_kernel_spmd(k.nc, in_maps, core_ids=list(range(8)))
    return np.stack([np.asarray(r["out"]) for r in res.results], axis=0).astype(np.float32)
```

```python
import numpy as np
import concourse.bass as bass
import concourse.mybir as mybir
from concourse.bass_utils import run_bass_kernel_spmd

F32 = mybir.dt.float32
F32R = mybir.dt.float32r
BF16 = mybir.dt.bfloat16
I32 = mybir.dt.int32
U32 = mybir.dt.uint32
ALU = mybir.AluOpType
AF = mybir.ActivationFunctionType
AX = mybir.AxisListType

EPOCH = 20000


class Buf:
    __slots__ = ("t", "name", "lastw", "reads", "wfill", "dsem", "dcount", "is_dram")

    def __init__(self, t, name, is_dram=False):
        self.t = t
        self.name = name
        self.lastw = None
        self.reads = []
        self.wfill = []
        self.is_dram = is_dram

    def __getitem__(self, idx):
        return self.t[idx]


class FW:
    def __init__(self, nc):
        self.nc = nc
        self.eng = {"pe": nc.tensor, "dve": nc.vector, "act": nc.scalar,
                    "pool": nc.gpsimd, "sp": nc.sync}
        self.sem = {}
        self.cnt = {}
        self.nsem = 0
        self.waited = {e: {} for e in self.eng}
        self.semobjs = {}
        self.ctx = []
        self.bctx = []
        self.last = {e: None for e in self.eng}
        self.pend = {e: [] for e in self.eng}
        self.store_q = "pool"
        self.dma_toks = []
        self.dma_pool = []
        self.dma_free = []
        for e in ("pe", "dve", "act", "pool"):
            self._new_epoch(e)

    def _mksem(self, name):
        cm = self.nc.semaphore(name)
        s = cm.__enter__()
        self.ctx.append(cm)
        self.nsem += 1
        self.semobjs[id(s)] = s
        return s

    def _new_epoch(self, e):
        self.sem[e] = self._mksem(f"s_{e}_{self.nsem}")
        self.cnt[e] = 0

    def sb(self, name, shape, dt=F32):
        self.uid = getattr(self, "uid", 0) + 1
        cm = self.nc.sbuf_tensor(f"{name}_u{self.uid}", shape, dt)
        t = cm.__enter__()
        self.bctx.append(cm)
        return Buf(t, name)

    def ps(self, name, shape, dt=F32):
        cm = self.nc.psum_tensor(name, shape, dt)
        t = cm.__enter__()
        self.ctx.append(cm)
        return Buf(t, name)

    def dram(self, name, shape, dt=F32, kind="Internal"):
        t = self.nc.dram_tensor(name, shape, dt, kind=kind)
        return Buf(t.ap(), name, is_dram=True)

    def _wait(self, e, tok):
        if tok is None:
            return
        sem, val, _ = tok
        w = self.waited[e]
        if w.get(id(sem), 0) >= val:
            return
        w[id(sem)] = val
        self.eng[e].wait_ge(sem, val)

    def _deps(self, e, reads, writes, accumulate=False, is_dma=False):
        for tok in self.pend[e]:
            self._wait(e, tok)
        self.pend[e] = []
        for b in reads:
            if b is None:
                continue
            if b.lastw is not None:
                self._wait(e, b.lastw)
            for tok in b.wfill:
                self._wait(e, tok)
        for b in writes:
            if b is None:
                continue
            fill = is_dma and b.lastw is not None and b.lastw[2] == "dmaq" and not b.reads
            if b.lastw is not None and not (accumulate and b.lastw[2] == e) and not fill:
                if b.lastw[2] != e or b.lastw[2] in ("sp", "poolq", "actq"):
                    self._wait(e, b.lastw)
                    for tok in b.wfill:
                        self._wait(e, tok)
            for tok in b.reads:
                if tok[2] != e:
                    self._wait(e, tok)

    def _commit(self, tok, reads, writes):
        for b in reads:
            if b is not None:
                b.reads.append(tok)
        for b in writes:
            if b is not None:
                if tok[2] == "dmaq" and b.lastw is not None and b.lastw[2] == "dmaq" and not b.reads:
                    b.wfill.append(b.lastw)
                else:
                    b.wfill = []
                b.lastw = tok
                b.reads = []

    def op(self, e, fn, reads=(), writes=(), accumulate=False):
        self._deps(e, reads, writes, accumulate)
        if self.cnt[e] >= EPOCH:
            self._new_epoch(e)
        inst = fn(self.eng[e])
        self.cnt[e] += 1
        inst.then_inc(self.sem[e], 1)
        tok = (self.sem[e], self.cnt[e], e)
        self.last[e] = tok
        self._commit(tok, reads, writes)
        return tok

    def _dma_sem(self):
        if self.dma_free:
            return self.dma_free.pop()
        s = [self._mksem(f"s_dma_{self.nsem}"), 0]
        self.dma_pool.append(s)
        return s

    def dma(self, q, fn, reads=(), writes=(), semslot=None):
        if q == "sp" and not [w for w in writes if w is not None] and getattr(self, "store_q", None):
            q = self.store_q
        self._deps(q, reads, writes, is_dma=True)
        if semslot is None:
            semslot = self._rot_sem(q)
        if semslot[1] > 0:
            self._wait(q, (semslot[0], semslot[1], "dmaq"))
        inst = fn(self.eng[q])
        semslot[1] += 16
        inst.then_inc(semslot[0], 16)
        tok = (semslot[0], semslot[1], "dmaq")
        self._commit(tok, reads, writes)
        self.dma_toks.append(tok)
        if len(self.dma_toks) > 64:
            self.dma_toks = self.dma_toks[-64:]
        return tok

    NROT = 16

    def _rot_sem(self, q="sp"):
        if not hasattr(self, "_rotq"):
            self._rotq = {}
        if q not in self._rotq:
            self._rotq[q] = {"sems": [[self._mksem(f"s_rot_{q}_{i}"), 0] for i in range(self.NROT)], "i": 0}
        pool = self._rotq[q]
        i = pool["i"]
        pool["i"] = (i + 1) % self.NROT
        return _RotSlot(pool["sems"], i)

    def barrier(self):
        toks = [t for t in self.last.values() if t is not None]
        toks += self.dma_toks
        for pool in getattr(self, "_rotq", {}).values():
            for s in pool["sems"]:
                if s[1] > 0:
                    toks.append((s[0], s[1], "dmaq"))
        for e in self.eng:
            self.pend[e] = list(toks)
        self.dma_toks = []

    def finish(self):
        self.barrier()
        for e in self.eng:
            for tok in self.pend[e]:
                self._wait(e, tok)
            self.pend[e] = []

    def mark(self):
        return len(self.bctx)

    def release(self, m):
        while len(self.bctx) > m:
            self.bctx.pop().__exit__(None, None, None)

    def close(self):
        self.release(0)
        for cm in reversed(self.ctx):
            cm.__exit__(None, None, None)
        self.ctx = []


class _RotSlot(list):
    def __init__(self, sems, i):
        super().__init__(sems[i])
        self.sems = sems
        self.i = i

    def __setitem__(self, k, v):
        super().__setitem__(k, v)
        self.sems[self.i][k] = v


T = 4096
D = 1024
NT = T // 128
EPS = 1e-6
MLA_SCALE = 192 ** -0.5
PI = float(np.pi)
TWO_PI = float(2 * np.pi)


class K:
    pass


def mm(fw, ob, o_ap, lb, l_ap, rb, r_ap, start, stop):
    return fw.op("pe", lambda e: e.matmul(o_ap, l_ap, r_ap, start=start, stop=stop),
                 reads=[lb, rb], writes=[ob], accumulate=not start)


def tr(fw, ob, o_ap, ib, i_ap, ident, id_ap, first=True):
    return fw.op("pe", lambda e: e.transpose(o_ap, i_ap, id_ap), reads=[ib, ident], writes=[ob],
                 accumulate=not first)


def build(stop="all", n_layers=2, debug=False):
    nc = bass.Bass("TRN2", target_bir_lowering=False)
    fw = FW(nc)
    k = K()

    def din(name, shape, dt=F32):
        return nc.dram_tensor(name, shape, dt, kind="ExternalInput").ap()

    x_in = din("x", [T, D])
    c_in = din("c", [128, 8])
    pos_in = din("pos", [1, T], I32)
    w_mod = din("w_mod", [2, D, 6 * D])
    b_mod = din("b_mod", [2, 6 * D])
    g_mix = din("g_mix", [2, D])
    g_ffn = din("g_ffn", [2, D])
    mla_w_in = din("mla_w_in", [D, 640])
    mla_gq = din("mla_gq", [128, 2])
    mla_gkv = din("mla_gkv", [128, 2])
    mla_wq = din("mla_wq", [256, 2048])
    mla_wkv = din("mla_wkv", [256, 2048])
    mla_wo = din("mla_wo", [D, D])
    w_router = din("w_router", [2, D, 32])
    b_router = din("b_router", [2, 32])
    w_gu = din("w_gu", [2 * 32 * 1024, 2 * D])
    b_gu = din("b_gu", [64, 2 * D])
    w_dn = din("w_dn", [2 * 32 * 1024, D])
    b_dn = din("b_dn", [64, D])
    g_final = din("g_final", [1, D])
    ssm_w_in = din("ssm_w_in", [D, 6176])
    ssm_cw = din("ssm_cw", [128, 32, 4])
    ssm_cb = din("ssm_cb", [128, 32])
    ssm_hv = din("ssm_hv", [32, 2])
    ssm_dexp = din("ssm_dexp", [1, 2048])
    ssm_gn = din("ssm_gn", [1, 2048])
    ssm_wo = din("ssm_wo", [2048, D])
    cst = din("cst", [128, 1024])
    invf = din("invf", [64, 2])
    out_d = nc.dram_tensor("out", [T, D], F32, kind="ExternalOutput").ap()
    dbg_d = nc.dram_tensor("dbg", [T, D], F32, kind="ExternalOutput").ap() if debug else None

    def dscr(name, shape, dt=F32):
        return nc.dram_tensor(name, shape, dt, kind="Internal").ap()

    xs = dscr("xs", [T, D])
    qT_d = dscr("qT_d", [8, 192, T], BF16)
    kT_d = dscr("kT_d", [8, 128, T], BF16)
    krT_d = dscr("krT_d", [64, T], BF16)
    v_d = dscr("v_d", [8, T, 128], BF16)
    oT_d = dscr("oT_d", [8, 128, T], BF16)
    yacc = dscr("yacc", [T, D])
    h2_d = dscr("h2_d", [T, D])
    xsort_d = dscr("xsort_d", [4 * T + 64 * 256, D])
    ysort_d = dscr("ysort_d", [4 * T + 64 * 256, D])
    sxs_d = dscr("sxs_d", [T, 2048])
    szs_d = dscr("szs_d", [T, 2048])
    sy_d = dscr("sy_d", [T, 2048])
    sbc_d = dscr("sbc_d", [16, 128, T], BF16)
    sac_d = dscr("sac_d", [32, T])
    synT_d = dscr("synT_d", [16, 128, T], BF16)

    C_ = fw.sb("cst_s", [128, 1024])
    fw.dma("sp", lambda e: e.dma_start(out=C_[:], in_=cst), writes=[C_])
    ident = C_
    ID = C_[:, 0:128]
    ONES = C_[:, 128:256]
    TRI = C_[:, 256:384]
    SL = C_[:, 384:512]
    onesr = fw.sb("onesr", [128, 128], F32R)
    fw.op("act", lambda e: e.activation(out=onesr[:], in_=C_[:, 128:256], func=AF.Copy), reads=[C_], writes=[onesr])
    trib = fw.sb("trib", [128, 128], BF16)
    fw.op("act", lambda e: e.activation(out=trib[:], in_=C_[:, 256:384], func=AF.Copy), reads=[C_], writes=[trib])
    epsc = fw.sb("epsc", [128, 1])
    fw.op("dve", lambda e: e.memset(epsc[:], EPS), writes=[epsc])
    PS = [fw.ps(f"ps{i}", [128, 512]) for i in range(8)]

    cond = fw.sb("cond", [128, 8])
    fw.dma("sp", lambda e: e.dma_start(out=cond[:], in_=c_in), writes=[cond])
    sg = fw.sb("sg", [128, 8])
    fw.op("act", lambda e: e.activation(out=sg[:], in_=cond[:], func=AF.Sigmoid), reads=[cond], writes=[sg])
    fw.op("dve", lambda e: e.tensor_tensor(out=cond[:], in0=cond[:], in1=sg[:], op=ALU.mult), reads=[cond, sg], writes=[cond])
    condb = fw.sb("condb", [128, 8, 128])
    for c in range(8):
        fw.op("dve", lambda e, c=c: e.tensor_scalar(out=condb[:, c, :], in0=C_[:, 128:256], scalar1=cond[:, c:c + 1],
                                                    scalar2=None, op0=ALU.mult), reads=[C_, cond], writes=[condb])
    MOD = fw.sb("MOD", [128, 6, D])

    def compute_mod(L):
        mkm = fw.mark()
        wmt = [fw.sb(f"wmt{i}", [128, 8, 512]) for i in range(2)]
        bmt = [fw.sb(f"bmt{i}", [128, 512]) for i in range(2)]
        gbc = fw.sb("gbc", [128, D])
        for n in range(12):
            wt = wmt[n % 2]
            bt = bmt[n % 2]
            fw.dma("sp", lambda e: e.dma_start(out=wt[:], in_=w_mod[L, :, n * 512:(n + 1) * 512].rearrange("(c p) n -> p c n", p=128)), writes=[wt])
            fw.dma("sp", lambda e: e.dma_start(out=bt[:], in_=b_mod[L:L + 1, n * 512:(n + 1) * 512].broadcast_to([128, 512])), writes=[bt])
            p = PS[n % 2]
            for c in range(8):
                mm(fw, p, p[:], condb, condb[:, c, :], wt, wt[:, c, :], c == 0, c == 7)
            fw.op("dve", lambda e: e.tensor_tensor(out=MOD[:, n // 2, (n % 2) * 512:(n % 2 + 1) * 512], in0=p[:], in1=bt[:], op=ALU.add),
                  reads=[p, bt], writes=[MOD])
        for (slot, g) in ((1, g_mix), (4, g_ffn)):
            fw.dma("sp", lambda e: e.dma_start(out=gbc[:], in_=g[L:L + 1, :].broadcast_to([128, D])), writes=[gbc])
            fw.op("dve", lambda e: e.scalar_tensor_tensor(out=MOD[:, slot, :], in0=MOD[:, slot, :], scalar=1.0, in1=gbc[:],
                                                          op0=ALU.add, op1=ALU.mult), reads=[MOD, gbc], writes=[MOD])
        fw.barrier()
        fw.release(mkm)

    xt = [fw.sb(f"xt{i}", [128, D]) for i in range(2)]
    ht = [fw.sb(f"ht{i}", [128, D]) for i in range(2)]
    junk = fw.sb("junk", [128, D])
    st = [fw.sb(f"st{i}", [128, 4]) for i in range(2)]

    def norm_mod(xb, hb, sb_, a_slot, s_slot):
        fw.op("act", lambda e: e.activation(out=junk[:], in_=xb[:], func=AF.Square, accum_out=sb_[:, 0:1]), reads=[xb], writes=[junk, sb_])
        fw.op("act", lambda e: e.activation(out=sb_[:, 1:2], in_=sb_[:, 0:1], func=AF.Sqrt, bias=epsc[:], scale=1.0 / D), reads=[sb_, epsc], writes=[sb_])
        fw.op("dve", lambda e: e.reciprocal(out=sb_[:, 2:3], in_=sb_[:, 1:2]), reads=[sb_], writes=[sb_])
        fw.op("dve", lambda e: e.scalar_tensor_tensor(out=hb[:], in0=xb[:], scalar=sb_[:, 2:3], in1=MOD[:, a_slot, :], op0=ALU.mult, op1=ALU.mult),
              reads=[xb, sb_, MOD], writes=[hb])
        if s_slot is not None:
            fw.op("dve", lambda e: e.tensor_tensor(out=hb[:], in0=hb[:], in1=MOD[:, s_slot, :], op=ALU.add), reads=[hb, MOD], writes=[hb])

    def transpose_to(hb, dstb, dst_fn, pa, pb):
        for half, p in ((0, pa), (1, pb)):
            for c in range(4):
                cc = half * 4 + c
                tr(fw, p, p[:, c * 128:(c + 1) * 128], hb, hb[:, cc * 128:(cc + 1) * 128], C_, ID, first=(c == 0))
            fw.op("act", lambda e, half=half, p=p: e.activation(out=dst_fn(half), in_=p[:].rearrange("p (c t) -> p c t", c=4), func=AF.Copy),
                  reads=[p], writes=[dstb])

    k.__dict__.update(locals())
    return k


def mla_layer(k, L, src_first):
    fw = k.fw; nc = k.nc; PS = k.PS; C_ = k.C_; MOD = k.MOD
    x_src = k.x_in if src_first else k.xs
    mk0 = fw.mark()
    w_in_s = fw.sb("mla_w_in_s", [128, 8, 640], F32R)
    for h2 in range(2):
        fw.dma("pool", lambda e: e.dma_start(out=w_in_s[:, h2 * 4:(h2 + 1) * 4, :], in_=k.mla_w_in[h2 * 512:(h2 + 1) * 512, :].rearrange("(c p) n -> p c n", p=128)), writes=[w_in_s])
    wq_s = fw.sb("wq_s", [128, 2, 2048], F32R)
    wkv_s = fw.sb("wkv_s", [128, 2, 2048], F32R)
    for c in range(2):
        fw.dma("pool", lambda e: e.dma_start(out=wq_s[:, c, :], in_=k.mla_wq[c * 128:(c + 1) * 128, :]), writes=[wq_s])
        fw.dma("pool", lambda e: e.dma_start(out=wkv_s[:, c, :], in_=k.mla_wkv[c * 128:(c + 1) * 128, :]), writes=[wkv_s])
    gq = fw.sb("gq", [128, 2]); gkv = fw.sb("gkv", [128, 2]); invf = fw.sb("invf_s", [64, 2])
    fw.dma("sp", lambda e: e.dma_start(out=gq[:], in_=k.mla_gq), writes=[gq])
    fw.dma("sp", lambda e: e.dma_start(out=gkv[:], in_=k.mla_gkv), writes=[gkv])
    fw.dma("sp", lambda e: e.dma_start(out=invf[:], in_=k.invf), writes=[invf])
    hT = fw.sb("hT", [128, 8, 512], F32R)
    sq = fw.sb("sq", [128, 2, 512], F32R)
    rstd = fw.sb("rstd", [128, 512])
    latn = fw.sb("latn", [128, 4, 512], F32R)
    posi = fw.sb("posi", [64, 512], I32)
    ang = fw.sb("ang", [64, 512]); kf = fw.sb("kf", [64, 512]); ki = fw.sb("ki", [64, 512], I32)
    rr = fw.sb("rr", [64, 512]); mwrap = fw.sb("mwrap", [64, 512])
    Ct = fw.sb("Ct", [64, 512]); St = fw.sb("St", [64, 512])
    t1 = fw.sb("t1", [64, 512]); t2 = fw.sb("t2", [64, 512])
    stq = [fw.sb(f"stq{i}", [128, 512], BF16) for i in range(2)]
    str_ = [fw.sb(f"str{i}", [64, 512], BF16) for i in range(2)]
    stv = [fw.sb(f"stv{i}", [128, 1024], BF16) for i in range(2)]

    def wrap_sin(dst, src, shift):
        fw.op("dve", lambda e: e.tensor_scalar(out=kf[:], in0=src[:], scalar1=shift, scalar2=1.0 / TWO_PI, op0=ALU.add, op1=ALU.mult), reads=[src], writes=[kf])
        fw.op("dve", lambda e: e.tensor_copy(out=ki[:], in_=kf[:]), reads=[kf], writes=[ki])
        fw.op("dve", lambda e: e.tensor_copy(out=kf[:], in_=ki[:]), reads=[ki], writes=[kf])
        fw.op("dve", lambda e: e.scalar_tensor_tensor(out=rr[:], in0=kf[:], scalar=-TWO_PI, in1=src[:], op0=ALU.mult, op1=ALU.add), reads=[kf, src], writes=[rr])
        if shift != 0.0:
            fw.op("dve", lambda e: e.tensor_scalar(out=rr[:], in0=rr[:], scalar1=shift, scalar2=None, op0=ALU.add), reads=[rr], writes=[rr])
        fw.op("dve", lambda e: e.tensor_scalar(out=mwrap[:], in0=rr[:], scalar1=PI, scalar2=-TWO_PI, op0=ALU.is_gt, op1=ALU.mult), reads=[rr], writes=[mwrap])
        fw.op("dve", lambda e: e.tensor_tensor(out=rr[:], in0=rr[:], in1=mwrap[:], op=ALU.add), reads=[rr, mwrap], writes=[rr])
        fw.op("dve", lambda e: e.tensor_scalar(out=mwrap[:], in0=rr[:], scalar1=-PI, scalar2=TWO_PI, op0=ALU.is_lt, op1=ALU.mult), reads=[rr], writes=[mwrap])
        fw.op("dve", lambda e: e.tensor_tensor(out=rr[:], in0=rr[:], in1=mwrap[:], op=ALU.add), reads=[rr, mwrap], writes=[rr])
        fw.op("dve", lambda e: e.tensor_scalar(out=rr[:], in0=rr[:], scalar1=PI, scalar2=-PI, op0=ALU.min, op1=ALU.max), reads=[rr], writes=[rr])
        fw.op("act", lambda e: e.activation(out=dst[:], in_=rr[:], func=AF.Sin), reads=[rr], writes=[dst])

    for j in range(T // 512):
        t0 = j * 512
        for r in range(4):
            xb = k.xt[r % 2]; hb = k.ht[r % 2]; sb_ = k.st[r % 2]
            fw.dma("sp", lambda e: e.dma_start(out=xb[:], in_=x_src[t0 + r * 128:t0 + (r + 1) * 128, :]), writes=[xb])
            if src_first:
                fw.dma("sp", lambda e: e.dma_start(out=k.xs[t0 + r * 128:t0 + (r + 1) * 128, :], in_=xb[:]), reads=[xb])
            k.norm_mod(xb, hb, sb_, 1, 0)
            k.transpose_to(hb, hT, lambda half, r=r: hT[:, half * 4:(half + 1) * 4, r * 128:(r + 1) * 128], PS[0], PS[1])
        fw.dma("sp", lambda e: e.dma_start(out=posi[:], in_=k.pos_in[0:1, t0:t0 + 512].broadcast_to([64, 512])), writes=[posi])
        fw.op("dve", lambda e: e.tensor_copy(out=ang[:], in_=posi[:]), reads=[posi], writes=[ang])
        fw.op("dve", lambda e: e.tensor_scalar(out=ang[:], in0=ang[:], scalar1=invf[:, 0:1], scalar2=None, op0=ALU.mult), reads=[ang, invf], writes=[ang])
        wrap_sin(St, ang, 0.0)
        wrap_sin(Ct, ang, PI / 2)
        fw.op("dve", lambda e: e.tensor_scalar(out=St[:], in0=St[:], scalar1=invf[:, 1:2], scalar2=None, op0=ALU.mult), reads=[St, invf], writes=[St])
        for oc in range(4):
            p = PS[2 + oc]
            for c in range(8):
                mm(fw, p, p[:], w_in_s, w_in_s[:, c, oc * 128:(oc + 1) * 128], hT, hT[:, c, :], c == 0, c == 7)
        for oc in range(2):
            p = PS[6 + oc]
            for c in range(8):
                mm(fw, p, p[0:64, :], w_in_s, w_in_s[:, c, 512 + oc * 64:512 + (oc + 1) * 64], hT, hT[:, c, :], c == 0, c == 7)
        for grp, gvec in ((0, gq), (1, gkv)):
            for c2 in range(2):
                p = PS[2 + grp * 2 + c2]
                fw.op("act", lambda e, p=p, c2=c2: e.activation(out=sq[:, c2, :], in_=p[:], func=AF.Square), reads=[p], writes=[sq])
            for c2 in range(2):
                mm(fw, PS[0], PS[0][:], k.onesr, k.onesr[:], sq, sq[:, c2, :], c2 == 0, c2 == 1)
            fw.op("act", lambda e: e.activation(out=rstd[:], in_=PS[0][:], func=AF.Sqrt, bias=k.epsc[:], scale=1.0 / 256), reads=[PS[0], k.epsc], writes=[rstd])
            fw.op("dve", lambda e: e.reciprocal(out=rstd[:], in_=rstd[:]), reads=[rstd], writes=[rstd])
            for c2 in range(2):
                p = PS[2 + grp * 2 + c2]
                fw.op("dve", lambda e, p=p, c2=c2, grp=grp, gvec=gvec: e.scalar_tensor_tensor(out=latn[:, grp * 2 + c2, :], in0=p[:], scalar=gvec[:, c2:c2 + 1], in1=rstd[:],
                                                                                     op0=ALU.mult, op1=ALU.mult), reads=[p, gvec, rstd], writes=[latn])
        sr = str_[0]
        fw.op("dve", lambda e: e.tensor_tensor(out=t1[:], in0=PS[6][0:64, :], in1=Ct[:], op=ALU.mult), reads=[PS[6], Ct], writes=[t1])
        fw.op("dve", lambda e: e.tensor_tensor(out=t2[:], in0=PS[7][0:64, :], in1=St[:], op=ALU.mult), reads=[PS[7], St], writes=[t2])
        fw.op("dve", lambda e: e.tensor_tensor(out=sr[:], in0=t1[:], in1=t2[:], op=ALU.add), reads=[t1, t2], writes=[sr])
        fw.dma("sp", lambda e: e.dma_start(out=k.krT_d[:, t0:t0 + 512], in_=sr[:]), reads=[sr])
        for h in range(8):
            pq = PS[2 + (h % 2) * 3]; pr = PS[3 + (h % 2) * 3]; prs = PS[4 + (h % 2) * 3]
            for c in range(2):
                mm(fw, pq, pq[:], wq_s, wq_s[:, c, h * 192:h * 192 + 128], latn, latn[:, c, :], c == 0, c == 1)
            for c in range(2):
                mm(fw, pr, pr[0:64, :], wq_s, wq_s[:, c, h * 192 + 128:h * 192 + 192], latn, latn[:, c, :], c == 0, c == 1)
            for c in range(2):
                mm(fw, prs, prs[0:64, :], wq_s, wq_s[:, c, 1536 + h * 64:1536 + (h + 1) * 64], latn, latn[:, c, :], c == 0, c == 1)
            sq_ = stq[h % 2]; sr = str_[(h + 1) % 2]
            fw.op("act", lambda e, pq=pq, sq_=sq_: e.activation(out=sq_[:], in_=pq[:], func=AF.Copy, scale=MLA_SCALE), reads=[pq], writes=[sq_])
            fw.dma("sp", lambda e, sq_=sq_, h=h: e.dma_start(out=k.qT_d[h, 0:128, t0:t0 + 512], in_=sq_[:]), reads=[sq_])
            fw.op("dve", lambda e, pr=pr: e.scalar_tensor_tensor(out=t1[:], in0=pr[0:64, :], scalar=MLA_SCALE, in1=Ct[:], op0=ALU.mult, op1=ALU.mult), reads=[pr, Ct], writes=[t1])
            fw.op("dve", lambda e, prs=prs: e.scalar_tensor_tensor(out=t2[:], in0=prs[0:64, :], scalar=MLA_SCALE, in1=St[:], op0=ALU.mult, op1=ALU.mult), reads=[prs, St], writes=[t2])
            fw.op("dve", lambda e, sr=sr: e.tensor_tensor(out=sr[:], in0=t1[:], in1=t2[:], op=ALU.add), reads=[t1, t2], writes=[sr])
            fw.dma("sp", lambda e, sr=sr, h=h: e.dma_start(out=k.qT_d[h, 128:192, t0:t0 + 512], in_=sr[:]), reads=[sr])
        for h in range(8):
            pk = PS[2 + (h % 2)]
            for c in range(2):
                mm(fw, pk, pk[:], wkv_s, wkv_s[:, c, h * 128:(h + 1) * 128], latn, latn[:, 2 + c, :], c == 0, c == 1)
            sk = stq[h % 2]
            fw.op("act", lambda e, pk=pk, sk=sk: e.activation(out=sk[:], in_=pk[:], func=AF.Copy), reads=[pk], writes=[sk])
            fw.dma("sp", lambda e, sk=sk, h=h: e.dma_start(out=k.kT_d[h, :, t0:t0 + 512], in_=sk[:]), reads=[sk])
        for r in range(4):
            sv = stv[r % 2]
            for half in range(2):
                p = PS[4 + half]
                for c in range(2):
                    mm(fw, p, p[:], latn, latn[:, 2 + c, r * 128:(r + 1) * 128], wkv_s, wkv_s[:, c, 1024 + half * 512:1024 + (half + 1) * 512], c == 0, c == 1)
                fw.op("act", lambda e, p=p, sv=sv, half=half: e.activation(out=sv[:, half * 512:(half + 1) * 512], in_=p[:], func=AF.Copy), reads=[p], writes=[sv])
            fw.dma("sp", lambda e, sv=sv, r=r: e.dma_start(out=k.v_d[:, t0 + r * 128:t0 + (r + 1) * 128, :].rearrange("h t d -> t h d"),
                                                            in_=sv[:].rearrange("t (h d) -> t h d", h=8)), reads=[sv])
    fw.barrier()
    fw.release(mk0)
    if k.stop == "A":
        return
    krT = fw.sb("krT", [64, T], BF16)
    fw.dma("sp", lambda e: e.dma_start(out=krT[:], in_=k.krT_d), writes=[krT])
    kTs = [fw.sb(f"kTs{i}", [128, T], BF16) for i in range(2)]
    vs = [fw.sb(f"vs{i}", [128, 32, 132], BF16) for i in range(2)]
    for i in range(2):
        fw.op("dve", lambda e, i=i: e.memset(vs[i][:, :, 128:129], 1.0), writes=[vs[i]])
    qn = [fw.sb(f"qn{i}", [128, 512], BF16) for i in range(2)]
    qr = [fw.sb(f"qr{i}", [64, 512], BF16) for i in range(2)]
    pT = [fw.sb(f"pT{i}", [128, 512], BF16) for i in range(3)]
    rs = fw.sb("rs", [128, 4])
    on = [fw.sb(f"on{i}", [128, 128]) for i in range(2)]
    oT = [fw.sb(f"oT{i}", [128, 512], BF16) for i in range(2)]
    blk = 0
    for h in range(8):
        kb = kTs[h % 2]; vb = vs[h % 2]
        fw.dma("sp", lambda e: e.dma_start(out=kb[:], in_=k.kT_d[h]), writes=[kb])
        for q4 in range(4):
            fw.dma("sp", lambda e, q4=q4: e.dma_start(out=vb[:, q4 * 8:(q4 + 1) * 8, 0:128],
                                                      in_=k.v_d[h, q4 * 1024:(q4 + 1) * 1024, :].rearrange("(c p) d -> p c d", p=128)), writes=[vb])
        for jq in range(8):
            qnb = qn[jq % 2]; qrb = qr[jq % 2]
            fw.dma("sp", lambda e: e.dma_start(out=qnb[:], in_=k.qT_d[h, 0:128, jq * 512:(jq + 1) * 512]), writes=[qnb])
            fw.dma("sp", lambda e: e.dma_start(out=qrb[:], in_=k.qT_d[h, 128:192, jq * 512:(jq + 1) * 512]), writes=[qrb])
            nk = 4 * jq + 4

            def emit_qk(kc_):
                q0_ = max(0, kc_ - 4 * jq) * 128
                s__ = PS[(blk + kc_) % 3]
                mm(fw, s__, s__[:, q0_:512], kb, kb[:, kc_ * 128:(kc_ + 1) * 128], qnb, qnb[:, q0_:512], True, False)
                mm(fw, s__, s__[:, q0_:512], krT, krT[:, kc_ * 128:(kc_ + 1) * 128], qrb, qrb[:, q0_:512], False, True)

            emit_qk(0)
            for kc in range(nk):
                r = max(0, kc - 4 * jq)
                q0 = r * 128
                sp_ = PS[(blk + kc) % 3]; pb = pT[(blk + kc) % 3]
                if kc + 1 < nk:
                    emit_qk(kc + 1)
                fw.op("act", lambda e, sp_=sp_, pb=pb, q0=q0: e.activation(out=pb[:, q0:512], in_=sp_[:, q0:512], func=AF.Exp), reads=[sp_], writes=[pb])
                if kc >= 4 * jq:
                    fw.op("dve", lambda e, pb=pb, q0=q0: e.tensor_tensor(out=pb[:, q0:q0 + 128], in0=pb[:, q0:q0 + 128], in1=k.trib[:], op=ALU.mult),
                          reads=[pb, k.trib], writes=[pb])
                for s_ in range(r, 4):
                    acc = PS[3 + s_]
                    mm(fw, acc, acc[:, 0:129], pb, pb[:, s_ * 128:(s_ + 1) * 128], vb, vb[:, kc, 0:129], kc == 0, kc == 4 * jq + s_)
            blk += nk
            ob = oT[jq % 2]
            for s_ in range(4):
                acc = PS[3 + s_]; onb = on[s_ % 2]
                fw.op("dve", lambda e, acc=acc, s_=s_: e.reciprocal(out=rs[:, s_:s_ + 1], in_=acc[:, 128:129]), reads=[acc], writes=[rs])
                fw.op("act", lambda e, acc=acc, onb=onb, s_=s_: e.activation(out=onb[:], in_=acc[:, 0:128], func=AF.Copy, scale=rs[:, s_:s_ + 1]), reads=[acc, rs], writes=[onb])
                tr(fw, PS[7], PS[7][:, s_ * 128:(s_ + 1) * 128], onb, onb[:], C_, k.ID, first=(s_ == 0))
            fw.op("act", lambda e, ob=ob: e.activation(out=ob[:], in_=PS[7][:], func=AF.Copy), reads=[PS[7]], writes=[ob])
            fw.dma("sp", lambda e, ob=ob: e.dma_start(out=k.oT_d[h, :, jq * 512:(jq + 1) * 512], in_=ob[:]), reads=[ob])
    fw.barrier()
    fw.release(mk0)
    if k.stop == "B":
        return


def mixer_out_and_moe_router(k, L, w_out_d, n_kc, oT_src):
    fw = k.fw; PS = k.PS; C_ = k.C_; MOD = k.MOD
    mk0 = fw.mark()
    wo = fw.sb(f"wo_{L}", [128, n_kc, D], BF16)
    for c in range(n_kc):
        fw.dma("pool", lambda e, c=c: e.dma_start(out=wo[:, c, :], in_=w_out_d[c * 128:(c + 1) * 128, :]), writes=[wo])
    oTt = [fw.sb(f"oTt{L}_{i}", [128, n_kc, 512], BF16) for i in range(2)]
    ytmp = fw.sb(f"ytmp{L}", [128, D])
    for j in range(T // 512):
        ob = oTt[j % 2]
        fw.dma("sp", lambda e: e.dma_start(out=ob[:], in_=oT_src[:, :, j * 512:(j + 1) * 512].rearrange("h p t -> p h t")), writes=[ob])
        for r in range(4):
            t0 = j * 512 + r * 128
            xb = k.xt[r % 2]
            fw.dma("sp", lambda e: e.dma_start(out=xb[:], in_=k.xs[t0:t0 + 128, :]), writes=[xb])
            for half in range(2):
                p = PS[half]
                for c in range(n_kc):
                    mm(fw, p, p[:], ob, ob[:, c, r * 128:(r + 1) * 128], wo, wo[:, c, half * 512:(half + 1) * 512], c == 0, c == n_kc - 1)
                fw.op("dve", lambda e, p=p, half=half: e.tensor_tensor(out=ytmp[:, half * 512:(half + 1) * 512], in0=p[:], in1=MOD[:, 2, half * 512:(half + 1) * 512], op=ALU.mult),
                      reads=[p, MOD], writes=[ytmp])
            fw.op("dve", lambda e: e.tensor_tensor(out=xb[:], in0=xb[:], in1=ytmp[:], op=ALU.add), reads=[xb, ytmp], writes=[xb])
            fw.dma("sp", lambda e: e.dma_start(out=k.xs[t0:t0 + 128, :], in_=xb[:]), reads=[xb])
    fw.barrier()
    fw.release(mk0)


def moe_layer(k, L):
    fw = k.fw; PS = k.PS; C_ = k.C_; MOD = k.MOD; nc = k.nc
    mk0 = fw.mark()
    yacc = k.yacc
    h2T = fw.sb("h2T", [128, 8, T], BF16)
    G_all = fw.sb("G_all", [128, NT, 32])
    wr = fw.sb("wr", [128, 8, 32])
    fw.dma("sp", lambda e: e.dma_start(out=wr[:], in_=k.w_router[L].rearrange("(c p) n -> p c n", p=128)), writes=[wr])
    brb = fw.sb("brb", [128, 32])
    fw.dma("sp", lambda e: e.dma_start(out=brb[:], in_=k.b_router[L:L + 1, :].broadcast_to([128, 32])), writes=[brb])
    h2f = fw.sb("h2f", [128, 8, 128])
    lg = fw.sb("lg", [128, 32]); m8 = fw.sb("m8", [128, 8]); msk = fw.sb("msk", [128, 32]); ex = fw.sb("ex", [128, 32])
    sm = fw.sb("sm", [128, 4])
    MS = ""
    for i in range(NT):
        xb = k.xt[i % 2]; hb = k.ht[i % 2]; sb_ = k.st[i % 2]
        fw.dma("sp", lambda e: e.dma_start(out=xb[:], in_=k.xs[i * 128:(i + 1) * 128, :]), writes=[xb])
        if MS == "DL":
            continue
        k.norm_mod(xb, hb, sb_, 4, 3)
        if MS == "D0":
            continue
        for half, p in ((0, PS[0]), (1, PS[1])):
            for c in range(4):
                cc = half * 4 + c
                tr(fw, p, p[:, c * 128:(c + 1) * 128], hb, hb[:, cc * 128:(cc + 1) * 128], C_, k.ID, first=(c == 0))
            fw.op("act", lambda e, half=half, p=p: e.activation(out=h2f[:, half * 4:(half + 1) * 4, :], in_=p[:].rearrange("p (c t) -> p c t", c=4), func=AF.Copy),
                  reads=[p], writes=[h2f])
        fw.op("dve", lambda e: e.tensor_copy(out=h2T[:, :, i * 128:(i + 1) * 128], in_=h2f[:]), reads=[h2f], writes=[h2T])
        if MS == "D1":
            continue
        pl = PS[2]
        for c in range(8):
            mm(fw, pl, pl[:, 0:32], h2f, h2f[:, c, :], wr, wr[:, c, :], c == 0, c == 7)
        fw.op("dve", lambda e: e.tensor_tensor(out=lg[:], in0=pl[:, 0:32], in1=brb[:], op=ALU.add), reads=[pl, brb], writes=[lg])
        if MS == "D2":
            continue
        fw.op("dve", lambda e: e.max(out=m8[:], in_=lg[:]), reads=[lg], writes=[m8])
        fw.op("dve", lambda e: e.tensor_scalar(out=msk[:], in0=lg[:], scalar1=m8[:, 3:4], scalar2=None, op0=ALU.is_ge), reads=[lg, m8], writes=[msk])
        fw.op("dve", lambda e: e.tensor_scalar(out=sm[:, 0:1], in0=m8[:, 0:1], scalar1=-1.0, scalar2=None, op0=ALU.mult), reads=[m8], writes=[sm])
        fw.op("act", lambda e: e.activation(out=ex[:], in_=lg[:], func=AF.Exp, bias=sm[:, 0:1], scale=1.0), reads=[lg, sm], writes=[ex])
        fw.op("dve", lambda e: e.tensor_tensor(out=ex[:], in0=ex[:], in1=msk[:], op=ALU.mult), reads=[ex, msk], writes=[ex])
        fw.op("dve", lambda e: e.reduce_sum(out=sm[:, 1:2], in_=ex[:], axis=AX.X), reads=[ex], writes=[sm])
        fw.op("dve", lambda e: e.reciprocal(out=sm[:, 2:3], in_=sm[:, 1:2]), reads=[sm], writes=[sm])
        fw.op("dve", lambda e: e.tensor_scalar(out=G_all[:, i, :], in0=ex[:], scalar1=sm[:, 2:3], scalar2=None, op0=ALU.mult), reads=[ex, sm], writes=[G_all])
    fw.barrier()
    if MS:
        fw.release(mk0)
        return
    wgu = fw.sb("wgu", [128, 8, 2048], BF16)
    wdn = fw.sb("wdn", [128, 8, 1024], BF16)
    bgu = fw.sb("bgu", [128, 16])
    bdb = fw.sb("bdb", [128, 1024])
    hact = fw.sb("hact", [128, 8, 512], BF16)
    g_ = fw.sb("g_", [128, 512]); sg_ = fw.sb("sg_", [128, 512]); l_ = fw.sb("l_", [128, 512])
    yo = [fw.sb(f"yo{i}", [128, 1024]) for i in range(2)]
    for ex_i in range(32):
        for c in range(8):
            fw.dma("pool", lambda e, c=c: e.dma_start(out=wgu[:, c, :], in_=k.w_gu[L, ex_i, c * 128:(c + 1) * 128, :]), writes=[wgu])
        for c in range(8):
            fw.dma("pool", lambda e, c=c: e.dma_start(out=wdn[:, c, :], in_=k.w_dn[L, ex_i, c * 128:(c + 1) * 128, :]), writes=[wdn])
        with nc.allow_non_contiguous_dma(reason="small bias relayout"):
            fw.dma("sp", lambda e: e.dma_start(out=bgu[:], in_=k.b_gu[L, ex_i, :].rearrange("(c p) -> p c", p=128)), writes=[bgu])
        fw.dma("sp", lambda e: e.dma_start(out=bdb[:], in_=k.b_dn[L, ex_i:ex_i + 1, :].broadcast_to([128, 1024])), writes=[bdb])
        for jt in range(T // 512):
            for fc in range(8):
                pg = PS[(fc % 2) * 2]; pl2 = PS[(fc % 2) * 2 + 1]
                for c in range(8):
                    mm(fw, pg, pg[:], wgu, wgu[:, c, fc * 128:(fc + 1) * 128], h2T, h2T[:, c, jt * 512:(jt + 1) * 512], c == 0, c == 7)
                for c in range(8):
                    mm(fw, pl2, pl2[:], wgu, wgu[:, c, 1024 + fc * 128:1024 + (fc + 1) * 128], h2T, h2T[:, c, jt * 512:(jt + 1) * 512], c == 0, c == 7)
                fw.op("dve", lambda e, pg=pg, fc=fc: e.tensor_scalar(out=g_[:], in0=pg[:], scalar1=bgu[:, fc:fc + 1], scalar2=7.0, op0=ALU.add, op1=ALU.min), reads=[pg, bgu], writes=[g_])
                fw.op("act", lambda e: e.activation(out=sg_[:], in_=g_[:], func=AF.Sigmoid, scale=1.702), reads=[g_], writes=[sg_])
                fw.op("dve", lambda e, pl2=pl2, fc=fc: e.tensor_scalar(out=l_[:], in0=pl2[:], scalar1=bgu[:, 8 + fc:9 + fc], scalar2=7.0, op0=ALU.add, op1=ALU.min), reads=[pl2, bgu], writes=[l_])
                fw.op("dve", lambda e: e.tensor_scalar(out=l_[:], in0=l_[:], scalar1=-7.0, scalar2=1.0, op0=ALU.max, op1=ALU.add), reads=[l_], writes=[l_])
                fw.op("dve", lambda e: e.tensor_tensor(out=g_[:], in0=g_[:], in1=sg_[:], op=ALU.mult), reads=[g_, sg_], writes=[g_])
                fw.op("dve", lambda e, fc=fc: e.tensor_tensor(out=hact[:, fc, :], in0=g_[:], in1=l_[:], op=ALU.mult), reads=[g_, l_], writes=[hact])
            for r in range(4):
                ti = jt * 4 + r
                yb = yo[r % 2]
                for half in range(2):
                    p = PS[4 + half]
                    for fc in range(8):
                        mm(fw, p, p[:], hact, hact[:, fc, r * 128:(r + 1) * 128], wdn, wdn[:, fc, half * 512:(half + 1) * 512], fc == 0, fc == 7)
                    fw.op("dve", lambda e, p=p, half=half, yb=yb: e.tensor_tensor(out=yb[:, half * 512:(half + 1) * 512], in0=p[:], in1=bdb[:, half * 512:(half + 1) * 512], op=ALU.add),
                          reads=[p, bdb], writes=[yb])
                fw.op("dve", lambda e, yb=yb, ti=ti: e.tensor_scalar(out=yb[:], in0=yb[:], scalar1=G_all[:, ti, ex_i:ex_i + 1], scalar2=None, op0=ALU.mult), reads=[yb, G_all], writes=[yb])
                if ex_i == 0:
                    fw.dma("sp", lambda e, yb=yb, ti=ti: e.dma_start(out=yacc[ti * 128:(ti + 1) * 128, :], in_=yb[:]), reads=[yb])
                else:
                    fw.dma("pool", lambda e, yb=yb, ti=ti: e.dma_start(out=yacc[ti * 128:(ti + 1) * 128, :], in_=yb[:], accum_op=ALU.add), reads=[yb])
        fw.barrier()
    for i in range(NT):
        xb = k.xt[i % 2]; yb = yo[i % 2]
        fw.dma("sp", lambda e: e.dma_start(out=xb[:], in_=k.xs[i * 128:(i + 1) * 128, :]), writes=[xb])
        fw.dma("sp", lambda e: e.dma_start(out=yb[:], in_=yacc[i * 128:(i + 1) * 128, :]), writes=[yb])
        fw.op("dve", lambda e: e.tensor_tensor(out=yb[:], in0=yb[:], in1=MOD[:, 5, :], op=ALU.mult), reads=[yb, MOD], writes=[yb])
        fw.op("dve", lambda e: e.tensor_tensor(out=xb[:], in0=xb[:], in1=yb[:], op=ALU.add), reads=[xb, yb], writes=[xb])
        fw.dma("sp", lambda e: e.dma_start(out=k.xs[i * 128:(i + 1) * 128, :], in_=xb[:]), reads=[xb])
    fw.barrier()
    fw.release(mk0)


def final_norm(k):
    fw = k.fw
    gfb = fw.sb("gfb", [128, D])
    fw.dma("sp", lambda e: e.dma_start(out=gfb[:], in_=k.g_final[0:1, :].broadcast_to([128, D])), writes=[gfb])
    fw.op("dve", lambda e: e.tensor_copy(out=k.MOD[:, 1, :], in_=gfb[:]), reads=[gfb], writes=[k.MOD])
    for i in range(NT):
        xb = k.xt[i % 2]; hb = k.ht[i % 2]; sb_ = k.st[i % 2]
        fw.dma("sp", lambda e: e.dma_start(out=xb[:], in_=k.xs[i * 128:(i + 1) * 128, :]), writes=[xb])
        k.norm_mod(xb, hb, sb_, 1, None)
        fw.dma("sp", lambda e: e.dma_start(out=k.out_d[i * 128:(i + 1) * 128, :], in_=hb[:]), reads=[hb])


def ssd_layer(k, L):
    fw = k.fw; nc = k.nc; PS = k.PS; C_ = k.C_; MOD = k.MOD
    mk0 = fw.mark()
    w_in = k.ssm_w_in
    fw.store_q = None
    dt_tm = fw.sb("dt_tm", [128, NT, 32]); nac_tm = fw.sb("nac_tm", [128, NT, 32])
    mk_a = fw.mark()
    dtT = fw.sb("dtT", [32, T]); adtT = fw.sb("adtT", [32, T])
    mkA = fw.mark()
    hT = fw.sb("s_hT", [128, 8, 512], F32R)
    wsl = [fw.sb(f"wsl{i}", [128, 8, 512], F32R) for i in range(2)]
    wdt = fw.sb("wdt", [128, 8, 32], F32R)
    fw.dma("pool", lambda e: e.dma_start(out=wdt[:], in_=w_in[:, 6144:6176].rearrange("(c p) n -> p c n", p=128)), writes=[wdt])
    cw = fw.sb("cw", [128, 32, 4]); cb = fw.sb("cb", [128, 32])
    fw.dma("sp", lambda e: e.dma_start(out=cw[:], in_=k.ssm_cw), writes=[cw])
    fw.dma("sp", lambda e: e.dma_start(out=cb[:], in_=k.ssm_cb), writes=[cb])
    hv = fw.sb("hv", [32, 4])
    fw.dma("sp", lambda e: e.dma_start(out=hv[:, 0:2], in_=k.ssm_hv), writes=[hv])
    fw.op("act", lambda e: e.activation(out=hv[:, 2:3], in_=hv[:, 1:2], func=AF.Exp), reads=[hv], writes=[hv])
    fw.op("dve", lambda e: e.tensor_scalar(out=hv[:, 3:4], in0=hv[:, 2:3], scalar1=-1.0, scalar2=None, op0=ALU.mult), reads=[hv], writes=[hv])
    halo = fw.sb("halo", [128, 32, 4])
    fw.op("dve", lambda e: e.memset(halo[:], 0.0), writes=[halo])
    ub = [fw.sb(f"ub{i}", [128, 516]) for i in range(4)]
    acc = [fw.sb(f"cacc{i}", [128, 512]) for i in range(4)]
    xact = [fw.sb(f"xact{i}", [128, 512]) for i in range(4)]
    bcst = [fw.sb(f"bcst{i}", [128, 512], BF16) for i in range(4)]
    xtm = [fw.sb(f"xtm{i}", [128, 4, 512]) for i in range(2)]
    zst = [fw.sb(f"zst{i}", [128, 512]) for i in range(2)]
    et = fw.sb("et", [32, 512])
    nslab = 0
    for j in range(T // 512):
        t0 = j * 512
        for r in range(4):
            xb = k.xt[r % 2]; hb = k.ht[r % 2]; sb_ = k.st[r % 2]
            fw.dma("pool", lambda e: e.dma_start(out=xb[:], in_=k.xs[t0 + r * 128:t0 + (r + 1) * 128, :]), writes=[xb])
            k.norm_mod(xb, hb, sb_, 1, 0)
            k.transpose_to(hb, hT, lambda half, r=r: hT[:, half * 4:(half + 1) * 4, r * 128:(r + 1) * 128], PS[0], PS[1])
        pd = PS[2]
        for c in range(8):
            mm(fw, pd, pd[0:32, :], wdt, wdt[:, c, :], hT, hT[:, c, :], c == 0, c == 7)
        fw.op("act", lambda e: e.activation(out=et[:], in_=pd[0:32, :], func=AF.Exp, bias=hv[:, 0:1], scale=1.0), reads=[pd, hv], writes=[et])
        fw.op("act", lambda e: e.activation(out=dtT[:, t0:t0 + 512], in_=et[:], func=AF.Ln, bias=1.0, scale=1.0), reads=[et], writes=[dtT])
        fw.op("dve", lambda e: e.tensor_scalar(out=adtT[:, t0:t0 + 512], in0=dtT[:, t0:t0 + 512], scalar1=hv[:, 3:4], scalar2=None, op0=ALU.mult), reads=[dtT, hv], writes=[adtT])
        pending = []
        for sl in range(8):
            wb = wsl[nslab % 2]; nslab += 1
            for hf in range(2):
                fw.dma("pool", lambda e, hf=hf: e.dma_start(out=wb[:, hf * 4:(hf + 1) * 4, :],
                                                             in_=w_in[hf * 512:(hf + 1) * 512, 2048 + sl * 512:2048 + (sl + 1) * 512].rearrange("(c p) n -> p c n", p=128)), writes=[wb])
            for c4 in range(4):
                cc = sl * 4 + c4
                p = PS[2 + (cc % 3)]
                for c in range(8):
                    mm(fw, p, p[:], wb, wb[:, c, c4 * 128:(c4 + 1) * 128], hT, hT[:, c, :], c == 0, c == 7)
                u = ub[cc % 4]; a_ = acc[cc % 4]; xa = xact[cc % 4]
                fw.op("act", lambda e, p=p, u=u: e.activation(out=u[:, 3:515], in_=p[:], func=AF.Copy), reads=[p], writes=[u])
                fw.op("dve", lambda e, u=u, cc=cc: e.tensor_copy(out=u[:, 0:3], in_=halo[:, cc, 0:3]), reads=[halo], writes=[u])
                fw.op("dve", lambda e, u=u, cc=cc: e.tensor_copy(out=halo[:, cc, 0:3], in_=u[:, 512:515]), reads=[u], writes=[halo])
                fw.op("act", lambda e, u=u, a_=a_, cc=cc: e.activation(out=a_[:], in_=u[:, 3:515], func=AF.Identity, scale=cw[:, cc, 3:4], bias=cb[:, cc:cc + 1]),
                      reads=[u, cw, cb], writes=[a_])
                for kk in range(3):
                    fw.op("dve", lambda e, u=u, a_=a_, cc=cc, kk=kk: e.scalar_tensor_tensor(out=a_[:], in0=u[:, kk:kk + 512], scalar=cw[:, cc, kk:kk + 1], in1=a_[:],
                                                                                          op0=ALU.mult, op1=ALU.add), reads=[u, cw, a_], writes=[a_])
                if cc < 16:
                    fw.op("act", lambda e, a_=a_, xa=xa: e.activation(out=xa[:], in_=a_[:], func=AF.Silu), reads=[a_], writes=[xa])
                    def fin(cc=cc, c4=c4, sl=sl, xa=xa, t0=t0):
                        xs_t = xtm[(cc // 4) % 2]
                        pt = PS[5 + (cc % 3)]
                        for r in range(4):
                            tr(fw, pt, pt[:, r * 128:(r + 1) * 128], xa, xa[:, r * 128:(r + 1) * 128], C_, k.ID, first=(r == 0))
                        fw.op("act", lambda e: e.activation(out=xs_t[:, :, c4 * 128:(c4 + 1) * 128], in_=pt[:].rearrange("p (r c) -> p r c", r=4), func=AF.Copy),
                              reads=[pt], writes=[xs_t])
                        if c4 == 3:
                            for r in range(4):
                                fw.dma("sp", lambda e, r=r: e.dma_start(out=k.sxs_d[t0 + r * 128:t0 + (r + 1) * 128, sl * 512:(sl + 1) * 512], in_=xs_t[:, r, :]), reads=[xs_t])
                    pending.append(fin)
                else:
                    bs = bcst[cc % 4]
                    fw.op("act", lambda e, a_=a_, bs=bs: e.activation(out=bs[:], in_=a_[:], func=AF.Silu), reads=[a_], writes=[bs])
                    fw.dma("sp", lambda e, bs=bs, cc=cc: e.dma_start(out=k.sbc_d[cc - 16, :, t0:t0 + 512], in_=bs[:]), reads=[bs])
                while len(pending) > 2 or (pending and cc >= 17):
                    pending.pop(0)()
        while pending:
            pending.pop(0)()
        for zs in range(4):
            wb = wsl[nslab % 2]; nslab += 1
            for hf in range(2):
                fw.dma("pool", lambda e, hf=hf: e.dma_start(out=wb[:, hf * 4:(hf + 1) * 4, :],
                                                             in_=w_in[hf * 512:(hf + 1) * 512, zs * 512:(zs + 1) * 512].rearrange("(c p) n -> p c n", p=128)), writes=[wb])
            for r in range(4):
                p = PS[3 + (r % 2)]
                for c in range(8):
                    mm(fw, p, p[:], hT, hT[:, c, r * 128:(r + 1) * 128], wb, wb[:, c, :], c == 0, c == 7)
                zt = zst[r % 2]
                fw.op("act", lambda e, p=p, zt=zt: e.activation(out=zt[:, 0:512], in_=p[:], func=AF.Silu), reads=[p], writes=[zt])
                fw.dma("sp", lambda e, zt=zt, r=r: e.dma_start(out=k.szs_d[t0 + r * 128:t0 + (r + 1) * 128, zs * 512:(zs + 1) * 512], in_=zt[:, 0:512]), reads=[zt])
    fw.barrier()
    fw.store_q = "pool"
    fw.release(mkA)
    ones32 = fw.sb("ones32", [32, T])
    fw.op("dve", lambda e: e.memset(ones32[:], 1.0), writes=[ones32])
    acT = fw.sb("acT", [32, T])
    fw.op("dve", lambda e: e.tensor_tensor_scan(out=acT[:], data0=ones32[:], data1=adtT[:], initial=0.0, op0=ALU.mult, op1=ALU.add), reads=[ones32, adtT], writes=[acT])
    fw.dma("sp", lambda e: e.dma_start(out=k.sac_d, in_=acT[:]), reads=[acT])
    fw.barrier()
    for c in range(NT):
        p = PS[c % 2]
        tr(fw, p, p[:, 0:32], dtT, dtT[:, c * 128:(c + 1) * 128], C_, C_[0:32, 0:32])
        tr(fw, p, p[:, 32:64], acT, acT[:, c * 128:(c + 1) * 128], C_, C_[0:32, 0:32], first=False)
        fw.op("act", lambda e, p=p, c=c: e.activation(out=dt_tm[:, c, :], in_=p[:, 0:32], func=AF.Copy), reads=[p], writes=[dt_tm])
        fw.op("act", lambda e, p=p, c=c: e.activation(out=nac_tm[:, c, :], in_=p[:, 32:64], func=AF.Copy, scale=-1.0), reads=[p], writes=[nac_tm])
    fw.barrier()
    fw.release(mk_a)
    mk1 = fw.mark()
    BT = [fw.sb(f"BT{i}", [128, T], BF16) for i in range(2)]
    CT = [fw.sb(f"CT{i}", [128, T], BF16) for i in range(2)]
    abc = [fw.sb(f"abc{i}", [128, T]) for i in range(4)]
    xsf = fw.sb("xsf", [128, NT, 64])
    V = [fw.sb(f"V{i}", [128, NT, 64], BF16) for i in range(4)]
    dec = [fw.sb(f"dec{i}", [128, 512]) for i in range(4)]
    pT = [fw.sb(f"spT{i}", [128, 512], BF16) for i in range(6)]
    ysb = [fw.sb(f"ysb{i}", [128, 4, 64]) for i in range(2)]
    blk = 0; nd = 0; npt = 0
    for g in range(8):
        Bb = BT[g % 2]; Cb = CT[g % 2]
        fw.dma("sp", lambda e: e.dma_start(out=Bb[:], in_=k.sbc_d[g]), writes=[Bb])
        fw.dma("sp", lambda e: e.dma_start(out=Cb[:], in_=k.sbc_d[8 + g]), writes=[Cb])
        for r in range(4):
            hh = g * 4 + r
            fw.dma("sp", lambda e, r=r, hh=hh: e.dma_start(out=abc[r][:], in_=k.sac_d[hh:hh + 1, :].broadcast_to([128, T])), writes=[abc[r]])
            for q4 in range(4):
                fw.dma("sp", lambda e, q4=q4, hh=hh: e.dma_start(out=xsf[:, q4 * 8:(q4 + 1) * 8, :],
                                                                 in_=k.sxs_d[q4 * 1024:(q4 + 1) * 1024, hh * 64:(hh + 1) * 64].rearrange("(c p) d -> p c d", p=128)), writes=[xsf])
            fw.op("dve", lambda e, r=r, hh=hh: e.tensor_tensor(out=V[r][:], in0=xsf[:], in1=dt_tm[:, :, hh:hh + 1].broadcast_to([128, NT, 64]), op=ALU.mult),
                  reads=[xsf, dt_tm], writes=[V[r]])
        for jq in range(8):
            nk = 4 * jq + 4

            def emit_st(kc_):
                q0_ = max(0, kc_ - 4 * jq) * 128
                s__ = PS[(blk + kc_) % 3]
                mm(fw, s__, s__[:, q0_:512], Bb, Bb[:, kc_ * 128:(kc_ + 1) * 128], Cb, Cb[:, jq * 512 + q0_:(jq + 1) * 512], True, True)

            emit_st(0)
            for kc in range(nk):
                r0 = max(0, kc - 4 * jq)
                q0 = r0 * 128
                diag = kc >= 4 * jq
                sp_ = PS[(blk + kc) % 3]
                if kc + 1 < nk:
                    emit_st(kc + 1)
                for r in range(4):
                    hh = g * 4 + r
                    d_ = dec[nd % 4]; nd += 1
                    pb = pT[npt % 6]; npt += 1
                    qa = jq * 512 + q0
                    if diag:
                        fw.op("dve", lambda e, d_=d_, r=r, qa=qa, hh=hh: e.tensor_scalar(out=d_[:, q0:q0 + 128], in0=abc[r][:, qa:qa + 128], scalar1=nac_tm[:, kc, hh:hh + 1], scalar2=0.0,
                                                                                       op0=ALU.add, op1=ALU.min), reads=[abc[r], nac_tm], writes=[d_])
                        fw.op("act", lambda e, d_=d_: e.activation(out=d_[:, q0:q0 + 128], in_=d_[:, q0:q0 + 128], func=AF.Exp), reads=[d_], writes=[d_])
                        fw.op("dve", lambda e, d_=d_: e.tensor_tensor(out=d_[:, q0:q0 + 128], in0=d_[:, q0:q0 + 128], in1=C_[:, 256:384], op=ALU.mult), reads=[d_, C_], writes=[d_])
                        if q0 + 128 < 512:
                            fw.op("act", lambda e, d_=d_, r=r, qa=qa, hh=hh: e.activation(out=d_[:, q0 + 128:512], in_=abc[r][:, qa + 128:(jq + 1) * 512], func=AF.Exp,
                                                                                     bias=nac_tm[:, kc, hh:hh + 1], scale=1.0), reads=[abc[r], nac_tm, d_], writes=[d_])
                    else:
                        fw.op("act", lambda e, d_=d_, r=r, qa=qa, hh=hh: e.activation(out=d_[:, q0:512], in_=abc[r][:, qa:(jq + 1) * 512], func=AF.Exp,
                                                                                 bias=nac_tm[:, kc, hh:hh + 1], scale=1.0), reads=[abc[r], nac_tm], writes=[d_])
                    fw.op("dve", lambda e, d_=d_, pb=pb, sp_=sp_: e.tensor_tensor(out=pb[:, q0:512], in0=sp_[:, q0:512], in1=d_[:, q0:512], op=ALU.mult), reads=[sp_, d_], writes=[pb])
                    ya = PS[3 + r]
                    for s_ in range(r0, 4):
                        fw.op("pe", lambda e, ya=ya, pb=pb, s_=s_, r=r: e.matmul(ya[:, s_ * 64:(s_ + 1) * 64], pb[:, s_ * 128:(s_ + 1) * 128], V[r][:, kc, :],
                                                                               start=(kc == 0), stop=(kc == 4 * jq + s_)),
                              reads=[pb, V[r]], writes=[ya], accumulate=True)
            blk += nk
            for r in range(4):
                hh = g * 4 + r
                ya = PS[3 + r]; yb = ysb[r % 2]
                fw.op("act", lambda e, ya=ya, yb=yb: e.activation(out=yb[:], in_=ya[:, 0:256].rearrange("p (s d) -> p s d", s=4), func=AF.Copy), reads=[ya], writes=[yb])
                fw.dma("sp", lambda e, yb=yb, hh=hh: e.dma_start(out=k.sy_d[jq * 512:(jq + 1) * 512, hh * 64:(hh + 1) * 64].rearrange("(s p) d -> p s d", p=128), in_=yb[:]), reads=[yb])
    fw.barrier()
    fw.release(mk1)
    dbc = fw.sb("dbc", [128, 2048]); gnb = fw.sb("gnb", [128, 2048])
    fw.dma("sp", lambda e: e.dma_start(out=dbc[:], in_=k.ssm_dexp[0:1, :].broadcast_to([128, 2048])), writes=[dbc])
    fw.dma("sp", lambda e: e.dma_start(out=gnb[:], in_=k.ssm_gn[0:1, :].broadcast_to([128, 2048])), writes=[gnb])
    yt = [fw.sb(f"yt{i}", [128, 2048]) for i in range(2)]
    xs2 = [fw.sb(f"xs2{i}", [128, 2048]) for i in range(2)]
    zz = [fw.sb(f"zz{i}", [128, 2048]) for i in range(2)]
    sqv = fw.sb("sqv", [128, 2048])
    gs = fw.sb("gs", [128, 16])
    ynT = [fw.sb(f"ynT{i}", [128, 16, 128], BF16) for i in range(2)]
    for i in range(NT):
        y_ = yt[i % 2]; x_ = xs2[i % 2]; z_ = zz[i % 2]
        fw.dma("sp", lambda e: e.dma_start(out=y_[:], in_=k.sy_d[i * 128:(i + 1) * 128, :]), writes=[y_])
        fw.dma("sp", lambda e: e.dma_start(out=x_[:], in_=k.sxs_d[i * 128:(i + 1) * 128, :]), writes=[x_])
        fw.dma("sp", lambda e: e.dma_start(out=z_[:], in_=k.szs_d[i * 128:(i + 1) * 128, :]), writes=[z_])
        fw.op("dve", lambda e: e.tensor_tensor(out=x_[:], in0=x_[:], in1=dbc[:], op=ALU.mult), reads=[x_, dbc], writes=[x_])
        fw.op("dve", lambda e: e.tensor_tensor(out=y_[:], in0=y_[:], in1=x_[:], op=ALU.add), reads=[y_, x_], writes=[y_])
        fw.op("dve", lambda e: e.tensor_tensor(out=y_[:], in0=y_[:], in1=z_[:], op=ALU.mult), reads=[y_, z_], writes=[y_])
        fw.op("act", lambda e: e.activation(out=sqv[:], in_=y_[:], func=AF.Square), reads=[y_], writes=[sqv])
        fw.op("dve", lambda e: e.tensor_reduce(out=gs[:, 0:8], in_=sqv[:].rearrange("p (g d) -> p g d", g=8), axis=AX.X, op=ALU.add), reads=[sqv], writes=[gs])
        fw.op("act", lambda e: e.activation(out=gs[:, 8:16], in_=gs[:, 0:8], func=AF.Sqrt, bias=k.epsc[:], scale=1.0 / 256), reads=[gs, k.epsc], writes=[gs])
        fw.op("dve", lambda e: e.reciprocal(out=gs[:, 8:16], in_=gs[:, 8:16]), reads=[gs], writes=[gs])
        for g in range(8):
            fw.op("dve", lambda e, g=g: e.scalar_tensor_tensor(out=y_[:, g * 256:(g + 1) * 256], in0=y_[:, g * 256:(g + 1) * 256], scalar=gs[:, 8 + g:9 + g],
                                                               in1=gnb[:, g * 256:(g + 1) * 256], op0=ALU.mult, op1=ALU.mult), reads=[y_, gs, gnb], writes=[y_])
        yT = ynT[i % 2]
        for q4 in range(4):
            p = PS[q4 % 2]
            for c in range(4):
                cc = q4 * 4 + c
                tr(fw, p, p[:, c * 128:(c + 1) * 128], y_, y_[:, cc * 128:(cc + 1) * 128], C_, k.ID, first=(c == 0))
            fw.op("act", lambda e, p=p, q4=q4: e.activation(out=yT[:, q4 * 4:(q4 + 1) * 4, :], in_=p[:].rearrange("p (c t) -> p c t", c=4), func=AF.Copy), reads=[p], writes=[yT])
        fw.dma("sp", lambda e: e.dma_start(out=k.synT_d[:, :, i * 128:(i + 1) * 128].rearrange("c p t -> p c t"), in_=yT[:]), reads=[yT])
    fw.barrier()
    fw.release(mk0)


MOE_B = 256
MOE_NB = 4 * T // MOE_B + 64


def moe_layer_sparse(k, L):
    fw = k.fw; PS = k.PS; C_ = k.C_; MOD = k.MOD; nc = k.nc
    NB = MOE_NB; B = MOE_B
    mk0 = fw.mark()
    S4_all = fw.sb("S4_all", [128, NT, 4], I32); G4_all = fw.sb("G4_all", [128, NT, 4])
    idx_w = fw.sb("idx_w", [128, 128], I32); idx_b = fw.sb("idx_b", [128, 128], I32)
    idx_g = [fw.sb(f"idx_g{i}", [128, 128], I32) for i in range(8)]
    mkD = fw.mark()
    G_all = fw.sb("G_all", [128, NT, 32]); M_all = fw.sb("M_all", [128, NT, 32]); R_all = fw.sb("R_all", [128, NT, 32])
    base = fw.sb("base", [128, 32])
    fw.op("dve", lambda e: e.memset(base[:], 0.0), writes=[base])
    wr = fw.sb("wr", [128, 8, 32])
    fw.dma("sp", lambda e: e.dma_start(out=wr[:], in_=k.w_router[L].rearrange("(c p) n -> p c n", p=128)), writes=[wr])
    brb = fw.sb("brb", [128, 32])
    fw.dma("sp", lambda e: e.dma_start(out=brb[:], in_=k.b_router[L:L + 1, :].broadcast_to([128, 32])), writes=[brb])
    h2f = fw.sb("h2f", [128, 8, 128])
    lg = fw.sb("lg", [128, 32]); m8 = fw.sb("m8", [128, 8]); ex = fw.sb("ex", [128, 32])
    sm = fw.sb("sm", [128, 4])
    for i in range(NT):
        xb = k.xt[i % 2]; hb = k.ht[i % 2]; sb_ = k.st[i % 2]
        fw.dma("sp", lambda e: e.dma_start(out=xb[:], in_=k.xs[i * 128:(i + 1) * 128, :]), writes=[xb])
        k.norm_mod(xb, hb, sb_, 4, 3)
        fw.dma("sp", lambda e: e.dma_start(out=k.h2_d[i * 128:(i + 1) * 128, :], in_=hb[:]), reads=[hb])
        for half, p in ((0, PS[0]), (1, PS[1])):
            for c in range(4):
                cc = half * 4 + c
                tr(fw, p, p[:, c * 128:(c + 1) * 128], hb, hb[:, cc * 128:(cc + 1) * 128], C_, k.ID, first=(c == 0))
            fw.op("act", lambda e, half=half, p=p: e.activation(out=h2f[:, half * 4:(half + 1) * 4, :], in_=p[:].rearrange("p (c t) -> p c t", c=4), func=AF.Copy),
                  reads=[p], writes=[h2f])
        pl = PS[2]
        for c in range(8):
            mm(fw, pl, pl[:, 0:32], h2f, h2f[:, c, :], wr, wr[:, c, :], c == 0, c == 7)
        msk = M_all
        fw.op("dve", lambda e: e.tensor_tensor(out=lg[:], in0=pl[:, 0:32], in1=brb[:], op=ALU.add), reads=[pl, brb], writes=[lg])
        fw.op("dve", lambda e: e.max(out=m8[:], in_=lg[:]), reads=[lg], writes=[m8])
        fw.op("dve", lambda e: e.tensor_scalar(out=M_all[:, i, :], in0=lg[:], scalar1=m8[:, 3:4], scalar2=None, op0=ALU.is_ge), reads=[lg, m8], writes=[M_all])
        fw.op("dve", lambda e: e.tensor_scalar(out=sm[:, 0:1], in0=m8[:, 0:1], scalar1=-1.0, scalar2=None, op0=ALU.mult), reads=[m8], writes=[sm])
        fw.op("act", lambda e: e.activation(out=ex[:], in_=lg[:], func=AF.Exp, bias=sm[:, 0:1], scale=1.0), reads=[lg, sm], writes=[ex])
        fw.op("dve", lambda e: e.tensor_tensor(out=ex[:], in0=ex[:], in1=M_all[:, i, :], op=ALU.mult), reads=[ex, M_all], writes=[ex])
        fw.op("dve", lambda e: e.reduce_sum(out=sm[:, 1:2], in_=ex[:], axis=AX.X), reads=[ex], writes=[sm])
        fw.op("dve", lambda e: e.reciprocal(out=sm[:, 2:3], in_=sm[:, 1:2]), reads=[sm], writes=[sm])
        fw.op("dve", lambda e: e.tensor_scalar(out=G_all[:, i, :], in0=ex[:], scalar1=sm[:, 2:3], scalar2=None, op0=ALU.mult), reads=[ex, sm], writes=[G_all])
        pr = PS[3]; pc = PS[4]
        mm(fw, pr, pr[:, 0:32], C_, C_[:, 384:512], M_all, M_all[:, i, :], True, True)
        mm(fw, pc, pc[:, 0:32], C_, C_[:, 128:256], M_all, M_all[:, i, :], True, True)
        fw.op("dve", lambda e: e.tensor_tensor(out=R_all[:, i, :], in0=pr[:, 0:32], in1=base[:], op=ALU.add), reads=[pr, base], writes=[R_all])
        fw.op("dve", lambda e: e.tensor_tensor(out=base[:], in0=pc[:, 0:32], in1=base[:], op=ALU.add), reads=[pc, base], writes=[base])
    ti = fw.sb("ti", [128, 32], I32); pf = fw.sb("pf", [128, 32]); pend = fw.sb("pend", [128, 32]); pst = fw.sb("pst", [128, 32])
    on32 = fw.sb("on32", [128, 32]); sl_ = fw.sb("sl_", [128, 32]); v_ = fw.sb("v_", [128, 32]); m8s = fw.sb("m8s", [128, 8]); jk = fw.sb("jk", [128, 32])
    fw.op("dve", lambda e: e.memset(on32[:], 1.0), writes=[on32])
    fw.op("dve", lambda e: e.tensor_scalar(out=pf[:], in0=base[:], scalar1=float(2 * B - 1), scalar2=None, op0=ALU.add), reads=[base], writes=[pf])
    fw.op("dve", lambda e: e.tensor_copy(out=ti[:], in_=pf[:]), reads=[pf], writes=[ti])
    sh = (2 * B).bit_length() - 1
    fw.op("dve", lambda e: e.tensor_scalar(out=ti[:], in0=ti[:], scalar1=sh, scalar2=sh, op0=ALU.logical_shift_right, op1=ALU.logical_shift_left), reads=[ti], writes=[ti])
    fw.op("dve", lambda e: e.tensor_copy(out=pf[:], in_=ti[:]), reads=[ti], writes=[pf])
    fw.op("dve", lambda e: e.tensor_tensor_scan(out=pend[:], data0=on32[:], data1=pf[:], initial=0.0, op0=ALU.mult, op1=ALU.add), reads=[on32, pf], writes=[pend])
    fw.op("dve", lambda e: e.tensor_tensor(out=pst[:], in0=pend[:], in1=pf[:], op=ALU.subtract), reads=[pend, pf], writes=[pst])
    for i in range(NT):
        fw.op("dve", lambda e: e.tensor_tensor(out=sl_[:], in0=R_all[:, i, :], in1=pst[:], op=ALU.add), reads=[R_all, pst], writes=[sl_])
        fw.op("dve", lambda e: e.scalar_tensor_tensor(out=v_[:], in0=sl_[:], scalar=1.0, in1=M_all[:, i, :], op0=ALU.add, op1=ALU.mult), reads=[sl_, M_all], writes=[v_])
        fw.op("dve", lambda e: e.max(out=m8s[:], in_=v_[:]), reads=[v_], writes=[m8s])
        fw.op("dve", lambda e: e.tensor_scalar(out=S4_all[:, i, :], in0=m8s[:, 0:4], scalar1=-1.0, scalar2=None, op0=ALU.add), reads=[m8s], writes=[S4_all])
        for kk in range(4):
            fw.op("dve", lambda e, kk=kk: e.scalar_tensor_tensor(out=jk[:], in0=v_[:], scalar=m8s[:, kk:kk + 1], in1=G_all[:, i, :], op0=ALU.is_equal, op1=ALU.mult,
                                                                accum_out=G4_all[:, i, kk:kk + 1]), reads=[v_, m8s, G_all], writes=[jk, G4_all])
    cmp_ = fw.sb("cmp_", [128, 32]); bec = fw.sb("bec", [128, 2]); ber = fw.sb("ber", [1, 136]); crow = fw.sb("crow", [1, 128]); vrow = fw.sb("vrow", [1, 128])
    fw.op("dve", lambda e: e.tensor_scalar(out=cmp_[:], in0=pend[:], scalar1=C_[:, 512:513], scalar2=None, op0=ALU.is_le), reads=[pend, C_], writes=[cmp_])
    fw.op("dve", lambda e: e.reduce_sum(out=bec[:, 0:1], in_=cmp_[:], axis=AX.X), reads=[cmp_], writes=[bec])
    fw.op("dve", lambda e: e.tensor_scalar(out=bec[:, 0:1], in0=bec[:, 0:1], scalar1=31.0, scalar2=None, op0=ALU.min), reads=[bec], writes=[bec])
    fw.op("dve", lambda e: e.tensor_scalar(out=bec[:, 1:2], in0=C_[:, 512:513], scalar1=pend[:, 31:32], scalar2=None, op0=ALU.is_lt), reads=[pend, C_], writes=[bec])
    pt_ = PS[5]
    tr(fw, pt_, pt_[0:1, 0:128], bec, bec[:, 0:1], C_, k.ID)
    tr(fw, pt_, pt_[0:1, 128:256], bec, bec[:, 1:2], C_, k.ID, first=False)
    fw.op("dve", lambda e: e.memset(ber[:], -1.0), writes=[ber])
    fw.op("act", lambda e: e.activation(out=ber[0:1, 4:132], in_=pt_[0:1, 0:128], func=AF.Copy), reads=[pt_], writes=[ber])
    fw.op("act", lambda e: e.activation(out=vrow[:], in_=pt_[0:1, 128:256], func=AF.Copy), reads=[pt_], writes=[vrow])
    fw.op("dve", lambda e: e.tensor_tensor(out=crow[:], in0=ber[0:1, 4:132], in1=ber[0:1, 0:128], op=ALU.not_equal), reads=[ber], writes=[crow])
    fw.op("dve", lambda e: e.tensor_tensor(out=crow[:], in0=crow[:], in1=vrow[:], op=ALU.mult), reads=[crow, vrow], writes=[crow])
    fw.op("dve", lambda e: e.tensor_tensor(out=crow[:], in0=crow[:], in1=C_[0:1, 640:768], op=ALU.mult), reads=[crow, C_], writes=[crow])
    BIG = 1.0e6
    pb1 = PS[6]; pb2 = PS[7]
    mm(fw, pb1, pb1[:, 0:128], C_, C_[0:1, 128:256], ber, ber[0:1, 4:132], True, True)
    mm(fw, pb2, pb2[:, 0:128], C_, C_[0:1, 128:256], crow, crow[0:1, :], True, True)
    cnd = fw.sb("cnd", [128, 128]); tw = fw.sb("tw", [128, 128]); tb = fw.sb("tb", [128, 128])
    fw.op("act", lambda e: e.activation(out=cnd[:], in_=pb2[:, 0:128], func=AF.Copy), reads=[pb2], writes=[cnd])
    fw.op("dve", lambda e: e.tensor_scalar(out=tb[:], in0=pb1[:, 0:128], scalar1=float(L * 32) - BIG, scalar2=None, op0=ALU.add), reads=[pb1], writes=[tb])
    fw.op("dve", lambda e: e.tensor_scalar(out=tw[:], in0=pb1[:, 0:128], scalar1=128.0, scalar2=float(L * 4096) - BIG, op0=ALU.mult, op1=ALU.add), reads=[pb1], writes=[tw])
    fw.op("dve", lambda e: e.tensor_scalar(out=tw[:], in0=tw[:], scalar1=C_[:, 513:514], scalar2=None, op0=ALU.add), reads=[tw, C_], writes=[tw])
    for t_, ix in ((tw, idx_w), (tb, idx_b)):
        fw.op("dve", lambda e, t_=t_: e.tensor_tensor(out=t_[:], in0=t_[:], in1=cnd[:], op=ALU.mult), reads=[t_, cnd], writes=[t_])
        fw.op("dve", lambda e, t_=t_, ix=ix: e.tensor_scalar(out=ix[:], in0=t_[:], scalar1=BIG, scalar2=None, op0=ALU.add), reads=[t_], writes=[ix])
    for c2 in range(8):
        fw.op("dve", lambda e, c2=c2: e.tensor_scalar(out=idx_g[c2][:], in0=tw[:], scalar1=8.0, scalar2=8 * BIG + c2, op0=ALU.mult, op1=ALU.add), reads=[tw], writes=[idx_g[c2]])
    fw.barrier()
    fw.release(mkD)
    MS = ""
    if MS == "E":
        dbt = k.xt[0]
        for j_, src in enumerate((idx_w, idx_b, idx_g[0], idx_g[1])):
            fw.op("dve", lambda e, j_=j_, src=src: e.tensor_copy(out=dbt[:, j_ * 128:(j_ + 1) * 128], in_=src[:]), reads=[src], writes=[dbt])
        fw.op("dve", lambda e: e.tensor_copy(out=dbt[:, 512:640], in_=S4_all[:].rearrange("p a b -> p (a b)")), reads=[S4_all], writes=[dbt])
        fw.op("dve", lambda e: e.tensor_copy(out=dbt[:, 640:768], in_=G4_all[:].rearrange("p a b -> p (a b)")), reads=[G4_all], writes=[dbt])
        fw.dma("sp", lambda e: e.dma_start(out=k.xs[0:128, :], in_=dbt[:]), reads=[dbt])
        fw.barrier()
        fw.release(mk0); return
    fw.store_q = None
    for i in range(NT):
        hb = k.ht[i % 2]
        fw.dma("sp", lambda e: e.dma_start(out=hb[:], in_=k.h2_d[i * 128:(i + 1) * 128, :]), writes=[hb])
        for kk in range(4):
            fw.dma("pool", lambda e, kk=kk: e.indirect_dma_start(out=k.xsort_d, out_offset=bass.IndirectOffsetOnAxis(ap=S4_all[:, i, kk:kk + 1], axis=0),
                                                                 in_=hb[:], in_offset=None), reads=[hb, S4_all])
    fw.barrier()
    if MS == "F":
        fw.release(mk0); return
    mkG = fw.mark()
    wgu = [fw.sb(f"wgu{i}", [128, 8, 2048], BF16) for i in range(2)]
    wdn = [fw.sb(f"wdn{i}", [128, 8, 1024], BF16) for i in range(2)]
    bgr = [fw.sb(f"bgr{i}", [128, 2048], BF16) for i in range(2)]
    bdb = [fw.sb(f"bdb{i}", [128, 1024]) for i in range(2)]
    onesb = fw.sb("onesb", [1, B], BF16)
    fw.op("dve", lambda e: e.memset(onesb[:], 1.0), writes=[onesb])
    xblk = [fw.sb(f"xblk{i}", [128, B // 128, 1024]) for i in range(1)]
    xT = [fw.sb(f"xT{i}", [128, 8, B], BF16) for i in range(2)]
    hact = fw.sb("hact", [128, 8, B], BF16)
    g_s = [fw.sb(f"g_{i}", [128, B]) for i in range(2)]; sg_s = [fw.sb(f"sg_{i}", [128, B]) for i in range(2)]; l_s = [fw.sb(f"l_{i}", [128, B]) for i in range(2)]
    yo = [fw.sb(f"yo{i}", [128, 1024]) for i in range(2)]
    bound_reg = nc.gpsimd.to_reg(2 * 32 * 1024 - 1)
    for bi in range(NB):
        pj = (bi // 2) % 2
        wg = wgu[pj]; wd = wdn[pj]; bg = bgr[pj]; bd = bdb[pj]
        NW = 2 * 32 * 128 - 1
        if bi % 2 == 0:
            for c in range(8):
                fw.dma("pool", lambda e, c=c: e.indirect_dma_start(out=wg[:, c, :], out_offset=None, in_=k.w_gu,
                                                                   in_offset=bass.IndirectOffsetOnAxis(ap=idx_g[c][:, bi:bi + 1], axis=0), bounds_check=bound_reg, oob_is_err=False),
                       reads=[idx_g[c]], writes=[wg])
            for c in range(8):
                fw.dma("pool", lambda e, c=c: e.indirect_dma_start(out=wd[:, c, :], out_offset=None, in_=k.w_dn,
                                                                   in_offset=bass.IndirectOffsetOnAxis(ap=idx_g[c][:, bi:bi + 1], axis=0), bounds_check=bound_reg, oob_is_err=False),
                       reads=[idx_g[c]], writes=[wd])
            fw.dma("pool", lambda e: e.indirect_dma_start(out=bg[:], out_offset=None, in_=k.b_gu, in_offset=bass.IndirectOffsetOnAxis(ap=idx_b[:, bi:bi + 1], axis=0),
                                                          bounds_check=bound_reg, oob_is_err=False), reads=[idx_b], writes=[bg])
            fw.dma("pool", lambda e: e.indirect_dma_start(out=bd[:], out_offset=None, in_=k.b_dn, in_offset=bass.IndirectOffsetOnAxis(ap=idx_b[:, bi:bi + 1], axis=0),
                                                          bounds_check=bound_reg, oob_is_err=False), reads=[idx_b], writes=[bd])
        xb = xblk[0]; xt_ = xT[bi % 2]

        def emit_xT(bj):
            xtj = xT[bj % 2]
            for rt in range(B // 128):
                for half in range(2):
                    p = PS[half]
                    for c in range(4):
                        cc = half * 4 + c
                        tr(fw, p, p[:, c * 128:(c + 1) * 128], xb, xb[:, rt, bass.ds(cc, 128, 8)], C_, k.ID, first=(c == 0))
                    fw.op("act", lambda e, half=half, p=p, rt=rt: e.activation(out=xtj[:, half * 4:(half + 1) * 4, rt * 128:(rt + 1) * 128], in_=p[:].rearrange("p (c t) -> p c t", c=4), func=AF.Copy),
                          reads=[p], writes=[xtj])
            if bj + 1 < NB:
                fw.dma("sp", lambda e: e.dma_start(out=xb[:], in_=k.xsort_d[(bj + 1) * B:(bj + 2) * B, :].rearrange("(r p) d -> p r d", p=128)), writes=[xb])

        if bi == 0:
            fw.dma("sp", lambda e: e.dma_start(out=xb[:], in_=k.xsort_d[0:B, :].rearrange("(r p) d -> p r d", p=128)), writes=[xb])
            emit_xT(0)
        for fc in range(8):
            pgl = PS[2 + (fc % 4)]
            g_ = g_s[fc % 2]; sg_ = sg_s[fc % 2]; l_ = l_s[fc % 2]
            for part, off in ((0, 0), (1, 1024)):
                o_ap = pgl[:, part * B:(part + 1) * B]
                for c in range(8):
                    mm(fw, pgl, o_ap, wg, wg[:, c, bass.ds(off + fc, 128, 8)], xt_, xt_[:, c, :], c == 0, False)
                mm(fw, pgl, o_ap, bg, bg[0:1, bass.ds(off + fc, 128, 8)], onesb, onesb[0:1, :], False, True)
            fw.op("dve", lambda e, pgl=pgl, g_=g_: e.tensor_scalar(out=g_[:], in0=pgl[:, 0:B], scalar1=7.0, scalar2=None, op0=ALU.min), reads=[pgl], writes=[g_])
            fw.op("act", lambda e: e.activation(out=sg_[:], in_=g_[:], func=AF.Sigmoid, scale=1.702), reads=[g_], writes=[sg_])
            fw.op("dve", lambda e, pgl=pgl: e.tensor_scalar(out=l_[:], in0=pgl[:, B:2 * B], scalar1=7.0, scalar2=-7.0, op0=ALU.min, op1=ALU.max), reads=[pgl], writes=[l_])
            fw.op("dve", lambda e: e.tensor_tensor(out=g_[:], in0=g_[:], in1=sg_[:], op=ALU.mult), reads=[g_, sg_], writes=[g_])
            fw.op("dve", lambda e, fc=fc: e.scalar_tensor_tensor(out=hact[:, fc, :], in0=l_[:], scalar=1.0, in1=g_[:], op0=ALU.add, op1=ALU.mult), reads=[g_, l_], writes=[hact])
        if bi + 1 < NB:
            emit_xT(bi + 1)
        for rt in range(B // 128):
            yb = yo[rt % 2]
            for half in range(2):
                p = PS[6 + half]
                for fc in range(8):
                    mm(fw, p, p[:], hact, hact[:, fc, rt * 128:(rt + 1) * 128], wd, wd[:, fc, half * 512:(half + 1) * 512], fc == 0, fc == 7)
                fw.op("dve", lambda e, p=p, half=half, yb=yb: e.tensor_tensor(out=yb[:, half * 512:(half + 1) * 512], in0=p[:], in1=bd[:, half * 512:(half + 1) * 512], op=ALU.add),
                      reads=[p, bd], writes=[yb])
            fw.dma("sp", lambda e, yb=yb, rt=rt: e.dma_start(out=k.ysort_d[bi * B + rt * 128:bi * B + (rt + 1) * 128, :], in_=yb[:]), reads=[yb])
    fw.barrier()
    fw.release(mkG)
    if MS == "G":
        fw.release(mk0); return
    fw.store_q = "act"
    yk = [[fw.sb(f"yk{i}_{kk}", [128, 1024]) for kk in range(4)] for i in range(2)]
    for i in range(NT):
        xb = k.xt[i % 2]; ys_ = yk[i % 2]
        fw.dma("sp", lambda e: e.dma_start(out=xb[:], in_=k.xs[i * 128:(i + 1) * 128, :]), writes=[xb])
        for kk in range(4):
            fw.dma("pool", lambda e, kk=kk: e.indirect_dma_start(out=ys_[kk][:], out_offset=None, in_=k.ysort_d,
                                                                 in_offset=bass.IndirectOffsetOnAxis(ap=S4_all[:, i, kk:kk + 1], axis=0)), reads=[S4_all], writes=[ys_[kk]])
        a0 = ys_[0]
        fw.op("dve", lambda e: e.tensor_scalar(out=a0[:], in0=a0[:], scalar1=G4_all[:, i, 0:1], scalar2=None, op0=ALU.mult), reads=[a0, G4_all], writes=[a0])
        for kk in range(1, 4):
            fw.op("dve", lambda e, kk=kk: e.scalar_tensor_tensor(out=a0[:], in0=ys_[kk][:], scalar=G4_all[:, i, kk:kk + 1], in1=a0[:], op0=ALU.mult, op1=ALU.add),
                  reads=[ys_[kk], G4_all, a0], writes=[a0])
        fw.op("dve", lambda e: e.tensor_tensor(out=a0[:], in0=a0[:], in1=MOD[:, 5, :], op=ALU.mult), reads=[a0, MOD], writes=[a0])
        fw.op("dve", lambda e: e.tensor_tensor(out=xb[:], in0=xb[:], in1=a0[:], op=ALU.add), reads=[xb, a0], writes=[xb])
        fw.dma("sp", lambda e: e.dma_start(out=k.xs[i * 128:(i + 1) * 128, :], in_=xb[:]), reads=[xb])
    fw.barrier()
    fw.store_q = "pool"
    fw.release(mk0)


def host_consts():
    cst = np.zeros((128, 1024), np.float32)
    cst[:, 0:128] = np.eye(128, dtype=np.float32)
    cst[:, 128:256] = 1.0
    i = np.arange(128)
    cst[:, 256:384] = (i[None, :] >= i[:, None]).astype(np.float32)
    cst[:, 384:512] = (i[:, None] < i[None, :]).astype(np.float32)
    cst[:, 512] = i * 256.0
    cst[:, 513] = i
    cst[:, 640:768] = (i % 2 == 0).astype(np.float32)[None, :]
    inv = (1.0 / (10000.0 ** (np.arange(0, 64, 2, dtype=np.float32) / 64))).astype(np.float32)
    invf = np.zeros((64, 2), np.float32)
    invf[:32, 0] = inv; invf[32:, 0] = inv
    invf[:32, 1] = -1.0; invf[32:, 1] = 1.0
    return cst, invf


def prep_core(inp, b):
    f = np.ascontiguousarray
    cst, invf = host_consts()
    m = {}
    m["x"] = f(inp["x"][b])
    m["c"] = f(inp["c"][b].reshape(8, 128).T)
    m["pos"] = f(inp["positions"][b].reshape(1, T).astype(np.int32))
    m["w_mod"] = inp["w_mod"]; m["b_mod"] = inp["b_mod"]
    m["g_mix"] = inp["g_mix_norm"]; m["g_ffn"] = inp["g_ffn_norm"]
    w_in = inp["mla_w_in"][0]
    kr = w_in[:, 512:576]
    m["mla_w_in"] = f(np.concatenate([w_in, kr[:, 32:], kr[:, :32]], axis=1))
    m["mla_gq"] = f(inp["mla_g_q"][0].reshape(2, 128).T)
    m["mla_gkv"] = f(inp["mla_g_kv"][0].reshape(2, 128).T)
    wq = inp["mla_w_q_up"][0].reshape(256, 8, 192)
    sw = np.concatenate([wq[:, :, 160:192], wq[:, :, 128:160]], axis=2)
    m["mla_wq"] = f(np.concatenate([wq.reshape(256, 1536), sw.reshape(256, 512)], axis=1))
    wkv = inp["mla_w_kv_up"][0].reshape(256, 8, 256)
    m["mla_wkv"] = f(np.concatenate([wkv[:, :, :128].reshape(256, 1024), wkv[:, :, 128:].reshape(256, 1024)], axis=1))
    m["mla_wo"] = inp["mla_w_out"][0]
    m["w_router"] = inp["moe_w_router"]; m["b_router"] = inp["moe_b_router"]
    m["w_gu"] = inp["moe_w_gate_up"].reshape(2 * 32 * 1024, 2048); m["b_gu"] = inp["moe_b_gate_up"].reshape(64, 2048)
    m["w_dn"] = inp["moe_w_down"].reshape(2 * 32 * 1024, 1024); m["b_dn"] = inp["moe_b_down"].reshape(64, 1024)
    m["g_final"] = f(inp["g_final"].reshape(1, D))
    m["ssm_w_in"] = inp["ssm_w_in"][0]
    m["ssm_cw"] = f(inp["ssm_conv_w"][0].reshape(4, 32, 128).transpose(2, 1, 0))
    m["ssm_cb"] = f(inp["ssm_conv_b"][0].reshape(32, 128).T)
    m["ssm_hv"] = f(np.stack([inp["ssm_dt_bias"][0], inp["ssm_a_log"][0]], axis=1))
    m["ssm_dexp"] = f(np.repeat(inp["ssm_d"][0], 64).reshape(1, 2048))
    m["ssm_gn"] = f(inp["ssm_g_norm"][0].reshape(1, 2048))
    m["ssm_wo"] = inp["ssm_w_out"][0]
    m["cst"] = cst; m["invf"] = invf
    return m

_CACHE = {}


def _program():
    if "k" not in _CACHE:
        k = build()
        k.stop = "all"
        k.compute_mod(0)
        mla_layer(k, 0, True)
        mixer_out_and_moe_router(k, 0, k.mla_wo, 8, k.oT_d)
        moe_layer_sparse(k, 0)
        k.compute_mod(1)
        ssd_layer(k, 1)
        mixer_out_and_moe_router(k, 1, k.ssm_wo, 16, k.synT_d)
        moe_layer_sparse(k, 1)
        final_norm(k)
        k.fw.finish()
        k.fw.close()
        _CACHE["k"] = k
    return _CACHE["k"]


def kernel(**inputs):
    inp = {kk: np.asarray(v) for kk, v in inputs.items()}
    k = _program()
    in_maps = [prep_core(inp, b) for b in range(8)]
    res = run_bass_kernel_spmd(k.nc, in_maps, core_ids=list(range(8)))
    return np.stack([np.asarray(r["out"]) for r in res.results], axis=0).astype(np.float32)
```
